# Optimizing a Trainium2 kernel written in Bass

```python
import math
import jax, jax.numpy as jnp
from jax import lax
import numpy as np

D_MODEL = 2048
BATCH = 16
SEQ = 2048
DEPTH = 4

HEAD_DIM = 128
BRANCH_W = D_MODEL // 2
N_BRANCH = 3
NSA_HEADS = BRANCH_W // HEAD_DIM
NSA_KV_HEADS = NSA_HEADS // 4
NSA_GROUP = NSA_HEADS // NSA_KV_HEADS
CMP_BLOCK = 32
CMP_STRIDE = 16
SEL_BLOCK = 64
SEL_TOPK = 16
WINDOW = 512
Q_BLOCK = 128
SEL_QCHUNK = 16
SB_HEADS = BRANCH_W // HEAD_DIM
HG_DK = 128
HG_DV = 128
HG_HEADS = BRANCH_W // HG_DV
HG_CHUNK = 64
D_FF = 256 * ((8 * D_MODEL // 3 + 255) // 256)
CONV_W = 3
REL_BUCKETS = 32
REL_MAX_DIST = 128
EPS = 1e-6
NEG = -1e30
TINY = 1e-30

NSA_Q = NSA_HEADS * HEAD_DIM
NSA_KV = NSA_KV_HEADS * HEAD_DIM
NSA_GATE = 3 * NSA_HEADS
SB_W = SB_HEADS * HEAD_DIM
HG_KW = HG_HEADS * HG_DK
HG_VW = HG_HEADS * HG_DV
SPLIT_SIZES = (NSA_Q,) + (NSA_KV,) * 6 + (NSA_GATE,) + (SB_W,) * 3 + (HG_KW, HG_KW, HG_VW, HG_VW, N_BRANCH * D_MODEL)
IN_COLS = sum(SPLIT_SIZES)

kernel_name = 'hybrid_nsa_stickbreaking_hgrn2_block'


def rmsnorm(x, g):
    xf = x.astype(jnp.float32)
    y = xf * lax.rsqrt(jnp.mean(xf * xf, axis=-1, keepdims=True) + EPS)
    return (y * g.astype(jnp.float32)).astype(x.dtype)


def masked_softmax(s, mask):
    s = jnp.where(mask, s, NEG)
    e = jnp.where(mask, jnp.exp(s - jnp.max(s, axis=-1, keepdims=True)), 0.0)
    return e / jnp.maximum(jnp.sum(e, axis=-1, keepdims=True), TINY)


def t5_bucket(dist):
    n = jnp.maximum(dist, 0)
    exact = REL_BUCKETS // 2
    big = exact + (jnp.log(jnp.maximum(n, 1).astype(jnp.float32) / exact)
                   / math.log(REL_MAX_DIST / exact) * (REL_BUCKETS - exact)).astype(jnp.int32)
    return jnp.where(n < exact, n, jnp.minimum(big, REL_BUCKETS - 1))


def nsa_mixer(q, kc, vc, ks, vs, kw, vw, g, q_gain, k_gain, cmp_pos, cmp_w1, cmp_w2, rel_bias):
    B, T, _ = q.shape
    G, R, Dh = NSA_KV_HEADS, NSA_GROUP, HEAD_DIM
    scale = HEAD_DIM ** -0.5
    q = rmsnorm(q.reshape(B, T, G, R, Dh), q_gain).transpose(0, 2, 3, 1, 4)
    kc, vc, ks, vs, kw, vw = (a.reshape(B, T, G, Dh).transpose(0, 2, 1, 3) for a in (kc, vc, ks, vs, kw, vw))
    ks = rmsnorm(ks, k_gain[1])
    kw = rmsnorm(kw, k_gain[2])
    tab_h = rel_bias.T
    tab_gr = tab_h.reshape(G, R, REL_BUCKETS)

    n_cmp = (T - CMP_BLOCK) // CMP_STRIDE + 1
    cidx = np.arange(n_cmp)[:, None] * CMP_STRIDE + np.arange(CMP_BLOCK)[None, :]

    def compress(a, j):
        blocks = a[:, :, cidx] + cmp_pos[j]
        hid = jax.nn.gelu(jnp.einsum('bgnld,lde->bgne', blocks, cmp_w1[j]))
        return hid @ cmp_w2[j]

    kcmp = rmsnorm(compress(kc, 0), k_gain[0])
    vcmp = compress(vc, 1)
    dist_c = np.arange(T)[:, None] - cidx[None, :, -1]
    bias_c = jnp.take(tab_h, t5_bucket(jnp.asarray(dist_c)), axis=1).reshape(G, R, T, n_cmp)
    s_c = jnp.einsum('bgrtd,bgnd->bgrtn', q, kcmp).astype(jnp.float32) * scale + bias_c
    p_cmp = masked_softmax(s_c, jnp.asarray(dist_c >= 0))
    o_cmp = jnp.einsum('bgrtn,bgnd->bgrtd', p_cmp, vcmp)

    n_sel = T // SEL_BLOCK
    c_start = cidx[:, 0]
    s_start = np.arange(n_sel) * SEL_BLOCK
    overlap = ((c_start[:, None] < s_start[None, :] + SEL_BLOCK)
               & (c_start[:, None] + CMP_BLOCK > s_start[None, :])).astype(np.float32)
    imp = jnp.einsum('bgrtn,nj->bgtj', p_cmp, jnp.asarray(overlap))
    qblk = np.arange(T) // SEL_BLOCK
    jb = np.arange(n_sel)
    causal_b = jb[None, :] <= qblk[:, None]
    forced = causal_b & ((jb[None, :] == 0) | (jb[None, :] >= qblk[:, None] - 1))
    score = jnp.where(jnp.asarray(forced), jnp.inf, jnp.where(jnp.asarray(causal_b), imp, -jnp.inf))
    n_top = min(SEL_TOPK, n_sel)
    top_val, top_idx = lax.top_k(score, n_top)
    sel_ok = top_val > -jnp.inf

    kb = ks.reshape(B, G, n_sel, SEL_BLOCK, Dh)
    vb = vs.reshape(B, G, n_sel, SEL_BLOCK, Dh)
    QC = SEL_QCHUNK
    nqc = T // QC
    q_c = q.reshape(B, G, R, nqc, QC, Dh).transpose(3, 0, 1, 2, 4, 5)
    idx_c = top_idx.reshape(B, G, nqc, QC, n_top).transpose(2, 0, 1, 3, 4)
    ok_c = sel_ok.reshape(B, G, nqc, QC, n_top).transpose(2, 0, 1, 3, 4)
    pos_c = jnp.arange(T, dtype=jnp.int32).reshape(nqc, QC)
    b_ix = jnp.arange(B)[:, None, None, None]
    g_ix = jnp.arange(G)[None, :, None, None]
    g6 = jnp.arange(G).reshape(1, G, 1, 1, 1, 1)
    r6 = jnp.arange(R).reshape(1, 1, R, 1, 1, 1)

    def sel_block(args):
        qq, ii, ok, pp = args
        kg = kb[b_ix, g_ix, ii]
        vg = vb[b_ix, g_ix, ii]
        kpos = ii[..., None] * SEL_BLOCK + jnp.arange(SEL_BLOCK, dtype=jnp.int32)
        dist = pp[None, None, :, None, None] - kpos
        mask = (ok[..., None] & (dist >= 0))[:, :, None]
        bias = tab_gr[g6, r6, t5_bucket(dist)[:, :, None]]
        s = jnp.einsum('bgrqd,bgqksd->bgrqks', qq, kg).astype(jnp.float32) * scale + bias
        flat = s.shape[:4] + (-1,)
        p = masked_softmax(s.reshape(flat), mask.reshape(mask.shape[:4] + (-1,)))
        return jnp.einsum('bgrqm,bgqmd->bgrqd', p, vg.reshape(B, G, QC, -1, Dh))

    o_sel = lax.map(sel_block, (q_c, idx_c, ok_c, pos_c))
    o_sel = o_sel.transpose(1, 2, 3, 0, 4, 5).reshape(B, G, R, T, Dh)

    nqb = T // Q_BLOCK
    widx = np.arange(nqb)[:, None] * Q_BLOCK + np.arange(Q_BLOCK + WINDOW)[None, :]
    kwin = jnp.pad(kw, ((0, 0), (0, 0), (WINDOW, 0), (0, 0)))[:, :, widx]
    vwin = jnp.pad(vw, ((0, 0), (0, 0), (WINDOW, 0), (0, 0)))[:, :, widx]
    qpos = np.arange(nqb)[:, None] * Q_BLOCK + np.arange(Q_BLOCK)[None, :]
    kpos = widx - WINDOW
    dist_w = qpos[:, :, None] - kpos[:, None, :]
    mask_w = (dist_w >= 0) & (dist_w < WINDOW) & (kpos[:, None, :] >= 0)
    bias_w = jnp.take(tab_h, t5_bucket(jnp.asarray(dist_w)), axis=1).reshape(G, R, nqb, Q_BLOCK, Q_BLOCK + WINDOW)
    s_w = jnp.einsum('bgrnqd,bgnkd->bgrnqk', q.reshape(B, G, R, nqb, Q_BLOCK, Dh), kwin).astype(jnp.float32) * scale
    p_w = masked_softmax(s_w + bias_w, jnp.asarray(mask_w))
    o_win = jnp.einsum('bgrnqk,bgnkd->bgrnqd', p_w, vwin).reshape(B, G, R, T, Dh)

    gate = jax.nn.sigmoid(g.astype(jnp.float32)).reshape(B, T, G, R, 3).transpose(0, 2, 3, 1, 4)
    o = gate[..., 0:1] * o_cmp + gate[..., 1:2] * o_sel + gate[..., 2:3] * o_win
    return o.transpose(0, 3, 1, 2, 4).reshape(B, T, NSA_HEADS * Dh)


def stick_breaking(q, k, v):
    B, T, _ = q.shape
    q, k, v = (a.reshape(B, T, SB_HEADS, HEAD_DIM).transpose(0, 2, 1, 3) for a in (q, k, v))
    scale = HEAD_DIM ** -0.5
    outs = []
    for i in range(T // Q_BLOCK):
        end = (i + 1) * Q_BLOCK
        z = jnp.einsum('bhqd,bhkd->bhqk', q[:, :, i * Q_BLOCK:end], k[:, :, :end]).astype(jnp.float32) * scale
        causal = jnp.asarray(np.arange(end)[None, :] < np.arange(i * Q_BLOCK, end)[:, None])
        log_1mb = jnp.where(causal, jax.nn.log_sigmoid(-z), 0.0)
        log_a = jax.nn.log_sigmoid(z) + lax.cumsum(log_1mb, axis=3, reverse=True) - log_1mb
        a = jnp.where(causal, jnp.exp(log_a), 0.0)
        outs.append(jnp.einsum('bhqk,bhkd->bhqd', a, v[:, :, :end]))
    o = jnp.concatenate(outs, axis=2)
    return o.transpose(0, 2, 1, 3).reshape(B, T, SB_HEADS * HEAD_DIM)


def hgrn2(q, f_pre, inp, g_out, lb, norm_gain):
    B, T, _ = q.shape
    H, C = HG_HEADS, HG_CHUNK
    nc = T // C
    f_pre = f_pre.astype(jnp.float32).reshape(B, T, H, HG_DK)
    lb = lb.reshape(H, HG_DK)
    log_f = jnp.logaddexp(jnp.log(lb), jnp.log1p(-lb) + jax.nn.log_sigmoid(f_pre))
    k = (1.0 - lb) * jax.nn.sigmoid(-f_pre)

    def chunks(a, d):
        return a.astype(jnp.float32).reshape(B, nc, C, H, d).transpose(1, 0, 3, 2, 4)

    xs = (chunks(q, HG_DK), chunks(k, HG_DK), chunks(inp, HG_DV), chunks(log_f, HG_DK))
    tri = jnp.asarray(np.tril(np.ones((C, C), dtype=bool)))

    def step(S, xs_c):
        qq, kk, vv, lf = xs_c
        b = jnp.cumsum(lf, axis=2)
        decay = jnp.exp(jnp.where(tri[:, :, None], b[:, :, :, None, :] - b[:, :, None, :, :], -jnp.inf))
        a = jnp.einsum('bhtd,bhsd,bhtsd->bhts', qq, kk, decay)
        o = jnp.einsum('bhts,bhse->bhte', a, vv) + jnp.einsum('bhtd,bhde->bhte', qq * jnp.exp(b), S)
        b_last = b[:, :, -1:, :]
        S = jnp.exp(b_last[:, :, 0, :, None]) * S + jnp.einsum('bhsd,bhse->bhde', kk * jnp.exp(b_last - b), vv)
        return S, o

    S0 = jnp.zeros((B, H, HG_DK, HG_DV), jnp.float32)
    _, o = lax.scan(step, S0, xs)
    o = o.transpose(1, 0, 3, 2, 4).reshape(B, T, H, HG_DV)
    gate = jax.nn.sigmoid(g_out.astype(jnp.float32).reshape(B, T, H, HG_DV))
    return (rmsnorm(o, norm_gain) * gate).reshape(B, T, H * HG_DV)


def conv_ffn(h, w_up, conv_w, conv_b, w_down):
    u = h @ w_up
    T = u.shape[1]
    up = jnp.pad(u, ((0, 0), (CONV_W - 1, 0), (0, 0)))
    c = conv_b
    for j in range(CONV_W):
        c = c + up[:, j:j + T] * conv_w[j]
    gate, val = jnp.split(c, 2, axis=-1)
    return (jax.nn.silu(gate) * val) @ w_down


def setup_inputs(seed: int = 0) -> dict:
    key = jax.random.key(seed)
    ks = jax.random.split(key, 18)

    def nrm(k, shape, scale):
        return jax.random.normal(k, shape, jnp.float32) * scale

    res = (2 * DEPTH) ** -0.5
    return {
        'x': nrm(ks[0], (BATCH, SEQ, D_MODEL), 1.0),
        'norm_attn': 1.0 + nrm(ks[1], (DEPTH, D_MODEL), 0.02),
        'w_in': nrm(ks[2], (DEPTH, D_MODEL, IN_COLS), D_MODEL ** -0.5),
        'nsa_q_gain': 1.0 + nrm(ks[3], (DEPTH, HEAD_DIM), 0.02),
        'nsa_k_gain': 1.0 + nrm(ks[4], (DEPTH, 3, HEAD_DIM), 0.02),
        'cmp_pos': nrm(ks[5], (DEPTH, 2, CMP_BLOCK, HEAD_DIM), 0.02),
        'cmp_w1': nrm(ks[6], (DEPTH, 2, CMP_BLOCK, HEAD_DIM, HEAD_DIM), (CMP_BLOCK * HEAD_DIM) ** -0.5),
        'cmp_w2': nrm(ks[7], (DEPTH, 2, HEAD_DIM, HEAD_DIM), HEAD_DIM ** -0.5),
        'rel_bias': nrm(ks[8], (REL_BUCKETS, NSA_HEADS), 0.1),
        'hg_lower_bound': nrm(ks[9], (DEPTH, HG_HEADS * HG_DK), 0.1),
        'hg_norm_gain': 1.0 + nrm(ks[10], (DEPTH, HG_DV), 0.02),
        'w_branch': nrm(ks[11], (DEPTH, N_BRANCH, BRANCH_W, D_MODEL), BRANCH_W ** -0.5),
        'w_out': nrm(ks[12], (DEPTH, D_MODEL, D_MODEL), D_MODEL ** -0.5 * res),
        'norm_ffn': 1.0 + nrm(ks[13], (DEPTH, D_MODEL), 0.02),
        'w_up': nrm(ks[14], (DEPTH, D_MODEL, 2 * D_FF), D_MODEL ** -0.5),
        'conv_w': nrm(ks[15], (DEPTH, CONV_W, 2 * D_FF), CONV_W ** -0.5),
        'conv_b': nrm(ks[16], (DEPTH, 2 * D_FF), 0.02),
        'w_down': nrm(ks[17], (DEPTH, D_FF, D_MODEL), D_FF ** -0.5 * res),
    }


def reference(x, norm_attn, w_in, nsa_q_gain, nsa_k_gain, cmp_pos, cmp_w1, cmp_w2, rel_bias,
              hg_lower_bound, hg_norm_gain, w_branch, w_out, norm_ffn, w_up, conv_w, conv_b, w_down):
    B, T, _ = x.shape
    lb_all = jnp.cumsum(jax.nn.softmax(hg_lower_bound.astype(jnp.float32), axis=0), axis=0)
    lb_all = jnp.maximum(lb_all - lb_all[0:1], 0.0)
    split_at = np.cumsum(SPLIT_SIZES)[:-1].tolist()
    for l in range(DEPTH):
        h = rmsnorm(x, norm_attn[l])
        (q_n, kc_n, vc_n, ks_n, vs_n, kw_n, vw_n, g_n, q_s, k_s, v_s,
         q_h, f_h, i_h, g_h, gates) = jnp.split(h @ w_in[l], split_at, axis=-1)
        o_nsa = nsa_mixer(q_n, kc_n, vc_n, ks_n, vs_n, kw_n, vw_n, g_n, nsa_q_gain[l], nsa_k_gain[l],
                          cmp_pos[l], cmp_w1[l], cmp_w2[l], rel_bias).astype(h.dtype)
        o_sb = stick_breaking(q_s, k_s, v_s).astype(h.dtype)
        o_hg = hgrn2(q_h, f_h, i_h, g_h, lb_all[l], hg_norm_gain[l]).astype(h.dtype)
        branches = jnp.stack([o_nsa, o_sb, o_hg], axis=2)
        y = jnp.einsum('btnc,ncd->btnd', branches, w_branch[l])
        gsig = jax.nn.sigmoid(gates.astype(jnp.float32)).reshape(B, T, N_BRANCH, D_MODEL)
        merged = jnp.sum(gsig * y, axis=2).astype(h.dtype)
        x = x + merged @ w_out[l]
        x = x + conv_ffn(rmsnorm(x, norm_ffn[l]), w_up[l], conv_w[l], conv_b[l], w_down[l])
    return x
```

```python
import numpy as np
from contextlib import ExitStack
import ml_dtypes
import concourse.bass as bass
import concourse.mybir as mybir
from concourse.bass_utils import run_bass_kernel_spmd

F32 = mybir.dt.float32
BF16 = mybir.dt.bfloat16
AF = mybir.ActivationFunctionType
ALU = mybir.AluOpType

D = 2048
T = 2048
NSEQ = 2
DEPTH = 4
HD = 128
IN_COLS = 15896
DFF = 5632
EPS = 1e-6
SCALE = HD ** -0.5
C_QN, C_KC, C_VC, C_KS, C_VS, C_KW, C_VW, C_G = 0, 1024, 1280, 1536, 1792, 2048, 2304, 2560
C_SQ, C_SK, C_SV = 2584, 3608, 4632
C_HQ, C_HF, C_HI, C_HG = 5656, 6680, 7704, 8728
C_MG = 9752


class Ctx:
    def __init__(self, nc):
        self.nc = nc
        self.E = dict(pe=nc.tensor, act=nc.scalar, dve=nc.vector, pool=nc.gpsimd, sp=nc.sync)
        self.sem = {n: nc.alloc_semaphore("s_" + n) for n in self.E}
        self.cnt = {n: 0 for n in self.E}
        self.seen = {n: {} for n in self.E}
        self.res = {}
        self.slots = {}
        self.nwait = 0

    def _slot(self, name):
        if name not in self.slots:
            self.slots[name] = [self.nc.alloc_semaphore("d_" + name), 0]
        return self.slots[name]

    def _need(self, reads, writes):
        need = {}

        def add(p, c):
            if need.get(p, 0) < c:
                need[p] = c
        for k in reads:
            r = self.res.get(k)
            if r and r[0]:
                add(*r[0])
        for k in writes:
            r = self.res.get(k)
            if r:
                if r[0]:
                    add(*r[0])
                for p, c in r[1].items():
                    add(p, c)
        return need

    def _wait(self, eng, need):
        for p, c in need.items():
            if p == eng and eng in ("pe",):
                continue
            if self.seen[eng].get(p, 0) >= c:
                continue
            if isinstance(p, tuple):
                self.E[eng].wait_ge(self.slots[p[1]][0], 16 * c)
            else:
                self.E[eng].wait_ge(self.sem[p], c)
            self.nwait += 1
            self.seen[eng][p] = c

    def _mark(self, who, c, reads, writes):
        for k in reads:
            r = self.res.setdefault(k, [None, {}])
            r[1][who] = c
        for k in writes:
            self.res[k] = [(who, c), {}]

    def op(self, eng, fn, reads=(), writes=()):
        self._wait(eng, self._need(reads, writes))
        ins = fn(self.E[eng])
        self.cnt[eng] += 1
        ins.then_inc(self.sem[eng], 1)
        self._mark(eng, self.cnt[eng], reads, writes)
        return ins

    def dma(self, q, slot, out, in_, reads=(), writes=(), **kw):
        self._wait(q, self._need(reads, writes))
        s = self._slot(slot)
        ins = self.E[q].dma_start(out=out, in_=in_, **kw)
        s[1] += 1
        ins.then_inc(s[0], 16)
        self._mark(("dma", slot), s[1], reads, writes)
        return ins

    def barrier(self):
        for eng in self.E:
            need = {}
            for n in self.E:
                if n != eng and self.cnt[n]:
                    need[n] = self.cnt[n]
            for name, (sem, cc) in self.slots.items():
                if cc:
                    need[("dma", name)] = cc
            self._wait(eng, need)

    def finish(self):
        for name, (sem, c) in self.slots.items():
            if c:
                self.E["sp"].wait_ge(sem, 16 * c)
        for n in self.E:
            if n != "sp" and self.cnt[n]:
                self.E["sp"].wait_ge(self.sem[n], self.cnt[n])


class Prog:
    def __init__(self, layers, debug=()):
        self.layers = list(layers)
        self.debug = set(debug)
        nc = self.nc = bass.Bass("TRN2", target_bir_lowering=False)
        self.c = Ctx(nc)
        self._n = 0
        L = len(self.layers)
        self.L = L
        di = lambda name, shape, dt=F32: nc.dram_tensor(name, list(shape), dt, kind="ExternalInput").ap()
        self.x = di("x", [NSEQ, T, D])
        self.w_in = di("w_in", [L, D, IN_COLS])
        self.norm_attn = di("norm_attn", [128, L, 16])
        self.hgain = di("hgain", [128, L, 4])
        self.ebias = di("ebias", [8, 128, 2688])
        self.cb = di("cb", [128, 8])
        self.addmask = di("addmask", [128, 8, 32])
        self.esel = di("esel", [32, 16, 128], BF16)
        self.ov33 = di("ov33", [128, 33], BF16)
        self.selg = di("selg", [24, 24, 128])
        self.cmp_w1 = di("cmp_w1", [L, 2, 32, 128, 128])
        self.cmp_w2 = di("cmp_w2", [L, 2, 128, 128])
        self.posT = di("posT", [128, L, 2, 32])
        self.hlb = di("hlb", [128, 4, 8])
        self.lcum = di("lcum", [128, L, 4])
        self.gn = di("gn", [128, L, 128])
        self.w_branch = di("w_branch", [L, 3072, D])
        self.w_out = di("w_out", [L, D, D])
        self.norm_ffn = di("norm_ffn", [128, L, 16])
        self.w_up = di("w_up", [L, D, 2 * DFF])
        self.convw = di("convw", [128, L, 3, 88])
        self.convb = di("convb", [128, L, 88])
        self.w_down = di("w_down", [L, DFF, D])
        self.wbr_b = self.scratch("wbr_b", [L, 3072, D], BF16)
        self.wout_b = self.scratch("wout_b", [L, D, D], BF16)
        self.wup_b = self.scratch("wup_b", [L, D, 2 * DFF], BF16)
        self.wdn_b = self.scratch("wdn_b", [L, DFF, D], BF16)
        self.XS = self.scratch("XS", [2, NSEQ, T, D], F32)
        self.EBD = self.scratch("EBD", [8, 128, 2688], F32)
        self.OT = self.scratch("OT", [24, 128, T], BF16)
        self.out = nc.dram_tensor("out", [NSEQ, T, D], F32, kind="ExternalOutput").ap()
        self.win_b = self.scratch("win_b", [L, D, IN_COLS], BF16)
        self.HT = self.scratch("HT", [128, 16, T], BF16)
        self.FM = self.scratch("FM", [40, 128, T], BF16)
        self.HF = self.scratch("HF", [8, 128, T], F32)
        self.GS = self.scratch("GS", [24, T], F32)
        self.TM = self.scratch("TM", [T, 3584], BF16)

    def scratch(self, name, shape, dt):
        kind = "ExternalOutput" if name in self.debug else "Internal"
        return self.nc.dram_tensor(name, list(shape), dt, kind=kind).ap()

    def sb(self, name, shape, dt):
        self._n += 1
        if getattr(self, "_stacks", None):
            return self._stacks[-1].enter_context(self.nc.sbuf_tensor(f"{name}_{self._n}", list(shape), dt))
        return self.nc.alloc_sbuf_tensor(f"{name}_{self._n}", list(shape), dt)

    def phase(self):
        prog = self

        class _P:
            def __enter__(self_):
                prog._stacks.append(ExitStack())
                return self_

            def __exit__(self_, *a):
                prog.c.barrier()
                prog._stacks.pop().close()
                return False
        return _P()

    def ps(self, name, shape, dt=F32):
        self._n += 1
        return self.nc.alloc_psum_tensor(f"{name}_{self._n}", list(shape), dt)

    def convert_weights(self, li):
        c = self.c
        for i in range(D // 128):
            src = self.w_in[li, i * 128:(i + 1) * 128, :].rearrange("p (a b) -> p a b", b=1987)
            dst = self.win_b[li, i * 128:(i + 1) * 128, :].rearrange("p (a b) -> p a b", b=1987)
            c.dma("pool", "cvt", dst, src, writes=[("win_b", li)])
        for srcw, dstw, rows, cols, key in ((self.w_branch, self.wbr_b, 3072, D, "wbr_b"), (self.w_out, self.wout_b, D, D, "wout_b"),
                                            (self.w_up, self.wup_b, D, 2 * DFF, "wup_b"), (self.w_down, self.wdn_b, DFF, D, "wdn_b")):
            for i in range(rows // 128):
                src = srcw[li, i * 128:(i + 1) * 128, :].rearrange("p (a b) -> p a b", b=1024)
                dst = dstw[li, i * 128:(i + 1) * 128, :].rearrange("p (a b) -> p a b", b=1024)
                c.dma("pool", "cvt", dst, src, writes=[(key, li)])

    def setup_consts(self):
        c = self.c
        self.ident = self.sb("ident", [128, 128], BF16)
        self.ones_b = self.sb("ones", [128, 128], BF16)
        self.gattn = self.sb("gattn", [128, self.L, 16], F32)
        idf = self.sb("idf", [128, 128], F32)
        c.op("pool", lambda e: e.memset(idf[:], 1.0), writes=["idf"])
        c.op("pool", lambda e: e.affine_select(out=idf[:], in_=idf[:], pattern=[[-1, 128]], compare_op=ALU.is_equal,
                                               fill=0.0, base=0, channel_multiplier=1), reads=["idf"], writes=["idf"])
        c.op("pool", lambda e: e.tensor_copy(out=self.ident[:], in_=idf[:]), reads=["idf"], writes=["ident"])
        c.op("pool", lambda e: e.memset(self.ones_b[:], 1.0), writes=["ones"])
        c.dma("sp", "cst", self.gattn[:], self.norm_attn[:, :, :], writes=["gattn"])
        self.hgain_t = self.sb("hgain", [128, self.L, 4], F32)
        c.dma("sp", "cst2", self.hgain_t[:], self.hgain[:, :, :], writes=["hgain"])

    def norm_tile(self, xt, kx, gains, dstT, col0, kdst):
        c = self.c
        ss, sq, xn = self.ss, self.sqj, self.xn
        c.op("act", lambda e: e.activation(out=sq[:], in_=xt, func=AF.Square, accum_out=ss[:, 0:1]), reads=[kx], writes=["sqj", "ss"])
        c.op("act", lambda e: e.activation(out=ss[:, 1:2], in_=ss[:, 0:1], func=AF.Sqrt, scale=1.0 / D, bias=self.eps_t[:, 0:1]),
             reads=["ss", "eps"], writes=["ss1"])
        c.op("dve", lambda e: e.reciprocal(out=ss[:, 2:3], in_=ss[:, 1:2]), reads=["ss1"], writes=["ss2"])
        c.op("dve", lambda e: e.tensor_scalar(out=xn[:], in0=xt, scalar1=ss[:, 2:3], scalar2=None, op0=ALU.mult), reads=[kx, "ss2"], writes=["xn"])
        for half in range(2):
            pt = self.ptr[half]
            kp = ("ptr", half)
            for j in range(8):
                kc = half * 8 + j
                c.op("pe", lambda e: e.transpose(out=pt[:, j, :], in_=xn[:, kc * 128:(kc + 1) * 128], identity=self.ident[:]),
                     reads=["xn", "ident"], writes=[kp])
            for j in range(8):
                kc = half * 8 + j
                dst = dstT[:, kc, col0:col0 + 128]
                g = gains[:, kc:kc + 1]
                if j % 2 == 0:
                    c.op("act", lambda e: e.activation(out=dst, in_=pt[:, j, :], func=AF.Copy, scale=g), reads=[kp, "gattn", "gffn"], writes=[kdst])
                else:
                    c.op("dve", lambda e: e.tensor_scalar(out=dst, in0=pt[:, j, :], scalar1=g, scalar2=None, op0=ALU.mult),
                         reads=[kp, "gattn", "gffn"], writes=[kdst])

    def phase_a(self, li, s, xsrc, xname):
        c = self.c
        for tt in range(16):
            xt = self.xt[tt % 2]
            kx = ("xt", tt % 2)
            c.dma("sp", f"xt{tt % 2}", xt[:], xsrc[s, tt * 128:(tt + 1) * 128, :], reads=[("X", xname, s, tt // 4)], writes=[kx])
            self.norm_tile(xt[:], kx, self.gattn[:, li, :], self.hT, tt * 128, ("hT", tt))

    def phase_b(self, li, s):
        c = self.c
        hT = self.hT
        hT_keys = [("hT", t) for t in range(16)]
        wsrc = self.win_b[li].rearrange("(kc p) n -> p kc n", p=128)
        nload = [0]

        def load_w(col, n):
            i = nload[0] % 2
            nload[0] += 1
            wt = self.wt[i]
            c.dma("sp", f"wt{i}", wt[:, :, 0:n], wsrc[:, :, col:col + n], reads=[("win_b", li)], writes=[("wt", i)])
            return wt, ("wt", i)

        nacc = [0]

        def next_acc():
            i = nacc[0] % 2
            nacc[0] += 1
            return self.acc[i], ("acc", i)

        nst = [0]
        fm_jobs = [
            (C_QN, 8, "hnorm", 0, 0), (C_KC, 2, "plain", 8, None), (C_VC, 2, "plain", 10, None),
            (C_KS, 2, "hnorm", 12, 2), (C_KW, 2, "hnorm", 14, 3),
            (C_SQ, 8, "plainS", 16, None), (C_SK, 8, "plain", 24, None), (C_HQ, 8, "plain", 32, None),
            (C_HF, 8, "plain32", 0, None),
        ]
        for col0, nch, kind, fm0, gi in fm_jobs:
            for c4 in range(0, nch, 4):
                ncc = min(4, nch - c4)
                wt, kw = load_w(col0 + c4 * 128, ncc * 128)
                for j in range(ncc):
                    ch = c4 + j
                    if kind == "plain32":
                        st, kst = self.fst32, "fst32"
                    else:
                        st, kst = self.fst[nst[0] % 2], ("fst", nst[0] % 2)
                        nst[0] += 1
                    for tb in range(4):
                        acc, ka = next_acc()
                        for kc in range(16):
                            c.op("pe", lambda e: e.matmul(acc[:], lhsT=wt[:, kc, j * 128:(j + 1) * 128], rhs=hT[:, kc, tb * 512:(tb + 1) * 512],
                                                          start=(kc == 0), stop=(kc == 15)),
                                 reads=[kw] + hT_keys, writes=[ka])
                        dst = st[:, tb * 512:(tb + 1) * 512]
                        if kind == "plainS":
                            c.op("act", lambda e: e.activation(out=dst, in_=acc[:], func=AF.Copy, scale=SCALE), reads=[ka], writes=[kst])
                        elif kind == "plain":
                            eng = "act" if tb % 2 == 0 else "dve"
                            if eng == "act":
                                c.op("act", lambda e: e.copy(out=dst, in_=acc[:]), reads=[ka], writes=[kst])
                            else:
                                c.op("dve", lambda e: e.tensor_copy(out=dst, in_=acc[:]), reads=[ka], writes=[kst])
                        elif kind == "plain32":
                            c.op("act", lambda e: e.copy(out=dst, in_=acc[:]), reads=[ka], writes=[kst])
                        else:
                            self.hnorm_block(acc, ka, dst, kst, self.hgain_t[:, li, gi:gi + 1])
                    if kind == "plain32":
                        c.dma("pool", "fst32", self.HF[ch, :, :], st[:], reads=[kst], writes=[("HF", ch)])
                    else:
                        c.dma("pool", f"fst{kst[1]}", self.FM[fm0 + ch, :, :], st[:], reads=[kst], writes=[("FM", fm0 + ch)])
        wt, kw = load_w(C_G, 24)
        for tb in range(4):
            acc, ka = next_acc()
            for kc in range(16):
                c.op("pe", lambda e: e.matmul(acc[0:24, :], lhsT=wt[:, kc, 0:24], rhs=hT[:, kc, tb * 512:(tb + 1) * 512],
                                              start=(kc == 0), stop=(kc == 15)), reads=[kw] + hT_keys, writes=[ka])
            c.op("act", lambda e: e.activation(out=self.gst[0:24, tb * 512:(tb + 1) * 512], in_=acc[0:24, :], func=AF.Sigmoid),
                 reads=[ka], writes=["gst"])
        c.dma("pool", "gst", self.GS[:, :], self.gst[0:24, :], reads=["gst"], writes=["GS"])
        tm_jobs = [(C_VS, 256, 0, False), (C_VW, 256, 256, False), (C_SV, 1024, 512, False),
                   (C_HI, 1024, 1536, False), (C_HG, 1024, 2560, True)]
        nts = [0]
        for col0, ncols, tm0, sig in tm_jobs:
            for c0 in range(0, ncols, 512):
                n = min(512, ncols - c0)
                wt, kw = load_w(col0 + c0, n)
                for tt in range(16):
                    acc, ka = next_acc()
                    for kc in range(16):
                        c.op("pe", lambda e: e.matmul(acc[:, 0:n], lhsT=hT[:, kc, tt * 128:(tt + 1) * 128], rhs=wt[:, kc, 0:n],
                                                      start=(kc == 0), stop=(kc == 15)), reads=[kw, ("hT", tt)], writes=[ka])
                    i = nts[0] % 2
                    nts[0] += 1
                    st, kst = self.tst[i], ("tst", i)
                    if sig:
                        c.op("act", lambda e: e.activation(out=st[:, 0:n], in_=acc[:, 0:n], func=AF.Sigmoid), reads=[ka], writes=[kst])
                    elif tt % 2 == 0:
                        c.op("act", lambda e: e.copy(out=st[:, 0:n], in_=acc[:, 0:n]), reads=[ka], writes=[kst])
                    else:
                        c.op("dve", lambda e: e.tensor_copy(out=st[:, 0:n], in_=acc[:, 0:n]), reads=[ka], writes=[kst])
                    c.dma("pool", f"tst{i}", self.TM[tt * 128:(tt + 1) * 128, tm0 + c0:tm0 + c0 + n], st[:, 0:n],
                          reads=[kst], writes=[("TM", tm0 + c0, tt)])

    def hnorm_block(self, acc, ka, dst, kdst, gain):
        c = self.c
        c.op("act", lambda e: e.activation(out=self.sqb[:], in_=acc[:], func=AF.Square), reads=[ka], writes=["sqb"])
        c.op("pe", lambda e: e.matmul(self.pn[:], lhsT=self.ones_b[:], rhs=self.sqb[:], start=True, stop=True),
             reads=["sqb", "ones"], writes=["pn"])
        c.op("act", lambda e: e.activation(out=self.rt[:], in_=self.pn[:], func=AF.Sqrt, scale=1.0 / HD, bias=self.eps_t[:, 0:1]),
             reads=["pn", "eps"], writes=["rt"])
        c.op("dve", lambda e: e.reciprocal(out=self.rt[:], in_=self.rt[:]), reads=["rt"], writes=["rt"])
        c.op("dve", lambda e: e.scalar_tensor_tensor(out=dst, in0=acc[:], scalar=gain, in1=self.rt[:], op0=ALU.mult, op1=ALU.mult),
             reads=[ka, "rt", "hgain"], writes=[kdst])

    def setup_tables(self):
        c = self.c
        self.cb_t = self.sb("cb", [128, 8], F32)
        self.negc = self.sb("negc", [128, 8], F32)
        c.dma("sp", "cst3", self.cb_t[:], self.cb[:, :], writes=["cb"])
        c.op("dve", lambda e: e.tensor_scalar(out=self.negc[:], in0=self.cb_t[:], scalar1=-1.0, scalar2=None, op0=ALU.mult),
             reads=["cb"], writes=["negc"])
        self.addmask_t = self.sb("addmask", [128, 8, 32], F32)
        c.dma("sp", "cst4", self.addmask_t[:], self.addmask[:, :, :], writes=["addmask"])
        self.esel_t = self.sb("esel", [32, 16, 128], BF16)
        c.dma("sp", "cst5", self.esel_t[:], self.esel[:, :, :], writes=["esel"])
        self.ov33_t = self.sb("ov33", [128, 33], BF16)
        c.dma("sp", "cst6", self.ov33_t[:], self.ov33[:, :], writes=["ov33"])
        self.selg_t = self.sb("selg", [24, 24, 128], F32)
        c.dma("sp", "cst7", self.selg_t[:], self.selg[:, :, :], writes=["selg"])
        self.posT_t = self.sb("posT", [128, self.L, 2, 32], F32)
        c.dma("sp", "cst8", self.posT_t[:], self.posT[:, :, :, :], writes=["posT"])
        self.posb = self.sb("posb", [128, self.L, 2, 32], BF16)
        c.op("dve", lambda e: e.tensor_copy(out=self.posb[:], in_=self.posT_t[:]), reads=["posT"], writes=["posb"])
        self.idf = self.sb("idf2", [128, 128], F32)
        c.op("pool", lambda e: e.memset(self.idf[:], 1.0), writes=["idf2"])
        c.op("pool", lambda e: e.affine_select(out=self.idf[:], in_=self.idf[:], pattern=[[-1, 128]], compare_op=ALU.is_equal,
                                               fill=0.0, base=0, channel_multiplier=1), reads=["idf2"], writes=["idf2"])
        with self.phase():
            eb = self.sb("ebl", [128, 2688], F32)
            for h in range(8):
                c.dma("sp", "ebl", eb[:], self.ebias[h, :, :], writes=["ebl"])
                c.op("act", lambda e: e.activation(out=eb[:], in_=eb[:], func=AF.Exp, bias=self.negc[:, h:h + 1]),
                     reads=["ebl", "negc"], writes=["ebl"])
                c.dma("sp", "ebs", self.EBD[h, :, :], eb[:], reads=["ebl"], writes=[("EBD", h)])

    def gelu_tanh(self, dst, kdst, src_ps, ksrc, bias, n):
        c = self.c
        x, x2 = self.gx, self.gx2
        c.op("act", lambda e: e.activation(out=x[:, 0:n], in_=src_ps, func=AF.Identity, bias=bias), reads=[ksrc, "cbias"], writes=["gx"])
        c.op("dve", lambda e: e.tensor_tensor(out=x2[:, 0:n], in0=x[:, 0:n], in1=x[:, 0:n], op=ALU.mult), reads=["gx"], writes=["gx2"])
        c.op("dve", lambda e: e.tensor_scalar(out=x2[:, 0:n], in0=x2[:, 0:n], scalar1=0.044715, scalar2=1.0, op0=ALU.mult, op1=ALU.add),
             reads=["gx2"], writes=["gx2"])
        c.op("dve", lambda e: e.tensor_tensor(out=x2[:, 0:n], in0=x2[:, 0:n], in1=x[:, 0:n], op=ALU.mult), reads=["gx", "gx2"], writes=["gx2"])
        c.op("act", lambda e: e.activation(out=x2[:, 0:n], in_=x2[:, 0:n], func=AF.Sigmoid, scale=1.5957691216), reads=["gx2"], writes=["gx2"])
        c.op("dve", lambda e: e.tensor_tensor(out=dst, in0=x2[:, 0:n], in1=x[:, 0:n], op=ALU.mult), reads=["gx", "gx2"], writes=[kdst])

    def compress(self, li, j, xT, kx, w1b, w2b, hid):
        c = self.c
        b5 = self.bk[5]
        k5 = ("bk", 5)
        c.dma("pool", "w1b", w1b[:], self.cmp_w1[li, j].rearrange("l d e -> d l e"), writes=["w1b"])
        c.dma("pool", "w2b", w2b[:], self.cmp_w2[li, j], writes=["w2b"])
        for l in range(32):
            c.op("pe", lambda e: e.matmul(b5[:, 0:127], lhsT=w1b[:, l, :], rhs=xT[:, l:l + 16 * 126 + 1:16], start=(l == 0), stop=(l == 31)),
                 reads=["w1b", kx], writes=[k5])
        for l in range(32):
            c.op("pe", lambda e: e.matmul(b5[:, 128:129], lhsT=w1b[:, l, :], rhs=self.posb[:, li, j, l:l + 1], start=False, stop=(l == 31)),
                 reads=["w1b", "posb"], writes=[k5])
        c.op("dve", lambda e: e.tensor_copy(out=self.cbias[:], in_=b5[:, 128:129]), reads=[k5], writes=["cbias"])
        c.op("pool", lambda e: e.memset(hid[:], 0.0), writes=["hid"])
        self.gelu_tanh(hid[:, 0:127], "hid", b5[:, 0:127], k5, self.cbias[:, 0:1], 127)

    def gate_bcast(self, krow, cols):
        c = self.c
        n = cols[1] - cols[0]
        c.op("pe", lambda e: e.matmul(self.bk[4][:, 0:n], lhsT=self.selg_t[:, krow, :], rhs=self.gsb[0:24, cols[0]:cols[1]], start=True, stop=True),
             reads=["selg", "gsb"], writes=[("bk", 4)])

    def attn_finish(self, h, br, G, oacc, first, guard=False):
        c = self.c
        w, tmp = self.wv, self.tmpv
        self.gate_bcast(h * 3 + br, (G * 512, (G + 1) * 512))
        if guard:
            c.op("dve", lambda e: e.tensor_scalar(out=w[:], in0=self.bk[3][:], scalar1=1e-30, scalar2=None, op0=ALU.max),
                 reads=[("bk", 3)], writes=["wv"])
            c.op("dve", lambda e: e.reciprocal(out=w[:], in_=w[:]), reads=["wv"], writes=["wv"])
        else:
            c.op("dve", lambda e: e.reciprocal(out=w[:], in_=self.bk[3][:]), reads=[("bk", 3)], writes=["wv"])
        c.op("dve", lambda e: e.tensor_tensor(out=w[:], in0=self.bk[4][:], in1=w[:], op=ALU.mult), reads=["wv", ("bk", 4)], writes=["wv"])
        dst = oacc[:, G * 512:(G + 1) * 512]
        ko = ("oacc", id(oacc), G)
        if first:
            c.op("dve", lambda e: e.tensor_tensor(out=dst, in0=self.bk[2][:], in1=w[:], op=ALU.mult), reads=["wv", ("bk", 2)], writes=[ko])
        else:
            c.op("dve", lambda e: e.tensor_tensor(out=tmp[:], in0=self.bk[2][:], in1=w[:], op=ALU.mult), reads=["wv", ("bk", 2)], writes=["tmpv"])
            c.op("pool", lambda e: e.tensor_tensor(out=dst, in0=dst, in1=tmp[:], op=ALU.add), reads=["tmpv", ko], writes=[ko])

    def banded_attn(self, h, G, qT, kq, kT, kk, V, kv, eb, keb, mode, nmT=None):
        c = self.c
        if mode == "win":
            kts = [kt for kt in range(max(0, 4 * G - 4), 4 * G + 4)]
        else:
            kts = list(range(0, 4 * G + 4))
        first = True
        for kt in kts:
            qlo = max(kt, 4 * G)
            qhi = min(kt + 4, 4 * G + 3) if mode == "win" else 4 * G + 3
            c0, c1 = (qlo - 4 * G) * 128, (qhi - 4 * G + 1) * 128
            n = c1 - c0
            i = self.nS % 2
            self.nS += 1
            S, kS = self.bk[i], ("bk", i)
            P, kP = self.P[i], ("P", i)
            use_mask = (mode == "sel" and G >= 2)
            c.op("pe", lambda e: e.matmul(S[:, 0:n], lhsT=kT[:, kt * 128:(kt + 1) * 128], rhs=qT[:, G * 512 + c0:G * 512 + c1],
                                          start=True, stop=not use_mask), reads=[kk, kq], writes=[kS])
            if use_mask:
                c.op("pe", lambda e: e.matmul(S[:, 0:n], lhsT=self.esel_t[:, kt, :], rhs=nmT[:, G * 512 + c0 - 1024:G * 512 + c1 - 1024],
                                              start=False, stop=True), reads=["esel", "nmT"], writes=[kS])
            c.op("act", lambda e: e.activation(out=P[:, 0:n], in_=S[:, 0:n], func=AF.Exp, scale=SCALE, bias=self.cb_t[:, h:h + 1]),
                 reads=[kS, "cb"], writes=[kP])
            for qb in range(qlo, qhi + 1):
                d = qb - kt
                if mode == "win":
                    ti = {0: 0, 1: 1, 4: 2}.get(d)
                    off = 0
                else:
                    ti = {0: 0, 1: 1}.get(d)
                    off = 384
                if ti is None:
                    continue
                sub = P[:, (qb - qlo) * 128:(qb - qlo + 1) * 128]
                c.op("pool", lambda e: e.tensor_tensor(out=sub, in0=sub, in1=eb[:, off + ti * 128:off + (ti + 1) * 128], op=ALU.mult),
                     reads=[kP, keb], writes=[kP])
            c.op("pe", lambda e: e.matmul(self.bk[2][:, c0:c1], lhsT=V[:, kt, :], rhs=P[:, 0:n], start=first, stop=(kt == kts[-1])),
                 reads=[kv, kP], writes=[("bk", 2)])
            c.op("pe", lambda e: e.matmul(self.bk[3][:, c0:c1], lhsT=self.ones_b[:], rhs=P[:, 0:n], start=first, stop=(kt == kts[-1])),
                 reads=["ones", kP], writes=[("bk", 3)])
            first = False

    def phase_nsa(self, li, s):
        c = self.c
        sb = self.sb
        self.gsb = sb("gsb", [32, T], F32)
        self.wv = sb("wv", [128, 512], F32)
        self.tmpv = sb("tmpv", [128, 512], F32)
        self.P = [sb("P", [128, 512], BF16) for _ in range(2)]
        self.gx = sb("gx", [128, 128], F32)
        self.gx2 = sb("gx2", [128, 128], F32)
        self.cbias = sb("cbias", [128, 1], F32)
        self.nS = 0
        w1b = sb("w1b", [128, 32, 128], BF16)
        w2b = sb("w2b", [128, 128], BF16)
        xk = sb("xk", [128, T], BF16)
        xv = sb("xv", [128, T], BF16)
        hid = sb("hid", [128, 128], BF16)
        kcmpT = sb("kcmpT", [128, 128], BF16)
        vcmp = sb("vcmp", [128, 1, 128], BF16)
        ksT = sb("ksT", [128, T], BF16)
        kwT = sb("kwT", [128, T], BF16)
        vs = sb("vs", [128, 16, 128], BF16)
        vw = sb("vw", [128, 16, 128], BF16)
        qT = [sb("qT", [128, T], BF16) for _ in range(4)]
        oacc = [sb("oacc", [128, T], F32) for _ in range(4)]
        obf = sb("obf", [128, T], BF16)
        eb = sb("eb", [128, 2688], F32)
        impacc = sb("impacc", [128, 8, 32], F32)
        imps = sb("imps", [128, 40], F32)
        sc = sb("sc", [128, 32], F32)
        sc2 = sb("sc2", [128, 32], F32)
        m8 = sb("m8", [128, 16], F32)
        nmT = sb("nmT", [32, T // 2], BF16)
        b5, k5 = self.bk[5], ("bk", 5)
        c.dma("sp", "gsb", self.gsb[0:24, :], self.GS[:, :], reads=["GS"], writes=["gsb"])
        for g in range(2):
            c.dma("sp", "xk", xk[:], self.FM[8 + g, :, :], reads=[("FM", 8 + g)], writes=["xk"])
            c.dma("sp", "xv", xv[:], self.FM[10 + g, :, :], reads=[("FM", 10 + g)], writes=["xv"])
            c.dma("sp", "ksT", ksT[:], self.FM[12 + g, :, :], reads=[("FM", 12 + g)], writes=["ksT"])
            c.dma("sp", "kwT", kwT[:], self.FM[14 + g, :, :], reads=[("FM", 14 + g)], writes=["kwT"])
            tmv = self.TM.rearrange("(kt p) n -> p kt n", p=128)
            c.dma("sp", "vs", vs[:], tmv[:, :, g * 128:(g + 1) * 128], reads=[("TM", 0, t) for t in range(16)], writes=["vs"])
            c.dma("sp", "vw", vw[:], tmv[:, :, 256 + g * 128:256 + (g + 1) * 128], reads=[("TM", 256, t) for t in range(16)], writes=["vw"])
            self.compress(li, 0, xk, "xk", w1b, w2b, hid)
            c.op("pe", lambda e: e.matmul(b5[:, 256:384], lhsT=w2b[:], rhs=hid[:], start=True, stop=True), reads=["w2b", "hid"], writes=[k5])
            c.op("act", lambda e: e.activation(out=self.sqb[:, 0:128], in_=b5[:, 256:384], func=AF.Square), reads=[k5], writes=["sqb"])
            c.op("pe", lambda e: e.matmul(self.bk[4][:, 0:128], lhsT=self.ones_b[:], rhs=self.sqb[:, 0:128], start=True, stop=True),
                 reads=["sqb", "ones"], writes=[("bk", 4)])
            c.op("act", lambda e: e.activation(out=self.rt[:, 0:128], in_=self.bk[4][:, 0:128], func=AF.Sqrt, scale=1.0 / HD, bias=self.eps_t[:, 0:1]),
                 reads=[("bk", 4), "eps"], writes=["rt"])
            c.op("dve", lambda e: e.reciprocal(out=self.rt[:, 0:128], in_=self.rt[:, 0:128]), reads=["rt"], writes=["rt"])
            c.op("dve", lambda e: e.scalar_tensor_tensor(out=kcmpT[:], in0=b5[:, 256:384], scalar=self.hgain_t[:, li, 1:2], in1=self.rt[:, 0:128],
                                                         op0=ALU.mult, op1=ALU.mult), reads=[k5, "rt", "hgain"], writes=["kcmpT"])
            self.compress(li, 1, xv, "xv", w1b, w2b, hid)
            c.op("pe", lambda e: e.matmul(b5[:, 256:384], lhsT=hid[:], rhs=w2b[:], start=True, stop=True), reads=["w2b", "hid"], writes=[k5])
            c.op("dve", lambda e: e.tensor_copy(out=vcmp[:, 0, :], in_=b5[:, 256:384]), reads=[k5], writes=["vcmp"])
            c.op("pool", lambda e: e.memset(impacc[:], 0.0), writes=["impacc"])
            for r in range(4):
                h = 4 * g + r
                c.dma("sp", f"qT{r}", qT[r][:], self.FM[h, :, :], reads=[("FM", h)], writes=[("qT", r)])
                c.dma("sp", "eb", eb[:], self.EBD[h, :, :], reads=[("EBD", h)], writes=["eb"])
                for G in range(4):
                    i = self.nS % 2
                    self.nS += 1
                    S, kS = self.bk[i], ("bk", i)
                    P, kP = self.P[i], ("P", i)
                    c.op("pe", lambda e: e.matmul(S[:], lhsT=kcmpT[:], rhs=qT[r][:, G * 512:(G + 1) * 512], start=True, stop=True),
                         reads=["kcmpT", ("qT", r)], writes=[kS])
                    c.op("act", lambda e: e.activation(out=self.tmpv[:], in_=S[:], func=AF.Exp, scale=SCALE, bias=self.cb_t[:, h:h + 1]),
                         reads=[kS, "cb"], writes=["tmpv"])
                    c.op("dve", lambda e: e.tensor_tensor(out=P[:], in0=self.tmpv[:], in1=eb[:, 640 + G * 512:640 + (G + 1) * 512], op=ALU.mult),
                         reads=["tmpv", "eb"], writes=[kP])
                    c.op("pe", lambda e: e.matmul(self.bk[2][:], lhsT=vcmp[:, 0, :], rhs=P[:], start=True, stop=True), reads=["vcmp", kP], writes=[("bk", 2)])
                    c.op("pe", lambda e: e.matmul(self.bk[3][:], lhsT=self.ones_b[:], rhs=P[:], start=True, stop=True), reads=["ones", kP], writes=[("bk", 3)])
                    if G >= 2:
                        for q4 in range(4):
                            tt = G * 4 + q4 - 8
                            c.op("pe", lambda e: e.matmul(b5[:, q4 * 64:q4 * 64 + 33], lhsT=P[:, q4 * 128:(q4 + 1) * 128], rhs=self.ov33_t[:],
                                                          start=True, stop=True), reads=["ov33", kP], writes=[k5])
                            c.op("dve", lambda e: e.reciprocal(out=imps[:, 32:33], in_=b5[:, q4 * 64 + 32:q4 * 64 + 33]), reads=[k5], writes=["imps"])
                            c.op("dve", lambda e: e.tensor_scalar(out=imps[:, 0:32], in0=b5[:, q4 * 64:q4 * 64 + 32], scalar1=imps[:, 32:33], scalar2=None,
                                                                  op0=ALU.mult), reads=[k5, "imps"], writes=["imps2"])
                            c.op("dve", lambda e: e.tensor_tensor(out=impacc[:, tt, :], in0=impacc[:, tt, :], in1=imps[:, 0:32], op=ALU.add),
                                 reads=["imps2", "impacc"], writes=["impacc"])
                    self.attn_finish(h, 0, G, oacc[r], True, guard=True)
            for tt in range(8):
                c.op("dve", lambda e: e.tensor_tensor(out=sc[:], in0=impacc[:, tt, :], in1=self.addmask_t[:, tt, :], op=ALU.add),
                     reads=["impacc", "addmask"], writes=["sc"])
                c.op("dve", lambda e: e.max(out=m8[:, 0:8], in_=sc[:]), reads=["sc"], writes=["m8"])
                c.op("dve", lambda e: e.match_replace(out=sc2[:], in_to_replace=m8[:, 0:8], in_values=sc[:], imm_value=-3e38),
                     reads=["sc", "m8"], writes=["sc2"])
                c.op("dve", lambda e: e.max(out=m8[:, 8:16], in_=sc2[:]), reads=["sc2"], writes=["m8b"])
                c.op("dve", lambda e: e.tensor_scalar(out=sc2[:], in0=sc[:], scalar1=m8[:, 15:16], scalar2=None, op0=ALU.is_ge),
                     reads=["sc", "m8b"], writes=["sc2"])
                c.op("dve", lambda e: e.tensor_scalar(out=sc2[:], in0=sc2[:], scalar1=30000.0, scalar2=-30000.0, op0=ALU.mult, op1=ALU.add),
                     reads=["sc2"], writes=["sc2"])
                c.op("pe", lambda e: e.transpose(out=b5[0:32, 384:512], in_=sc2[:], identity=self.idf[:]), reads=["sc2", "idf2"], writes=[k5])
                c.op("act", lambda e: e.copy(out=nmT[:, tt * 128:(tt + 1) * 128], in_=b5[0:32, 384:512]), reads=[k5], writes=["nmT"])
            for r in range(4):
                h = 4 * g + r
                c.dma("sp", "eb", eb[:], self.EBD[h, :, :], reads=[("EBD", h)], writes=["eb"])
                for G in range(4):
                    self.banded_attn(h, G, qT[r], ("qT", r), ksT, "ksT", vs, "vs", eb, "eb", "sel", nmT)
                    self.attn_finish(h, 1, G, oacc[r], False)
                    self.banded_attn(h, G, qT[r], ("qT", r), kwT, "kwT", vw, "vw", eb, "eb", "win")
                    self.attn_finish(h, 2, G, oacc[r], False)
                c.op("act", lambda e: e.copy(out=obf[:], in_=oacc[r][:]), reads=[("oacc", id(oacc[r]), G) for G in range(4)], writes=["obf"])
                c.dma("pool", "obf", self.OT[h, :, :], obf[:], reads=["obf"], writes=[("OT", h)])

    def setup_tables2(self):
        c = self.c
        L = self.L
        self.trineg = self.sb("trineg", [128, 128], BF16)
        self.mstrict = self.sb("mstrict", [128, 128], BF16)
        self.masku = self.sb("masku", [128, 64], F32)
        self.rst = self.sb("rst", [128, T], F32)
        tmp = self.sb("tmpc", [128, 128], F32)
        c.op("pool", lambda e: e.memset(tmp[:], -1.0), writes=["tmpc"])
        c.op("pool", lambda e: e.affine_select(out=tmp[:], in_=tmp[:], pattern=[[-1, 128]], compare_op=ALU.is_ge, fill=0.0, base=0, channel_multiplier=1),
             reads=["tmpc"], writes=["tmpc"])
        c.op("pool", lambda e: e.tensor_copy(out=self.trineg[:], in_=tmp[:]), reads=["tmpc"], writes=["trineg"])
        c.op("pool", lambda e: e.memset(tmp[:], 1.0), reads=["tmpc"], writes=["tmpc"])
        c.op("pool", lambda e: e.affine_select(out=tmp[:], in_=tmp[:], pattern=[[1, 128]], compare_op=ALU.is_gt, fill=0.0, base=0, channel_multiplier=-1),
             reads=["tmpc"], writes=["tmpc"])
        c.op("pool", lambda e: e.tensor_copy(out=self.mstrict[:], in_=tmp[:]), reads=["tmpc"], writes=["mstrict"])
        c.op("pool", lambda e: e.memset(self.masku[:], 1.0), writes=["masku"])
        for p0 in (0, 64):
            c.op("pool", lambda e: e.affine_select(out=self.masku[p0:p0 + 64, :], in_=self.masku[p0:p0 + 64, :], pattern=[[1, 64]], compare_op=ALU.is_ge,
                                                   fill=0.0, base=0, channel_multiplier=-1), reads=["masku"], writes=["masku"])
        c.op("pool", lambda e: e.memset(self.rst[:], 1.0), writes=["rst"])
        c.op("pool", lambda e: e.memset(self.rst[:].rearrange("p (c t) -> p c t", t=64)[:, :, 0:1], 0.0), reads=["rst"], writes=["rst"])
        hl = self.sb("hl", [128, 4, 8], F32)
        lc = self.sb("lc", [128, L, 4], F32)
        self.lb = self.sb("lb", [128, L, 8], F32)
        self.oml = self.sb("oml", [128, L, 8], F32)
        ssum = self.sb("ssum", [128, 8], F32)
        c.dma("sp", "cst9", hl[:], self.hlb[:, :, :], writes=["hl"])
        c.dma("sp", "cst10", lc[:], self.lcum[:, :, :], writes=["lc"])
        c.op("act", lambda e: e.activation(out=hl[:], in_=hl[:], func=AF.Exp), reads=["hl"], writes=["hl"])
        c.op("dve", lambda e: e.tensor_tensor(out=ssum[:], in0=hl[:, 0, :], in1=hl[:, 1, :], op=ALU.add), reads=["hl"], writes=["ssum"])
        c.op("dve", lambda e: e.tensor_tensor(out=ssum[:], in0=ssum[:], in1=hl[:, 2, :], op=ALU.add), reads=["hl", "ssum"], writes=["ssum"])
        c.op("dve", lambda e: e.tensor_tensor(out=ssum[:], in0=ssum[:], in1=hl[:, 3, :], op=ALU.add), reads=["hl", "ssum"], writes=["ssum"])
        c.op("dve", lambda e: e.reciprocal(out=ssum[:], in_=ssum[:]), reads=["ssum"], writes=["ssum"])
        for l4 in range(4):
            c.op("dve", lambda e: e.tensor_tensor(out=hl[:, l4, :], in0=hl[:, l4, :], in1=ssum[:], op=ALU.mult), reads=["hl", "ssum"], writes=["hl"])
        for li in range(L):
            c.op("dve", lambda e: e.tensor_scalar(out=self.lb[:, li, :], in0=hl[:, 0, :], scalar1=lc[:, li, 0:1], scalar2=None, op0=ALU.mult),
                 reads=["hl", "lc"], writes=["lb"])
            for l4 in range(1, 4):
                c.op("dve", lambda e: e.scalar_tensor_tensor(out=self.lb[:, li, :], in0=hl[:, l4, :], scalar=lc[:, li, l4:l4 + 1], in1=self.lb[:, li, :],
                                                             op0=ALU.mult, op1=ALU.add), reads=["hl", "lc", "lb"], writes=["lb"])
        c.op("dve", lambda e: e.tensor_scalar(out=self.oml[:], in0=self.lb[:], scalar1=-1.0, scalar2=1.0, op0=ALU.mult, op1=ALU.add),
             reads=["lb"], writes=["oml"])
        self.gn_t = self.sb("gn", [128, L, 128], F32)
        c.dma("sp", "cst11", self.gn_t[:], self.gn[:, :, :], writes=["gn"])
        self.gffn = self.sb("gffn", [128, L, 16], F32)
        c.dma("sp", "cst12", self.gffn[:], self.norm_ffn[:, :, :], writes=["gffn"])
        self.convw_t = self.sb("convw", [128, L, 3, 88], F32)
        c.dma("sp", "cst13", self.convw_t[:], self.convw[:, :, :, :], writes=["convw"])
        self.convb_t = self.sb("convb", [128, L, 88], F32)
        c.dma("sp", "cst14", self.convb_t[:], self.convb[:, :, :], writes=["convb"])

    def phase_sb(self, li, s):
        c = self.c
        sb = self.sb
        qT = sb("sqT", [128, T], BF16)
        kT = sb("skT", [128, T], BF16)
        V = sb("sV", [128, 16, 128], BF16)
        R = sb("sR", [128, 512], F32)
        e32 = sb("se32", [128, 512], F32)
        arg = sb("sarg", [128, 512], F32)
        spb = [sb("spb", [128, 512], BF16) for _ in range(2)]
        A = [sb("sA", [128, 512], BF16) for _ in range(2)]
        obf = sb("sobf", [128, T], BF16)
        tmv = self.TM.rearrange("(kt p) n -> p kt n", p=128)
        n_it = 0
        for h in range(8):
            c.dma("sp", "sqT", qT[:], self.FM[16 + h, :, :], reads=[("FM", 16 + h)], writes=["sqT"])
            c.dma("sp", "skT", kT[:], self.FM[24 + h, :, :], reads=[("FM", 24 + h)], writes=["skT"])
            c.dma("sp", "sV", V[:], tmv[:, :, 512 + h * 128:512 + (h + 1) * 128],
                  reads=[("TM", 512 + (h // 4) * 512, t) for t in range(16)], writes=["sV"])
            for G in range(4):
                c.op("pool", lambda e: e.memset(R[:], 0.0), writes=["sR"])
                kts = list(range(4 * G + 3, -1, -1))
                for kt in kts:
                    qlo = max(kt, 4 * G)
                    c0, c1 = (qlo - 4 * G) * 128, 512
                    n = c1 - c0
                    i = n_it % 2
                    n_it += 1
                    S, kS = self.bk[i], ("bk", i)
                    sp_, ksp = spb[i], ("spb", i)
                    A_, kA = A[i], ("sA", i)
                    diag = kt >= 4 * G
                    c.op("pe", lambda e: e.matmul(S[:, 0:n], lhsT=kT[:, kt * 128:(kt + 1) * 128], rhs=qT[:, G * 512 + c0:G * 512 + c1], start=True, stop=False),
                         reads=["skT", "sqT"], writes=[kS])
                    c.op("act", lambda e: e.activation(out=e32[:, 0:n], in_=S[:, 0:n], func=AF.Exp), reads=[kS], writes=["se32"])
                    c.op("act", lambda e: e.activation(out=sp_[:, 0:n], in_=e32[:, 0:n], func=AF.Ln, bias=1.0), reads=["se32"], writes=[ksp])
                    if diag:
                        c.op("pool", lambda e: e.tensor_tensor(out=sp_[:, 0:128], in0=sp_[:, 0:128], in1=self.mstrict[:], op=ALU.mult),
                             reads=[ksp, "mstrict"], writes=[ksp])
                    c.op("pe", lambda e: e.matmul(S[:, 0:n], lhsT=self.trineg[:], rhs=sp_[:, 0:n], start=False, stop=True),
                         reads=["trineg", ksp], writes=[kS])
                    c.op("pe", lambda e: e.matmul(self.bk[3][:, 0:n], lhsT=self.ones_b[:], rhs=sp_[:, 0:n], start=True, stop=True),
                         reads=["ones", ksp], writes=[("bk", 3)])
                    c.op("dve", lambda e: e.tensor_tensor(out=arg[:, 0:n], in0=S[:, 0:n], in1=R[:, c0:c1], op=ALU.subtract),
                         reads=[kS, "sR"], writes=["sarg"])
                    c.op("act", lambda e: e.activation(out=A_[:, 0:n], in_=arg[:, 0:n], func=AF.Exp), reads=["sarg"], writes=[kA])
                    if diag:
                        c.op("pool", lambda e: e.tensor_tensor(out=A_[:, 0:128], in0=A_[:, 0:128], in1=self.mstrict[:], op=ALU.mult),
                             reads=[kA, "mstrict"], writes=[kA])
                    c.op("dve", lambda e: e.tensor_tensor(out=R[:, c0:c1], in0=R[:, c0:c1], in1=self.bk[3][:, 0:n], op=ALU.add),
                         reads=["sR", ("bk", 3)], writes=["sR"])
                    c.op("pe", lambda e: e.matmul(self.bk[2][:, c0:c1], lhsT=V[:, kt, :], rhs=A_[:, 0:n], start=(kt == kts[0]), stop=(kt == kts[-1])),
                         reads=["sV", kA], writes=[("bk", 2)])
                c.op("act", lambda e: e.copy(out=obf[:, G * 512:(G + 1) * 512], in_=self.bk[2][:]), reads=[("bk", 2)], writes=["sobf"])
            c.dma("pool", "sobf", self.OT[8 + h, :, :], obf[:], reads=["sobf"], writes=[("OT", 8 + h)])

    def phase_hg(self, li, s):
        c = self.c
        sb = self.sb
        qf = sb("hq", [128, T], BF16)
        F = sb("hF", [128, T], F32)
        LF = sb("hLF", [128, T], F32)
        Kk = sb("hK", [128, T], F32)
        b = sb("hb", [128, T], F32)
        d1 = sb("hd1", [128, T], F32)
        E = sb("hE", [128, T], F32)
        qp = sb("hqp", [128, T], BF16)
        kp = sb("hkp", [128, T], BF16)
        kd = sb("hkd", [128, T], BF16)
        kdT = sb("hkdT", [128, 16, 128], BF16)
        V = sb("hV", [128, 16, 128], BF16)
        Gs = sb("hGs", [128, 16, 128], BF16)
        emid = sb("hemid", [128, 32], F32)
        elast = sb("helast", [128, 32], F32)
        S = sb("hS", [128, 128], F32)
        Sb = sb("hSb", [128, 128], BF16)
        am = sb("ham", [128, 64], BF16)
        ss = sb("hss", [128, 4], F32)
        junk = sb("hjunk", [128, 128], F32)
        y = sb("hy", [128, 128], F32)
        yb = sb("hyb", [128, 128], BF16)
        ohT = sb("hohT", [128, T], BF16)
        tmv = self.TM.rearrange("(kt p) n -> p kt n", p=128)
        c3 = lambda a: a[:].rearrange("p (c t) -> p c t", t=64)
        for h in range(8):
            c.dma("sp", "hq", qf[:], self.FM[32 + h, :, :], reads=[("FM", 32 + h)], writes=["hq"])
            c.dma("sp", "hF", F[:], self.HF[h, :, :], reads=[("HF", h)], writes=["hF"])
            c.dma("sp", "hV", V[:], tmv[:, :, 1536 + h * 128:1536 + (h + 1) * 128],
                  reads=[("TM", 1536 + (h // 4) * 512, t) for t in range(16)], writes=["hV"])
            c.dma("sp", "hGs", Gs[:], tmv[:, :, 2560 + h * 128:2560 + (h + 1) * 128],
                  reads=[("TM", 2560 + (h // 4) * 512, t) for t in range(16)], writes=["hGs"])
            c.op("act", lambda e: e.activation(out=F[:], in_=F[:], func=AF.Sigmoid), reads=["hF"], writes=["hF"])
            c.op("dve", lambda e: e.tensor_scalar(out=F[:], in0=F[:], scalar1=self.oml[:, li, h:h + 1], scalar2=self.lb[:, li, h:h + 1],
                                                  op0=ALU.mult, op1=ALU.add), reads=["hF", "oml", "lb"], writes=["hF"])
            c.op("act", lambda e: e.activation(out=LF[:], in_=F[:], func=AF.Ln), reads=["hF"], writes=["hLF"])
            c.op("dve", lambda e: e.tensor_scalar(out=Kk[:], in0=F[:], scalar1=-1.0, scalar2=1.0, op0=ALU.mult, op1=ALU.add), reads=["hF"], writes=["hK"])
            c.op("dve", lambda e: e.tensor_tensor_scan(out=b[:], data0=self.rst[:], data1=LF[:], initial=0.0, op0=ALU.mult, op1=ALU.add),
                 reads=["hLF", "rst"], writes=["hb"])
            bmid = c3(b)[:, :, 31:32]
            blast = c3(b)[:, :, 63:64]
            c.op("dve", lambda e: e.tensor_tensor(out=c3(d1), in0=c3(b), in1=bmid.broadcast_to([128, 32, 64]), op=ALU.subtract), reads=["hb"], writes=["hd1"])
            c.op("act", lambda e: e.activation(out=E[:], in_=d1[:], func=AF.Exp), reads=["hd1"], writes=["hE"])
            c.op("dve", lambda e: e.tensor_tensor(out=qp[:], in0=qf[:], in1=E[:], op=ALU.mult), reads=["hq", "hE"], writes=["hqp"])
            c.op("act", lambda e: e.activation(out=E[:], in_=d1[:], func=AF.Exp, scale=-1.0), reads=["hd1", "hqp"], writes=["hE"])
            c.op("dve", lambda e: e.tensor_tensor(out=kp[:], in0=Kk[:], in1=E[:], op=ALU.mult), reads=["hK", "hE"], writes=["hkp"])
            c.op("dve", lambda e: e.tensor_tensor(out=c3(d1), in0=blast.broadcast_to([128, 32, 64]), in1=c3(b), op=ALU.subtract), reads=["hb", "hkp"], writes=["hd1"])
            c.op("act", lambda e: e.activation(out=E[:], in_=d1[:], func=AF.Exp), reads=["hd1", "hkp"], writes=["hE"])
            c.op("dve", lambda e: e.tensor_tensor(out=kd[:], in0=Kk[:], in1=E[:], op=ALU.mult), reads=["hK", "hE"], writes=["hkd"])
            c.op("act", lambda e: e.activation(out=emid[:].rearrange("p (c o) -> p c o", o=1), in_=bmid, func=AF.Exp), reads=["hb"], writes=["hemid"])
            c.op("act", lambda e: e.activation(out=elast[:].rearrange("p (c o) -> p c o", o=1), in_=blast, func=AF.Exp), reads=["hb"], writes=["helast"])
            for tt in range(16):
                pt = self.ptr[tt % 2]
                kp_ = ("ptr", tt % 2)
                c.op("pe", lambda e: e.transpose(out=pt[:, 0, :], in_=kd[:, tt * 128:(tt + 1) * 128], identity=self.ident[:]), reads=["hkd", "ident"], writes=[kp_])
                c.op("act", lambda e: e.copy(out=kdT[:, tt, :], in_=pt[:, 0, :]), reads=[kp_], writes=["hkdT"])
            c.op("pool", lambda e: e.memset(S[:], 0.0), writes=["hS"])
            for ch in range(32):
                tt, half = ch // 2, ch % 2
                p0 = half * 64
                cs = slice(ch * 64, (ch + 1) * 64)
                aps, ka = (self.bk[0], ("bk", 0)) if ch % 2 == 0 else (self.bk[5], ("bk", 5))
                O, kO = (self.bk[1], ("bk", 1)) if tt % 2 == 0 else (self.bk[4], ("bk", 4))
                c.op("pe", lambda e: e.matmul(aps[p0:p0 + 64, 0:64], lhsT=kp[:, cs], rhs=qp[:, cs], start=True, stop=True), reads=["hkp", "hqp"], writes=[ka])
                c.op("dve", lambda e: e.tensor_tensor(out=am[p0:p0 + 64, :], in0=aps[p0:p0 + 64, 0:64], in1=self.masku[p0:p0 + 64, :], op=ALU.mult),
                     reads=[ka, "masku"], writes=[("ham", half)])
                c.op("act", lambda e: e.activation(out=Sb[:], in_=S[:], func=AF.Copy, scale=emid[:, ch:ch + 1]), reads=["hS", "hemid"], writes=["hSb"])
                c.op("pe", lambda e: e.matmul(O[p0:p0 + 64, 0:128], lhsT=am[p0:p0 + 64, :], rhs=V[p0:p0 + 64, tt, :], start=True, stop=False),
                     reads=[("ham", half), "hV"], writes=[kO])
                c.op("pe", lambda e: e.matmul(O[p0:p0 + 64, 0:128], lhsT=qp[:, cs], rhs=Sb[:], start=False, stop=True), reads=["hqp", "hSb"], writes=[kO])
                c.op("pe", lambda e: e.matmul(self.bk[3][:, 0:128], lhsT=kdT[p0:p0 + 64, tt, :], rhs=V[p0:p0 + 64, tt, :], start=True, stop=True),
                     reads=["hkdT", "hV"], writes=[("bk", 3)])
                c.op("dve", lambda e: e.scalar_tensor_tensor(out=S[:], in0=S[:], scalar=elast[:, ch:ch + 1], in1=self.bk[3][:, 0:128], op0=ALU.mult, op1=ALU.add),
                     reads=["hS", "helast", ("bk", 3)], writes=["hS"])
                if half == 1:
                    c.op("act", lambda e: e.activation(out=junk[:], in_=O[:, 0:128], func=AF.Square, accum_out=ss[:, 0:1]), reads=[kO], writes=["hjunk", "hss"])
                    c.op("act", lambda e: e.activation(out=ss[:, 1:2], in_=ss[:, 0:1], func=AF.Sqrt, scale=1.0 / HD, bias=self.eps_t[:, 0:1]),
                         reads=["hss", "eps"], writes=["hss1"])
                    c.op("dve", lambda e: e.reciprocal(out=ss[:, 2:3], in_=ss[:, 1:2]), reads=["hss1"], writes=["hss2"])
                    c.op("dve", lambda e: e.scalar_tensor_tensor(out=y[:], in0=O[:, 0:128], scalar=ss[:, 2:3], in1=self.gn_t[:, li, :], op0=ALU.mult, op1=ALU.mult),
                         reads=[kO, "hss2", "gn"], writes=["hy"])
                    c.op("dve", lambda e: e.tensor_tensor(out=yb[:], in0=y[:], in1=Gs[:, tt, :], op=ALU.mult), reads=["hy", "hGs"], writes=["hyb"])
                    pt = self.ptr[tt % 2]
                    kp_ = ("ptr", tt % 2)
                    c.op("pe", lambda e: e.transpose(out=pt[:, 1, :], in_=yb[:], identity=self.ident[:]), reads=["hyb", "ident"], writes=[kp_])
                    c.op("act", lambda e: e.copy(out=ohT[:, tt * 128:(tt + 1) * 128], in_=pt[:, 1, :]), reads=[kp_], writes=["hohT"])
            c.dma("pool", "hohT", self.OT[16 + h, :, :], ohT[:], reads=["hohT"], writes=[("OT", 16 + h)])

    def phase_d(self, li, s, tb, xsrc, xsname, xdst, xdname, carry):
        c = self.c
        sb = self.sb
        t0 = tb * 512
        xblk = sb("xblk", [128, 4, D], F32)
        h2T = sb("h2T", [128, 16, 512], BF16)
        kxb = "xblk"
        c.dma("sp", "xblk", xblk[:], xsrc[s, t0:t0 + 512, :].rearrange("(ts p) n -> p ts n", p=128), reads=[("X", xsname, s, tb)], writes=[kxb])
        nacc = [0]

        def next_acc():
            i = nacc[0] % 2
            nacc[0] += 1
            return self.bk[i], ("bk", i)
        with self.phase():
            hb = sb("hb", [128, 16, 512], BF16)
            ob = sb("ob", [128, 24, 512], BF16)
            sg = [sb("sg", [128, 4, 512], BF16) for _ in range(3)]
            mT = sb("mT", [128, 16, 512], BF16)
            macc = sb("macc", [128, 4, 512], F32)
            tmp = sb("mtmp", [128, 512], F32)
            wt = [sb("wtd", [128, 16, 512], BF16) for _ in range(2)]
            self.xn = sb("xn", [128, D], BF16)
            self.sqj = sb("sqj", [128, D], BF16)
            self.ss = sb("ss", [128, 4], F32)
            nl = [0]

            def load(view, n_k, key):
                i = nl[0] % 2
                nl[0] += 1
                c.dma("sp", f"wtd{i}", wt[i][:, 0:n_k, :], view, reads=[key], writes=[("wtd", i)])
                return wt[i], ("wtd", i)
            c.dma("sp", "hb", hb[:], self.HT[:, :, t0:t0 + 512], reads=["HT"], writes=["hb"])
            c.dma("sp", "ob", ob[:], self.OT[:, :, t0:t0 + 512].rearrange("c p t -> p c t"), reads=[("OT", k) for k in range(24)], writes=["ob"])
            win = self.win_b[li].rearrange("(kc p) n -> p kc n", p=128)
            for dg in range(4):
                for br in range(3):
                    col = C_MG + br * 2048 + dg * 512
                    w, kw = load(win[:, :, col:col + 512], 16, ("win_b", li))
                    for j in range(4):
                        acc, ka = next_acc()
                        for kc in range(16):
                            c.op("pe", lambda e: e.matmul(acc[:], lhsT=w[:, kc, j * 128:(j + 1) * 128], rhs=hb[:, kc, :], start=(kc == 0), stop=(kc == 15)),
                                 reads=[kw, "hb"], writes=[ka])
                        c.op("act", lambda e: e.activation(out=sg[br][:, j, :], in_=acc[:], func=AF.Sigmoid), reads=[ka], writes=[("sg", br, j)])
                for br in range(3):
                    wb = self.wbr_b[li, br * 1024:(br + 1) * 1024, :].rearrange("(kc p) n -> p kc n", p=128)
                    w, kw = load(wb[:, :, dg * 512:(dg + 1) * 512], 8, ("wbr_b", li))
                    for j in range(4):
                        acc, ka = next_acc()
                        for c8 in range(8):
                            c.op("pe", lambda e: e.matmul(acc[:], lhsT=w[:, c8, j * 128:(j + 1) * 128], rhs=ob[:, br * 8 + c8, :], start=(c8 == 0), stop=(c8 == 7)),
                                 reads=[kw, "ob"], writes=[ka])
                        if br == 0:
                            c.op("dve", lambda e: e.tensor_tensor(out=macc[:, j, :], in0=acc[:], in1=sg[0][:, j, :], op=ALU.mult),
                                 reads=[ka, ("sg", 0, j)], writes=[("macc", j)])
                        else:
                            c.op("dve", lambda e: e.tensor_tensor(out=tmp[:], in0=acc[:], in1=sg[br][:, j, :], op=ALU.mult),
                                 reads=[ka, ("sg", br, j)], writes=["mtmp"])
                            c.op("pool", lambda e: e.tensor_tensor(out=macc[:, j, :], in0=macc[:, j, :], in1=tmp[:], op=ALU.add),
                                 reads=["mtmp", ("macc", j)], writes=[("macc", j)])
                        if br == 2:
                            c.op("act", lambda e: e.copy(out=mT[:, dg * 4 + j, :], in_=macc[:, j, :]), reads=[("macc", j)], writes=["mT"])
            wo = self.wout_b[li].rearrange("(kc p) n -> p kc n", p=128)
            for ng in range(4):
                w, kw = load(wo[:, :, ng * 512:(ng + 1) * 512], 16, ("wout_b", li))
                for ts in range(4):
                    acc, ka = next_acc()
                    for kc in range(16):
                        c.op("pe", lambda e: e.matmul(acc[:], lhsT=mT[:, kc, ts * 128:(ts + 1) * 128], rhs=w[:, kc, :], start=(kc == 0), stop=(kc == 15)),
                             reads=[kw, "mT"], writes=[ka])
                    xs = xblk[:, ts, ng * 512:(ng + 1) * 512]
                    c.op("dve", lambda e: e.tensor_tensor(out=xs, in0=acc[:], in1=xs, op=ALU.add), reads=[ka, kxb], writes=[kxb])
            for ts in range(4):
                self.norm_tile(xblk[:, ts, :], kxb, self.gffn[:, li, :], h2T, ts * 128, "h2T")
        with self.phase():
            aT = sb("aT", [128, 44, 512], BF16)
            wt = [sb("wtf", [128, 16, 512], BF16) for _ in range(3)]
            ub = [sb("ub", [128, 514], F32) for _ in range(2)]
            cc = [sb("cc", [128, 512], F32) for _ in range(2)]
            nl = [0]

            def load3(view, n_k, key):
                i = nl[0] % 3
                nl[0] += 1
                c.dma("sp", f"wtf{i}", wt[i][:, 0:n_k, :], view, reads=[key], writes=[("wtf", i)])
                return wt[i], ("wtf", i)
            wu = self.wup_b[li].rearrange("(kc p) n -> p kc n", p=128)
            for i4 in range(11):
                wg_, kwg = load3(wu[:, :, i4 * 512:(i4 + 1) * 512], 16, ("wup_b", li))
                wv_, kwv = load3(wu[:, :, DFF + i4 * 512:DFF + (i4 + 1) * 512], 16, ("wup_b", li))
                for j in range(4):
                    i = i4 * 4 + j
                    for side, (w, kw) in enumerate(((wg_, kwg), (wv_, kwv))):
                        ch = i + 44 * side
                        acc, ka = self.bk[2 + side], ("bk", 2 + side)
                        for kc in range(16):
                            c.op("pe", lambda e: e.matmul(acc[:], lhsT=w[:, kc, j * 128:(j + 1) * 128], rhs=h2T[:, kc, :], start=(kc == 0), stop=(kc == 15)),
                                 reads=[kw, "h2T"], writes=[ka])
                        u, ku = ub[side], ("ub", side)
                        cv, kc_ = cc[side], ("cc", side)
                        c.op("pool", lambda e: e.tensor_copy(out=u[:, 0:2], in_=carry[:, ch, :]), reads=[("carry", ch)], writes=[ku])
                        c.op("act", lambda e: e.copy(out=u[:, 2:514], in_=acc[:]), reads=[ka], writes=[ku])
                        c.op("pool", lambda e: e.tensor_copy(out=carry[:, ch, :], in_=u[:, 512:514]), reads=[ku], writes=[("carry", ch)])
                        cw = self.convw_t
                        c.op("pool", lambda e: e.tensor_scalar(out=cv[:], in0=u[:, 2:514], scalar1=cw[:, li, 2, ch:ch + 1], scalar2=self.convb_t[:, li, ch:ch + 1],
                                                               op0=ALU.mult, op1=ALU.add), reads=[ku, "convw", "convb"], writes=[kc_])
                        c.op("dve", lambda e: e.scalar_tensor_tensor(out=cv[:], in0=u[:, 1:513], scalar=cw[:, li, 1, ch:ch + 1], in1=cv[:], op0=ALU.mult, op1=ALU.add),
                             reads=[ku, "convw", kc_], writes=[kc_])
                        c.op("dve", lambda e: e.scalar_tensor_tensor(out=cv[:], in0=u[:, 0:512], scalar=cw[:, li, 0, ch:ch + 1], in1=cv[:], op0=ALU.mult, op1=ALU.add),
                             reads=[ku, "convw", kc_], writes=[kc_])
                    c.op("act", lambda e: e.activation(out=cc[0][:], in_=cc[0][:], func=AF.Silu), reads=[("cc", 0)], writes=[("cc", 0)])
                    c.op("dve", lambda e: e.tensor_tensor(out=aT[:, i, :], in0=cc[0][:], in1=cc[1][:], op=ALU.mult), reads=[("cc", 0), ("cc", 1)], writes=["aT"])
            wd = self.wdn_b[li].rearrange("(kc p) n -> p kc n", p=128)
            for ng in range(4):
                for kg in range(3):
                    nk = 16 if kg < 2 else 12
                    w, kw = load3(wd[:, kg * 16:kg * 16 + nk, ng * 512:(ng + 1) * 512], nk, ("wdn_b", li))
                    for ts in range(4):
                        acc, ka = self.bk[ts], ("bk", ts)
                        for k2 in range(nk):
                            kc = kg * 16 + k2
                            c.op("pe", lambda e: e.matmul(acc[:], lhsT=aT[:, kc, ts * 128:(ts + 1) * 128], rhs=w[:, k2, :], start=(kc == 0), stop=(kc == 43)),
                                 reads=[kw, "aT"], writes=[ka])
                for ts in range(4):
                    xs = xblk[:, ts, ng * 512:(ng + 1) * 512]
                    c.op("dve", lambda e: e.tensor_tensor(out=xs, in0=self.bk[ts][:], in1=xs, op=ALU.add), reads=[("bk", ts), kxb], writes=[kxb])
            c.dma("pool", "xout", xdst[s, t0:t0 + 512, :].rearrange("(ts p) n -> p ts n", p=128), xblk[:], reads=[kxb], writes=[("X", xdname, s, tb)])

    def build(self):
        c = self.c
        self._stacks = []
        self.setup_consts()
        self.eps_t = self.sb("eps", [128, 1], F32)
        c.op("pool", lambda e: e.memset(self.eps_t[:], EPS), writes=["eps"])
        self.ptr = [self.ps("ptr", [128, 8, 128], BF16) for _ in range(2)]
        self.bk = [self.ps("bk", [128, 512], F32) for _ in range(6)]
        self.acc = self.bk[0:2]
        self.pn = self.bk[2]
        self.sqb = self.sb("sqb", [128, 512], BF16)
        self.rt = self.sb("rt", [128, 512], F32)
        self.setup_tables()
        self.setup_tables2()
        for li in range(self.L):
            self.convert_weights(li)
        for li in range(self.L):
            xsrc, xsname = (self.x, "xin") if li == 0 else (self.XS[(li - 1) % 2], f"XS{(li - 1) % 2}")
            xdst, xdname = (self.out, "out") if li == self.L - 1 else (self.XS[li % 2], f"XS{li % 2}")
            for s in range(NSEQ):
                with self.phase():
                    self.hT = self.sb("hT", [128, 16, T], BF16)
                    self.xt = [self.sb("xt", [128, D], F32) for _ in range(2)]
                    self.xn = self.sb("xn", [128, D], BF16)
                    self.sqj = self.sb("sqj", [128, D], BF16)
                    self.ss = self.sb("ss", [128, 4], F32)
                    self.wt = [self.sb("wt", [128, 16, 512], BF16) for _ in range(2)]
                    self.fst = [self.sb("fst", [128, T], BF16) for _ in range(2)]
                    self.fst32 = self.sb("fst32", [128, T], F32)
                    self.tst = [self.sb("tst", [128, 512], BF16) for _ in range(2)]
                    self.gst = self.sb("gst", [32, T], F32)
                    self.phase_a(li, s, xsrc, xsname)
                    c.dma("pool", "hts", self.HT[:, :, :], self.hT[:], reads=[("hT", t) for t in range(16)], writes=["HT"])
                    self.phase_b(li, s)
                if "stopB" in self.debug:
                    continue
                with self.phase():
                    self.phase_nsa(li, s)
                with self.phase():
                    self.phase_sb(li, s)
                with self.phase():
                    self.phase_hg(li, s)
                if "stopC" in self.debug:
                    continue
                with self.phase():
                    carry = self.sb("carry", [128, 88, 2], F32)
                    c.op("pool", lambda e: e.memset(carry[:], 0.0), writes=[("carry", ch) for ch in range(88)])
                    for tb in range(4):
                        with self.phase():
                            self.phase_d(li, s, tb, xsrc, xsname, xdst, xdname, carry)
        c.finish()
        return self.nc


def _t5_bucket(dist):
    import math
    n = np.maximum(dist, 0)
    big = 16 + (np.log(np.maximum(n, 1).astype(np.float32) / np.float32(16)) / np.float32(math.log(8.0)) * np.float32(16)).astype(np.int32)
    return np.where(n < 16, n, np.minimum(big, 31))


def _static_tables(rel_bias):
    i = np.arange(128)[:, None]
    j = np.arange(128)[None, :]
    blocks = []
    for delta in (0, 128, 512):
        dist = delta + j - i
        blocks.append((dist, (dist >= 0) & (dist < 512)))
    for delta in (0, 128):
        dist = delta + j - i
        blocks.append((dist, dist >= 0))
    t = np.arange(T)[None, :]
    dist = t - 16 * i - 31
    blocks.append((dist, (dist >= 0) & (i < 127)))
    dist_all = np.concatenate([b[0] for b in blocks], axis=1)
    valid = np.concatenate([b[1] for b in blocks], axis=1)
    bucket = _t5_bucket(dist_all)
    ebias = np.empty((8, 128, dist_all.shape[1]), np.float32)
    for h in range(8):
        ebias[h] = np.where(valid, rel_bias[bucket, h], np.float32(-1e30))
    cb = np.ascontiguousarray(np.broadcast_to(rel_bias[31][None, :], (128, 8))).astype(np.float32)
    tpos = (8 + np.arange(8))[None, :, None] * 128 + np.arange(128)[:, None, None]
    qblk = tpos // 64
    jb = np.arange(32)[None, None, :]
    forced = (jb <= qblk) & ((jb == 0) | (jb >= qblk - 1))
    addmask = np.where(forced, 1e30, np.where(jb <= qblk, 0.0, -1e30)).astype(np.float32)
    esel = (np.arange(32)[:, None, None] == 2 * np.arange(16)[None, :, None] + (np.arange(128) // 64)[None, None, :])
    c_start = np.arange(127) * 16
    s_start = np.arange(32) * 64
    overlap = ((c_start[:, None] < s_start[None, :] + 64) & (c_start[:, None] + 32 > s_start[None, :]))
    ov33 = np.zeros((128, 33), np.float32)
    ov33[:127, :32] = overlap
    ov33[:, 32] = 1.0
    selg = (np.arange(24)[:, None, None] == np.arange(24)[None, :, None]) & np.ones((1, 1, 128), bool)
    return dict(ebias=ebias, cb=cb, addmask=addmask, esel=esel.astype(ml_dtypes.bfloat16),
                ov33=ov33.astype(ml_dtypes.bfloat16), selg=selg.astype(np.float32))


def prep_shared(inputs, layers):
    ls = list(layers)
    f = lambda a: np.ascontiguousarray(np.asarray(a, dtype=np.float32))
    d = _static_tables(f(inputs["rel_bias"]))
    d["w_in"] = f(inputs["w_in"])[ls]
    d["norm_attn"] = f(np.asarray(inputs["norm_attn"])[ls].reshape(len(ls), 16, 128).transpose(2, 0, 1))
    hg = np.concatenate([np.asarray(inputs["nsa_q_gain"])[ls][:, None, :], np.asarray(inputs["nsa_k_gain"])[ls]], axis=1)
    d["hgain"] = f(hg.transpose(2, 0, 1))
    d["cmp_w1"] = f(inputs["cmp_w1"])[ls]
    d["cmp_w2"] = f(inputs["cmp_w2"])[ls]
    d["posT"] = f(np.asarray(inputs["cmp_pos"])[ls].transpose(3, 0, 1, 2))
    d["hlb"] = f(np.asarray(inputs["hg_lower_bound"]).reshape(4, 8, 128).transpose(2, 0, 1))
    lcum = np.zeros((128, len(ls), 4), np.float32)
    for i, l in enumerate(ls):
        lcum[:, i, 1:l + 1] = 1.0
    d["lcum"] = lcum
    d["gn"] = f(np.broadcast_to(np.asarray(inputs["hg_norm_gain"])[ls][None, :, :], (128, len(ls), 128)))
    d["w_branch"] = f(np.asarray(inputs["w_branch"])[ls].reshape(len(ls), 3072, D))
    d["w_out"] = f(inputs["w_out"])[ls]
    d["norm_ffn"] = f(np.asarray(inputs["norm_ffn"])[ls].reshape(len(ls), 16, 128).transpose(2, 0, 1))
    d["w_up"] = f(inputs["w_up"])[ls]
    d["convw"] = f(np.asarray(inputs["conv_w"])[ls].reshape(len(ls), 3, 88, 128).transpose(3, 0, 1, 2))
    d["convb"] = f(np.asarray(inputs["conv_b"])[ls].reshape(len(ls), 88, 128).transpose(2, 0, 1))
    d["w_down"] = f(inputs["w_down"])[ls]
    return d


_PROGS = {}


def _get_prog(n_layers):
    if n_layers not in _PROGS:
        p = Prog(list(range(n_layers)))
        _PROGS[n_layers] = p.build()
    return _PROGS[n_layers]


FUSED = False


def kernel(**inputs):
    x = np.ascontiguousarray(np.asarray(inputs["x"], dtype=np.float32))
    n_cores = 8
    if FUSED:
        groups = [list(range(DEPTH))]
    else:
        groups = [[l] for l in range(DEPTH)]
    cur = x
    for ls in groups:
        nc = _get_prog(len(ls))
        shared = prep_shared(inputs, ls)
        in_maps = [dict(shared, x=np.ascontiguousarray(cur[NSEQ * i:NSEQ * (i + 1)])) for i in range(n_cores)]
        res = run_bass_kernel_spmd(nc, in_maps, core_ids=list(range(n_cores)))
        cur = np.concatenate([np.asarray(r["out"], dtype=np.float32) for r in res.results], axis=0)
    return cur
```

```python
import numpy as np
from contextlib import ExitStack
import ml_dtypes
import concourse.bass as bass
import concourse.mybir as mybir
from concourse.bass_utils import run_bass_kernel_spmd

F32 = mybir.dt.float32
BF16 = mybir.dt.bfloat16
AF = mybir.ActivationFunctionType
ALU = mybir.AluOpType

D = 2048
T = 2048
NSEQ = 2
DEPTH = 4
HD = 128
IN_COLS = 15896
DFF = 5632
EPS = 1e-6
SCALE = HD ** -0.5
C_QN, C_KC, C_VC, C_KS, C_VS, C_KW, C_VW, C_G = 0, 1024, 1280, 1536, 1792, 2048, 2304, 2560
C_SQ, C_SK, C_SV = 2584, 3608, 4632
C_HQ, C_HF, C_HI, C_HG = 5656, 6680, 7704, 8728
C_MG = 9752


class Ctx:
    def __init__(self, nc):
        self.nc = nc
        self.E = dict(pe=nc.tensor, act=nc.scalar, dve=nc.vector, pool=nc.gpsimd, sp=nc.sync)
        self.sem = {n: nc.alloc_semaphore("s_" + n) for n in self.E}
        self.cnt = {n: 0 for n in self.E}
        self.seen = {n: {} for n in self.E}
        self.res = {}
        self.slots = {}
        self.nwait = 0

    def _slot(self, name):
        if name not in self.slots:
            self.slots[name] = [self.nc.alloc_semaphore("d_" + name), 0]
        return self.slots[name]

    def _need(self, reads, writes):
        need = {}

        def add(p, c):
            if need.get(p, 0) < c:
                need[p] = c
        for k in reads:
            r = self.res.get(k)
            if r and r[0]:
                add(*r[0])
        for k in writes:
            r = self.res.get(k)
            if r:
                if r[0]:
                    add(*r[0])
                for p, c in r[1].items():
                    add(p, c)
        return need

    def _wait(self, eng, need):
        for p, c in need.items():
            if p == eng and eng in ("pe",):
                continue
            if self.seen[eng].get(p, 0) >= c:
                continue
            if isinstance(p, tuple):
                self.E[eng].wait_ge(self.slots[p[1]][0], 16 * c)
            else:
                self.E[eng].wait_ge(self.sem[p], c)
            self.nwait += 1
            self.seen[eng][p] = c

    def _mark(self, who, c, reads, writes):
        for k in reads:
            r = self.res.setdefault(k, [None, {}])
            r[1][who] = c
        for k in writes:
            self.res[k] = [(who, c), {}]

    def op(self, eng, fn, reads=(), writes=()):
        self._wait(eng, self._need(reads, writes))
        ins = fn(self.E[eng])
        self.cnt[eng] += 1
        ins.then_inc(self.sem[eng], 1)
        self._mark(eng, self.cnt[eng], reads, writes)
        return ins

    def dma(self, q, slot, out, in_, reads=(), writes=(), **kw):
        self._wait(q, self._need(reads, writes))
        s = self._slot(slot)
        ins = self.E[q].dma_start(out=out, in_=in_, **kw)
        s[1] += 1
        ins.then_inc(s[0], 16)
        self._mark(("dma", slot), s[1], reads, writes)
        return ins

    def barrier(self):
        for eng in self.E:
            need = {}
            for n in self.E:
                if n != eng and self.cnt[n]:
                    need[n] = self.cnt[n]
            for name, (sem, cc) in self.slots.items():
                if cc:
                    need[("dma", name)] = cc
            self._wait(eng, need)

    def finish(self):
        for name, (sem, c) in self.slots.items():
            if c:
                self.E["sp"].wait_ge(sem, 16 * c)
        for n in self.E:
            if n != "sp" and self.cnt[n]:
                self.E["sp"].wait_ge(self.sem[n], self.cnt[n])


class Prog:
    def __init__(self, layers, debug=()):
        self.layers = list(layers)
        self.debug = set(debug)
        nc = self.nc = bass.Bass("TRN2", target_bir_lowering=False)
        self.c = Ctx(nc)
        self._n = 0
        L = len(self.layers)
        self.L = L
        di = lambda name, shape, dt=F32: nc.dram_tensor(name, list(shape), dt, kind="ExternalInput").ap()
        self.x = di("x", [NSEQ, T, D])
        self.w_in = di("w_in", [L, D, IN_COLS])
        self.norm_attn = di("norm_attn", [128, L, 16])
        self.hgain = di("hgain", [128, L, 4])
        self.ebias = di("ebias", [8, 128, 2688])
        self.cb = di("cb", [128, 8])
        self.addmask = di("addmask", [128, 8, 32])
        self.esel = di("esel", [32, 16, 128], BF16)
        self.ov33 = di("ov33", [128, 33], BF16)
        self.selg = di("selg", [24, 24, 128])
        self.cmp_w1 = di("cmp_w1", [L, 2, 32, 128, 128])
        self.cmp_w2 = di("cmp_w2", [L, 2, 128, 128])
        self.posT = di("posT", [128, L, 2, 32])
        self.hlb = di("hlb", [128, 4, 8])
        self.lcum = di("lcum", [128, L, 4])
        self.gn = di("gn", [128, L, 128])
        self.w_branch = di("w_branch", [L, 3072, D])
        self.w_out = di("w_out", [L, D, D])
        self.norm_ffn = di("norm_ffn", [128, L, 16])
        self.w_up = di("w_up", [L, D, 2 * DFF])
        self.convw = di("convw", [128, L, 3, 88])
        self.convb = di("convb", [128, L, 88])
        self.w_down = di("w_down", [L, DFF, D])
        self.wbr_b = self.scratch("wbr_b", [L, 3072, D], BF16)
        self.wout_b = self.scratch("wout_b", [L, D, D], BF16)
        self.wup_b = self.scratch("wup_b", [L, D, 2 * DFF], BF16)
        self.wdn_b = self.scratch("wdn_b", [L, DFF, D], BF16)
        self.XS = self.scratch("XS", [2, NSEQ, T, D], F32)
        self.EBD = self.scratch("EBD", [8, 128, 2688], F32)
        self.OT = self.scratch("OT", [24, 128, T], BF16)
        self.out = nc.dram_tensor("out", [NSEQ, T, D], F32, kind="ExternalOutput").ap()
        self.win_b = self.scratch("win_b", [L, D, IN_COLS], BF16)
        self.HT = self.scratch("HT", [128, 16, T], BF16)
        self.FM = self.scratch("FM", [40, 128, T], BF16)
        self.HF = self.scratch("HF", [8, 128, T], F32)
        self.GS = self.scratch("GS", [24, T], F32)
        self.TM = self.scratch("TM", [T, 3584], BF16)

    def scratch(self, name, shape, dt):
        kind = "ExternalOutput" if name in self.debug else "Internal"
        return self.nc.dram_tensor(name, list(shape), dt, kind=kind).ap()

    def sb(self, name, shape, dt):
        self._n += 1
        if getattr(self, "_stacks", None):
            return self._stacks[-1].enter_context(self.nc.sbuf_tensor(f"{name}_{self._n}", list(shape), dt))
        return self.nc.alloc_sbuf_tensor(f"{name}_{self._n}", list(shape), dt)

    def phase(self):
        prog = self

        class _P:
            def __enter__(self_):
                prog._stacks.append(ExitStack())
                return self_

            def __exit__(self_, *a):
                prog.c.barrier()
                prog._stacks.pop().close()
                return False
        return _P()

    def ps(self, name, shape, dt=F32):
        self._n += 1
        return self.nc.alloc_psum_tensor(f"{name}_{self._n}", list(shape), dt)

    def convert_pieces(self, li):
        c = self.c
        pieces = []

        def mk(dst, src, key):
            return lambda: c.dma("pool", "cvt", dst, src, writes=[key])
        for i in range(D // 128):
            src = self.w_in[li, i * 128:(i + 1) * 128, :].rearrange("p (a b) -> p a b", b=1987)
            dst = self.win_b[li, i * 128:(i + 1) * 128, :].rearrange("p (a b) -> p a b", b=1987)
            pieces.append(mk(dst, src, ("win_b", li)))
        for srcw, dstw, rows, key in ((self.w_branch, self.wbr_b, 3072, "wbr_b"), (self.w_out, self.wout_b, D, "wout_b"),
                                      (self.w_up, self.wup_b, D, "wup_b"), (self.w_down, self.wdn_b, DFF, "wdn_b")):
            for i in range(rows // 128):
                src = srcw[li, i * 128:(i + 1) * 128, :].rearrange("p (a b) -> p a b", b=1024)
                dst = dstw[li, i * 128:(i + 1) * 128, :].rearrange("p (a b) -> p a b", b=1024)
                pieces.append(mk(dst, src, (key, li)))
        return pieces

    def cvt_some(self, n=1):
        for _ in range(n):
            if self.cvt_queue:
                self.cvt_queue.pop(0)()

    def setup_consts(self):
        c = self.c
        self.ident = self.sb("ident", [128, 128], BF16)
        self.ones_b = self.sb("ones", [128, 128], BF16)
        self.gattn = self.sb("gattn", [128, self.L, 16], F32)
        idf = self.sb("idf", [128, 128], F32)
        c.op("pool", lambda e: e.memset(idf[:], 1.0), writes=["idf"])
        c.op("pool", lambda e: e.affine_select(out=idf[:], in_=idf[:], pattern=[[-1, 128]], compare_op=ALU.is_equal,
                                               fill=0.0, base=0, channel_multiplier=1), reads=["idf"], writes=["idf"])
        c.op("pool", lambda e: e.tensor_copy(out=self.ident[:], in_=idf[:]), reads=["idf"], writes=["ident"])
        c.op("pool", lambda e: e.memset(self.ones_b[:], 1.0), writes=["ones"])
        c.dma("sp", "cst", self.gattn[:], self.norm_attn[:, :, :], writes=["gattn"])
        self.hgain_t = self.sb("hgain", [128, self.L, 4], F32)
        c.dma("sp", "cst2", self.hgain_t[:], self.hgain[:, :, :], writes=["hgain"])

    def norm_tile(self, xt, kx, gains, dstT, col0, kdst):
        c = self.c
        ss, sq, xn = self.ss, self.sqj, self.xn
        c.op("act", lambda e: e.activation(out=sq[:], in_=xt, func=AF.Square, accum_out=ss[:, 0:1]), reads=[kx], writes=["sqj", "ss"])
        c.op("act", lambda e: e.activation(out=ss[:, 1:2], in_=ss[:, 0:1], func=AF.Sqrt, scale=1.0 / D, bias=self.eps_t[:, 0:1]),
             reads=["ss", "eps"], writes=["ss1"])
        c.op("dve", lambda e: e.reciprocal(out=ss[:, 2:3], in_=ss[:, 1:2]), reads=["ss1"], writes=["ss2"])
        c.op("dve", lambda e: e.tensor_scalar(out=xn[:], in0=xt, scalar1=ss[:, 2:3], scalar2=None, op0=ALU.mult), reads=[kx, "ss2"], writes=["xn"])
        for half in range(2):
            pt = self.ptr[half]
            kp = ("ptr", half)
            for j in range(8):
                kc = half * 8 + j
                c.op("pe", lambda e: e.transpose(out=pt[:, j, :], in_=xn[:, kc * 128:(kc + 1) * 128], identity=self.ident[:]),
                     reads=["xn", "ident"], writes=[kp])
            for j in range(8):
                kc = half * 8 + j
                dst = dstT[:, kc, col0:col0 + 128]
                g = gains[:, kc:kc + 1]
                if j % 2 == 0:
                    c.op("act", lambda e: e.activation(out=dst, in_=pt[:, j, :], func=AF.Copy, scale=g), reads=[kp, "gattn", "gffn"], writes=[kdst])
                else:
                    c.op("dve", lambda e: e.tensor_scalar(out=dst, in0=pt[:, j, :], scalar1=g, scalar2=None, op0=ALU.mult),
                         reads=[kp, "gattn", "gffn"], writes=[kdst])

    def phase_a(self, li, s, xsrc, xname):
        c = self.c
        for tt in range(16):
            xt = self.xt[tt % 2]
            kx = ("xt", tt % 2)
            c.dma("sp", f"xt{tt % 2}", xt[:], xsrc[s, tt * 128:(tt + 1) * 128, :], reads=[("X", xname, s, tt // 4)], writes=[kx])
            self.norm_tile(xt[:], kx, self.gattn[:, li, :], self.hT, tt * 128, ("hT", tt))

    def phase_b(self, li, s):
        c = self.c
        hT = self.hT
        hT_keys = [("hT", t) for t in range(16)]
        wsrc = self.win_b[li].rearrange("(kc p) n -> p kc n", p=128)
        nload = [0]

        def load_w(col, n):
            i = nload[0] % 2
            nload[0] += 1
            wt = self.wt[i]
            c.dma("sp", f"wt{i}", wt[:, :, 0:n], wsrc[:, :, col:col + n], reads=[("win_b", li)], writes=[("wt", i)])
            return wt, ("wt", i)

        nacc = [0]

        def next_acc():
            i = nacc[0] % 2
            nacc[0] += 1
            return self.acc[i], ("acc", i)

        nst = [0]
        fm_jobs = [
            (C_QN, 8, "hnorm", 0, 0), (C_KC, 2, "plain", 8, None), (C_VC, 2, "plain", 10, None),
            (C_KS, 2, "hnorm", 12, 2), (C_KW, 2, "hnorm", 14, 3),
            (C_SQ, 8, "plainS", 16, None), (C_SK, 8, "plain", 24, None), (C_HQ, 8, "plain", 32, None),
            (C_HF, 8, "plain32", 0, None),
        ]
        for col0, nch, kind, fm0, gi in fm_jobs:
            for c4 in range(0, nch, 4):
                ncc = min(4, nch - c4)
                wt, kw = load_w(col0 + c4 * 128, ncc * 128)
                for j in range(ncc):
                    ch = c4 + j
                    if kind == "plain32":
                        st, kst = self.fst32, "fst32"
                    else:
                        st, kst = self.fst[nst[0] % 2], ("fst", nst[0] % 2)
                        nst[0] += 1
                    for tb in range(4):
                        acc, ka = next_acc()
                        for kc in range(16):
                            c.op("pe", lambda e: e.matmul(acc[:], lhsT=wt[:, kc, j * 128:(j + 1) * 128], rhs=hT[:, kc, tb * 512:(tb + 1) * 512],
                                                          start=(kc == 0), stop=(kc == 15)),
                                 reads=[kw] + hT_keys, writes=[ka])
                        dst = st[:, tb * 512:(tb + 1) * 512]
                        if kind == "plainS":
                            c.op("act", lambda e: e.activation(out=dst, in_=acc[:], func=AF.Copy, scale=SCALE), reads=[ka], writes=[kst])
                        elif kind == "plain":
                            eng = "act" if tb % 2 == 0 else "dve"
                            if eng == "act":
                                c.op("act", lambda e: e.copy(out=dst, in_=acc[:]), reads=[ka], writes=[kst])
                            else:
                                c.op("dve", lambda e: e.tensor_copy(out=dst, in_=acc[:]), reads=[ka], writes=[kst])
                        elif kind == "plain32":
                            c.op("act", lambda e: e.copy(out=dst, in_=acc[:]), reads=[ka], writes=[kst])
                        else:
                            self.hnorm_block(acc, ka, dst, kst, self.hgain_t[:, li, gi:gi + 1])
                    if kind == "plain32":
                        c.dma("pool", "fst32", self.HF[ch, :, :], st[:], reads=[kst], writes=[("HF", ch)])
                    else:
                        c.dma("pool", f"fst{kst[1]}", self.FM[fm0 + ch, :, :], st[:], reads=[kst], writes=[("FM", fm0 + ch)])
        wt, kw = load_w(C_G, 24)
        for tb in range(4):
            acc, ka = next_acc()
            for kc in range(16):
                c.op("pe", lambda e: e.matmul(acc[0:24, :], lhsT=wt[:, kc, 0:24], rhs=hT[:, kc, tb * 512:(tb + 1) * 512],
                                              start=(kc == 0), stop=(kc == 15)), reads=[kw] + hT_keys, writes=[ka])
            c.op("act", lambda e: e.activation(out=self.gst[0:24, tb * 512:(tb + 1) * 512], in_=acc[0:24, :], func=AF.Sigmoid),
                 reads=[ka], writes=["gst"])
        c.dma("pool", "gst", self.GS[:, :], self.gst[0:24, :], reads=["gst"], writes=["GS"])
        tm_jobs = [(C_VS, 256, 0, False), (C_VW, 256, 256, False), (C_SV, 1024, 512, False),
                   (C_HI, 1024, 1536, False), (C_HG, 1024, 2560, True)]
        nts = [0]
        for col0, ncols, tm0, sig in tm_jobs:
            for c0 in range(0, ncols, 512):
                n = min(512, ncols - c0)
                wt, kw = load_w(col0 + c0, n)
                for tt in range(16):
                    acc, ka = next_acc()
                    for kc in range(16):
                        c.op("pe", lambda e: e.matmul(acc[:, 0:n], lhsT=hT[:, kc, tt * 128:(tt + 1) * 128], rhs=wt[:, kc, 0:n],
                                                      start=(kc == 0), stop=(kc == 15)), reads=[kw, ("hT", tt)], writes=[ka])
                    i = nts[0] % 2
                    nts[0] += 1
                    st, kst = self.tst[i], ("tst", i)
                    if sig:
                        c.op("act", lambda e: e.activation(out=st[:, 0:n], in_=acc[:, 0:n], func=AF.Sigmoid), reads=[ka], writes=[kst])
                    elif tt % 2 == 0:
                        c.op("act", lambda e: e.copy(out=st[:, 0:n], in_=acc[:, 0:n]), reads=[ka], writes=[kst])
                    else:
                        c.op("dve", lambda e: e.tensor_copy(out=st[:, 0:n], in_=acc[:, 0:n]), reads=[ka], writes=[kst])
                    c.dma("pool", f"tst{i}", self.TM[tt * 128:(tt + 1) * 128, tm0 + c0:tm0 + c0 + n], st[:, 0:n],
                          reads=[kst], writes=[("TM", tm0 + c0, tt)])

    def hnorm_block(self, acc, ka, dst, kdst, gain):
        c = self.c
        c.op("act", lambda e: e.activation(out=self.sqb[:], in_=acc[:], func=AF.Square), reads=[ka], writes=["sqb"])
        c.op("pe", lambda e: e.matmul(self.pn[:], lhsT=self.ones_b[:], rhs=self.sqb[:], start=True, stop=True),
             reads=["sqb", "ones"], writes=["pn"])
        c.op("act", lambda e: e.activation(out=self.rt[:], in_=self.pn[:], func=AF.Sqrt, scale=1.0 / HD, bias=self.eps_t[:, 0:1]),
             reads=["pn", "eps"], writes=["rt"])
        c.op("dve", lambda e: e.reciprocal(out=self.rt[:], in_=self.rt[:]), reads=["rt"], writes=["rt"])
        c.op("dve", lambda e: e.scalar_tensor_tensor(out=dst, in0=acc[:], scalar=gain, in1=self.rt[:], op0=ALU.mult, op1=ALU.mult),
             reads=[ka, "rt", "hgain"], writes=[kdst])

    def setup_tables(self):
        c = self.c
        self.cb_t = self.sb("cb", [128, 8], F32)
        self.negc = self.sb("negc", [128, 8], F32)
        c.dma("sp", "cst3", self.cb_t[:], self.cb[:, :], writes=["cb"])
        c.op("dve", lambda e: e.tensor_scalar(out=self.negc[:], in0=self.cb_t[:], scalar1=-1.0, scalar2=None, op0=ALU.mult),
             reads=["cb"], writes=["negc"])
        self.addmask_t = self.sb("addmask", [128, 8, 32], F32)
        c.dma("sp", "cst4", self.addmask_t[:], self.addmask[:, :, :], writes=["addmask"])
        self.esel_t = self.sb("esel", [32, 16, 128], BF16)
        c.dma("sp", "cst5", self.esel_t[:], self.esel[:, :, :], writes=["esel"])
        self.ov33_t = self.sb("ov33", [128, 33], BF16)
        c.dma("sp", "cst6", self.ov33_t[:], self.ov33[:, :], writes=["ov33"])
        self.selg_t = self.sb("selg", [24, 24, 128], F32)
        c.dma("sp", "cst7", self.selg_t[:], self.selg[:, :, :], writes=["selg"])
        self.posT_t = self.sb("posT", [128, self.L, 2, 32], F32)
        c.dma("sp", "cst8", self.posT_t[:], self.posT[:, :, :, :], writes=["posT"])
        self.posb = self.sb("posb", [128, self.L, 2, 32], BF16)
        c.op("dve", lambda e: e.tensor_copy(out=self.posb[:], in_=self.posT_t[:]), reads=["posT"], writes=["posb"])
        self.idf = self.sb("idf2", [128, 128], F32)
        c.op("pool", lambda e: e.memset(self.idf[:], 1.0), writes=["idf2"])
        c.op("pool", lambda e: e.affine_select(out=self.idf[:], in_=self.idf[:], pattern=[[-1, 128]], compare_op=ALU.is_equal,
                                               fill=0.0, base=0, channel_multiplier=1), reads=["idf2"], writes=["idf2"])
        with self.phase():
            eb = self.sb("ebl", [128, 2688], F32)
            for h in range(8):
                c.dma("sp", "ebl", eb[:], self.ebias[h, :, :], writes=["ebl"])
                c.op("act", lambda e: e.activation(out=eb[:], in_=eb[:], func=AF.Exp, bias=self.negc[:, h:h + 1]),
                     reads=["ebl", "negc"], writes=["ebl"])
                c.dma("sp", "ebs", self.EBD[h, :, :], eb[:], reads=["ebl"], writes=[("EBD", h)])

    def gelu_tanh(self, dst, kdst, src_ps, ksrc, bias, n):
        c = self.c
        x, x2 = self.gx, self.gx2
        c.op("act", lambda e: e.activation(out=x[:, 0:n], in_=src_ps, func=AF.Identity, bias=bias), reads=[ksrc, "cbias"], writes=["gx"])
        c.op("dve", lambda e: e.tensor_tensor(out=x2[:, 0:n], in0=x[:, 0:n], in1=x[:, 0:n], op=ALU.mult), reads=["gx"], writes=["gx2"])
        c.op("dve", lambda e: e.tensor_scalar(out=x2[:, 0:n], in0=x2[:, 0:n], scalar1=0.044715, scalar2=1.0, op0=ALU.mult, op1=ALU.add),
             reads=["gx2"], writes=["gx2"])
        c.op("dve", lambda e: e.tensor_tensor(out=x2[:, 0:n], in0=x2[:, 0:n], in1=x[:, 0:n], op=ALU.mult), reads=["gx", "gx2"], writes=["gx2"])
        c.op("act", lambda e: e.activation(out=x2[:, 0:n], in_=x2[:, 0:n], func=AF.Sigmoid, scale=1.5957691216), reads=["gx2"], writes=["gx2"])
        c.op("dve", lambda e: e.tensor_tensor(out=dst, in0=x2[:, 0:n], in1=x[:, 0:n], op=ALU.mult), reads=["gx", "gx2"], writes=[kdst])

    def compress(self, li, j, xT, kx, w1b, w2b, hid):
        c = self.c
        b5 = self.bk[5]
        k5 = ("bk", 5)
        c.dma("pool", "w1b", w1b[:], self.cmp_w1[li, j].rearrange("l d e -> d l e"), writes=["w1b"])
        c.dma("pool", "w2b", w2b[:], self.cmp_w2[li, j], writes=["w2b"])
        for l in range(32):
            c.op("pe", lambda e: e.matmul(b5[:, 0:127], lhsT=w1b[:, l, :], rhs=xT[:, l:l + 16 * 126 + 1:16], start=(l == 0), stop=(l == 31)),
                 reads=["w1b", kx], writes=[k5])
        for l in range(32):
            c.op("pe", lambda e: e.matmul(b5[:, 128:129], lhsT=w1b[:, l, :], rhs=self.posb[:, li, j, l:l + 1], start=False, stop=(l == 31)),
                 reads=["w1b", "posb"], writes=[k5])
        c.op("dve", lambda e: e.tensor_copy(out=self.cbias[:], in_=b5[:, 128:129]), reads=[k5], writes=["cbias"])
        c.op("pool", lambda e: e.memset(hid[:], 0.0), writes=["hid"])
        self.gelu_tanh(hid[:, 0:127], "hid", b5[:, 0:127], k5, self.cbias[:, 0:1], 127)

    def gate_bcast(self, krow, cols):
        c = self.c
        n = cols[1] - cols[0]
        c.op("pe", lambda e: e.matmul(self.bk[4][:, 0:n], lhsT=self.selg_t[:, krow, :], rhs=self.gsb[0:24, cols[0]:cols[1]], start=True, stop=True),
             reads=["selg", "gsb"], writes=[("bk", 4)])

    def attn_finish(self, h, br, G, oacc, first, guard=False):
        c = self.c
        w, tmp = self.wv, self.tmpv
        self.gate_bcast(h * 3 + br, (G * 512, (G + 1) * 512))
        if guard:
            c.op("dve", lambda e: e.tensor_scalar(out=w[:], in0=self.bk[3][:], scalar1=1e-30, scalar2=None, op0=ALU.max),
                 reads=[("bk", 3)], writes=["wv"])
            c.op("dve", lambda e: e.reciprocal(out=w[:], in_=w[:]), reads=["wv"], writes=["wv"])
        else:
            c.op("dve", lambda e: e.reciprocal(out=w[:], in_=self.bk[3][:]), reads=[("bk", 3)], writes=["wv"])
        c.op("dve", lambda e: e.tensor_tensor(out=w[:], in0=self.bk[4][:], in1=w[:], op=ALU.mult), reads=["wv", ("bk", 4)], writes=["wv"])
        dst = oacc[:, G * 512:(G + 1) * 512]
        ko = ("oacc", id(oacc), G)
        if first:
            c.op("dve", lambda e: e.tensor_tensor(out=dst, in0=self.bk[2][:], in1=w[:], op=ALU.mult), reads=["wv", ("bk", 2)], writes=[ko])
        else:
            c.op("dve", lambda e: e.tensor_tensor(out=tmp[:], in0=self.bk[2][:], in1=w[:], op=ALU.mult), reads=["wv", ("bk", 2)], writes=["tmpv"])
            c.op("pool", lambda e: e.tensor_tensor(out=dst, in0=dst, in1=tmp[:], op=ALU.add), reads=["tmpv", ko], writes=[ko])

    def banded_attn(self, h, G, qT, kq, kT, kk, V, kv, eb, keb, mode, nmT=None):
        c = self.c
        if mode == "win":
            kts = [kt for kt in range(max(0, 4 * G - 4), 4 * G + 4)]
        else:
            kts = list(range(0, 4 * G + 4))
        first = True
        for kt in kts:
            qlo = max(kt, 4 * G)
            qhi = min(kt + 4, 4 * G + 3) if mode == "win" else 4 * G + 3
            c0, c1 = (qlo - 4 * G) * 128, (qhi - 4 * G + 1) * 128
            n = c1 - c0
            i = self.nS % 2
            self.nS += 1
            S, kS = self.bk[i], ("bk", i)
            P, kP = self.P[i], ("P", i)
            use_mask = (mode == "sel" and G >= 2)
            c.op("pe", lambda e: e.matmul(S[:, 0:n], lhsT=kT[:, kt * 128:(kt + 1) * 128], rhs=qT[:, G * 512 + c0:G * 512 + c1],
                                          start=True, stop=not use_mask), reads=[kk, kq], writes=[kS])
            if use_mask:
                c.op("pe", lambda e: e.matmul(S[:, 0:n], lhsT=self.esel_t[:, kt, :], rhs=nmT[:, G * 512 + c0 - 1024:G * 512 + c1 - 1024],
                                              start=False, stop=True), reads=["esel", "nmT"], writes=[kS])
            c.op("act", lambda e: e.activation(out=P[:, 0:n], in_=S[:, 0:n], func=AF.Exp, scale=SCALE, bias=self.cb_t[:, h:h + 1]),
                 reads=[kS, "cb"], writes=[kP])
            for qb in range(qlo, qhi + 1):
                d = qb - kt
                if mode == "win":
                    ti = {0: 0, 1: 1, 4: 2}.get(d)
                    off = 0
                else:
                    ti = {0: 0, 1: 1}.get(d)
                    off = 384
                if ti is None:
                    continue
                sub = P[:, (qb - qlo) * 128:(qb - qlo + 1) * 128]
                c.op("pool", lambda e: e.tensor_tensor(out=sub, in0=sub, in1=eb[:, off + ti * 128:off + (ti + 1) * 128], op=ALU.mult),
                     reads=[kP, keb], writes=[kP])
            c.op("pe", lambda e: e.matmul(self.bk[2][:, c0:c1], lhsT=V[:, kt, :], rhs=P[:, 0:n], start=first, stop=(kt == kts[-1])),
                 reads=[kv, kP], writes=[("bk", 2)])
            c.op("pe", lambda e: e.matmul(self.bk[3][:, c0:c1], lhsT=self.ones_b[:], rhs=P[:, 0:n], start=first, stop=(kt == kts[-1])),
                 reads=["ones", kP], writes=[("bk", 3)])
            first = False

    def phase_nsa(self, li, s):
        c = self.c
        sb = self.sb
        self.gsb = sb("gsb", [32, T], F32)
        self.wv = sb("wv", [128, 512], F32)
        self.tmpv = sb("tmpv", [128, 512], F32)
        self.P = [sb("P", [128, 512], BF16) for _ in range(2)]
        self.gx = sb("gx", [128, 128], F32)
        self.gx2 = sb("gx2", [128, 128], F32)
        self.cbias = sb("cbias", [128, 1], F32)
        self.nS = 0
        w1b = sb("w1b", [128, 32, 128], BF16)
        w2b = sb("w2b", [128, 128], BF16)
        xk = sb("xk", [128, T], BF16)
        xv = sb("xv", [128, T], BF16)
        hid = sb("hid", [128, 128], BF16)
        kcmpT = sb("kcmpT", [128, 128], BF16)
        vcmp = sb("vcmp", [128, 1, 128], BF16)
        ksT = sb("ksT", [128, T], BF16)
        kwT = sb("kwT", [128, T], BF16)
        vs = sb("vs", [128, 16, 128], BF16)
        vw = sb("vw", [128, 16, 128], BF16)
        qT = [sb("qT", [128, T], BF16) for _ in range(4)]
        oacc = [sb("oacc", [128, T], F32) for _ in range(4)]
        obf = sb("obf", [128, T], BF16)
        eb = sb("eb", [128, 2688], F32)
        impacc = sb("impacc", [128, 8, 32], F32)
        imps = sb("imps", [128, 40], F32)
        sc = sb("sc", [128, 32], F32)
        sc2 = sb("sc2", [128, 32], F32)
        m8 = sb("m8", [128, 16], F32)
        nmT = sb("nmT", [32, T // 2], BF16)
        b5, k5 = self.bk[5], ("bk", 5)
        c.dma("sp", "gsb", self.gsb[0:24, :], self.GS[:, :], reads=["GS"], writes=["gsb"])
        for g in range(2):
            c.dma("sp", "xk", xk[:], self.FM[8 + g, :, :], reads=[("FM", 8 + g)], writes=["xk"])
            c.dma("sp", "xv", xv[:], self.FM[10 + g, :, :], reads=[("FM", 10 + g)], writes=["xv"])
            c.dma("sp", "ksT", ksT[:], self.FM[12 + g, :, :], reads=[("FM", 12 + g)], writes=["ksT"])
            c.dma("sp", "kwT", kwT[:], self.FM[14 + g, :, :], reads=[("FM", 14 + g)], writes=["kwT"])
            tmv = self.TM.rearrange("(kt p) n -> p kt n", p=128)
            c.dma("sp", "vs", vs[:], tmv[:, :, g * 128:(g + 1) * 128], reads=[("TM", 0, t) for t in range(16)], writes=["vs"])
            c.dma("sp", "vw", vw[:], tmv[:, :, 256 + g * 128:256 + (g + 1) * 128], reads=[("TM", 256, t) for t in range(16)], writes=["vw"])
            self.compress(li, 0, xk, "xk", w1b, w2b, hid)
            c.op("pe", lambda e: e.matmul(b5[:, 256:384], lhsT=w2b[:], rhs=hid[:], start=True, stop=True), reads=["w2b", "hid"], writes=[k5])
            c.op("act", lambda e: e.activation(out=self.sqb[:, 0:128], in_=b5[:, 256:384], func=AF.Square), reads=[k5], writes=["sqb"])
            c.op("pe", lambda e: e.matmul(self.bk[4][:, 0:128], lhsT=self.ones_b[:], rhs=self.sqb[:, 0:128], start=True, stop=True),
                 reads=["sqb", "ones"], writes=[("bk", 4)])
            c.op("act", lambda e: e.activation(out=self.rt[:, 0:128], in_=self.bk[4][:, 0:128], func=AF.Sqrt, scale=1.0 / HD, bias=self.eps_t[:, 0:1]),
                 reads=[("bk", 4), "eps"], writes=["rt"])
            c.op("dve", lambda e: e.reciprocal(out=self.rt[:, 0:128], in_=self.rt[:, 0:128]), reads=["rt"], writes=["rt"])
            c.op("dve", lambda e: e.scalar_tensor_tensor(out=kcmpT[:], in0=b5[:, 256:384], scalar=self.hgain_t[:, li, 1:2], in1=self.rt[:, 0:128],
                                                         op0=ALU.mult, op1=ALU.mult), reads=[k5, "rt", "hgain"], writes=["kcmpT"])
            self.compress(li, 1, xv, "xv", w1b, w2b, hid)
            c.op("pe", lambda e: e.matmul(b5[:, 256:384], lhsT=hid[:], rhs=w2b[:], start=True, stop=True), reads=["w2b", "hid"], writes=[k5])
            c.op("dve", lambda e: e.tensor_copy(out=vcmp[:, 0, :], in_=b5[:, 256:384]), reads=[k5], writes=["vcmp"])
            c.op("pool", lambda e: e.memset(impacc[:], 0.0), writes=["impacc"])
            for r in range(4):
                h = 4 * g + r
                c.dma("sp", f"qT{r}", qT[r][:], self.FM[h, :, :], reads=[("FM", h)], writes=[("qT", r)])
                c.dma("sp", "eb", eb[:], self.EBD[h, :, :], reads=[("EBD", h)], writes=["eb"])
                for G in range(4):
                    i = self.nS % 2
                    self.nS += 1
                    S, kS = self.bk[i], ("bk", i)
                    P, kP = self.P[i], ("P", i)
                    c.op("pe", lambda e: e.matmul(S[:], lhsT=kcmpT[:], rhs=qT[r][:, G * 512:(G + 1) * 512], start=True, stop=True),
                         reads=["kcmpT", ("qT", r)], writes=[kS])
                    c.op("act", lambda e: e.activation(out=self.tmpv[:], in_=S[:], func=AF.Exp, scale=SCALE, bias=self.cb_t[:, h:h + 1]),
                         reads=[kS, "cb"], writes=["tmpv"])
                    c.op("dve", lambda e: e.tensor_tensor(out=P[:], in0=self.tmpv[:], in1=eb[:, 640 + G * 512:640 + (G + 1) * 512], op=ALU.mult),
                         reads=["tmpv", "eb"], writes=[kP])
                    c.op("pe", lambda e: e.matmul(self.bk[2][:], lhsT=vcmp[:, 0, :], rhs=P[:], start=True, stop=True), reads=["vcmp", kP], writes=[("bk", 2)])
                    c.op("pe", lambda e: e.matmul(self.bk[3][:], lhsT=self.ones_b[:], rhs=P[:], start=True, stop=True), reads=["ones", kP], writes=[("bk", 3)])
                    if G >= 2:
                        for q4 in range(4):
                            tt = G * 4 + q4 - 8
                            c.op("pe", lambda e: e.matmul(b5[:, q4 * 64:q4 * 64 + 33], lhsT=P[:, q4 * 128:(q4 + 1) * 128], rhs=self.ov33_t[:],
                                                          start=True, stop=True), reads=["ov33", kP], writes=[k5])
                            c.op("dve", lambda e: e.reciprocal(out=imps[:, 32:33], in_=b5[:, q4 * 64 + 32:q4 * 64 + 33]), reads=[k5], writes=["imps"])
                            c.op("dve", lambda e: e.tensor_scalar(out=imps[:, 0:32], in0=b5[:, q4 * 64:q4 * 64 + 32], scalar1=imps[:, 32:33], scalar2=None,
                                                                  op0=ALU.mult), reads=[k5, "imps"], writes=["imps2"])
                            c.op("dve", lambda e: e.tensor_tensor(out=impacc[:, tt, :], in0=impacc[:, tt, :], in1=imps[:, 0:32], op=ALU.add),
                                 reads=["imps2", "impacc"], writes=["impacc"])
                    self.attn_finish(h, 0, G, oacc[r], True, guard=True)
            for tt in range(8):
                c.op("dve", lambda e: e.tensor_tensor(out=sc[:], in0=impacc[:, tt, :], in1=self.addmask_t[:, tt, :], op=ALU.add),
                     reads=["impacc", "addmask"], writes=["sc"])
                c.op("dve", lambda e: e.max(out=m8[:, 0:8], in_=sc[:]), reads=["sc"], writes=["m8"])
                c.op("dve", lambda e: e.match_replace(out=sc2[:], in_to_replace=m8[:, 0:8], in_values=sc[:], imm_value=-3e38),
                     reads=["sc", "m8"], writes=["sc2"])
                c.op("dve", lambda e: e.max(out=m8[:, 8:16], in_=sc2[:]), reads=["sc2"], writes=["m8b"])
                c.op("dve", lambda e: e.tensor_scalar(out=sc2[:], in0=sc[:], scalar1=m8[:, 15:16], scalar2=None, op0=ALU.is_ge),
                     reads=["sc", "m8b"], writes=["sc2"])
                c.op("dve", lambda e: e.tensor_scalar(out=sc2[:], in0=sc2[:], scalar1=30000.0, scalar2=-30000.0, op0=ALU.mult, op1=ALU.add),
                     reads=["sc2"], writes=["sc2"])
                c.op("pe", lambda e: e.transpose(out=b5[0:32, 384:512], in_=sc2[:], identity=self.idf[:]), reads=["sc2", "idf2"], writes=[k5])
                c.op("act", lambda e: e.copy(out=nmT[:, tt * 128:(tt + 1) * 128], in_=b5[0:32, 384:512]), reads=[k5], writes=["nmT"])
            for r in range(4):
                h = 4 * g + r
                c.dma("sp", "eb", eb[:], self.EBD[h, :, :], reads=[("EBD", h)], writes=["eb"])
                for G in range(4):
                    self.banded_attn(h, G, qT[r], ("qT", r), ksT, "ksT", vs, "vs", eb, "eb", "sel", nmT)
                    self.attn_finish(h, 1, G, oacc[r], False)
                    self.banded_attn(h, G, qT[r], ("qT", r), kwT, "kwT", vw, "vw", eb, "eb", "win")
                    self.attn_finish(h, 2, G, oacc[r], False)
                c.op("act", lambda e: e.copy(out=obf[:], in_=oacc[r][:]), reads=[("oacc", id(oacc[r]), G) for G in range(4)], writes=["obf"])
                c.dma("pool", "obf", self.OT[h, :, :], obf[:], reads=["obf"], writes=[("OT", h)])

    def setup_tables2(self):
        c = self.c
        L = self.L
        self.trineg = self.sb("trineg", [128, 128], BF16)
        self.mstrict = self.sb("mstrict", [128, 128], BF16)
        self.masku = self.sb("masku", [128, 64], F32)
        self.rst = self.sb("rst", [128, T], F32)
        tmp = self.sb("tmpc", [128, 128], F32)
        c.op("pool", lambda e: e.memset(tmp[:], -1.0), writes=["tmpc"])
        c.op("pool", lambda e: e.affine_select(out=tmp[:], in_=tmp[:], pattern=[[-1, 128]], compare_op=ALU.is_ge, fill=0.0, base=0, channel_multiplier=1),
             reads=["tmpc"], writes=["tmpc"])
        c.op("pool", lambda e: e.tensor_copy(out=self.trineg[:], in_=tmp[:]), reads=["tmpc"], writes=["trineg"])
        c.op("pool", lambda e: e.memset(tmp[:], 1.0), reads=["tmpc"], writes=["tmpc"])
        c.op("pool", lambda e: e.affine_select(out=tmp[:], in_=tmp[:], pattern=[[1, 128]], compare_op=ALU.is_gt, fill=0.0, base=0, channel_multiplier=-1),
             reads=["tmpc"], writes=["tmpc"])
        c.op("pool", lambda e: e.tensor_copy(out=self.mstrict[:], in_=tmp[:]), reads=["tmpc"], writes=["mstrict"])
        c.op("pool", lambda e: e.memset(self.masku[:], 1.0), writes=["masku"])
        for p0 in (0, 64):
            c.op("pool", lambda e: e.affine_select(out=self.masku[p0:p0 + 64, :], in_=self.masku[p0:p0 + 64, :], pattern=[[1, 64]], compare_op=ALU.is_ge,
                                                   fill=0.0, base=0, channel_multiplier=-1), reads=["masku"], writes=["masku"])
        c.op("pool", lambda e: e.memset(self.rst[:], 1.0), writes=["rst"])
        c.op("pool", lambda e: e.memset(self.rst[:].rearrange("p (c t) -> p c t", t=64)[:, :, 0:1], 0.0), reads=["rst"], writes=["rst"])
        hl = self.sb("hl", [128, 4, 8], F32)
        lc = self.sb("lc", [128, L, 4], F32)
        self.lb = self.sb("lb", [128, L, 8], F32)
        self.oml = self.sb("oml", [128, L, 8], F32)
        ssum = self.sb("ssum", [128, 8], F32)
        c.dma("sp", "cst9", hl[:], self.hlb[:, :, :], writes=["hl"])
        c.dma("sp", "cst10", lc[:], self.lcum[:, :, :], writes=["lc"])
        c.op("act", lambda e: e.activation(out=hl[:], in_=hl[:], func=AF.Exp), reads=["hl"], writes=["hl"])
        c.op("dve", lambda e: e.tensor_tensor(out=ssum[:], in0=hl[:, 0, :], in1=hl[:, 1, :], op=ALU.add), reads=["hl"], writes=["ssum"])
        c.op("dve", lambda e: e.tensor_tensor(out=ssum[:], in0=ssum[:], in1=hl[:, 2, :], op=ALU.add), reads=["hl", "ssum"], writes=["ssum"])
        c.op("dve", lambda e: e.tensor_tensor(out=ssum[:], in0=ssum[:], in1=hl[:, 3, :], op=ALU.add), reads=["hl", "ssum"], writes=["ssum"])
        c.op("dve", lambda e: e.reciprocal(out=ssum[:], in_=ssum[:]), reads=["ssum"], writes=["ssum"])
        for l4 in range(4):
            c.op("dve", lambda e: e.tensor_tensor(out=hl[:, l4, :], in0=hl[:, l4, :], in1=ssum[:], op=ALU.mult), reads=["hl", "ssum"], writes=["hl"])
        for li in range(L):
            c.op("dve", lambda e: e.tensor_scalar(out=self.lb[:, li, :], in0=hl[:, 0, :], scalar1=lc[:, li, 0:1], scalar2=None, op0=ALU.mult),
                 reads=["hl", "lc"], writes=["lb"])
            for l4 in range(1, 4):
                c.op("dve", lambda e: e.scalar_tensor_tensor(out=self.lb[:, li, :], in0=hl[:, l4, :], scalar=lc[:, li, l4:l4 + 1], in1=self.lb[:, li, :],
                                                             op0=ALU.mult, op1=ALU.add), reads=["hl", "lc", "lb"], writes=["lb"])
        c.op("dve", lambda e: e.tensor_scalar(out=self.oml[:], in0=self.lb[:], scalar1=-1.0, scalar2=1.0, op0=ALU.mult, op1=ALU.add),
             reads=["lb"], writes=["oml"])
        self.gn_t = self.sb("gn", [128, L, 128], F32)
        c.dma("sp", "cst11", self.gn_t[:], self.gn[:, :, :], writes=["gn"])
        self.gffn = self.sb("gffn", [128, L, 16], F32)
        c.dma("sp", "cst12", self.gffn[:], self.norm_ffn[:, :, :], writes=["gffn"])
        self.convw_t = self.sb("convw", [128, L, 3, 88], F32)
        c.dma("sp", "cst13", self.convw_t[:], self.convw[:, :, :, :], writes=["convw"])
        self.convb_t = self.sb("convb", [128, L, 88], F32)
        c.dma("sp", "cst14", self.convb_t[:], self.convb[:, :, :], writes=["convb"])

    def phase_sb(self, li, s):
        c = self.c
        sb = self.sb
        qT = sb("sqT", [128, T], BF16)
        kT = sb("skT", [128, T], BF16)
        V = sb("sV", [128, 16, 128], BF16)
        R = sb("sR", [128, 512], F32)
        e32 = sb("se32", [128, 512], F32)
        arg = sb("sarg", [128, 512], F32)
        spb = [sb("spb", [128, 512], BF16) for _ in range(2)]
        A = [sb("sA", [128, 512], BF16) for _ in range(2)]
        obf = sb("sobf", [128, T], BF16)
        tmv = self.TM.rearrange("(kt p) n -> p kt n", p=128)
        n_it = 0
        for h in range(8):
            c.dma("sp", "sqT", qT[:], self.FM[16 + h, :, :], reads=[("FM", 16 + h)], writes=["sqT"])
            c.dma("sp", "skT", kT[:], self.FM[24 + h, :, :], reads=[("FM", 24 + h)], writes=["skT"])
            c.dma("sp", "sV", V[:], tmv[:, :, 512 + h * 128:512 + (h + 1) * 128],
                  reads=[("TM", 512 + (h // 4) * 512, t) for t in range(16)], writes=["sV"])
            for G in range(4):
                c.op("pool", lambda e: e.memset(R[:], 0.0), writes=["sR"])
                kts = list(range(4 * G + 3, -1, -1))
                for kt in kts:
                    qlo = max(kt, 4 * G)
                    c0, c1 = (qlo - 4 * G) * 128, 512
                    n = c1 - c0
                    i = n_it % 2
                    n_it += 1
                    S, kS = self.bk[i], ("bk", i)
                    sp_, ksp = spb[i], ("spb", i)
                    A_, kA = A[i], ("sA", i)
                    diag = kt >= 4 * G
                    c.op("pe", lambda e: e.matmul(S[:, 0:n], lhsT=kT[:, kt * 128:(kt + 1) * 128], rhs=qT[:, G * 512 + c0:G * 512 + c1], start=True, stop=False),
                         reads=["skT", "sqT"], writes=[kS])
                    c.op("act", lambda e: e.activation(out=e32[:, 0:n], in_=S[:, 0:n], func=AF.Exp), reads=[kS], writes=["se32"])
                    c.op("act", lambda e: e.activation(out=sp_[:, 0:n], in_=e32[:, 0:n], func=AF.Ln, bias=1.0), reads=["se32"], writes=[ksp])
                    if diag:
                        c.op("pool", lambda e: e.tensor_tensor(out=sp_[:, 0:128], in0=sp_[:, 0:128], in1=self.mstrict[:], op=ALU.mult),
                             reads=[ksp, "mstrict"], writes=[ksp])
                    c.op("pe", lambda e: e.matmul(S[:, 0:n], lhsT=self.trineg[:], rhs=sp_[:, 0:n], start=False, stop=True),
                         reads=["trineg", ksp], writes=[kS])
                    c.op("pe", lambda e: e.matmul(self.bk[3][:, 0:n], lhsT=self.ones_b[:], rhs=sp_[:, 0:n], start=True, stop=True),
                         reads=["ones", ksp], writes=[("bk", 3)])
                    c.op("dve", lambda e: e.tensor_tensor(out=arg[:, 0:n], in0=S[:, 0:n], in1=R[:, c0:c1], op=ALU.subtract),
                         reads=[kS, "sR"], writes=["sarg"])
                    c.op("act", lambda e: e.activation(out=A_[:, 0:n], in_=arg[:, 0:n], func=AF.Exp), reads=["sarg"], writes=[kA])
                    if diag:
                        c.op("pool", lambda e: e.tensor_tensor(out=A_[:, 0:128], in0=A_[:, 0:128], in1=self.mstrict[:], op=ALU.mult),
                             reads=[kA, "mstrict"], writes=[kA])
                    c.op("dve", lambda e: e.tensor_tensor(out=R[:, c0:c1], in0=R[:, c0:c1], in1=self.bk[3][:, 0:n], op=ALU.add),
                         reads=["sR", ("bk", 3)], writes=["sR"])
                    c.op("pe", lambda e: e.matmul(self.bk[2][:, c0:c1], lhsT=V[:, kt, :], rhs=A_[:, 0:n], start=(kt == kts[0]), stop=(kt == kts[-1])),
                         reads=["sV", kA], writes=[("bk", 2)])
                c.op("act", lambda e: e.copy(out=obf[:, G * 512:(G + 1) * 512], in_=self.bk[2][:]), reads=[("bk", 2)], writes=["sobf"])
            c.dma("pool", "sobf", self.OT[8 + h, :, :], obf[:], reads=["sobf"], writes=[("OT", 8 + h)])

    def phase_hg(self, li, s):
        c = self.c
        sb = self.sb
        qf = sb("hq", [128, T], BF16)
        F = sb("hF", [128, T], F32)
        LF = sb("hLF", [128, T], F32)
        Kk = sb("hK", [128, T], F32)
        b = sb("hb", [128, T], F32)
        d1 = sb("hd1", [128, T], F32)
        E = sb("hE", [128, T], F32)
        qp = sb("hqp", [128, T], BF16)
        kp = sb("hkp", [128, T], BF16)
        kd = sb("hkd", [128, T], BF16)
        kdT = sb("hkdT", [128, 16, 128], BF16)
        V = sb("hV", [128, 16, 128], BF16)
        Gs = sb("hGs", [128, 16, 128], BF16)
        emid = sb("hemid", [128, 32], F32)
        elast = sb("helast", [128, 32], F32)
        S = sb("hS", [128, 128], F32)
        Sb = sb("hSb", [128, 128], BF16)
        am = sb("ham", [128, 64], BF16)
        ss = sb("hss", [128, 4], F32)
        junk = sb("hjunk", [128, 128], F32)
        y = sb("hy", [128, 128], F32)
        yb = sb("hyb", [128, 128], BF16)
        ohT = sb("hohT", [128, T], BF16)
        tmv = self.TM.rearrange("(kt p) n -> p kt n", p=128)
        c3 = lambda a: a[:].rearrange("p (c t) -> p c t", t=64)
        for h in range(8):
            c.dma("sp", "hq", qf[:], self.FM[32 + h, :, :], reads=[("FM", 32 + h)], writes=["hq"])
            c.dma("sp", "hF", F[:], self.HF[h, :, :], reads=[("HF", h)], writes=["hF"])
            c.dma("sp", "hV", V[:], tmv[:, :, 1536 + h * 128:1536 + (h + 1) * 128],
                  reads=[("TM", 1536 + (h // 4) * 512, t) for t in range(16)], writes=["hV"])
            c.dma("sp", "hGs", Gs[:], tmv[:, :, 2560 + h * 128:2560 + (h + 1) * 128],
                  reads=[("TM", 2560 + (h // 4) * 512, t) for t in range(16)], writes=["hGs"])
            c.op("act", lambda e: e.activation(out=F[:], in_=F[:], func=AF.Sigmoid), reads=["hF"], writes=["hF"])
            c.op("dve", lambda e: e.tensor_scalar(out=F[:], in0=F[:], scalar1=self.oml[:, li, h:h + 1], scalar2=self.lb[:, li, h:h + 1],
                                                  op0=ALU.mult, op1=ALU.add), reads=["hF", "oml", "lb"], writes=["hF"])
            c.op("act", lambda e: e.activation(out=LF[:], in_=F[:], func=AF.Ln), reads=["hF"], writes=["hLF"])
            c.op("dve", lambda e: e.tensor_scalar(out=Kk[:], in0=F[:], scalar1=-1.0, scalar2=1.0, op0=ALU.mult, op1=ALU.add), reads=["hF"], writes=["hK"])
            c.op("dve", lambda e: e.tensor_tensor_scan(out=b[:], data0=self.rst[:], data1=LF[:], initial=0.0, op0=ALU.mult, op1=ALU.add),
                 reads=["hLF", "rst"], writes=["hb"])
            bmid = c3(b)[:, :, 31:32]
            blast = c3(b)[:, :, 63:64]
            c.op("dve", lambda e: e.tensor_tensor(out=c3(d1), in0=c3(b), in1=bmid.broadcast_to([128, 32, 64]), op=ALU.subtract), reads=["hb"], writes=["hd1"])
            c.op("act", lambda e: e.activation(out=E[:], in_=d1[:], func=AF.Exp), reads=["hd1"], writes=["hE"])
            c.op("dve", lambda e: e.tensor_tensor(out=qp[:], in0=qf[:], in1=E[:], op=ALU.mult), reads=["hq", "hE"], writes=["hqp"])
            c.op("act", lambda e: e.activation(out=E[:], in_=d1[:], func=AF.Exp, scale=-1.0), reads=["hd1", "hqp"], writes=["hE"])
            c.op("dve", lambda e: e.tensor_tensor(out=kp[:], in0=Kk[:], in1=E[:], op=ALU.mult), reads=["hK", "hE"], writes=["hkp"])
            c.op("dve", lambda e: e.tensor_tensor(out=c3(d1), in0=blast.broadcast_to([128, 32, 64]), in1=c3(b), op=ALU.subtract), reads=["hb", "hkp"], writes=["hd1"])
            c.op("act", lambda e: e.activation(out=E[:], in_=d1[:], func=AF.Exp), reads=["hd1", "hkp"], writes=["hE"])
            c.op("dve", lambda e: e.tensor_tensor(out=kd[:], in0=Kk[:], in1=E[:], op=ALU.mult), reads=["hK", "hE"], writes=["hkd"])
            c.op("act", lambda e: e.activation(out=emid[:].rearrange("p (c o) -> p c o", o=1), in_=bmid, func=AF.Exp), reads=["hb"], writes=["hemid"])
            c.op("act", lambda e: e.activation(out=elast[:].rearrange("p (c o) -> p c o", o=1), in_=blast, func=AF.Exp), reads=["hb"], writes=["helast"])
            for tt in range(16):
                pt = self.ptr[tt % 2]
                kp_ = ("ptr", tt % 2)
                c.op("pe", lambda e: e.transpose(out=pt[:, 0, :], in_=kd[:, tt * 128:(tt + 1) * 128], identity=self.ident[:]), reads=["hkd", "ident"], writes=[kp_])
                c.op("act", lambda e: e.copy(out=kdT[:, tt, :], in_=pt[:, 0, :]), reads=[kp_], writes=["hkdT"])
            c.op("pool", lambda e: e.memset(S[:], 0.0), writes=["hS"])
            for ch in range(32):
                tt, half = ch // 2, ch % 2
                p0 = half * 64
                cs = slice(ch * 64, (ch + 1) * 64)
                aps, ka = (self.bk[0], ("bk", 0)) if ch % 2 == 0 else (self.bk[5], ("bk", 5))
                O, kO = (self.bk[1], ("bk", 1)) if tt % 2 == 0 else (self.bk[4], ("bk", 4))
                c.op("pe", lambda e: e.matmul(aps[p0:p0 + 64, 0:64], lhsT=kp[:, cs], rhs=qp[:, cs], start=True, stop=True), reads=["hkp", "hqp"], writes=[ka])
                c.op("dve", lambda e: e.tensor_tensor(out=am[p0:p0 + 64, :], in0=aps[p0:p0 + 64, 0:64], in1=self.masku[p0:p0 + 64, :], op=ALU.mult),
                     reads=[ka, "masku"], writes=[("ham", half)])
                c.op("act", lambda e: e.activation(out=Sb[:], in_=S[:], func=AF.Copy, scale=emid[:, ch:ch + 1]), reads=["hS", "hemid"], writes=["hSb"])
                c.op("pe", lambda e: e.matmul(O[p0:p0 + 64, 0:128], lhsT=am[p0:p0 + 64, :], rhs=V[p0:p0 + 64, tt, :], start=True, stop=False),
                     reads=[("ham", half), "hV"], writes=[kO])
                c.op("pe", lambda e: e.matmul(O[p0:p0 + 64, 0:128], lhsT=qp[:, cs], rhs=Sb[:], start=False, stop=True), reads=["hqp", "hSb"], writes=[kO])
                c.op("pe", lambda e: e.matmul(self.bk[3][:, 0:128], lhsT=kdT[p0:p0 + 64, tt, :], rhs=V[p0:p0 + 64, tt, :], start=True, stop=True),
                     reads=["hkdT", "hV"], writes=[("bk", 3)])
                c.op("dve", lambda e: e.scalar_tensor_tensor(out=S[:], in0=S[:], scalar=elast[:, ch:ch + 1], in1=self.bk[3][:, 0:128], op0=ALU.mult, op1=ALU.add),
                     reads=["hS", "helast", ("bk", 3)], writes=["hS"])
                if half == 1:
                    c.op("act", lambda e: e.activation(out=junk[:], in_=O[:, 0:128], func=AF.Square, accum_out=ss[:, 0:1]), reads=[kO], writes=["hjunk", "hss"])
                    c.op("act", lambda e: e.activation(out=ss[:, 1:2], in_=ss[:, 0:1], func=AF.Sqrt, scale=1.0 / HD, bias=self.eps_t[:, 0:1]),
                         reads=["hss", "eps"], writes=["hss1"])
                    c.op("dve", lambda e: e.reciprocal(out=ss[:, 2:3], in_=ss[:, 1:2]), reads=["hss1"], writes=["hss2"])
                    c.op("dve", lambda e: e.scalar_tensor_tensor(out=y[:], in0=O[:, 0:128], scalar=ss[:, 2:3], in1=self.gn_t[:, li, :], op0=ALU.mult, op1=ALU.mult),
                         reads=[kO, "hss2", "gn"], writes=["hy"])
                    c.op("dve", lambda e: e.tensor_tensor(out=yb[:], in0=y[:], in1=Gs[:, tt, :], op=ALU.mult), reads=["hy", "hGs"], writes=["hyb"])
                    pt = self.ptr[tt % 2]
                    kp_ = ("ptr", tt % 2)
                    c.op("pe", lambda e: e.transpose(out=pt[:, 1, :], in_=yb[:], identity=self.ident[:]), reads=["hyb", "ident"], writes=[kp_])
                    c.op("act", lambda e: e.copy(out=ohT[:, tt * 128:(tt + 1) * 128], in_=pt[:, 1, :]), reads=[kp_], writes=["hohT"])
            c.dma("pool", "hohT", self.OT[16 + h, :, :], ohT[:], reads=["hohT"], writes=[("OT", 16 + h)])

    def phase_d(self, li, s, tb, xsrc, xsname, xdst, xdname, carry):
        c = self.c
        sb = self.sb
        t0 = tb * 512
        xblk = sb("xblk", [128, 4, D], F32)
        h2T = sb("h2T", [128, 16, 512], BF16)
        kxb = "xblk"
        c.dma("sp", "xblk", xblk[:], xsrc[s, t0:t0 + 512, :].rearrange("(ts p) n -> p ts n", p=128), reads=[("X", xsname, s, tb)], writes=[kxb])
        nacc = [0]

        def next_acc():
            i = nacc[0] % 2
            nacc[0] += 1
            return self.bk[i], ("bk", i)
        with self.phase():
            hb = sb("hb", [128, 16, 512], BF16)
            ob = sb("ob", [128, 24, 512], BF16)
            sg = [sb("sg", [128, 4, 512], BF16) for _ in range(3)]
            mT = sb("mT", [128, 16, 512], BF16)
            macc = sb("macc", [128, 4, 512], F32)
            tmp = sb("mtmp", [128, 512], F32)
            wt = [sb("wtd", [128, 16, 512], BF16) for _ in range(2)]
            self.xn = sb("xn", [128, D], BF16)
            self.sqj = sb("sqj", [128, D], BF16)
            self.ss = sb("ss", [128, 4], F32)
            nl = [0]

            def load(view, n_k, key):
                i = nl[0] % 2
                nl[0] += 1
                c.dma("sp", f"wtd{i}", wt[i][:, 0:n_k, :], view, reads=[key], writes=[("wtd", i)])
                return wt[i], ("wtd", i)
            c.dma("sp", "hb", hb[:], self.HT[:, :, t0:t0 + 512], reads=["HT"], writes=["hb"])
            c.dma("sp", "ob", ob[:], self.OT[:, :, t0:t0 + 512].rearrange("c p t -> p c t"), reads=[("OT", k) for k in range(24)], writes=["ob"])
            win = self.win_b[li].rearrange("(kc p) n -> p kc n", p=128)
            for dg in range(4):
                self.cvt_some(1)
                for br in range(3):
                    col = C_MG + br * 2048 + dg * 512
                    w, kw = load(win[:, :, col:col + 512], 16, ("win_b", li))
                    for j in range(4):
                        acc, ka = next_acc()
                        for kc in range(16):
                            c.op("pe", lambda e: e.matmul(acc[:], lhsT=w[:, kc, j * 128:(j + 1) * 128], rhs=hb[:, kc, :], start=(kc == 0), stop=(kc == 15)),
                                 reads=[kw, "hb"], writes=[ka])
                        c.op("act", lambda e: e.activation(out=sg[br][:, j, :], in_=acc[:], func=AF.Sigmoid), reads=[ka], writes=[("sg", br, j)])
                for br in range(3):
                    wb = self.wbr_b[li, br * 1024:(br + 1) * 1024, :].rearrange("(kc p) n -> p kc n", p=128)
                    w, kw = load(wb[:, :, dg * 512:(dg + 1) * 512], 8, ("wbr_b", li))
                    for j in range(4):
                        acc, ka = next_acc()
                        for c8 in range(8):
                            c.op("pe", lambda e: e.matmul(acc[:], lhsT=w[:, c8, j * 128:(j + 1) * 128], rhs=ob[:, br * 8 + c8, :], start=(c8 == 0), stop=(c8 == 7)),
                                 reads=[kw, "ob"], writes=[ka])
                        if br == 0:
                            c.op("dve", lambda e: e.tensor_tensor(out=macc[:, j, :], in0=acc[:], in1=sg[0][:, j, :], op=ALU.mult),
                                 reads=[ka, ("sg", 0, j)], writes=[("macc", j)])
                        else:
                            c.op("dve", lambda e: e.tensor_tensor(out=tmp[:], in0=acc[:], in1=sg[br][:, j, :], op=ALU.mult),
                                 reads=[ka, ("sg", br, j)], writes=["mtmp"])
                            c.op("pool", lambda e: e.tensor_tensor(out=macc[:, j, :], in0=macc[:, j, :], in1=tmp[:], op=ALU.add),
                                 reads=["mtmp", ("macc", j)], writes=[("macc", j)])
                        if br == 2:
                            c.op("act", lambda e: e.copy(out=mT[:, dg * 4 + j, :], in_=macc[:, j, :]), reads=[("macc", j)], writes=["mT"])
            wo = self.wout_b[li].rearrange("(kc p) n -> p kc n", p=128)
            for ng in range(4):
                w, kw = load(wo[:, :, ng * 512:(ng + 1) * 512], 16, ("wout_b", li))
                for ts in range(4):
                    acc, ka = next_acc()
                    for kc in range(16):
                        c.op("pe", lambda e: e.matmul(acc[:], lhsT=mT[:, kc, ts * 128:(ts + 1) * 128], rhs=w[:, kc, :], start=(kc == 0), stop=(kc == 15)),
                             reads=[kw, "mT"], writes=[ka])
                    xs = xblk[:, ts, ng * 512:(ng + 1) * 512]
                    c.op("dve", lambda e: e.tensor_tensor(out=xs, in0=acc[:], in1=xs, op=ALU.add), reads=[ka, kxb], writes=[kxb])
            for ts in range(4):
                self.norm_tile(xblk[:, ts, :], kxb, self.gffn[:, li, :], h2T, ts * 128, "h2T")
        with self.phase():
            aT = sb("aT", [128, 44, 512], BF16)
            wt = [sb("wtf", [128, 16, 512], BF16) for _ in range(3)]
            ub = [sb("ub", [128, 514], F32) for _ in range(2)]
            cc = [sb("cc", [128, 512], F32) for _ in range(2)]
            nl = [0]

            def load3(view, n_k, key):
                i = nl[0] % 3
                nl[0] += 1
                c.dma("sp", f"wtf{i}", wt[i][:, 0:n_k, :], view, reads=[key], writes=[("wtf", i)])
                return wt[i], ("wtf", i)
            wu = self.wup_b[li].rearrange("(kc p) n -> p kc n", p=128)
            for i4 in range(11):
                self.cvt_some(1)
                wg_, kwg = load3(wu[:, :, i4 * 512:(i4 + 1) * 512], 16, ("wup_b", li))
                wv_, kwv = load3(wu[:, :, DFF + i4 * 512:DFF + (i4 + 1) * 512], 16, ("wup_b", li))
                for j in range(4):
                    i = i4 * 4 + j
                    for side, (w, kw) in enumerate(((wg_, kwg), (wv_, kwv))):
                        ch = i + 44 * side
                        acc, ka = self.bk[2 + side], ("bk", 2 + side)
                        for kc in range(16):
                            c.op("pe", lambda e: e.matmul(acc[:], lhsT=w[:, kc, j * 128:(j + 1) * 128], rhs=h2T[:, kc, :], start=(kc == 0), stop=(kc == 15)),
                                 reads=[kw, "h2T"], writes=[ka])
                        u, ku = ub[side], ("ub", side)
                        cv, kc_ = cc[side], ("cc", side)
                        c.op("pool", lambda e: e.tensor_copy(out=u[:, 0:2], in_=carry[:, ch, :]), reads=[("carry", ch)], writes=[ku])
                        c.op("act", lambda e: e.copy(out=u[:, 2:514], in_=acc[:]), reads=[ka], writes=[ku])
                        c.op("pool", lambda e: e.tensor_copy(out=carry[:, ch, :], in_=u[:, 512:514]), reads=[ku], writes=[("carry", ch)])
                        cw = self.convw_t
                        c.op("pool", lambda e: e.tensor_scalar(out=cv[:], in0=u[:, 2:514], scalar1=cw[:, li, 2, ch:ch + 1], scalar2=self.convb_t[:, li, ch:ch + 1],
                                                               op0=ALU.mult, op1=ALU.add), reads=[ku, "convw", "convb"], writes=[kc_])
                        c.op("dve", lambda e: e.scalar_tensor_tensor(out=cv[:], in0=u[:, 1:513], scalar=cw[:, li, 1, ch:ch + 1], in1=cv[:], op0=ALU.mult, op1=ALU.add),
                             reads=[ku, "convw", kc_], writes=[kc_])
                        c.op("dve", lambda e: e.scalar_tensor_tensor(out=cv[:], in0=u[:, 0:512], scalar=cw[:, li, 0, ch:ch + 1], in1=cv[:], op0=ALU.mult, op1=ALU.add),
                             reads=[ku, "convw", kc_], writes=[kc_])
                    c.op("act", lambda e: e.activation(out=cc[0][:], in_=cc[0][:], func=AF.Silu), reads=[("cc", 0)], writes=[("cc", 0)])
                    c.op("dve", lambda e: e.tensor_tensor(out=aT[:, i, :], in0=cc[0][:], in1=cc[1][:], op=ALU.mult), reads=[("cc", 0), ("cc", 1)], writes=["aT"])
            wd = self.wdn_b[li].rearrange("(kc p) n -> p kc n", p=128)
            for ng in range(4):
                for kg in range(3):
                    nk = 16 if kg < 2 else 12
                    w, kw = load3(wd[:, kg * 16:kg * 16 + nk, ng * 512:(ng + 1) * 512], nk, ("wdn_b", li))
                    for ts in range(4):
                        acc, ka = self.bk[ts], ("bk", ts)
                        for k2 in range(nk):
                            kc = kg * 16 + k2
                            c.op("pe", lambda e: e.matmul(acc[:], lhsT=aT[:, kc, ts * 128:(ts + 1) * 128], rhs=w[:, k2, :], start=(kc == 0), stop=(kc == 43)),
                                 reads=[kw, "aT"], writes=[ka])
                for ts in range(4):
                    xs = xblk[:, ts, ng * 512:(ng + 1) * 512]
                    c.op("dve", lambda e: e.tensor_tensor(out=xs, in0=self.bk[ts][:], in1=xs, op=ALU.add), reads=[("bk", ts), kxb], writes=[kxb])
            c.dma("pool", "xout", xdst[s, t0:t0 + 512, :].rearrange("(ts p) n -> p ts n", p=128), xblk[:], reads=[kxb], writes=[("X", xdname, s, tb)])

    def build(self):
        c = self.c
        self._stacks = []
        self.setup_consts()
        self.eps_t = self.sb("eps", [128, 1], F32)
        c.op("pool", lambda e: e.memset(self.eps_t[:], EPS), writes=["eps"])
        self.ptr = [self.ps("ptr", [128, 8, 128], BF16) for _ in range(2)]
        self.bk = [self.ps("bk", [128, 512], F32) for _ in range(6)]
        self.acc = self.bk[0:2]
        self.pn = self.bk[2]
        self.sqb = self.sb("sqb", [128, 512], BF16)
        self.rt = self.sb("rt", [128, 512], F32)
        self.setup_tables()
        self.setup_tables2()
        self.cvt_queue = []
        for piece in self.convert_pieces(0):
            piece()
        for li in range(self.L):
            xsrc, xsname = (self.x, "xin") if li == 0 else (self.XS[(li - 1) % 2], f"XS{(li - 1) % 2}")
            xdst, xdname = (self.out, "out") if li == self.L - 1 else (self.XS[li % 2], f"XS{li % 2}")
            if li + 1 < self.L:
                self.cvt_queue = self.convert_pieces(li + 1)
            for s in range(NSEQ):
                with self.phase():
                    self.hT = self.sb("hT", [128, 16, T], BF16)
                    self.xt = [self.sb("xt", [128, D], F32) for _ in range(2)]
                    self.xn = self.sb("xn", [128, D], BF16)
                    self.sqj = self.sb("sqj", [128, D], BF16)
                    self.ss = self.sb("ss", [128, 4], F32)
                    self.wt = [self.sb("wt", [128, 16, 512], BF16) for _ in range(2)]
                    self.fst = [self.sb("fst", [128, T], BF16) for _ in range(2)]
                    self.fst32 = self.sb("fst32", [128, T], F32)
                    self.tst = [self.sb("tst", [128, 512], BF16) for _ in range(2)]
                    self.gst = self.sb("gst", [32, T], F32)
                    self.phase_a(li, s, xsrc, xsname)
                    c.dma("pool", "hts", self.HT[:, :, :], self.hT[:], reads=[("hT", t) for t in range(16)], writes=["HT"])
                    self.phase_b(li, s)
                if "stopB" in self.debug:
                    continue
                with self.phase():
                    self.phase_nsa(li, s)
                with self.phase():
                    self.phase_sb(li, s)
                with self.phase():
                    self.phase_hg(li, s)
                if "stopC" in self.debug:
                    continue
                with self.phase():
                    carry = self.sb("carry", [128, 88, 2], F32)
                    c.op("pool", lambda e: e.memset(carry[:], 0.0), writes=[("carry", ch) for ch in range(88)])
                    for tb in range(4):
                        with self.phase():
                            self.phase_d(li, s, tb, xsrc, xsname, xdst, xdname, carry)
            self.cvt_some(1000)
        c.finish()
        return self.nc


def _t5_bucket(dist):
    import math
    n = np.maximum(dist, 0)
    big = 16 + (np.log(np.maximum(n, 1).astype(np.float32) / np.float32(16)) / np.float32(math.log(8.0)) * np.float32(16)).astype(np.int32)
    return np.where(n < 16, n, np.minimum(big, 31))


def _static_tables(rel_bias):
    i = np.arange(128)[:, None]
    j = np.arange(128)[None, :]
    blocks = []
    for delta in (0, 128, 512):
        dist = delta + j - i
        blocks.append((dist, (dist >= 0) & (dist < 512)))
    for delta in (0, 128):
        dist = delta + j - i
        blocks.append((dist, dist >= 0))
    t = np.arange(T)[None, :]
    dist = t - 16 * i - 31
    blocks.append((dist, (dist >= 0) & (i < 127)))
    dist_all = np.concatenate([b[0] for b in blocks], axis=1)
    valid = np.concatenate([b[1] for b in blocks], axis=1)
    bucket = _t5_bucket(dist_all)
    ebias = np.empty((8, 128, dist_all.shape[1]), np.float32)
    for h in range(8):
        ebias[h] = np.where(valid, rel_bias[bucket, h], np.float32(-1e30))
    cb = np.ascontiguousarray(np.broadcast_to(rel_bias[31][None, :], (128, 8))).astype(np.float32)
    tpos = (8 + np.arange(8))[None, :, None] * 128 + np.arange(128)[:, None, None]
    qblk = tpos // 64
    jb = np.arange(32)[None, None, :]
    forced = (jb <= qblk) & ((jb == 0) | (jb >= qblk - 1))
    addmask = np.where(forced, 1e30, np.where(jb <= qblk, 0.0, -1e30)).astype(np.float32)
    esel = (np.arange(32)[:, None, None] == 2 * np.arange(16)[None, :, None] + (np.arange(128) // 64)[None, None, :])
    c_start = np.arange(127) * 16
    s_start = np.arange(32) * 64
    overlap = ((c_start[:, None] < s_start[None, :] + 64) & (c_start[:, None] + 32 > s_start[None, :]))
    ov33 = np.zeros((128, 33), np.float32)
    ov33[:127, :32] = overlap
    ov33[:, 32] = 1.0
    selg = (np.arange(24)[:, None, None] == np.arange(24)[None, :, None]) & np.ones((1, 1, 128), bool)
    return dict(ebias=ebias, cb=cb, addmask=addmask, esel=esel.astype(ml_dtypes.bfloat16),
                ov33=ov33.astype(ml_dtypes.bfloat16), selg=selg.astype(np.float32))


def prep_shared(inputs, layers):
    ls = list(layers)
    f = lambda a: np.ascontiguousarray(np.asarray(a, dtype=np.float32))
    d = _static_tables(f(inputs["rel_bias"]))
    d["w_in"] = f(inputs["w_in"])[ls]
    d["norm_attn"] = f(np.asarray(inputs["norm_attn"])[ls].reshape(len(ls), 16, 128).transpose(2, 0, 1))
    hg = np.concatenate([np.asarray(inputs["nsa_q_gain"])[ls][:, None, :], np.asarray(inputs["nsa_k_gain"])[ls]], axis=1)
    d["hgain"] = f(hg.transpose(2, 0, 1))
    d["cmp_w1"] = f(inputs["cmp_w1"])[ls]
    d["cmp_w2"] = f(inputs["cmp_w2"])[ls]
    d["posT"] = f(np.asarray(inputs["cmp_pos"])[ls].transpose(3, 0, 1, 2))
    d["hlb"] = f(np.asarray(inputs["hg_lower_bound"]).reshape(4, 8, 128).transpose(2, 0, 1))
    lcum = np.zeros((128, len(ls), 4), np.float32)
    for i, l in enumerate(ls):
        lcum[:, i, 1:l + 1] = 1.0
    d["lcum"] = lcum
    d["gn"] = f(np.broadcast_to(np.asarray(inputs["hg_norm_gain"])[ls][None, :, :], (128, len(ls), 128)))
    d["w_branch"] = f(np.asarray(inputs["w_branch"])[ls].reshape(len(ls), 3072, D))
    d["w_out"] = f(inputs["w_out"])[ls]
    d["norm_ffn"] = f(np.asarray(inputs["norm_ffn"])[ls].reshape(len(ls), 16, 128).transpose(2, 0, 1))
    d["w_up"] = f(inputs["w_up"])[ls]
    d["convw"] = f(np.asarray(inputs["conv_w"])[ls].reshape(len(ls), 3, 88, 128).transpose(3, 0, 1, 2))
    d["convb"] = f(np.asarray(inputs["conv_b"])[ls].reshape(len(ls), 88, 128).transpose(2, 0, 1))
    d["w_down"] = f(inputs["w_down"])[ls]
    return d


_PROGS = {}


def _get_prog(n_layers):
    if n_layers not in _PROGS:
        p = Prog(list(range(n_layers)))
        _PROGS[n_layers] = p.build()
    return _PROGS[n_layers]


FUSED = True


def kernel(**inputs):
    x = np.ascontiguousarray(np.asarray(inputs["x"], dtype=np.float32))
    n_cores = 8
    if FUSED:
        groups = [list(range(DEPTH))]
    else:
        groups = [[l] for l in range(DEPTH)]
    cur = x
    for ls in groups:
        nc = _get_prog(len(ls))
        shared = prep_shared(inputs, ls)
        in_maps = [dict(shared, x=np.ascontiguousarray(cur[NSEQ * i:NSEQ * (i + 1)])) for i in range(n_cores)]
        res = run_bass_kernel_spmd(nc, in_maps, core_ids=list(range(n_cores)))
        cur = np.concatenate([np.asarray(r["out"], dtype=np.float32) for r in res.results], axis=0)
    return cur
```

```python
import numpy as np
from contextlib import ExitStack
import ml_dtypes
import concourse.bass as bass
import concourse.mybir as mybir
from concourse.bass_utils import run_bass_kernel_spmd

F32 = mybir.dt.float32
BF16 = mybir.dt.bfloat16
AF = mybir.ActivationFunctionType
ALU = mybir.AluOpType

D = 2048
T = 2048
NSEQ = 2
DEPTH = 4
HD = 128
IN_COLS = 15896
DFF = 5632
EPS = 1e-6
SCALE = HD ** -0.5
C_QN, C_KC, C_VC, C_KS, C_VS, C_KW, C_VW, C_G = 0, 1024, 1280, 1536, 1792, 2048, 2304, 2560
C_SQ, C_SK, C_SV = 2584, 3608, 4632
C_HQ, C_HF, C_HI, C_HG = 5656, 6680, 7704, 8728
C_MG = 9752


class Ctx:
    def __init__(self, nc):
        self.nc = nc
        self.E = dict(pe=nc.tensor, act=nc.scalar, dve=nc.vector, pool=nc.gpsimd, sp=nc.sync)
        self.sem = {n: nc.alloc_semaphore("s_" + n) for n in self.E}
        self.cnt = {n: 0 for n in self.E}
        self.seen = {n: {} for n in self.E}
        self.res = {}
        self.slots = {}
        self.nwait = 0

    def _slot(self, name):
        if name not in self.slots:
            self.slots[name] = [self.nc.alloc_semaphore("d_" + name), 0]
        return self.slots[name]

    def _need(self, reads, writes):
        need = {}

        def add(p, c):
            if need.get(p, 0) < c:
                need[p] = c
        for k in reads:
            r = self.res.get(k)
            if r and r[0]:
                add(*r[0])
        for k in writes:
            r = self.res.get(k)
            if r:
                if r[0]:
                    add(*r[0])
                for p, c in r[1].items():
                    add(p, c)
        return need

    def _wait(self, eng, need):
        for p, c in need.items():
            if p == eng and eng in ("pe",):
                continue
            if self.seen[eng].get(p, 0) >= c:
                continue
            if isinstance(p, tuple):
                self.E[eng].wait_ge(self.slots[p[1]][0], 16 * c)
            else:
                self.E[eng].wait_ge(self.sem[p], c)
            self.nwait += 1
            self.seen[eng][p] = c

    def _mark(self, who, c, reads, writes):
        for k in reads:
            r = self.res.setdefault(k, [None, {}])
            r[1][who] = c
        for k in writes:
            self.res[k] = [(who, c), {}]

    def op(self, eng, fn, reads=(), writes=()):
        self._wait(eng, self._need(reads, writes))
        ins = fn(self.E[eng])
        self.cnt[eng] += 1
        ins.then_inc(self.sem[eng], 1)
        self._mark(eng, self.cnt[eng], reads, writes)
        return ins

    def dma(self, q, slot, out, in_, reads=(), writes=(), **kw):
        self._wait(q, self._need(reads, writes))
        s = self._slot(slot)
        ins = self.E[q].dma_start(out=out, in_=in_, **kw)
        s[1] += 1
        ins.then_inc(s[0], 16)
        self._mark(("dma", slot), s[1], reads, writes)
        return ins

    def barrier(self):
        for eng in self.E:
            need = {}
            for n in self.E:
                if n != eng and self.cnt[n]:
                    need[n] = self.cnt[n]
            for name, (sem, cc) in self.slots.items():
                if cc:
                    need[("dma", name)] = cc
            self._wait(eng, need)

    def finish(self):
        for name, (sem, c) in self.slots.items():
            if c:
                self.E["sp"].wait_ge(sem, 16 * c)
        for n in self.E:
            if n != "sp" and self.cnt[n]:
                self.E["sp"].wait_ge(self.sem[n], self.cnt[n])


class Prog:
    def __init__(self, layers, debug=()):
        self.layers = list(layers)
        self.debug = set(debug)
        nc = self.nc = bass.Bass("TRN2", target_bir_lowering=False)
        self.c = Ctx(nc)
        self._n = 0
        L = len(self.layers)
        self.L = L
        di = lambda name, shape, dt=F32: nc.dram_tensor(name, list(shape), dt, kind="ExternalInput").ap()
        self.x = di("x", [NSEQ, T, D])
        self.w_in = di("w_in", [L, D, IN_COLS])
        self.norm_attn = di("norm_attn", [128, L, 16])
        self.hgain = di("hgain", [128, L, 4])
        self.ebias = di("ebias", [8, 128, 2688])
        self.cb = di("cb", [128, 8])
        self.addmask = di("addmask", [128, 8, 32])
        self.esel = di("esel", [32, 16, 128], BF16)
        self.ov33 = di("ov33", [128, 33], BF16)
        self.selg = di("selg", [24, 24, 128])
        self.cmp_w1 = di("cmp_w1", [L, 2, 32, 128, 128])
        self.cmp_w2 = di("cmp_w2", [L, 2, 128, 128])
        self.posT = di("posT", [128, L, 2, 32])
        self.hlb = di("hlb", [128, 4, 8])
        self.lcum = di("lcum", [128, L, 4])
        self.gn = di("gn", [128, L, 128])
        self.w_branch = di("w_branch", [L, 3072, D])
        self.w_out = di("w_out", [L, D, D])
        self.norm_ffn = di("norm_ffn", [128, L, 16])
        self.w_up = di("w_up", [L, D, 2 * DFF])
        self.convw = di("convw", [128, L, 3, 88])
        self.convb = di("convb", [128, L, 88])
        self.w_down = di("w_down", [L, DFF, D])
        self.wbr_b = self.scratch("wbr_b", [L, 3072, D], BF16)
        self.wout_b = self.scratch("wout_b", [L, D, D], BF16)
        self.wup_b = self.scratch("wup_b", [L, D, 2 * DFF], BF16)
        self.wdn_b = self.scratch("wdn_b", [L, DFF, D], BF16)
        self.XS = self.scratch("XS", [2, NSEQ, T, D], F32)
        self.EBD = self.scratch("EBD", [8, 128, 2688], F32)
        self.EBA = self.scratch("EBA", [8, 128, 640], BF16)
        self.OT = self.scratch("OT", [24, 128, T], BF16)
        self.out = nc.dram_tensor("out", [NSEQ, T, D], F32, kind="ExternalOutput").ap()
        self.win_b = self.scratch("win_b", [L, D, IN_COLS], BF16)
        self.HT = self.scratch("HT", [128, 16, T], BF16)
        self.FM = self.scratch("FM", [40, 128, T], BF16)
        self.HF = self.scratch("HF", [8, 128, T], F32)
        self.GS = self.scratch("GS", [24, T], F32)
        self.TM = self.scratch("TM", [T, 3584], BF16)

    def scratch(self, name, shape, dt):
        kind = "ExternalOutput" if name in self.debug else "Internal"
        return self.nc.dram_tensor(name, list(shape), dt, kind=kind).ap()

    def sb(self, name, shape, dt):
        self._n += 1
        if getattr(self, "_stacks", None):
            return self._stacks[-1].enter_context(self.nc.sbuf_tensor(f"{name}_{self._n}", list(shape), dt))
        return self.nc.alloc_sbuf_tensor(f"{name}_{self._n}", list(shape), dt)

    def phase(self):
        prog = self

        class _P:
            def __enter__(self_):
                prog._stacks.append(ExitStack())
                return self_

            def __exit__(self_, *a):
                prog.c.barrier()
                prog._stacks.pop().close()
                return False
        return _P()

    def ps(self, name, shape, dt=F32):
        self._n += 1
        return self.nc.alloc_psum_tensor(f"{name}_{self._n}", list(shape), dt)

    def convert_pieces(self, li):
        c = self.c
        pieces = []

        def mk(dst, src, key):
            return lambda: c.dma("pool", "cvt_" + key[0], dst, src, writes=[key])
        for i in range(D // 128):
            src = self.w_in[li, i * 128:(i + 1) * 128, :].rearrange("p (a b) -> p a b", b=1987)
            dst = self.win_b[li, i * 128:(i + 1) * 128, :].rearrange("p (a b) -> p a b", b=1987)
            pieces.append(mk(dst, src, ("win_b", li)))
        for srcw, dstw, rows, key in ((self.w_branch, self.wbr_b, 3072, "wbr_b"), (self.w_out, self.wout_b, D, "wout_b"),
                                      (self.w_up, self.wup_b, D, "wup_b"), (self.w_down, self.wdn_b, DFF, "wdn_b")):
            for i in range(rows // 128):
                src = srcw[li, i * 128:(i + 1) * 128, :].rearrange("p (a b) -> p a b", b=1024)
                dst = dstw[li, i * 128:(i + 1) * 128, :].rearrange("p (a b) -> p a b", b=1024)
                pieces.append(mk(dst, src, (key, li)))
        return pieces

    def cvt_some(self, n=1):
        for _ in range(n):
            if self.cvt_queue:
                self.cvt_queue.pop(0)()

    def setup_consts(self):
        c = self.c
        self.ident = self.sb("ident", [128, 128], BF16)
        self.ones_b = self.sb("ones", [128, 128], BF16)
        self.gattn = self.sb("gattn", [128, self.L, 16], F32)
        idf = self.sb("idf", [128, 128], F32)
        c.op("pool", lambda e: e.memset(idf[:], 1.0), writes=["idf"])
        c.op("pool", lambda e: e.affine_select(out=idf[:], in_=idf[:], pattern=[[-1, 128]], compare_op=ALU.is_equal,
                                               fill=0.0, base=0, channel_multiplier=1), reads=["idf"], writes=["idf"])
        c.op("pool", lambda e: e.tensor_copy(out=self.ident[:], in_=idf[:]), reads=["idf"], writes=["ident"])
        c.op("pool", lambda e: e.memset(self.ones_b[:], 1.0), writes=["ones"])
        c.dma("sp", "cst", self.gattn[:], self.norm_attn[:, :, :], writes=["gattn"])
        self.hgain_t = self.sb("hgain", [128, self.L, 4], F32)
        c.dma("sp", "cst2", self.hgain_t[:], self.hgain[:, :, :], writes=["hgain"])

    def norm_tile(self, xt, kx, gains, dstT, col0, kdst):
        c = self.c
        self.nnorm = getattr(self, "nnorm", 0) + 1
        b0, b1, b2 = self.nnorm % len(self.ss), self.nnorm % len(self.sqj), self.nnorm % len(self.xn)
        ss, sq, xn = self.ss[b0], self.sqj[b1], self.xn[b2]
        kss, ksq, kxn = ("ss", b0), ("sqj", b1), ("xn", b2)
        c.op("act", lambda e: e.activation(out=sq[:], in_=xt, func=AF.Square, accum_out=ss[:, 0:1]), reads=[kx], writes=[ksq, kss])
        c.op("act", lambda e: e.activation(out=ss[:, 1:2], in_=ss[:, 0:1], func=AF.Sqrt, scale=1.0 / D, bias=self.eps_t[:, 0:1]),
             reads=[kss, "eps"], writes=[kss])
        c.op("dve", lambda e: e.reciprocal(out=ss[:, 2:3], in_=ss[:, 1:2]), reads=[kss], writes=[kss])
        c.op("dve", lambda e: e.tensor_scalar(out=xn[:], in0=xt, scalar1=ss[:, 2:3], scalar2=None, op0=ALU.mult), reads=[kx, kss], writes=[kxn])
        for half in range(2):
            pt = self.ptr[half]
            kp = ("ptr", half)
            for j in range(8):
                kc = half * 8 + j
                c.op("pe", lambda e: e.transpose(out=pt[:, j, :], in_=xn[:, kc * 128:(kc + 1) * 128], identity=self.ident[:]),
                     reads=[kxn, "ident"], writes=[kp])
            for j in range(8):
                kc = half * 8 + j
                dst = dstT[:, kc, col0:col0 + 128]
                g = gains[:, kc:kc + 1]
                if j % 2 == 0:
                    c.op("act", lambda e: e.activation(out=dst, in_=pt[:, j, :], func=AF.Copy, scale=g), reads=[kp, "gattn", "gffn"], writes=[kdst])
                else:
                    c.op("dve", lambda e: e.tensor_scalar(out=dst, in0=pt[:, j, :], scalar1=g, scalar2=None, op0=ALU.mult),
                         reads=[kp, "gattn", "gffn"], writes=[kdst])

    def phase_a(self, li, s, xsrc, xname):
        c = self.c
        for tt in range(16):
            xt = self.xt[tt % 3]
            kx = ("xt", tt % 3)
            c.dma("sp", f"xt{tt % 3}", xt[:], xsrc[s, tt * 128:(tt + 1) * 128, :], reads=[("X", xname, s, tt // 4)], writes=[kx])
            self.norm_tile(xt[:], kx, self.gattn[:, li, :], self.hT, tt * 128, ("hT", tt))

    def phase_b(self, li, s):
        c = self.c
        hT = self.hT
        hT_keys = [("hT", t) for t in range(16)]
        wsrc = self.win_b[li].rearrange("(kc p) n -> p kc n", p=128)
        nload = [0]

        def load_w(col, n):
            i = nload[0] % 2
            nload[0] += 1
            wt = self.wt[i]
            c.dma("sp", f"wt{i}", wt[:, :, 0:n], wsrc[:, :, col:col + n], reads=[("win_b", li)], writes=[("wt", i)])
            return wt, ("wt", i)

        nacc = [0]

        def next_acc():
            i = nacc[0] % 2
            nacc[0] += 1
            return self.acc[i], ("acc", i)

        nst = [0]
        fm_jobs = [
            (C_QN, 8, "hnorm", 0, 0), (C_KC, 2, "plain", 8, None), (C_VC, 2, "plain", 10, None),
            (C_KS, 2, "hnorm", 12, 2), (C_KW, 2, "hnorm", 14, 3),
            (C_SQ, 8, "plainS", 16, None), (C_SK, 8, "plain", 24, None), (C_HQ, 8, "plain", 32, None),
            (C_HF, 8, "plain32", 0, None),
        ]
        for col0, nch, kind, fm0, gi in fm_jobs:
            for c4 in range(0, nch, 4):
                ncc = min(4, nch - c4)
                wt, kw = load_w(col0 + c4 * 128, ncc * 128)
                for j in range(ncc):
                    ch = c4 + j
                    if kind == "plain32":
                        st, kst = self.fst32, "fst32"
                    else:
                        st, kst = self.fst[nst[0] % 2], ("fst", nst[0] % 2)
                        nst[0] += 1
                    for tb in range(4):
                        acc, ka = next_acc()
                        for kc in range(16):
                            c.op("pe", lambda e: e.matmul(acc[:], lhsT=wt[:, kc, j * 128:(j + 1) * 128], rhs=hT[:, kc, tb * 512:(tb + 1) * 512],
                                                          start=(kc == 0), stop=(kc == 15)),
                                 reads=[kw] + hT_keys, writes=[ka])
                        dst = st[:, tb * 512:(tb + 1) * 512]
                        if kind == "plainS":
                            c.op("act", lambda e: e.activation(out=dst, in_=acc[:], func=AF.Copy, scale=SCALE), reads=[ka], writes=[kst])
                        elif kind == "plain":
                            eng = "act" if tb % 2 == 0 else "dve"
                            if eng == "act":
                                c.op("act", lambda e: e.copy(out=dst, in_=acc[:]), reads=[ka], writes=[kst])
                            else:
                                c.op("dve", lambda e: e.tensor_copy(out=dst, in_=acc[:]), reads=[ka], writes=[kst])
                        elif kind == "plain32":
                            c.op("act", lambda e: e.copy(out=dst, in_=acc[:]), reads=[ka], writes=[kst])
                        else:
                            self.hnorm_block(acc, ka, dst, kst, self.hgain_t[:, li, gi:gi + 1])
                    if kind == "plain32":
                        c.dma("act", "fst32", self.HF[ch, :, :], st[:], reads=[kst], writes=[("HF", ch)])
                    else:
                        c.dma("act", f"fst{kst[1]}", self.FM[fm0 + ch, :, :], st[:], reads=[kst], writes=[("FM", fm0 + ch)])
        wt, kw = load_w(C_G, 24)
        for tb in range(4):
            acc, ka = next_acc()
            for kc in range(16):
                c.op("pe", lambda e: e.matmul(acc[0:24, :], lhsT=wt[:, kc, 0:24], rhs=hT[:, kc, tb * 512:(tb + 1) * 512],
                                              start=(kc == 0), stop=(kc == 15)), reads=[kw] + hT_keys, writes=[ka])
            c.op("act", lambda e: e.activation(out=self.gst[0:24, tb * 512:(tb + 1) * 512], in_=acc[0:24, :], func=AF.Sigmoid),
                 reads=[ka], writes=["gst"])
        c.dma("act", "gst", self.GS[:, :], self.gst[0:24, :], reads=["gst"], writes=["GS"])
        tm_jobs = [(C_VS, 256, 0, False), (C_VW, 256, 256, False), (C_SV, 1024, 512, False),
                   (C_HI, 1024, 1536, False), (C_HG, 1024, 2560, True)]
        nts = [0]
        for col0, ncols, tm0, sig in tm_jobs:
            for c0 in range(0, ncols, 512):
                n = min(512, ncols - c0)
                wt, kw = load_w(col0 + c0, n)
                for tt in range(16):
                    acc, ka = next_acc()
                    for kc in range(16):
                        c.op("pe", lambda e: e.matmul(acc[:, 0:n], lhsT=hT[:, kc, tt * 128:(tt + 1) * 128], rhs=wt[:, kc, 0:n],
                                                      start=(kc == 0), stop=(kc == 15)), reads=[kw, ("hT", tt)], writes=[ka])
                    i = nts[0] % 2
                    nts[0] += 1
                    st, kst = self.tst[i], ("tst", i)
                    if sig:
                        c.op("act", lambda e: e.activation(out=st[:, 0:n], in_=acc[:, 0:n], func=AF.Sigmoid), reads=[ka], writes=[kst])
                    elif tt % 2 == 0:
                        c.op("act", lambda e: e.copy(out=st[:, 0:n], in_=acc[:, 0:n]), reads=[ka], writes=[kst])
                    else:
                        c.op("dve", lambda e: e.tensor_copy(out=st[:, 0:n], in_=acc[:, 0:n]), reads=[ka], writes=[kst])
                    c.dma("act", f"tst{i}", self.TM[tt * 128:(tt + 1) * 128, tm0 + c0:tm0 + c0 + n], st[:, 0:n],
                          reads=[kst], writes=[("TM", tm0 + c0, tt)])

    def hnorm_block(self, acc, ka, dst, kdst, gain):
        c = self.c
        c.op("act", lambda e: e.activation(out=self.sqb[:], in_=acc[:], func=AF.Square), reads=[ka], writes=["sqb"])
        c.op("pe", lambda e: e.matmul(self.pn[:], lhsT=self.ones_b[:], rhs=self.sqb[:], start=True, stop=True),
             reads=["sqb", "ones"], writes=["pn"])
        c.op("act", lambda e: e.activation(out=self.rt[:], in_=self.pn[:], func=AF.Sqrt, scale=1.0 / HD, bias=self.eps_t[:, 0:1]),
             reads=["pn", "eps"], writes=["rt"])
        c.op("dve", lambda e: e.reciprocal(out=self.rt[:], in_=self.rt[:]), reads=["rt"], writes=["rt"])
        c.op("dve", lambda e: e.scalar_tensor_tensor(out=dst, in0=acc[:], scalar=gain, in1=self.rt[:], op0=ALU.mult, op1=ALU.mult),
             reads=[ka, "rt", "hgain"], writes=[kdst])

    def setup_tables(self):
        c = self.c
        self.cb_t = self.sb("cb", [128, 8], F32)
        self.negc = self.sb("negc", [128, 8], F32)
        c.dma("sp", "cst3", self.cb_t[:], self.cb[:, :], writes=["cb"])
        c.op("dve", lambda e: e.tensor_scalar(out=self.negc[:], in0=self.cb_t[:], scalar1=-1.0, scalar2=None, op0=ALU.mult),
             reads=["cb"], writes=["negc"])
        self.addmask_t = self.sb("addmask", [128, 8, 32], F32)
        c.dma("sp", "cst4", self.addmask_t[:], self.addmask[:, :, :], writes=["addmask"])
        self.esel_t = self.sb("esel", [32, 16, 128], BF16)
        c.dma("sp", "cst5", self.esel_t[:], self.esel[:, :, :], writes=["esel"])
        self.ov33_t = self.sb("ov33", [128, 33], BF16)
        c.dma("sp", "cst6", self.ov33_t[:], self.ov33[:, :], writes=["ov33"])
        self.posT_t = self.sb("posT", [128, self.L, 2, 32], F32)
        c.dma("sp", "cst8", self.posT_t[:], self.posT[:, :, :, :], writes=["posT"])
        self.posb = self.sb("posb", [128, self.L, 2, 32], BF16)
        c.op("dve", lambda e: e.tensor_copy(out=self.posb[:], in_=self.posT_t[:]), reads=["posT"], writes=["posb"])
        self.idf = self.sb("idf2", [128, 128], F32)
        c.op("pool", lambda e: e.memset(self.idf[:], 1.0), writes=["idf2"])
        c.op("pool", lambda e: e.affine_select(out=self.idf[:], in_=self.idf[:], pattern=[[-1, 128]], compare_op=ALU.is_equal,
                                               fill=0.0, base=0, channel_multiplier=1), reads=["idf2"], writes=["idf2"])
        with self.phase():
            eb = self.sb("ebl", [128, 2688], F32)
            eba_s = self.sb("eba_s", [128, 640], BF16)
            for h in range(8):
                c.dma("sp", "ebl", eb[:], self.ebias[h, :, :], writes=["ebl"])
                c.op("dve", lambda e: e.tensor_scalar(out=eba_s[:], in0=eb[:, 0:640], scalar1=self.negc[:, h:h + 1], scalar2=1.0 / SCALE,
                                                      op0=ALU.add, op1=ALU.mult), reads=["ebl", "negc"], writes=["eba_s"])
                c.dma("sp", "ebas", self.EBA[h, :, :], eba_s[:], reads=["eba_s"], writes=[("EBA", h)])
                c.op("act", lambda e: e.activation(out=eb[:], in_=eb[:], func=AF.Exp, bias=self.negc[:, h:h + 1]),
                     reads=["ebl", "negc"], writes=["ebl"])
                c.dma("sp", "ebs", self.EBD[h, :, :], eb[:], reads=["ebl"], writes=[("EBD", h)])

    def gelu_tanh(self, dst, kdst, src_ps, ksrc, bias, n):
        c = self.c
        x, x2 = self.gx, self.gx2
        c.op("act", lambda e: e.activation(out=x[:, 0:n], in_=src_ps, func=AF.Identity, bias=bias), reads=[ksrc, "cbias"], writes=["gx"])
        c.op("dve", lambda e: e.tensor_tensor(out=x2[:, 0:n], in0=x[:, 0:n], in1=x[:, 0:n], op=ALU.mult), reads=["gx"], writes=["gx2"])
        c.op("dve", lambda e: e.tensor_scalar(out=x2[:, 0:n], in0=x2[:, 0:n], scalar1=0.044715, scalar2=1.0, op0=ALU.mult, op1=ALU.add),
             reads=["gx2"], writes=["gx2"])
        c.op("dve", lambda e: e.tensor_tensor(out=x2[:, 0:n], in0=x2[:, 0:n], in1=x[:, 0:n], op=ALU.mult), reads=["gx", "gx2"], writes=["gx2"])
        c.op("act", lambda e: e.activation(out=x2[:, 0:n], in_=x2[:, 0:n], func=AF.Sigmoid, scale=1.5957691216), reads=["gx2"], writes=["gx2"])
        c.op("dve", lambda e: e.tensor_tensor(out=dst, in0=x2[:, 0:n], in1=x[:, 0:n], op=ALU.mult), reads=["gx", "gx2"], writes=[kdst])

    def compress(self, li, j, xT, kx, w1b, w2b, hid):
        c = self.c
        b5 = self.bk[5]
        k5 = ("bk", 5)
        c.dma("pool", "w1b", w1b[:], self.cmp_w1[li, j].rearrange("l d e -> d l e"), writes=["w1b"])
        c.dma("pool", "w2b", w2b[:], self.cmp_w2[li, j], writes=["w2b"])
        for l in range(32):
            c.op("pe", lambda e: e.matmul(b5[:, 0:127], lhsT=w1b[:, l, :], rhs=xT[:, l:l + 16 * 126 + 1:16], start=(l == 0), stop=(l == 31)),
                 reads=["w1b", kx], writes=[k5])
        for l in range(32):
            c.op("pe", lambda e: e.matmul(b5[:, 128:129], lhsT=w1b[:, l, :], rhs=self.posb[:, li, j, l:l + 1], start=False, stop=(l == 31)),
                 reads=["w1b", "posb"], writes=[k5])
        c.op("dve", lambda e: e.tensor_copy(out=self.cbias[:], in_=b5[:, 128:129]), reads=[k5], writes=["cbias"])
        c.op("pool", lambda e: e.memset(hid[:], 0.0), writes=["hid"])
        self.gelu_tanh(hid[:, 0:127], "hid", b5[:, 0:127], k5, self.cbias[:, 0:1], 127)

    def gate_bcast(self, krow, cols):
        c = self.c
        n = cols[1] - cols[0]
        c.op("pe", lambda e: e.matmul(self.bk[4][:, 0:n], lhsT=self.selg_t[:, krow, :], rhs=self.gsb[0:24, cols[0]:cols[1]], start=True, stop=True),
             reads=["selg", "gsb"], writes=[("bk", 4)])

    def attn_finish(self, h, br, G, oacc, first, guard=False, uz=(2, 3)):
        c = self.c
        U, kU = self.bk[uz[0]], ("bk", uz[0])
        Z, kZ = self.bk[uz[1]], ("bk", uz[1])
        i = self.nfin % 2
        self.nfin += 1
        w, kw_ = self.wvs[i], ("wv", i)
        tmp, kt_ = self.tmps[i], ("tmpv", i)
        gb = self.gbs[br][:, G * 512:(G + 1) * 512]
        if guard:
            c.op("dve", lambda e: e.tensor_scalar(out=w[:], in0=Z[:], scalar1=1e-30, scalar2=None, op0=ALU.max), reads=[kZ], writes=[kw_])
            c.op("dve", lambda e: e.reciprocal(out=w[:], in_=w[:]), reads=[kw_], writes=[kw_])
        else:
            c.op("dve", lambda e: e.reciprocal(out=w[:], in_=Z[:]), reads=[kZ], writes=[kw_])
        c.op("pool", lambda e: e.tensor_tensor(out=w[:], in0=gb, in1=w[:], op=ALU.mult), reads=[kw_, ("gbs", br)], writes=[kw_])
        dst = oacc[:, G * 512:(G + 1) * 512]
        ko = ("oacc", id(oacc), G)
        if first:
            c.op("dve", lambda e: e.tensor_tensor(out=dst, in0=U[:], in1=w[:], op=ALU.mult), reads=[kw_, kU], writes=[ko])
        else:
            c.op("dve", lambda e: e.tensor_tensor(out=tmp[:], in0=U[:], in1=w[:], op=ALU.mult), reads=[kw_, kU], writes=[kt_])
            c.op("pool", lambda e: e.tensor_tensor(out=dst, in0=dst, in1=tmp[:], op=ALU.add), reads=[kt_, ko], writes=[ko])

    def gate_rows(self, h, brs):
        c = self.c
        for br in brs:
            row = h * 3 + br
            c.dma("sp", f"gbs{br}", self.gbs[br][:], self.GS[row:row + 1, :].partition_broadcast(128), reads=["GS"], writes=[("gbs", br)])

    def banded_attn(self, h, G, qT, kq, kT, kk, V, kv, eb, keb, mode, nmT=None, uz=(2, 3)):
        c = self.c
        if mode == "win":
            kts = [kt for kt in range(max(0, 4 * G - 4), 4 * G + 4)]
        else:
            kts = list(range(0, 4 * G + 4))
        use_mask = (mode == "sel" and G >= 2)
        U, kU = self.bk[uz[0]], ("bk", uz[0])
        Z, kZ = self.bk[uz[1]], ("bk", uz[1])
        info = []
        for kt in kts:
            qlo = max(kt, 4 * G)
            qhi = min(kt + 4, 4 * G + 3) if mode == "win" else 4 * G + 3
            c0, c1 = (qlo - 4 * G) * 128, (qhi - 4 * G + 1) * 128
            i = self.nS % 2
            self.nS += 1
            info.append((kt, qlo, qhi, c0, c1, i))

        def stage_a(k):
            kt, qlo, qhi, c0, c1, i = info[k]
            n = c1 - c0
            S, kS = self.bk[i], ("bk", i)
            P, kP = self.P[i], ("P", i)
            subs = []
            for qb in range(qlo, qhi + 1):
                d = qb - kt
                if mode == "win":
                    ti = {0: 0, 1: 1, 4: 2}.get(d)
                    off = 0
                else:
                    ti = {0: 0, 1: 1}.get(d)
                    off = 384
                if ti is None:
                    continue
                subs.append(((qb - qlo) * 128, off + ti * 128))
            c.op("pe", lambda e: e.matmul(S[:, 0:n], lhsT=kT[:, kt * 128:(kt + 1) * 128], rhs=qT[:, G * 512 + c0:G * 512 + c1],
                                          start=True, stop=False), reads=[kk, kq], writes=[kS])
            if use_mask:
                c.op("pe", lambda e: e.matmul(S[:, 0:n], lhsT=self.esel_t[:, kt, :], rhs=nmT[:, G * 512 + c0 - 1024:G * 512 + c1 - 1024],
                                              start=False, stop=False), reads=["esel", "nmT"], writes=[kS])
            for (pc, tc) in subs:
                c.op("pe", lambda e: e.matmul(S[:, pc:pc + 128], lhsT=self.ident[:], rhs=self.eba[:, tc:tc + 128], start=False, stop=False),
                     reads=["ident", "eba"], writes=[kS])
            c.op("pe", lambda e: e.matmul(S[0:32, 0:2], lhsT=self.esel_t[:, 0, 0:32], rhs=self.zero32[:, 0:2], start=False, stop=True),
                 reads=["esel", "zero32"], writes=[kS])
            c.op("act", lambda e: e.activation(out=P[:, 0:n], in_=S[:, 0:n], func=AF.Exp, scale=SCALE, bias=self.cb_t[:, h:h + 1]),
                 reads=[kS, "cb"], writes=[kP])

        def stage_b(k):
            kt, qlo, qhi, c0, c1, i = info[k]
            n = c1 - c0
            P, kP = self.P[i], ("P", i)
            c.op("pe", lambda e: e.matmul(U[:, c0:c1], lhsT=V[:, kt, :], rhs=P[:, 0:n], start=(k == 0), stop=(k == len(info) - 1)),
                 reads=[kv, kP], writes=[kU])
            c.op("pe", lambda e: e.matmul(Z[:, c0:c1], lhsT=self.ones_b[:], rhs=P[:, 0:n], start=(k == 0), stop=(k == len(info) - 1)),
                 reads=["ones", kP], writes=[kZ])
        stage_a(0)
        for k in range(len(info)):
            if k + 1 < len(info):
                stage_a(k + 1)
            stage_b(k)

    def phase_nsa(self, li, s):
        c = self.c
        sb = self.sb
        self.gsb = sb("gsb", [32, T], F32)
        self.wvs = [sb("wv", [128, 512], F32) for _ in range(2)]
        self.tmps = [sb("tmpv", [128, 512], F32) for _ in range(2)]
        self.tmpv = self.tmps[0]
        self.gbs = [sb("gbs", [128, T], F32) for _ in range(3)]
        self.nfin = 0
        self.P = [sb("P", [128, 512], BF16) for _ in range(2)]
        self.gx = sb("gx", [128, 128], F32)
        self.gx2 = sb("gx2", [128, 128], F32)
        self.cbias = sb("cbias", [128, 1], F32)
        self.nS = 0
        self.cexp = sb("cexp", [128, 512], F32)
        w1b = sb("w1b", [128, 32, 128], BF16)
        w2b = sb("w2b", [128, 128], BF16)
        xk = sb("xk", [128, T], BF16)
        xv = sb("xv", [128, T], BF16)
        hid = sb("hid", [128, 128], BF16)
        kcmpT = sb("kcmpT", [128, 128], BF16)
        vcmp = sb("vcmp", [128, 1, 128], BF16)
        ksT = sb("ksT", [128, T], BF16)
        kwT = sb("kwT", [128, T], BF16)
        vs = sb("vs", [128, 16, 128], BF16)
        vw = sb("vw", [128, 16, 128], BF16)
        qT = [sb("qT", [128, T], BF16) for _ in range(4)]
        oacc = [sb("oacc", [128, T], F32) for _ in range(4)]
        obf = sb("obf", [128, T], BF16)
        eb = sb("eb", [128, 2688], F32)
        eba4 = sb("eba", [128, 4, 640], BF16)
        self.zero32 = sb("zero32", [32, 2], BF16)
        c.op("pool", lambda e: e.memset(self.zero32[:], 0.0), writes=["zero32"])
        impacc = sb("impacc", [128, 8, 32], F32)
        imps = sb("imps", [128, 40], F32)
        sc = sb("sc", [128, 32], F32)
        sc2 = sb("sc2", [128, 32], F32)
        m8 = sb("m8", [128, 16], F32)
        nmT = sb("nmT", [32, T // 2], BF16)
        b5, k5 = self.bk[5], ("bk", 5)
        c.dma("sp", "gsb", self.gsb[0:24, :], self.GS[:, :], reads=["GS"], writes=["gsb"])
        for g in range(2):
            c.dma("sp", "xk", xk[:], self.FM[8 + g, :, :], reads=[("FM", 8 + g)], writes=["xk"])
            c.dma("sp", "xv", xv[:], self.FM[10 + g, :, :], reads=[("FM", 10 + g)], writes=["xv"])
            c.dma("sp", "ksT", ksT[:], self.FM[12 + g, :, :], reads=[("FM", 12 + g)], writes=["ksT"])
            c.dma("sp", "kwT", kwT[:], self.FM[14 + g, :, :], reads=[("FM", 14 + g)], writes=["kwT"])
            tmv = self.TM.rearrange("(kt p) n -> p kt n", p=128)
            c.dma("sp", "vs", vs[:], tmv[:, :, g * 128:(g + 1) * 128], reads=[("TM", 0, t) for t in range(16)], writes=["vs"])
            c.dma("sp", "vw", vw[:], tmv[:, :, 256 + g * 128:256 + (g + 1) * 128], reads=[("TM", 256, t) for t in range(16)], writes=["vw"])
            self.compress(li, 0, xk, "xk", w1b, w2b, hid)
            c.op("pe", lambda e: e.matmul(b5[:, 256:384], lhsT=w2b[:], rhs=hid[:], start=True, stop=True), reads=["w2b", "hid"], writes=[k5])
            c.op("act", lambda e: e.activation(out=self.sqb[:, 0:128], in_=b5[:, 256:384], func=AF.Square), reads=[k5], writes=["sqb"])
            c.op("pe", lambda e: e.matmul(self.bk[4][:, 0:128], lhsT=self.ones_b[:], rhs=self.sqb[:, 0:128], start=True, stop=True),
                 reads=["sqb", "ones"], writes=[("bk", 4)])
            c.op("act", lambda e: e.activation(out=self.rt[:, 0:128], in_=self.bk[4][:, 0:128], func=AF.Sqrt, scale=1.0 / HD, bias=self.eps_t[:, 0:1]),
                 reads=[("bk", 4), "eps"], writes=["rt"])
            c.op("dve", lambda e: e.reciprocal(out=self.rt[:, 0:128], in_=self.rt[:, 0:128]), reads=["rt"], writes=["rt"])
            c.op("dve", lambda e: e.scalar_tensor_tensor(out=kcmpT[:], in0=b5[:, 256:384], scalar=self.hgain_t[:, li, 1:2], in1=self.rt[:, 0:128],
                                                         op0=ALU.mult, op1=ALU.mult), reads=[k5, "rt", "hgain"], writes=["kcmpT"])
            self.compress(li, 1, xv, "xv", w1b, w2b, hid)
            c.op("pe", lambda e: e.matmul(b5[:, 256:384], lhsT=hid[:], rhs=w2b[:], start=True, stop=True), reads=["w2b", "hid"], writes=[k5])
            c.op("dve", lambda e: e.tensor_copy(out=vcmp[:, 0, :], in_=b5[:, 256:384]), reads=[k5], writes=["vcmp"])
            c.op("pool", lambda e: e.memset(impacc[:], 0.0), writes=["impacc"])
            for r in range(4):
                h = 4 * g + r
                c.dma("sp", f"qT{r}", qT[r][:], self.FM[h, :, :], reads=[("FM", h)], writes=[("qT", r)])
                c.dma("sp", "eb", eb[:], self.EBD[h, :, :], reads=[("EBD", h)], writes=["eb"])
                self.gate_rows(h, [0])
                for G in range(4):
                    i = self.nS % 2
                    self.nS += 1
                    S, kS = self.bk[i], ("bk", i)
                    P, kP = self.P[i], ("P", i)
                    c.op("pe", lambda e: e.matmul(S[:], lhsT=kcmpT[:], rhs=qT[r][:, G * 512:(G + 1) * 512], start=True, stop=True),
                         reads=["kcmpT", ("qT", r)], writes=[kS])
                    c.op("act", lambda e: e.activation(out=self.cexp[:], in_=S[:], func=AF.Exp, scale=SCALE, bias=self.cb_t[:, h:h + 1]),
                         reads=[kS, "cb"], writes=["cexp"])
                    c.op("dve", lambda e: e.tensor_tensor(out=P[:], in0=self.cexp[:], in1=eb[:, 640 + G * 512:640 + (G + 1) * 512], op=ALU.mult),
                         reads=["cexp", "eb"], writes=[kP])
                    c.op("pe", lambda e: e.matmul(self.bk[2][:], lhsT=vcmp[:, 0, :], rhs=P[:], start=True, stop=True), reads=["vcmp", kP], writes=[("bk", 2)])
                    c.op("pe", lambda e: e.matmul(self.bk[3][:], lhsT=self.ones_b[:], rhs=P[:], start=True, stop=True), reads=["ones", kP], writes=[("bk", 3)])
                    if G >= 2:
                        for q4 in range(4):
                            tt = G * 4 + q4 - 8
                            c.op("pe", lambda e: e.matmul(b5[:, q4 * 64:q4 * 64 + 33], lhsT=P[:, q4 * 128:(q4 + 1) * 128], rhs=self.ov33_t[:],
                                                          start=True, stop=True), reads=["ov33", kP], writes=[k5])
                            c.op("dve", lambda e: e.reciprocal(out=imps[:, 32:33], in_=b5[:, q4 * 64 + 32:q4 * 64 + 33]), reads=[k5], writes=["imps"])
                            c.op("dve", lambda e: e.tensor_scalar(out=imps[:, 0:32], in0=b5[:, q4 * 64:q4 * 64 + 32], scalar1=imps[:, 32:33], scalar2=None,
                                                                  op0=ALU.mult), reads=[k5, "imps"], writes=["imps2"])
                            c.op("dve", lambda e: e.tensor_tensor(out=impacc[:, tt, :], in0=impacc[:, tt, :], in1=imps[:, 0:32], op=ALU.add),
                                 reads=["imps2", "impacc"], writes=["impacc"])
                    self.attn_finish(h, 0, G, oacc[r], True, guard=True)
            for tt in range(8):
                c.op("dve", lambda e: e.tensor_tensor(out=sc[:], in0=impacc[:, tt, :], in1=self.addmask_t[:, tt, :], op=ALU.add),
                     reads=["impacc", "addmask"], writes=["sc"])
                c.op("dve", lambda e: e.max(out=m8[:, 0:8], in_=sc[:]), reads=["sc"], writes=["m8"])
                c.op("dve", lambda e: e.match_replace(out=sc2[:], in_to_replace=m8[:, 0:8], in_values=sc[:], imm_value=-3e38),
                     reads=["sc", "m8"], writes=["sc2"])
                c.op("dve", lambda e: e.max(out=m8[:, 8:16], in_=sc2[:]), reads=["sc2"], writes=["m8b"])
                c.op("dve", lambda e: e.tensor_scalar(out=sc2[:], in0=sc[:], scalar1=m8[:, 15:16], scalar2=None, op0=ALU.is_ge),
                     reads=["sc", "m8b"], writes=["sc2"])
                c.op("dve", lambda e: e.tensor_scalar(out=sc2[:], in0=sc2[:], scalar1=30000.0, scalar2=-30000.0, op0=ALU.mult, op1=ALU.add),
                     reads=["sc2"], writes=["sc2"])
                c.op("pe", lambda e: e.transpose(out=b5[0:32, 384:512], in_=sc2[:], identity=self.idf[:]), reads=["sc2", "idf2"], writes=[k5])
                c.op("act", lambda e: e.copy(out=nmT[:, tt * 128:(tt + 1) * 128], in_=b5[0:32, 384:512]), reads=[k5], writes=["nmT"])
            c.dma("sp", "eba", eba4[:], self.EBA[4 * g:4 * g + 4, :, :].rearrange("h p n -> p h n"), reads=[("EBA", 4 * g + r_) for r_ in range(4)], writes=["eba"])
            for r in range(4):
                h = 4 * g + r
                c.dma("sp", "eb", eb[:], self.EBD[h, :, :], reads=[("EBD", h)], writes=["eb"])
                self.gate_rows(h, [1, 2])
                self.eba = eba4[:, r, :]
                for G in range(4):
                    self.banded_attn(h, G, qT[r], ("qT", r), ksT, "ksT", vs, "vs", eb, "eb", "sel", nmT, uz=(2, 3))
                    self.banded_attn(h, G, qT[r], ("qT", r), kwT, "kwT", vw, "vw", eb, "eb", "win", uz=(4, 5))
                    self.attn_finish(h, 1, G, oacc[r], False, uz=(2, 3))
                    self.attn_finish(h, 2, G, oacc[r], False, uz=(4, 5))
                c.op("act", lambda e: e.copy(out=obf[:], in_=oacc[r][:]), reads=[("oacc", id(oacc[r]), G) for G in range(4)], writes=["obf"])
                c.dma("sp", "obf", self.OT[h, :, :], obf[:], reads=["obf"], writes=[("OT", h)])

    def setup_tables2(self):
        c = self.c
        L = self.L
        self.trineg = self.sb("trineg", [128, 128], BF16)
        self.mstrict = self.sb("mstrict", [128, 128], BF16)
        self.masku = self.sb("masku", [128, 64], F32)
        self.rst = self.sb("rst", [128, T], F32)
        tmp = self.sb("tmpc", [128, 128], F32)
        c.op("pool", lambda e: e.memset(tmp[:], -1.0), writes=["tmpc"])
        c.op("pool", lambda e: e.affine_select(out=tmp[:], in_=tmp[:], pattern=[[-1, 128]], compare_op=ALU.is_ge, fill=0.0, base=0, channel_multiplier=1),
             reads=["tmpc"], writes=["tmpc"])
        c.op("pool", lambda e: e.tensor_copy(out=self.trineg[:], in_=tmp[:]), reads=["tmpc"], writes=["trineg"])
        c.op("pool", lambda e: e.memset(tmp[:], 1.0), reads=["tmpc"], writes=["tmpc"])
        c.op("pool", lambda e: e.affine_select(out=tmp[:], in_=tmp[:], pattern=[[1, 128]], compare_op=ALU.is_gt, fill=0.0, base=0, channel_multiplier=-1),
             reads=["tmpc"], writes=["tmpc"])
        c.op("pool", lambda e: e.tensor_copy(out=self.mstrict[:], in_=tmp[:]), reads=["tmpc"], writes=["mstrict"])
        self.mneg = self.sb("mneg", [128, 128], BF16)
        c.op("pool", lambda e: e.memset(tmp[:], -30000.0), reads=["tmpc"], writes=["tmpc"])
        c.op("pool", lambda e: e.affine_select(out=tmp[:], in_=tmp[:], pattern=[[-1, 128]], compare_op=ALU.is_ge, fill=0.0, base=0, channel_multiplier=1),
             reads=["tmpc"], writes=["tmpc"])
        c.op("pool", lambda e: e.tensor_copy(out=self.mneg[:], in_=tmp[:]), reads=["tmpc"], writes=["mneg"])
        c.op("pool", lambda e: e.memset(self.masku[:], 1.0), writes=["masku"])
        for p0 in (0, 64):
            c.op("pool", lambda e: e.affine_select(out=self.masku[p0:p0 + 64, :], in_=self.masku[p0:p0 + 64, :], pattern=[[1, 64]], compare_op=ALU.is_ge,
                                                   fill=0.0, base=0, channel_multiplier=-1), reads=["masku"], writes=["masku"])
        c.op("pool", lambda e: e.memset(self.rst[:], 1.0), writes=["rst"])
        c.op("pool", lambda e: e.memset(self.rst[:].rearrange("p (c t) -> p c t", t=64)[:, :, 0:1], 0.0), reads=["rst"], writes=["rst"])
        hl = self.sb("hl", [128, 4, 8], F32)
        lc = self.sb("lc", [128, L, 4], F32)
        self.lb = self.sb("lb", [128, L, 8], F32)
        self.oml = self.sb("oml", [128, L, 8], F32)
        ssum = self.sb("ssum", [128, 8], F32)
        c.dma("sp", "cst9", hl[:], self.hlb[:, :, :], writes=["hl"])
        c.dma("sp", "cst10", lc[:], self.lcum[:, :, :], writes=["lc"])
        c.op("act", lambda e: e.activation(out=hl[:], in_=hl[:], func=AF.Exp), reads=["hl"], writes=["hl"])
        c.op("dve", lambda e: e.tensor_tensor(out=ssum[:], in0=hl[:, 0, :], in1=hl[:, 1, :], op=ALU.add), reads=["hl"], writes=["ssum"])
        c.op("dve", lambda e: e.tensor_tensor(out=ssum[:], in0=ssum[:], in1=hl[:, 2, :], op=ALU.add), reads=["hl", "ssum"], writes=["ssum"])
        c.op("dve", lambda e: e.tensor_tensor(out=ssum[:], in0=ssum[:], in1=hl[:, 3, :], op=ALU.add), reads=["hl", "ssum"], writes=["ssum"])
        c.op("dve", lambda e: e.reciprocal(out=ssum[:], in_=ssum[:]), reads=["ssum"], writes=["ssum"])
        for l4 in range(4):
            c.op("dve", lambda e: e.tensor_tensor(out=hl[:, l4, :], in0=hl[:, l4, :], in1=ssum[:], op=ALU.mult), reads=["hl", "ssum"], writes=["hl"])
        for li in range(L):
            c.op("dve", lambda e: e.tensor_scalar(out=self.lb[:, li, :], in0=hl[:, 0, :], scalar1=lc[:, li, 0:1], scalar2=None, op0=ALU.mult),
                 reads=["hl", "lc"], writes=["lb"])
            for l4 in range(1, 4):
                c.op("dve", lambda e: e.scalar_tensor_tensor(out=self.lb[:, li, :], in0=hl[:, l4, :], scalar=lc[:, li, l4:l4 + 1], in1=self.lb[:, li, :],
                                                             op0=ALU.mult, op1=ALU.add), reads=["hl", "lc", "lb"], writes=["lb"])
        c.op("dve", lambda e: e.tensor_scalar(out=self.oml[:], in0=self.lb[:], scalar1=-1.0, scalar2=1.0, op0=ALU.mult, op1=ALU.add),
             reads=["lb"], writes=["oml"])
        self.gn_t = self.sb("gn", [128, L, 128], F32)
        c.dma("sp", "cst11", self.gn_t[:], self.gn[:, :, :], writes=["gn"])
        self.gffn = self.sb("gffn", [128, L, 16], F32)
        c.dma("sp", "cst12", self.gffn[:], self.norm_ffn[:, :, :], writes=["gffn"])
        self.convw_t = self.sb("convw", [128, L, 3, 88], F32)
        c.dma("sp", "cst13", self.convw_t[:], self.convw[:, :, :, :], writes=["convw"])
        self.convb_t = self.sb("convb", [128, L, 88], F32)
        c.dma("sp", "cst14", self.convb_t[:], self.convb[:, :, :], writes=["convb"])

    def phase_sb(self, li, s):
        c = self.c
        sb = self.sb
        qTs = [sb("sqT", [128, T], BF16) for _ in range(2)]
        kTs = [sb("skT", [128, T], BF16) for _ in range(2)]
        Vs = [sb("sV", [128, 16, 128], BF16) for _ in range(2)]
        R = sb("sR", [128, 4, 512], F32)
        e32 = [sb("se32", [128, 512], F32) for _ in range(2)]
        arg = sb("sarg", [128, 512], F32)
        spb = [sb("spb", [128, 512], BF16) for _ in range(2)]
        A = [sb("sA", [128, 512], BF16) for _ in range(2)]
        obf = [sb("sobf", [128, T], BF16) for _ in range(2)]
        tmv = self.TM.rearrange("(kt p) n -> p kt n", p=128)

        def load_head(h):
            b = h % 2
            c.dma("sp", f"sqT{b}", qTs[b][:], self.FM[16 + h, :, :], reads=[("FM", 16 + h)], writes=[("sqT", b)])
            c.dma("sp", f"skT{b}", kTs[b][:], self.FM[24 + h, :, :], reads=[("FM", 24 + h)], writes=[("skT", b)])
            c.dma("sp", f"sV{b}", Vs[b][:], tmv[:, :, 512 + h * 128:512 + (h + 1) * 128],
                  reads=[("TM", 512 + (h // 4) * 512, t) for t in range(16)], writes=[("sV", b)])
        load_head(0)
        blocks = []
        for h in range(8):
            for G in range(4):
                kts = list(range(4 * G + 3, -1, -1))
                for kt in kts:
                    blocks.append((h, G, kt, kt == kts[0], kt == kts[-1]))

        def stage_a(k):
            h, G, kt, firstk, lastk = blocks[k]
            hb = h % 2
            qT, kT = qTs[hb], kTs[hb]
            if firstk:
                c.op("pool", lambda e: e.memset(R[:, G, :], 0.0), writes=[("sR", G)])
            qlo = max(kt, 4 * G)
            c0 = (qlo - 4 * G) * 128
            n = 512 - c0
            i = k % 2
            S, kS = self.bk[i], ("bk", i)
            sp_, ksp = spb[i], ("spb", i)
            diag = kt >= 4 * G
            c.op("pe", lambda e: e.matmul(S[:, 0:n], lhsT=kT[:, kt * 128:(kt + 1) * 128], rhs=qT[:, G * 512 + c0:(G + 1) * 512], start=True, stop=False),
                 reads=[("skT", hb), ("sqT", hb)], writes=[kS])
            if diag:
                c.op("pe", lambda e: e.matmul(S[:, 0:128], lhsT=self.ident[:], rhs=self.mneg[:], start=False, stop=False),
                     reads=["ident", "mneg"], writes=[kS])
            c.op("act", lambda e: e.activation(out=e32[i][:, 0:n], in_=S[:, 0:n], func=AF.Exp), reads=[kS], writes=[("se32", i)])
            c.op("act", lambda e: e.activation(out=sp_[:, 0:n], in_=e32[i][:, 0:n], func=AF.Ln, bias=1.0), reads=[("se32", i)], writes=[ksp])

        def stage_b(k):
            h, G, kt, firstk, lastk = blocks[k]
            hb = h % 2
            V = Vs[hb]
            qlo = max(kt, 4 * G)
            c0 = (qlo - 4 * G) * 128
            n = 512 - c0
            i = k % 2
            S, kS = self.bk[i], ("bk", i)
            sp_, ksp = spb[i], ("spb", i)
            A_, kA = A[i], ("sA", i)
            U, kU = (self.bk[2], ("bk", 2)) if G % 2 == 0 else (self.bk[4], ("bk", 4))
            Cs, kCs = (self.bk[3], ("bk", 3)) if k % 2 == 0 else (self.bk[5], ("bk", 5))
            diag = kt >= 4 * G
            if G == 0 and firstk and h + 1 < 8:
                load_head(h + 1)
            c.op("pe", lambda e: e.matmul(S[:, 0:n], lhsT=self.trineg[:], rhs=sp_[:, 0:n], start=False, stop=True), reads=["trineg", ksp], writes=[kS])
            c.op("pe", lambda e: e.matmul(Cs[:, 0:n], lhsT=self.ones_b[:], rhs=sp_[:, 0:n], start=True, stop=True), reads=["ones", ksp], writes=[kCs])
            c.op("dve", lambda e: e.tensor_tensor(out=arg[:, 0:n], in0=S[:, 0:n], in1=R[:, G, c0:512], op=ALU.subtract), reads=[kS, ("sR", G)], writes=["sarg"])
            c.op("act", lambda e: e.activation(out=A_[:, 0:n], in_=arg[:, 0:n], func=AF.Exp), reads=["sarg"], writes=[kA])
            if not lastk:
                c.op("dve", lambda e: e.tensor_tensor(out=R[:, G, c0:512], in0=R[:, G, c0:512], in1=Cs[:, 0:n], op=ALU.add), reads=[("sR", G), kCs], writes=[("sR", G)])
            c.op("pe", lambda e: e.matmul(U[:, c0:512], lhsT=V[:, kt, :], rhs=A_[:, 0:n], start=firstk, stop=lastk), reads=[("sV", hb), kA], writes=[kU])
            if lastk:
                ob = obf[hb]
                c.op("act", lambda e: e.copy(out=ob[:, G * 512:(G + 1) * 512], in_=U[:]), reads=[kU], writes=[("sobf", hb)])
                if G == 3:
                    c.dma("sp", f"sobf{hb}", self.OT[8 + h, :, :], ob[:], reads=[("sobf", hb)], writes=[("OT", 8 + h)])
        stage_a(0)
        for k in range(len(blocks)):
            if k + 1 < len(blocks):
                stage_a(k + 1)
            stage_b(k)

    def phase_hg(self, li, s):
        c = self.c
        sb = self.sb
        qf = sb("hq", [128, T], BF16)
        F = sb("hF", [128, T], F32)
        LF = sb("hLF", [128, T], F32)
        Kk = sb("hK", [128, T], F32)
        b = sb("hb", [128, T], F32)
        d1 = sb("hd1", [128, T], F32)
        E = sb("hE", [128, T], F32)
        qp = sb("hqp", [128, T], BF16)
        kp = sb("hkp", [128, T], BF16)
        kd = sb("hkd", [128, T], BF16)
        kdT = sb("hkdT", [128, 16, 128], BF16)
        V = sb("hV", [128, 16, 128], BF16)
        Gs = sb("hGs", [128, 16, 128], BF16)
        emid = sb("hemid", [128, 32], F32)
        elast = sb("helast", [128, 32], F32)
        S = sb("hS", [128, 128], F32)
        Sb = sb("hSb", [128, 128], BF16)
        am = sb("ham", [128, 64], BF16)
        ss = sb("hss", [128, 4], F32)
        junk = sb("hjunk", [128, 128], F32)
        y = sb("hy", [128, 128], F32)
        ybs = [sb("hyb", [128, 128], BF16) for _ in range(2)]
        ohT = sb("hohT", [128, T], BF16)
        tmv = self.TM.rearrange("(kt p) n -> p kt n", p=128)
        c3 = lambda a: a[:].rearrange("p (c t) -> p c t", t=64)
        for h in range(8):
            c.dma("sp", "hq", qf[:], self.FM[32 + h, :, :], reads=[("FM", 32 + h)], writes=["hq"])
            c.dma("sp", "hF", F[:], self.HF[h, :, :], reads=[("HF", h)], writes=["hF"])
            c.dma("sp", "hV", V[:], tmv[:, :, 1536 + h * 128:1536 + (h + 1) * 128],
                  reads=[("TM", 1536 + (h // 4) * 512, t) for t in range(16)], writes=["hV"])
            c.dma("sp", "hGs", Gs[:], tmv[:, :, 2560 + h * 128:2560 + (h + 1) * 128],
                  reads=[("TM", 2560 + (h // 4) * 512, t) for t in range(16)], writes=["hGs"])
            c.op("act", lambda e: e.activation(out=F[:], in_=F[:], func=AF.Sigmoid), reads=["hF"], writes=["hF"])
            c.op("dve", lambda e: e.tensor_scalar(out=F[:], in0=F[:], scalar1=self.oml[:, li, h:h + 1], scalar2=self.lb[:, li, h:h + 1],
                                                  op0=ALU.mult, op1=ALU.add), reads=["hF", "oml", "lb"], writes=["hF"])
            c.op("act", lambda e: e.activation(out=LF[:], in_=F[:], func=AF.Ln), reads=["hF"], writes=["hLF"])
            c.op("dve", lambda e: e.tensor_scalar(out=Kk[:], in0=F[:], scalar1=-1.0, scalar2=1.0, op0=ALU.mult, op1=ALU.add), reads=["hF"], writes=["hK"])
            c.op("dve", lambda e: e.tensor_tensor_scan(out=b[:], data0=self.rst[:], data1=LF[:], initial=0.0, op0=ALU.mult, op1=ALU.add),
                 reads=["hLF", "rst"], writes=["hb"])
            bmid = c3(b)[:, :, 31:32]
            blast = c3(b)[:, :, 63:64]
            c.op("dve", lambda e: e.tensor_tensor(out=c3(d1), in0=c3(b), in1=bmid.broadcast_to([128, 32, 64]), op=ALU.subtract), reads=["hb"], writes=["hd1"])
            c.op("act", lambda e: e.activation(out=E[:], in_=d1[:], func=AF.Exp), reads=["hd1"], writes=["hE"])
            c.op("dve", lambda e: e.tensor_tensor(out=qp[:], in0=qf[:], in1=E[:], op=ALU.mult), reads=["hq", "hE"], writes=["hqp"])
            c.op("act", lambda e: e.activation(out=E[:], in_=d1[:], func=AF.Exp, scale=-1.0), reads=["hd1", "hqp"], writes=["hE"])
            c.op("dve", lambda e: e.tensor_tensor(out=kp[:], in0=Kk[:], in1=E[:], op=ALU.mult), reads=["hK", "hE"], writes=["hkp"])
            c.op("dve", lambda e: e.tensor_tensor(out=c3(d1), in0=blast.broadcast_to([128, 32, 64]), in1=c3(b), op=ALU.subtract), reads=["hb", "hkp"], writes=["hd1"])
            c.op("act", lambda e: e.activation(out=E[:], in_=d1[:], func=AF.Exp), reads=["hd1", "hkp"], writes=["hE"])
            c.op("dve", lambda e: e.tensor_tensor(out=kd[:], in0=Kk[:], in1=E[:], op=ALU.mult), reads=["hK", "hE"], writes=["hkd"])
            c.op("act", lambda e: e.activation(out=emid[:].rearrange("p (c o) -> p c o", o=1), in_=bmid, func=AF.Exp), reads=["hb"], writes=["hemid"])
            c.op("act", lambda e: e.activation(out=elast[:].rearrange("p (c o) -> p c o", o=1), in_=blast, func=AF.Exp), reads=["hb"], writes=["helast"])
            for tt in range(16):
                pt = self.ptr[tt % 2]
                kp_ = ("ptr", tt % 2)
                c.op("pe", lambda e: e.transpose(out=pt[:, 0, :], in_=kd[:, tt * 128:(tt + 1) * 128], identity=self.ident[:]), reads=["hkd", "ident"], writes=[kp_])
                c.op("act", lambda e: e.copy(out=kdT[:, tt, :], in_=pt[:, 0, :]), reads=[kp_], writes=["hkdT"])
            c.op("pool", lambda e: e.memset(S[:], 0.0), writes=["hS"])
            def pre(ch):
                tt, half = ch // 2, ch % 2
                p0 = half * 64
                cs = slice(ch * 64, (ch + 1) * 64)
                aps, ka = (self.bk[0], ("bk", 0)) if ch % 2 == 0 else (self.bk[5], ("bk", 5))
                KV, kKV = (self.bk[3], ("bk", 3)) if ch % 2 == 0 else (self.bk[2], ("bk", 2))
                c.op("pe", lambda e: e.matmul(aps[p0:p0 + 64, 0:64], lhsT=kp[:, cs], rhs=qp[:, cs], start=True, stop=True), reads=["hkp", "hqp"], writes=[ka])
                c.op("dve", lambda e: e.tensor_tensor(out=am[p0:p0 + 64, :], in0=aps[p0:p0 + 64, 0:64], in1=self.masku[p0:p0 + 64, :], op=ALU.mult),
                     reads=[ka, "masku"], writes=[("ham", half)])
                c.op("pe", lambda e: e.matmul(KV[:, 0:128], lhsT=kdT[p0:p0 + 64, tt, :], rhs=V[p0:p0 + 64, tt, :], start=True, stop=True),
                     reads=["hkdT", "hV"], writes=[kKV])

            def main(ch):
                tt, half = ch // 2, ch % 2
                p0 = half * 64
                cs = slice(ch * 64, (ch + 1) * 64)
                KV, kKV = (self.bk[3], ("bk", 3)) if ch % 2 == 0 else (self.bk[2], ("bk", 2))
                O, kO = (self.bk[1], ("bk", 1)) if tt % 2 == 0 else (self.bk[4], ("bk", 4))
                c.op("act", lambda e: e.activation(out=Sb[:], in_=S[:], func=AF.Copy, scale=emid[:, ch:ch + 1]), reads=["hS", "hemid"], writes=["hSb"])
                c.op("pe", lambda e: e.matmul(O[p0:p0 + 64, 0:128], lhsT=am[p0:p0 + 64, :], rhs=V[p0:p0 + 64, tt, :], start=True, stop=False),
                     reads=[("ham", half), "hV"], writes=[kO])
                c.op("pe", lambda e: e.matmul(O[p0:p0 + 64, 0:128], lhsT=qp[:, cs], rhs=Sb[:], start=False, stop=True), reads=["hqp", "hSb"], writes=[kO])
                c.op("dve", lambda e: e.scalar_tensor_tensor(out=S[:], in0=S[:], scalar=elast[:, ch:ch + 1], in1=KV[:, 0:128], op0=ALU.mult, op1=ALU.add),
                     reads=["hS", "helast", kKV], writes=["hS"])
                if half == 1:
                    c.op("act", lambda e: e.activation(out=junk[:], in_=O[:, 0:128], func=AF.Square, accum_out=ss[:, 0:1]), reads=[kO], writes=["hjunk", "hss"])
                    c.op("act", lambda e: e.activation(out=ss[:, 1:2], in_=ss[:, 0:1], func=AF.Sqrt, scale=1.0 / HD, bias=self.eps_t[:, 0:1]),
                         reads=["hss", "eps"], writes=["hss1"])
                    c.op("dve", lambda e: e.reciprocal(out=ss[:, 2:3], in_=ss[:, 1:2]), reads=["hss1"], writes=["hss2"])
                    c.op("dve", lambda e: e.scalar_tensor_tensor(out=y[:], in0=O[:, 0:128], scalar=ss[:, 2:3], in1=self.gn_t[:, li, :], op0=ALU.mult, op1=ALU.mult),
                         reads=[kO, "hss2", "gn"], writes=["hy"])
                    ybt = ybs[tt % 2]
                    c.op("dve", lambda e: e.tensor_tensor(out=ybt[:], in0=y[:], in1=Gs[:, tt, :], op=ALU.mult), reads=["hy", "hGs"], writes=[("hyb", tt % 2)])

            def fin(tt):
                pt = self.ptr[tt % 2]
                kp_ = ("ptr", tt % 2)
                c.op("pe", lambda e: e.transpose(out=pt[:, 1, :], in_=ybs[tt % 2][:], identity=self.ident[:]), reads=[("hyb", tt % 2), "ident"], writes=[kp_])
                c.op("act", lambda e: e.copy(out=ohT[:, tt * 128:(tt + 1) * 128], in_=pt[:, 1, :]), reads=[kp_], writes=["hohT"])
            pre(0)
            for ch in range(32):
                if ch + 1 < 32:
                    pre(ch + 1)
                main(ch)
                if ch % 2 == 0 and ch >= 2:
                    fin(ch // 2 - 1)
            fin(15)
            c.dma("sp", "hohT", self.OT[16 + h, :, :], ohT[:], reads=["hohT"], writes=[("OT", 16 + h)])

    def phase_d(self, li, s, tb, xsrc, xsname, xdst, xdname, carry):
        c = self.c
        sb = self.sb
        t0 = tb * 512
        xblk = sb("xblk", [128, 4, D], F32)
        h2T = sb("h2T", [128, 16, 512], BF16)
        kxb = "xblk"
        c.dma("sp", "xblk", xblk[:], xsrc[s, t0:t0 + 512, :].rearrange("(ts p) n -> p ts n", p=128), reads=[("X", xsname, s, tb)], writes=[kxb])
        nacc = [0]

        def next_acc():
            i = nacc[0] % 2
            nacc[0] += 1
            return self.bk[i], ("bk", i)
        with self.phase():
            hb = sb("hb", [128, 16, 512], BF16)
            ob = sb("ob", [128, 24, 512], BF16)
            sg = [sb("sg", [128, 4, 512], BF16) for _ in range(3)]
            mT = sb("mT", [128, 16, 512], BF16)
            macc = sb("macc", [128, 4, 512], F32)
            tmp = sb("mtmp", [128, 512], F32)
            wt = [sb("wtd", [128, 16, 512], BF16) for _ in range(2)]
            self.xn = [sb("xn", [128, D], BF16)]
            self.sqj = [sb("sqj", [128, D], BF16)]
            self.ss = [sb("ss", [128, 4], F32) for _ in range(2)]
            nl = [0]

            def load(view, n_k, key):
                i = nl[0] % 2
                nl[0] += 1
                c.dma("sp", f"wtd{i}", wt[i][:, 0:n_k, :], view, reads=[key], writes=[("wtd", i)])
                return wt[i], ("wtd", i)
            c.dma("sp", "hb", hb[:], self.HT[:, :, t0:t0 + 512], reads=["HT"], writes=["hb"])
            c.dma("sp", "ob", ob[:], self.OT[:, :, t0:t0 + 512].rearrange("c p t -> p c t"), reads=[("OT", k) for k in range(24)], writes=["ob"])
            win = self.win_b[li].rearrange("(kc p) n -> p kc n", p=128)
            for dg in range(4):
                self.cvt_some(1)
                for br in range(3):
                    col = C_MG + br * 2048 + dg * 512
                    w, kw = load(win[:, :, col:col + 512], 16, ("win_b", li))
                    for j in range(4):
                        acc, ka = next_acc()
                        for kc in range(16):
                            c.op("pe", lambda e: e.matmul(acc[:], lhsT=w[:, kc, j * 128:(j + 1) * 128], rhs=hb[:, kc, :], start=(kc == 0), stop=(kc == 15)),
                                 reads=[kw, "hb"], writes=[ka])
                        c.op("act", lambda e: e.activation(out=sg[br][:, j, :], in_=acc[:], func=AF.Sigmoid), reads=[ka], writes=[("sg", br, j)])
                for br in range(3):
                    wb = self.wbr_b[li, br * 1024:(br + 1) * 1024, :].rearrange("(kc p) n -> p kc n", p=128)
                    w, kw = load(wb[:, :, dg * 512:(dg + 1) * 512], 8, ("wbr_b", li))
                    for j in range(4):
                        acc, ka = next_acc()
                        for c8 in range(8):
                            c.op("pe", lambda e: e.matmul(acc[:], lhsT=w[:, c8, j * 128:(j + 1) * 128], rhs=ob[:, br * 8 + c8, :], start=(c8 == 0), stop=(c8 == 7)),
                                 reads=[kw, "ob"], writes=[ka])
                        if br == 0:
                            c.op("dve", lambda e: e.tensor_tensor(out=macc[:, j, :], in0=acc[:], in1=sg[0][:, j, :], op=ALU.mult),
                                 reads=[ka, ("sg", 0, j)], writes=[("macc", j)])
                        else:
                            c.op("dve", lambda e: e.tensor_tensor(out=tmp[:], in0=acc[:], in1=sg[br][:, j, :], op=ALU.mult),
                                 reads=[ka, ("sg", br, j)], writes=["mtmp"])
                            c.op("pool", lambda e: e.tensor_tensor(out=macc[:, j, :], in0=macc[:, j, :], in1=tmp[:], op=ALU.add),
                                 reads=["mtmp", ("macc", j)], writes=[("macc", j)])
                        if br == 2:
                            c.op("act", lambda e: e.copy(out=mT[:, dg * 4 + j, :], in_=macc[:, j, :]), reads=[("macc", j)], writes=["mT"])
            wo = self.wout_b[li].rearrange("(kc p) n -> p kc n", p=128)
            for ng in range(4):
                w, kw = load(wo[:, :, ng * 512:(ng + 1) * 512], 16, ("wout_b", li))
                for ts in range(4):
                    acc, ka = next_acc()
                    for kc in range(16):
                        c.op("pe", lambda e: e.matmul(acc[:], lhsT=mT[:, kc, ts * 128:(ts + 1) * 128], rhs=w[:, kc, :], start=(kc == 0), stop=(kc == 15)),
                             reads=[kw, "mT"], writes=[ka])
                    xs = xblk[:, ts, ng * 512:(ng + 1) * 512]
                    c.op("dve", lambda e: e.tensor_tensor(out=xs, in0=acc[:], in1=xs, op=ALU.add), reads=[ka, kxb], writes=[kxb])
            for ts in range(4):
                self.norm_tile(xblk[:, ts, :], kxb, self.gffn[:, li, :], h2T, ts * 128, "h2T")
        with self.phase():
            aT = sb("aT", [128, 44, 512], BF16)
            wt = [sb("wtf", [128, 16, 512], BF16) for _ in range(3)]
            ub = [sb("ub", [128, 514], F32) for _ in range(2)]
            cc = [sb("cc", [128, 512], F32) for _ in range(2)]
            nl = [0]

            def load3(view, n_k, key):
                i = nl[0] % 3
                nl[0] += 1
                c.dma("sp", f"wtf{i}", wt[i][:, 0:n_k, :], view, reads=[key], writes=[("wtf", i)])
                return wt[i], ("wtf", i)
            wu = self.wup_b[li].rearrange("(kc p) n -> p kc n", p=128)
            for i4 in range(11):
                self.cvt_some(1)
                wg_, kwg = load3(wu[:, :, i4 * 512:(i4 + 1) * 512], 16, ("wup_b", li))
                wv_, kwv = load3(wu[:, :, DFF + i4 * 512:DFF + (i4 + 1) * 512], 16, ("wup_b", li))
                for j in range(4):
                    i = i4 * 4 + j
                    for side, (w, kw) in enumerate(((wg_, kwg), (wv_, kwv))):
                        ch = i + 44 * side
                        acc, ka = self.bk[2 + side], ("bk", 2 + side)
                        for kc in range(16):
                            c.op("pe", lambda e: e.matmul(acc[:], lhsT=w[:, kc, j * 128:(j + 1) * 128], rhs=h2T[:, kc, :], start=(kc == 0), stop=(kc == 15)),
                                 reads=[kw, "h2T"], writes=[ka])
                        u, ku = ub[side], ("ub", side)
                        cv, kc_ = cc[side], ("cc", side)
                        c.op("pool", lambda e: e.tensor_copy(out=u[:, 0:2], in_=carry[:, ch, :]), reads=[("carry", ch)], writes=[ku])
                        c.op("act", lambda e: e.copy(out=u[:, 2:514], in_=acc[:]), reads=[ka], writes=[ku])
                        c.op("pool", lambda e: e.tensor_copy(out=carry[:, ch, :], in_=u[:, 512:514]), reads=[ku], writes=[("carry", ch)])
                        cw = self.convw_t
                        c.op("pool", lambda e: e.tensor_scalar(out=cv[:], in0=u[:, 2:514], scalar1=cw[:, li, 2, ch:ch + 1], scalar2=self.convb_t[:, li, ch:ch + 1],
                                                               op0=ALU.mult, op1=ALU.add), reads=[ku, "convw", "convb"], writes=[kc_])
                        c.op("dve", lambda e: e.scalar_tensor_tensor(out=cv[:], in0=u[:, 1:513], scalar=cw[:, li, 1, ch:ch + 1], in1=cv[:], op0=ALU.mult, op1=ALU.add),
                             reads=[ku, "convw", kc_], writes=[kc_])
                        c.op("dve", lambda e: e.scalar_tensor_tensor(out=cv[:], in0=u[:, 0:512], scalar=cw[:, li, 0, ch:ch + 1], in1=cv[:], op0=ALU.mult, op1=ALU.add),
                             reads=[ku, "convw", kc_], writes=[kc_])
                    c.op("act", lambda e: e.activation(out=cc[0][:], in_=cc[0][:], func=AF.Silu), reads=[("cc", 0)], writes=[("cc", 0)])
                    c.op("dve", lambda e: e.tensor_tensor(out=aT[:, i, :], in0=cc[0][:], in1=cc[1][:], op=ALU.mult), reads=[("cc", 0), ("cc", 1)], writes=["aT"])
            wd = self.wdn_b[li].rearrange("(kc p) n -> p kc n", p=128)
            for ng in range(4):
                for kg in range(3):
                    nk = 16 if kg < 2 else 12
                    w, kw = load3(wd[:, kg * 16:kg * 16 + nk, ng * 512:(ng + 1) * 512], nk, ("wdn_b", li))
                    for ts in range(4):
                        acc, ka = self.bk[ts], ("bk", ts)
                        for k2 in range(nk):
                            kc = kg * 16 + k2
                            c.op("pe", lambda e: e.matmul(acc[:], lhsT=aT[:, kc, ts * 128:(ts + 1) * 128], rhs=w[:, k2, :], start=(kc == 0), stop=(kc == 43)),
                                 reads=[kw, "aT"], writes=[ka])
                for ts in range(4):
                    xs = xblk[:, ts, ng * 512:(ng + 1) * 512]
                    c.op("dve", lambda e: e.tensor_tensor(out=xs, in0=self.bk[ts][:], in1=xs, op=ALU.add), reads=[("bk", ts), kxb], writes=[kxb])
            c.dma("sp", "xout", xdst[s, t0:t0 + 512, :].rearrange("(ts p) n -> p ts n", p=128), xblk[:], reads=[kxb], writes=[("X", xdname, s, tb)])

    def build(self):
        c = self.c
        self._stacks = []
        self.setup_consts()
        self.eps_t = self.sb("eps", [128, 1], F32)
        c.op("pool", lambda e: e.memset(self.eps_t[:], EPS), writes=["eps"])
        self.ptr = [self.ps("ptr", [128, 8, 128], BF16) for _ in range(2)]
        self.bk = [self.ps("bk", [128, 512], F32) for _ in range(6)]
        self.acc = self.bk[0:2]
        self.pn = self.bk[2]
        self.sqb = self.sb("sqb", [128, 512], BF16)
        self.rt = self.sb("rt", [128, 512], F32)
        self.setup_tables()
        self.setup_tables2()
        self.cvt_queue = []
        for piece in self.convert_pieces(0):
            piece()
        for li in range(self.L):
            xsrc, xsname = (self.x, "xin") if li == 0 else (self.XS[(li - 1) % 2], f"XS{(li - 1) % 2}")
            xdst, xdname = (self.out, "out") if li == self.L - 1 else (self.XS[li % 2], f"XS{li % 2}")
            if li + 1 < self.L:
                self.cvt_queue = self.convert_pieces(li + 1)
            for s in range(NSEQ):
                with self.phase():
                    self.hT = self.sb("hT", [128, 16, T], BF16)
                    self.xt = [self.sb("xt", [128, D], F32) for _ in range(3)]
                    self.xn = [self.sb("xn", [128, D], BF16) for _ in range(2)]
                    self.sqj = [self.sb("sqj", [128, D], BF16) for _ in range(2)]
                    self.ss = [self.sb("ss", [128, 4], F32) for _ in range(2)]
                    self.wt = [self.sb("wt", [128, 16, 512], BF16) for _ in range(2)]
                    self.fst = [self.sb("fst", [128, T], BF16) for _ in range(2)]
                    self.fst32 = self.sb("fst32", [128, T], F32)
                    self.tst = [self.sb("tst", [128, 512], BF16) for _ in range(2)]
                    self.gst = self.sb("gst", [32, T], F32)
                    self.phase_a(li, s, xsrc, xsname)
                    c.dma("sp", "hts", self.HT[:, :, :], self.hT[:], reads=[("hT", t) for t in range(16)], writes=["HT"])
                    self.phase_b(li, s)
                if "stopB" in self.debug:
                    continue
                with self.phase():
                    self.phase_nsa(li, s)
                with self.phase():
                    self.phase_sb(li, s)
                with self.phase():
                    self.phase_hg(li, s)
                if "stopC" in self.debug:
                    continue
                with self.phase():
                    carry = self.sb("carry", [128, 88, 2], F32)
                    c.op("pool", lambda e: e.memset(carry[:], 0.0), writes=[("carry", ch) for ch in range(88)])
                    for tb in range(4):
                        with self.phase():
                            self.phase_d(li, s, tb, xsrc, xsname, xdst, xdname, carry)
            self.cvt_some(1000)
        c.finish()
        return self.nc


def _t5_bucket(dist):
    import math
    n = np.maximum(dist, 0)
    big = 16 + (np.log(np.maximum(n, 1).astype(np.float32) / np.float32(16)) / np.float32(math.log(8.0)) * np.float32(16)).astype(np.int32)
    return np.where(n < 16, n, np.minimum(big, 31))


def _static_tables(rel_bias):
    i = np.arange(128)[:, None]
    j = np.arange(128)[None, :]
    blocks = []
    for delta in (0, 128, 512):
        dist = delta + j - i
        blocks.append((dist, (dist >= 0) & (dist < 512)))
    for delta in (0, 128):
        dist = delta + j - i
        blocks.append((dist, dist >= 0))
    t = np.arange(T)[None, :]
    dist = t - 16 * i - 31
    blocks.append((dist, (dist >= 0) & (i < 127)))
    dist_all = np.concatenate([b[0] for b in blocks], axis=1)
    valid = np.concatenate([b[1] for b in blocks], axis=1)
    bucket = _t5_bucket(dist_all)
    ebias = np.empty((8, 128, dist_all.shape[1]), np.float32)
    for h in range(8):
        ebias[h] = np.where(valid, rel_bias[bucket, h], np.float32(-1e30))
    cb = np.ascontiguousarray(np.broadcast_to(rel_bias[31][None, :], (128, 8))).astype(np.float32)
    tpos = (8 + np.arange(8))[None, :, None] * 128 + np.arange(128)[:, None, None]
    qblk = tpos // 64
    jb = np.arange(32)[None, None, :]
    forced = (jb <= qblk) & ((jb == 0) | (jb >= qblk - 1))
    addmask = np.where(forced, 1e30, np.where(jb <= qblk, 0.0, -1e30)).astype(np.float32)
    esel = (np.arange(32)[:, None, None] == 2 * np.arange(16)[None, :, None] + (np.arange(128) // 64)[None, None, :])
    c_start = np.arange(127) * 16
    s_start = np.arange(32) * 64
    overlap = ((c_start[:, None] < s_start[None, :] + 64) & (c_start[:, None] + 32 > s_start[None, :]))
    ov33 = np.zeros((128, 33), np.float32)
    ov33[:127, :32] = overlap
    ov33[:, 32] = 1.0
    selg = (np.arange(24)[:, None, None] == np.arange(24)[None, :, None]) & np.ones((1, 1, 128), bool)
    return dict(ebias=ebias, cb=cb, addmask=addmask, esel=esel.astype(ml_dtypes.bfloat16),
                ov33=ov33.astype(ml_dtypes.bfloat16), selg=selg.astype(np.float32))


def prep_shared(inputs, layers):
    ls = list(layers)
    f = lambda a: np.ascontiguousarray(np.asarray(a, dtype=np.float32))
    d = _static_tables(f(inputs["rel_bias"]))
    d["w_in"] = f(inputs["w_in"])[ls]
    d["norm_attn"] = f(np.asarray(inputs["norm_attn"])[ls].reshape(len(ls), 16, 128).transpose(2, 0, 1))
    hg = np.concatenate([np.asarray(inputs["nsa_q_gain"])[ls][:, None, :], np.asarray(inputs["nsa_k_gain"])[ls]], axis=1)
    d["hgain"] = f(hg.transpose(2, 0, 1))
    d["cmp_w1"] = f(inputs["cmp_w1"])[ls]
    d["cmp_w2"] = f(inputs["cmp_w2"])[ls]
    d["posT"] = f(np.asarray(inputs["cmp_pos"])[ls].transpose(3, 0, 1, 2))
    d["hlb"] = f(np.asarray(inputs["hg_lower_bound"]).reshape(4, 8, 128).transpose(2, 0, 1))
    lcum = np.zeros((128, len(ls), 4), np.float32)
    for i, l in enumerate(ls):
        lcum[:, i, 1:l + 1] = 1.0
    d["lcum"] = lcum
    d["gn"] = f(np.broadcast_to(np.asarray(inputs["hg_norm_gain"])[ls][None, :, :], (128, len(ls), 128)))
    d["w_branch"] = f(np.asarray(inputs["w_branch"])[ls].reshape(len(ls), 3072, D))
    d["w_out"] = f(inputs["w_out"])[ls]
    d["norm_ffn"] = f(np.asarray(inputs["norm_ffn"])[ls].reshape(len(ls), 16, 128).transpose(2, 0, 1))
    d["w_up"] = f(inputs["w_up"])[ls]
    d["convw"] = f(np.asarray(inputs["conv_w"])[ls].reshape(len(ls), 3, 88, 128).transpose(3, 0, 1, 2))
    d["convb"] = f(np.asarray(inputs["conv_b"])[ls].reshape(len(ls), 88, 128).transpose(2, 0, 1))
    d["w_down"] = f(inputs["w_down"])[ls]
    return d


_PROGS = {}


def _get_prog(n_layers):
    if n_layers not in _PROGS:
        p = Prog(list(range(n_layers)))
        _PROGS[n_layers] = p.build()
    return _PROGS[n_layers]


FUSED = True


def kernel(**inputs):
    x = np.ascontiguousarray(np.asarray(inputs["x"], dtype=np.float32))
    n_cores = 8
    if FUSED:
        groups = [list(range(DEPTH))]
    else:
        groups = [[l] for l in range(DEPTH)]
    cur = x
    for ls in groups:
        nc = _get_prog(len(ls))
        shared = prep_shared(inputs, ls)
        in_maps = [dict(shared, x=np.ascontiguousarray(cur[NSEQ * i:NSEQ * (i + 1)])) for i in range(n_cores)]
        res = run_bass_kernel_spmd(nc, in_maps, core_ids=list(range(n_cores)))
        cur = np.concatenate([np.asarray(r["out"], dtype=np.float32) for r in res.results], axis=0)
    return cur
```

```python
import numpy as np
from contextlib import ExitStack
import ml_dtypes
import concourse.bass as bass
import concourse.mybir as mybir
from concourse.bass_utils import run_bass_kernel_spmd

F32 = mybir.dt.float32
BF16 = mybir.dt.bfloat16
AF = mybir.ActivationFunctionType
ALU = mybir.AluOpType

D = 2048
T = 2048
NSEQ = 2
DEPTH = 4
HD = 128
IN_COLS = 15896
DFF = 5632
EPS = 1e-6
SCALE = HD ** -0.5
C_QN, C_KC, C_VC, C_KS, C_VS, C_KW, C_VW, C_G = 0, 1024, 1280, 1536, 1792, 2048, 2304, 2560
C_SQ, C_SK, C_SV = 2584, 3608, 4632
C_HQ, C_HF, C_HI, C_HG = 5656, 6680, 7704, 8728
C_MG = 9752


class Ctx:
    def __init__(self, nc):
        self.nc = nc
        self.E = dict(pe=nc.tensor, act=nc.scalar, dve=nc.vector, pool=nc.gpsimd, sp=nc.sync)
        self.sem = {n: nc.alloc_semaphore("s_" + n) for n in self.E}
        self.cnt = {n: 0 for n in self.E}
        self.seen = {n: {} for n in self.E}
        self.res = {}
        self.slots = {}
        self.nwait = 0

    def _slot(self, name):
        if name not in self.slots:
            self.slots[name] = [self.nc.alloc_semaphore("d_" + name), 0]
        return self.slots[name]

    def _need(self, reads, writes):
        need = {}

        def add(p, c):
            if need.get(p, 0) < c:
                need[p] = c
        for k in reads:
            r = self.res.get(k)
            if r and r[0]:
                add(*r[0])
        for k in writes:
            r = self.res.get(k)
            if r:
                if r[0]:
                    add(*r[0])
                for p, c in r[1].items():
                    add(p, c)
        return need

    def _wait(self, eng, need):
        for p, c in need.items():
            if p == eng and eng in ("pe",):
                continue
            if self.seen[eng].get(p, 0) >= c:
                continue
            if isinstance(p, tuple):
                self.E[eng].wait_ge(self.slots[p[1]][0], 16 * c)
            else:
                self.E[eng].wait_ge(self.sem[p], c)
            self.nwait += 1
            self.seen[eng][p] = c

    def _mark(self, who, c, reads, writes):
        for k in reads:
            r = self.res.setdefault(k, [None, {}])
            r[1][who] = c
        for k in writes:
            self.res[k] = [(who, c), {}]

    def op(self, eng, fn, reads=(), writes=()):
        need = self._need(reads, writes)
        pend = []
        for p, cnt_ in need.items():
            if p == eng and eng in ("pe",):
                continue
            if self.seen[eng].get(p, 0) >= cnt_:
                continue
            pend.append((p, cnt_))
        attach = pend.pop() if pend else None
        self._wait(eng, dict(pend))
        ins = fn(self.E[eng])
        if attach is not None:
            p, cnt_ = attach
            if isinstance(p, tuple):
                ins._wait_ge(self.slots[p[1]][0], 16 * cnt_)
            else:
                ins._wait_ge(self.sem[p], cnt_)
            self.seen[eng][p] = cnt_
            self.nwait += 1
        self.cnt[eng] += 1
        ins.then_inc(self.sem[eng], 1)
        self._mark(eng, self.cnt[eng], reads, writes)
        return ins

    def dma(self, q, slot, out, in_, reads=(), writes=(), **kw):
        self._wait(q, self._need(reads, writes))
        s = self._slot(slot)
        ins = self.E[q].dma_start(out=out, in_=in_, **kw)
        s[1] += 1
        ins.then_inc(s[0], 16)
        self._mark(("dma", slot), s[1], reads, writes)
        return ins

    def barrier(self):
        for eng in self.E:
            need = {}
            for n in self.E:
                if n != eng and self.cnt[n]:
                    need[n] = self.cnt[n]
            for name, (sem, cc) in self.slots.items():
                if cc:
                    need[("dma", name)] = cc
            self._wait(eng, need)

    def finish(self):
        for name, (sem, c) in self.slots.items():
            if c:
                self.E["sp"].wait_ge(sem, 16 * c)
        for n in self.E:
            if n != "sp" and self.cnt[n]:
                self.E["sp"].wait_ge(self.sem[n], self.cnt[n])


class Prog:
    def __init__(self, layers, debug=()):
        self.layers = list(layers)
        self.debug = set(debug)
        nc = self.nc = bass.Bass("TRN2", target_bir_lowering=False)
        self.c = Ctx(nc)
        self._n = 0
        L = len(self.layers)
        self.L = L
        di = lambda name, shape, dt=F32: nc.dram_tensor(name, list(shape), dt, kind="ExternalInput").ap()
        self.x = di("x", [NSEQ, T, D])
        self.w_in = di("w_in", [L, D, IN_COLS])
        self.norm_attn = di("norm_attn", [128, L, 16])
        self.hgain = di("hgain", [128, L, 4])
        self.ebias = di("ebias", [8, 128, 2688])
        self.cb = di("cb", [128, 8])
        self.addmask = di("addmask", [128, 8, 32])
        self.esel = di("esel", [32, 16, 128], BF16)
        self.ov33 = di("ov33", [128, 33], BF16)
        self.selg = di("selg", [24, 24, 128])
        self.cmp_w1 = di("cmp_w1", [L, 2, 32, 128, 128])
        self.cmp_w2 = di("cmp_w2", [L, 2, 128, 128])
        self.posT = di("posT", [128, L, 2, 32])
        self.hlb = di("hlb", [128, 4, 8])
        self.lcum = di("lcum", [128, L, 4])
        self.gn = di("gn", [128, L, 128])
        self.w_branch = di("w_branch", [L, 3072, D])
        self.w_out = di("w_out", [L, D, D])
        self.norm_ffn = di("norm_ffn", [128, L, 16])
        self.w_up = di("w_up", [L, D, 2 * DFF])
        self.convw = di("convw", [128, L, 3, 88])
        self.convb = di("convb", [128, L, 88])
        self.w_down = di("w_down", [L, DFF, D])
        self.wbr_b = self.scratch("wbr_b", [L, 3072, D], BF16)
        self.wout_b = self.scratch("wout_b", [L, D, D], BF16)
        self.wup_b = self.scratch("wup_b", [L, D, 2 * DFF], BF16)
        self.wdn_b = self.scratch("wdn_b", [L, DFF, D], BF16)
        self.XS = self.scratch("XS", [2, NSEQ, T, D], F32)
        self.EBD = self.scratch("EBD", [8, 128, 2688], F32)
        self.EBA = self.scratch("EBA", [8, 128, 640], BF16)
        self.OT = self.scratch("OT", [24, 128, T], BF16)
        self.out = nc.dram_tensor("out", [NSEQ, T, D], F32, kind="ExternalOutput").ap()
        self.win_b = self.scratch("win_b", [L, D, IN_COLS], BF16)
        self.HT = self.scratch("HT", [128, 16, T], BF16)
        self.FM = self.scratch("FM", [40, 128, T], BF16)
        self.HF = self.scratch("HF", [8, 128, T], F32)
        self.GS = self.scratch("GS", [24, T], F32)
        self.TM = self.scratch("TM", [T, 3584], BF16)

    def scratch(self, name, shape, dt):
        kind = "ExternalOutput" if name in self.debug else "Internal"
        return self.nc.dram_tensor(name, list(shape), dt, kind=kind).ap()

    def sb(self, name, shape, dt):
        self._n += 1
        if getattr(self, "_stacks", None):
            return self._stacks[-1].enter_context(self.nc.sbuf_tensor(f"{name}_{self._n}", list(shape), dt))
        return self.nc.alloc_sbuf_tensor(f"{name}_{self._n}", list(shape), dt)

    def phase(self):
        prog = self

        class _P:
            def __enter__(self_):
                prog._stacks.append(ExitStack())
                return self_

            def __exit__(self_, *a):
                prog.c.barrier()
                prog._stacks.pop().close()
                return False
        return _P()

    def ps(self, name, shape, dt=F32):
        self._n += 1
        return self.nc.alloc_psum_tensor(f"{name}_{self._n}", list(shape), dt)

    def convert_pieces(self, li):
        c = self.c
        pieces = []

        def mk(dst, src, key):
            return lambda: c.dma("pool", "cvt_" + key[0], dst, src, writes=[key])
        for i in range(D // 128):
            src = self.w_in[li, i * 128:(i + 1) * 128, :].rearrange("p (a b) -> p a b", b=1987)
            dst = self.win_b[li, i * 128:(i + 1) * 128, :].rearrange("p (a b) -> p a b", b=1987)
            pieces.append(mk(dst, src, ("win_b", li)))
        for srcw, dstw, rows, key in ((self.w_branch, self.wbr_b, 3072, "wbr_b"), (self.w_out, self.wout_b, D, "wout_b"),
                                      (self.w_up, self.wup_b, D, "wup_b"), (self.w_down, self.wdn_b, DFF, "wdn_b")):
            for i in range(rows // 128):
                src = srcw[li, i * 128:(i + 1) * 128, :].rearrange("p (a b) -> p a b", b=1024)
                dst = dstw[li, i * 128:(i + 1) * 128, :].rearrange("p (a b) -> p a b", b=1024)
                pieces.append(mk(dst, src, (key, li)))
        return pieces

    def cvt_some(self, n=1):
        for _ in range(n):
            if self.cvt_queue:
                self.cvt_queue.pop(0)()

    def setup_consts(self):
        c = self.c
        self.ident = self.sb("ident", [128, 128], BF16)
        self.ones_b = self.sb("ones", [128, 128], BF16)
        self.gattn = self.sb("gattn", [128, self.L, 16], F32)
        idf = self.sb("idf", [128, 128], F32)
        c.op("pool", lambda e: e.memset(idf[:], 1.0), writes=["idf"])
        c.op("pool", lambda e: e.affine_select(out=idf[:], in_=idf[:], pattern=[[-1, 128]], compare_op=ALU.is_equal,
                                               fill=0.0, base=0, channel_multiplier=1), reads=["idf"], writes=["idf"])
        c.op("pool", lambda e: e.tensor_copy(out=self.ident[:], in_=idf[:]), reads=["idf"], writes=["ident"])
        c.op("pool", lambda e: e.memset(self.ones_b[:], 1.0), writes=["ones"])
        c.dma("sp", "cst", self.gattn[:], self.norm_attn[:, :, :], writes=["gattn"])
        self.hgain_t = self.sb("hgain", [128, self.L, 4], F32)
        c.dma("sp", "cst2", self.hgain_t[:], self.hgain[:, :, :], writes=["hgain"])

    def norm_tile(self, xt, kx, gains, dstT, col0, kdst):
        c = self.c
        self.nnorm = getattr(self, "nnorm", 0) + 1
        b0, b1, b2 = self.nnorm % len(self.ss), self.nnorm % len(self.sqj), self.nnorm % len(self.xn)
        ss, sq, xn = self.ss[b0], self.sqj[b1], self.xn[b2]
        kss, ksq, kxn = ("ss", b0), ("sqj", b1), ("xn", b2)
        c.op("act", lambda e: e.activation(out=sq[:], in_=xt, func=AF.Square, accum_out=ss[:, 0:1]), reads=[kx], writes=[ksq, kss])
        c.op("act", lambda e: e.activation(out=ss[:, 1:2], in_=ss[:, 0:1], func=AF.Sqrt, scale=1.0 / D, bias=self.eps_t[:, 0:1]),
             reads=[kss, "eps"], writes=[kss])
        c.op("dve", lambda e: e.reciprocal(out=ss[:, 2:3], in_=ss[:, 1:2]), reads=[kss], writes=[kss])
        c.op("dve", lambda e: e.tensor_scalar(out=xn[:], in0=xt, scalar1=ss[:, 2:3], scalar2=None, op0=ALU.mult), reads=[kx, kss], writes=[kxn])
        for half in range(2):
            pt = self.ptr[half]
            kp = ("ptr", half)
            for j in range(8):
                kc = half * 8 + j
                c.op("pe", lambda e: e.transpose(out=pt[:, j, :], in_=xn[:, kc * 128:(kc + 1) * 128], identity=self.ident[:]),
                     reads=[kxn, "ident"], writes=[kp])
            for j in range(8):
                kc = half * 8 + j
                dst = dstT[:, kc, col0:col0 + 128]
                g = gains[:, kc:kc + 1]
                if j % 2 == 0:
                    c.op("act", lambda e: e.activation(out=dst, in_=pt[:, j, :], func=AF.Copy, scale=g), reads=[kp, "gattn", "gffn"], writes=[kdst])
                else:
                    c.op("dve", lambda e: e.tensor_scalar(out=dst, in0=pt[:, j, :], scalar1=g, scalar2=None, op0=ALU.mult),
                         reads=[kp, "gattn", "gffn"], writes=[kdst])

    def phase_a(self, li, s, xsrc, xname):
        c = self.c
        for tt in range(16):
            xt = self.xt[tt % 3]
            kx = ("xt", tt % 3)
            c.dma("sp", f"xt{tt % 3}", xt[:], xsrc[s, tt * 128:(tt + 1) * 128, :], reads=[("X", xname, s, tt // 4)], writes=[kx])
            self.norm_tile(xt[:], kx, self.gattn[:, li, :], self.hT, tt * 128, ("hT", tt))

    def phase_b(self, li, s):
        c = self.c
        hT = self.hT
        hT_keys = [("hT", t) for t in range(16)]
        wsrc = self.win_b[li].rearrange("(kc p) n -> p kc n", p=128)
        nload = [0]

        def load_w(col, n):
            i = nload[0] % 2
            nload[0] += 1
            wt = self.wt[i]
            c.dma("sp", f"wt{i}", wt[:, :, 0:n], wsrc[:, :, col:col + n], reads=[("win_b", li)], writes=[("wt", i)])
            return wt, ("wt", i)

        nacc = [0]

        def next_acc():
            i = nacc[0] % 2
            nacc[0] += 1
            return self.acc[i], ("acc", i)

        nst = [0]
        fm_jobs = [
            (C_QN, 8, "hnorm", 0, 0), (C_KC, 2, "plain", 8, None), (C_VC, 2, "plain", 10, None),
            (C_KS, 2, "hnorm", 12, 2), (C_KW, 2, "hnorm", 14, 3),
            (C_SQ, 8, "plainS", 16, None), (C_SK, 8, "plain", 24, None), (C_HQ, 8, "plain", 32, None),
            (C_HF, 8, "plain32", 0, None),
        ]
        for col0, nch, kind, fm0, gi in fm_jobs:
            for c4 in range(0, nch, 4):
                ncc = min(4, nch - c4)
                wt, kw = load_w(col0 + c4 * 128, ncc * 128)
                for j in range(ncc):
                    ch = c4 + j
                    if kind == "plain32":
                        st, kst = self.fst32, "fst32"
                    else:
                        st, kst = self.fst[nst[0] % 2], ("fst", nst[0] % 2)
                        nst[0] += 1
                    for tb in range(4):
                        acc, ka = next_acc()
                        for kc in range(16):
                            c.op("pe", lambda e: e.matmul(acc[:], lhsT=wt[:, kc, j * 128:(j + 1) * 128], rhs=hT[:, kc, tb * 512:(tb + 1) * 512],
                                                          start=(kc == 0), stop=(kc == 15)),
                                 reads=[kw] + hT_keys, writes=[ka])
                        dst = st[:, tb * 512:(tb + 1) * 512]
                        if kind == "plainS":
                            c.op("act", lambda e: e.activation(out=dst, in_=acc[:], func=AF.Copy, scale=SCALE), reads=[ka], writes=[kst])
                        elif kind == "plain":
                            eng = "act" if tb % 2 == 0 else "dve"
                            if eng == "act":
                                c.op("act", lambda e: e.copy(out=dst, in_=acc[:]), reads=[ka], writes=[kst])
                            else:
                                c.op("dve", lambda e: e.tensor_copy(out=dst, in_=acc[:]), reads=[ka], writes=[kst])
                        elif kind == "plain32":
                            c.op("act", lambda e: e.copy(out=dst, in_=acc[:]), reads=[ka], writes=[kst])
                        else:
                            self.hnorm_block(acc, ka, dst, kst, self.hgain_t[:, li, gi:gi + 1])
                    if kind == "plain32":
                        c.dma("act", "fst32", self.HF[ch, :, :], st[:], reads=[kst], writes=[("HF", ch)])
                    else:
                        c.dma("act", f"fst{kst[1]}", self.FM[fm0 + ch, :, :], st[:], reads=[kst], writes=[("FM", fm0 + ch)])
        wt, kw = load_w(C_G, 24)
        for tb in range(4):
            acc, ka = next_acc()
            for kc in range(16):
                c.op("pe", lambda e: e.matmul(acc[0:24, :], lhsT=wt[:, kc, 0:24], rhs=hT[:, kc, tb * 512:(tb + 1) * 512],
                                              start=(kc == 0), stop=(kc == 15)), reads=[kw] + hT_keys, writes=[ka])
            c.op("act", lambda e: e.activation(out=self.gst[0:24, tb * 512:(tb + 1) * 512], in_=acc[0:24, :], func=AF.Sigmoid),
                 reads=[ka], writes=["gst"])
        c.dma("act", "gst", self.GS[:, :], self.gst[0:24, :], reads=["gst"], writes=["GS"])
        tm_jobs = [(C_VS, 256, 0, False), (C_VW, 256, 256, False), (C_SV, 1024, 512, False),
                   (C_HI, 1024, 1536, False), (C_HG, 1024, 2560, True)]
        nts = [0]
        for col0, ncols, tm0, sig in tm_jobs:
            for c0 in range(0, ncols, 512):
                n = min(512, ncols - c0)
                wt, kw = load_w(col0 + c0, n)
                for tt in range(16):
                    acc, ka = next_acc()
                    for kc in range(16):
                        c.op("pe", lambda e: e.matmul(acc[:, 0:n], lhsT=hT[:, kc, tt * 128:(tt + 1) * 128], rhs=wt[:, kc, 0:n],
                                                      start=(kc == 0), stop=(kc == 15)), reads=[kw, ("hT", tt)], writes=[ka])
                    i = nts[0] % 2
                    nts[0] += 1
                    st, kst = self.tst[i], ("tst", i)
                    if sig:
                        c.op("act", lambda e: e.activation(out=st[:, 0:n], in_=acc[:, 0:n], func=AF.Sigmoid), reads=[ka], writes=[kst])
                    elif tt % 2 == 0:
                        c.op("act", lambda e: e.copy(out=st[:, 0:n], in_=acc[:, 0:n]), reads=[ka], writes=[kst])
                    else:
                        c.op("dve", lambda e: e.tensor_copy(out=st[:, 0:n], in_=acc[:, 0:n]), reads=[ka], writes=[kst])
                    c.dma("act", f"tst{i}", self.TM[tt * 128:(tt + 1) * 128, tm0 + c0:tm0 + c0 + n], st[:, 0:n],
                          reads=[kst], writes=[("TM", tm0 + c0, tt)])

    def hnorm_block(self, acc, ka, dst, kdst, gain):
        c = self.c
        c.op("act", lambda e: e.activation(out=self.sqb[:], in_=acc[:], func=AF.Square), reads=[ka], writes=["sqb"])
        c.op("pe", lambda e: e.matmul(self.pn[:], lhsT=self.ones_b[:], rhs=self.sqb[:], start=True, stop=True),
             reads=["sqb", "ones"], writes=["pn"])
        c.op("act", lambda e: e.activation(out=self.rt[:], in_=self.pn[:], func=AF.Sqrt, scale=1.0 / HD, bias=self.eps_t[:, 0:1]),
             reads=["pn", "eps"], writes=["rt"])
        c.op("dve", lambda e: e.reciprocal(out=self.rt[:], in_=self.rt[:]), reads=["rt"], writes=["rt"])
        c.op("dve", lambda e: e.scalar_tensor_tensor(out=dst, in0=acc[:], scalar=gain, in1=self.rt[:], op0=ALU.mult, op1=ALU.mult),
             reads=[ka, "rt", "hgain"], writes=[kdst])

    def setup_tables(self):
        c = self.c
        self.cb_t = self.sb("cb", [128, 8], F32)
        self.negc = self.sb("negc", [128, 8], F32)
        c.dma("sp", "cst3", self.cb_t[:], self.cb[:, :], writes=["cb"])
        c.op("dve", lambda e: e.tensor_scalar(out=self.negc[:], in0=self.cb_t[:], scalar1=-1.0, scalar2=None, op0=ALU.mult),
             reads=["cb"], writes=["negc"])
        self.addmask_t = self.sb("addmask", [128, 8, 32], F32)
        c.dma("sp", "cst4", self.addmask_t[:], self.addmask[:, :, :], writes=["addmask"])
        self.esel_t = self.sb("esel", [32, 16, 128], BF16)
        c.dma("sp", "cst5", self.esel_t[:], self.esel[:, :, :], writes=["esel"])
        self.ov33_t = self.sb("ov33", [128, 33], BF16)
        c.dma("sp", "cst6", self.ov33_t[:], self.ov33[:, :], writes=["ov33"])
        self.posT_t = self.sb("posT", [128, self.L, 2, 32], F32)
        c.dma("sp", "cst8", self.posT_t[:], self.posT[:, :, :, :], writes=["posT"])
        self.posb = self.sb("posb", [128, self.L, 2, 32], BF16)
        c.op("dve", lambda e: e.tensor_copy(out=self.posb[:], in_=self.posT_t[:]), reads=["posT"], writes=["posb"])
        self.idf = self.sb("idf2", [128, 128], F32)
        c.op("pool", lambda e: e.memset(self.idf[:], 1.0), writes=["idf2"])
        c.op("pool", lambda e: e.affine_select(out=self.idf[:], in_=self.idf[:], pattern=[[-1, 128]], compare_op=ALU.is_equal,
                                               fill=0.0, base=0, channel_multiplier=1), reads=["idf2"], writes=["idf2"])
        with self.phase():
            eb = self.sb("ebl", [128, 2688], F32)
            eba_s = self.sb("eba_s", [128, 640], BF16)
            for h in range(8):
                c.dma("sp", "ebl", eb[:], self.ebias[h, :, :], writes=["ebl"])
                c.op("dve", lambda e: e.tensor_scalar(out=eba_s[:], in0=eb[:, 0:640], scalar1=self.negc[:, h:h + 1], scalar2=1.0 / SCALE,
                                                      op0=ALU.add, op1=ALU.mult), reads=["ebl", "negc"], writes=["eba_s"])
                c.dma("sp", "ebas", self.EBA[h, :, :], eba_s[:], reads=["eba_s"], writes=[("EBA", h)])
                c.op("act", lambda e: e.activation(out=eb[:], in_=eb[:], func=AF.Exp, bias=self.negc[:, h:h + 1]),
                     reads=["ebl", "negc"], writes=["ebl"])
                c.dma("sp", "ebs", self.EBD[h, :, :], eb[:], reads=["ebl"], writes=[("EBD", h)])

    def gelu_tanh(self, dst, kdst, src_ps, ksrc, bias, n):
        c = self.c
        x, x2 = self.gx, self.gx2
        c.op("act", lambda e: e.activation(out=x[:, 0:n], in_=src_ps, func=AF.Identity, bias=bias), reads=[ksrc, "cbias"], writes=["gx"])
        c.op("dve", lambda e: e.tensor_tensor(out=x2[:, 0:n], in0=x[:, 0:n], in1=x[:, 0:n], op=ALU.mult), reads=["gx"], writes=["gx2"])
        c.op("dve", lambda e: e.tensor_scalar(out=x2[:, 0:n], in0=x2[:, 0:n], scalar1=0.044715, scalar2=1.0, op0=ALU.mult, op1=ALU.add),
             reads=["gx2"], writes=["gx2"])
        c.op("dve", lambda e: e.tensor_tensor(out=x2[:, 0:n], in0=x2[:, 0:n], in1=x[:, 0:n], op=ALU.mult), reads=["gx", "gx2"], writes=["gx2"])
        c.op("act", lambda e: e.activation(out=x2[:, 0:n], in_=x2[:, 0:n], func=AF.Sigmoid, scale=1.5957691216), reads=["gx2"], writes=["gx2"])
        c.op("dve", lambda e: e.tensor_tensor(out=dst, in0=x2[:, 0:n], in1=x[:, 0:n], op=ALU.mult), reads=["gx", "gx2"], writes=[kdst])

    def compress(self, li, j, xT, kx, w1b, w2b, hid):
        c = self.c
        b5 = self.bk[5]
        k5 = ("bk", 5)
        c.dma("pool", "w1b", w1b[:], self.cmp_w1[li, j].rearrange("l d e -> d l e"), writes=["w1b"])
        c.dma("pool", "w2b", w2b[:], self.cmp_w2[li, j], writes=["w2b"])
        for l in range(32):
            c.op("pe", lambda e: e.matmul(b5[:, 0:127], lhsT=w1b[:, l, :], rhs=xT[:, l:l + 16 * 126 + 1:16], start=(l == 0), stop=(l == 31)),
                 reads=["w1b", kx], writes=[k5])
        for l in range(32):
            c.op("pe", lambda e: e.matmul(b5[:, 128:129], lhsT=w1b[:, l, :], rhs=self.posb[:, li, j, l:l + 1], start=False, stop=(l == 31)),
                 reads=["w1b", "posb"], writes=[k5])
        c.op("dve", lambda e: e.tensor_copy(out=self.cbias[:], in_=b5[:, 128:129]), reads=[k5], writes=["cbias"])
        c.op("pool", lambda e: e.memset(hid[:], 0.0), writes=["hid"])
        self.gelu_tanh(hid[:, 0:127], "hid", b5[:, 0:127], k5, self.cbias[:, 0:1], 127)

    def gate_bcast(self, krow, cols):
        c = self.c
        n = cols[1] - cols[0]
        c.op("pe", lambda e: e.matmul(self.bk[4][:, 0:n], lhsT=self.selg_t[:, krow, :], rhs=self.gsb[0:24, cols[0]:cols[1]], start=True, stop=True),
             reads=["selg", "gsb"], writes=[("bk", 4)])

    def attn_finish(self, h, br, G, oacc, first, guard=False, uz=(2, 3)):
        c = self.c
        U, kU = self.bk[uz[0]], ("bk", uz[0])
        Z, kZ = self.bk[uz[1]], ("bk", uz[1])
        i = self.nfin % 2
        self.nfin += 1
        w, kw_ = self.wvs[i], ("wv", i)
        tmp, kt_ = self.tmps[i], ("tmpv", i)
        gb = self.gbs[br][:, G * 512:(G + 1) * 512]
        if guard:
            c.op("dve", lambda e: e.tensor_scalar(out=w[:], in0=Z[:], scalar1=1e-30, scalar2=None, op0=ALU.max), reads=[kZ], writes=[kw_])
            c.op("dve", lambda e: e.reciprocal(out=w[:], in_=w[:]), reads=[kw_], writes=[kw_])
        else:
            c.op("dve", lambda e: e.reciprocal(out=w[:], in_=Z[:]), reads=[kZ], writes=[kw_])
        c.op("pool", lambda e: e.tensor_tensor(out=w[:], in0=gb, in1=w[:], op=ALU.mult), reads=[kw_, ("gbs", br)], writes=[kw_])
        dst = oacc[:, G * 512:(G + 1) * 512]
        ko = ("oacc", id(oacc), G)
        if first:
            c.op("dve", lambda e: e.tensor_tensor(out=dst, in0=U[:], in1=w[:], op=ALU.mult), reads=[kw_, kU], writes=[ko])
        else:
            c.op("dve", lambda e: e.tensor_tensor(out=tmp[:], in0=U[:], in1=w[:], op=ALU.mult), reads=[kw_, kU], writes=[kt_])
            c.op("pool", lambda e: e.tensor_tensor(out=dst, in0=dst, in1=tmp[:], op=ALU.add), reads=[kt_, ko], writes=[ko])

    def gate_rows(self, h, brs):
        c = self.c
        for br in brs:
            row = h * 3 + br
            c.dma("sp", f"gbs{br}", self.gbs[br][:], self.GS[row:row + 1, :].partition_broadcast(128), reads=["GS"], writes=[("gbs", br)])

    def banded_attn(self, h, G, qT, kq, kT, kk, V, kv, eb, keb, mode, nmT=None, uz=(2, 3)):
        c = self.c
        if mode == "win":
            kts = [kt for kt in range(max(0, 4 * G - 4), 4 * G + 4)]
        else:
            kts = list(range(0, 4 * G + 4))
        use_mask = (mode == "sel" and G >= 2)
        U, kU = self.bk[uz[0]], ("bk", uz[0])
        Z, kZ = self.bk[uz[1]], ("bk", uz[1])
        info = []
        for kt in kts:
            qlo = max(kt, 4 * G)
            qhi = min(kt + 4, 4 * G + 3) if mode == "win" else 4 * G + 3
            c0, c1 = (qlo - 4 * G) * 128, (qhi - 4 * G + 1) * 128
            i = self.nS % 2
            self.nS += 1
            info.append((kt, qlo, qhi, c0, c1, i))

        def stage_a(k):
            kt, qlo, qhi, c0, c1, i = info[k]
            n = c1 - c0
            S, kS = self.bk[i], ("bk", i)
            P, kP = self.P[i], ("P", i)
            subs = []
            for qb in range(qlo, qhi + 1):
                d = qb - kt
                if mode == "win":
                    ti = {0: 0, 1: 1, 4: 2}.get(d)
                    off = 0
                else:
                    ti = {0: 0, 1: 1}.get(d)
                    off = 384
                if ti is None:
                    continue
                subs.append(((qb - qlo) * 128, off + ti * 128))
            c.op("pe", lambda e: e.matmul(S[:, 0:n], lhsT=kT[:, kt * 128:(kt + 1) * 128], rhs=qT[:, G * 512 + c0:G * 512 + c1],
                                          start=True, stop=False), reads=[kk, kq], writes=[kS])
            if use_mask:
                c.op("pe", lambda e: e.matmul(S[:, 0:n], lhsT=self.esel_t[:, kt, :], rhs=nmT[:, G * 512 + c0 - 1024:G * 512 + c1 - 1024],
                                              start=False, stop=False), reads=["esel", "nmT"], writes=[kS])
            for (pc, tc) in subs:
                c.op("pe", lambda e: e.matmul(S[:, pc:pc + 128], lhsT=self.ident[:], rhs=self.eba[:, tc:tc + 128], start=False, stop=False),
                     reads=["ident", "eba"], writes=[kS])
            c.op("pe", lambda e: e.matmul(S[0:32, 0:2], lhsT=self.esel_t[:, 0, 0:32], rhs=self.zero32[:, 0:2], start=False, stop=True),
                 reads=["esel", "zero32"], writes=[kS])
            c.op("act", lambda e: e.activation(out=P[:, 0:n], in_=S[:, 0:n], func=AF.Exp, scale=SCALE, bias=self.cb_t[:, h:h + 1]),
                 reads=[kS, "cb"], writes=[kP])

        def stage_b(k):
            kt, qlo, qhi, c0, c1, i = info[k]
            n = c1 - c0
            P, kP = self.P[i], ("P", i)
            c.op("pe", lambda e: e.matmul(U[:, c0:c1], lhsT=V[:, kt, :], rhs=P[:, 0:n], start=(k == 0), stop=(k == len(info) - 1)),
                 reads=[kv, kP], writes=[kU])
            c.op("pe", lambda e: e.matmul(Z[:, c0:c1], lhsT=self.ones_b[:], rhs=P[:, 0:n], start=(k == 0), stop=(k == len(info) - 1)),
                 reads=["ones", kP], writes=[kZ])
        stage_a(0)
        for k in range(len(info)):
            if k + 1 < len(info):
                stage_a(k + 1)
            stage_b(k)

    def phase_nsa(self, li, s):
        c = self.c
        sb = self.sb
        self.gsb = sb("gsb", [32, T], F32)
        self.wvs = [sb("wv", [128, 512], F32) for _ in range(2)]
        self.tmps = [sb("tmpv", [128, 512], F32) for _ in range(2)]
        self.tmpv = self.tmps[0]
        self.gbs = [sb("gbs", [128, T], F32) for _ in range(3)]
        self.nfin = 0
        self.P = [sb("P", [128, 512], BF16) for _ in range(2)]
        self.gx = sb("gx", [128, 128], F32)
        self.gx2 = sb("gx2", [128, 128], F32)
        self.cbias = sb("cbias", [128, 1], F32)
        self.nS = 0
        self.cexp = sb("cexp", [128, 512], F32)
        w1b = sb("w1b", [128, 32, 128], BF16)
        w2b = sb("w2b", [128, 128], BF16)
        xk = sb("xk", [128, T], BF16)
        xv = sb("xv", [128, T], BF16)
        hid = sb("hid", [128, 128], BF16)
        kcmpT = sb("kcmpT", [128, 128], BF16)
        vcmp = sb("vcmp", [128, 1, 128], BF16)
        ksT = sb("ksT", [128, T], BF16)
        kwT = sb("kwT", [128, T], BF16)
        vs = sb("vs", [128, 16, 128], BF16)
        vw = sb("vw", [128, 16, 128], BF16)
        qT = [sb("qT", [128, T], BF16) for _ in range(4)]
        oacc = [sb("oacc", [128, T], F32) for _ in range(4)]
        obf = sb("obf", [128, T], BF16)
        eb = sb("eb", [128, 2688], F32)
        eba4 = sb("eba", [128, 4, 640], BF16)
        self.zero32 = sb("zero32", [32, 2], BF16)
        c.op("pool", lambda e: e.memset(self.zero32[:], 0.0), writes=["zero32"])
        impacc = sb("impacc", [128, 8, 32], F32)
        imps = sb("imps", [128, 40], F32)
        sc = sb("sc", [128, 32], F32)
        sc2 = sb("sc2", [128, 32], F32)
        m8 = sb("m8", [128, 16], F32)
        nmT = sb("nmT", [32, T // 2], BF16)
        b5, k5 = self.bk[5], ("bk", 5)
        c.dma("sp", "gsb", self.gsb[0:24, :], self.GS[:, :], reads=["GS"], writes=["gsb"])
        for g in range(2):
            c.dma("sp", "xk", xk[:], self.FM[8 + g, :, :], reads=[("FM", 8 + g)], writes=["xk"])
            c.dma("sp", "xv", xv[:], self.FM[10 + g, :, :], reads=[("FM", 10 + g)], writes=["xv"])
            c.dma("sp", "ksT", ksT[:], self.FM[12 + g, :, :], reads=[("FM", 12 + g)], writes=["ksT"])
            c.dma("sp", "kwT", kwT[:], self.FM[14 + g, :, :], reads=[("FM", 14 + g)], writes=["kwT"])
            tmv = self.TM.rearrange("(kt p) n -> p kt n", p=128)
            c.dma("sp", "vs", vs[:], tmv[:, :, g * 128:(g + 1) * 128], reads=[("TM", 0, t) for t in range(16)], writes=["vs"])
            c.dma("sp", "vw", vw[:], tmv[:, :, 256 + g * 128:256 + (g + 1) * 128], reads=[("TM", 256, t) for t in range(16)], writes=["vw"])
            self.compress(li, 0, xk, "xk", w1b, w2b, hid)
            c.op("pe", lambda e: e.matmul(b5[:, 256:384], lhsT=w2b[:], rhs=hid[:], start=True, stop=True), reads=["w2b", "hid"], writes=[k5])
            c.op("act", lambda e: e.activation(out=self.sqb[:, 0:128], in_=b5[:, 256:384], func=AF.Square), reads=[k5], writes=["sqb"])
            c.op("pe", lambda e: e.matmul(self.bk[4][:, 0:128], lhsT=self.ones_b[:], rhs=self.sqb[:, 0:128], start=True, stop=True),
                 reads=["sqb", "ones"], writes=[("bk", 4)])
            c.op("act", lambda e: e.activation(out=self.rt[:, 0:128], in_=self.bk[4][:, 0:128], func=AF.Sqrt, scale=1.0 / HD, bias=self.eps_t[:, 0:1]),
                 reads=[("bk", 4), "eps"], writes=["rt"])
            c.op("dve", lambda e: e.reciprocal(out=self.rt[:, 0:128], in_=self.rt[:, 0:128]), reads=["rt"], writes=["rt"])
            c.op("dve", lambda e: e.scalar_tensor_tensor(out=kcmpT[:], in0=b5[:, 256:384], scalar=self.hgain_t[:, li, 1:2], in1=self.rt[:, 0:128],
                                                         op0=ALU.mult, op1=ALU.mult), reads=[k5, "rt", "hgain"], writes=["kcmpT"])
            self.compress(li, 1, xv, "xv", w1b, w2b, hid)
            c.op("pe", lambda e: e.matmul(b5[:, 256:384], lhsT=hid[:], rhs=w2b[:], start=True, stop=True), reads=["w2b", "hid"], writes=[k5])
            c.op("dve", lambda e: e.tensor_copy(out=vcmp[:, 0, :], in_=b5[:, 256:384]), reads=[k5], writes=["vcmp"])
            c.op("pool", lambda e: e.memset(impacc[:], 0.0), writes=["impacc"])
            for r in range(4):
                h = 4 * g + r
                c.dma("sp", f"qT{r}", qT[r][:], self.FM[h, :, :], reads=[("FM", h)], writes=[("qT", r)])
                c.dma("sp", "eb", eb[:], self.EBD[h, :, :], reads=[("EBD", h)], writes=["eb"])
                self.gate_rows(h, [0])
                for G in range(4):
                    i = self.nS % 2
                    self.nS += 1
                    S, kS = self.bk[i], ("bk", i)
                    P, kP = self.P[i], ("P", i)
                    c.op("pe", lambda e: e.matmul(S[:], lhsT=kcmpT[:], rhs=qT[r][:, G * 512:(G + 1) * 512], start=True, stop=True),
                         reads=["kcmpT", ("qT", r)], writes=[kS])
                    c.op("act", lambda e: e.activation(out=self.cexp[:], in_=S[:], func=AF.Exp, scale=SCALE, bias=self.cb_t[:, h:h + 1]),
                         reads=[kS, "cb"], writes=["cexp"])
                    c.op("dve", lambda e: e.tensor_tensor(out=P[:], in0=self.cexp[:], in1=eb[:, 640 + G * 512:640 + (G + 1) * 512], op=ALU.mult),
                         reads=["cexp", "eb"], writes=[kP])
                    c.op("pe", lambda e: e.matmul(self.bk[2][:], lhsT=vcmp[:, 0, :], rhs=P[:], start=True, stop=True), reads=["vcmp", kP], writes=[("bk", 2)])
                    c.op("pe", lambda e: e.matmul(self.bk[3][:], lhsT=self.ones_b[:], rhs=P[:], start=True, stop=True), reads=["ones", kP], writes=[("bk", 3)])
                    if G >= 2:
                        for q4 in range(4):
                            tt = G * 4 + q4 - 8
                            c.op("pe", lambda e: e.matmul(b5[:, q4 * 64:q4 * 64 + 33], lhsT=P[:, q4 * 128:(q4 + 1) * 128], rhs=self.ov33_t[:],
                                                          start=True, stop=True), reads=["ov33", kP], writes=[k5])
                            c.op("dve", lambda e: e.reciprocal(out=imps[:, 32:33], in_=b5[:, q4 * 64 + 32:q4 * 64 + 33]), reads=[k5], writes=["imps"])
                            c.op("dve", lambda e: e.tensor_scalar(out=imps[:, 0:32], in0=b5[:, q4 * 64:q4 * 64 + 32], scalar1=imps[:, 32:33], scalar2=None,
                                                                  op0=ALU.mult), reads=[k5, "imps"], writes=["imps2"])
                            c.op("dve", lambda e: e.tensor_tensor(out=impacc[:, tt, :], in0=impacc[:, tt, :], in1=imps[:, 0:32], op=ALU.add),
                                 reads=["imps2", "impacc"], writes=["impacc"])
                    self.attn_finish(h, 0, G, oacc[r], True, guard=True)
            for tt in range(8):
                c.op("dve", lambda e: e.tensor_tensor(out=sc[:], in0=impacc[:, tt, :], in1=self.addmask_t[:, tt, :], op=ALU.add),
                     reads=["impacc", "addmask"], writes=["sc"])
                c.op("dve", lambda e: e.max(out=m8[:, 0:8], in_=sc[:]), reads=["sc"], writes=["m8"])
                c.op("dve", lambda e: e.match_replace(out=sc2[:], in_to_replace=m8[:, 0:8], in_values=sc[:], imm_value=-3e38),
                     reads=["sc", "m8"], writes=["sc2"])
                c.op("dve", lambda e: e.max(out=m8[:, 8:16], in_=sc2[:]), reads=["sc2"], writes=["m8b"])
                c.op("dve", lambda e: e.tensor_scalar(out=sc2[:], in0=sc[:], scalar1=m8[:, 15:16], scalar2=None, op0=ALU.is_ge),
                     reads=["sc", "m8b"], writes=["sc2"])
                c.op("dve", lambda e: e.tensor_scalar(out=sc2[:], in0=sc2[:], scalar1=30000.0, scalar2=-30000.0, op0=ALU.mult, op1=ALU.add),
                     reads=["sc2"], writes=["sc2"])
                c.op("pe", lambda e: e.transpose(out=b5[0:32, 384:512], in_=sc2[:], identity=self.idf[:]), reads=["sc2", "idf2"], writes=[k5])
                c.op("act", lambda e: e.copy(out=nmT[:, tt * 128:(tt + 1) * 128], in_=b5[0:32, 384:512]), reads=[k5], writes=["nmT"])
            c.dma("sp", "eba", eba4[:], self.EBA[4 * g:4 * g + 4, :, :].rearrange("h p n -> p h n"), reads=[("EBA", 4 * g + r_) for r_ in range(4)], writes=["eba"])
            for r in range(4):
                h = 4 * g + r
                c.dma("sp", "eb", eb[:], self.EBD[h, :, :], reads=[("EBD", h)], writes=["eb"])
                self.gate_rows(h, [1, 2])
                self.eba = eba4[:, r, :]
                for G in range(4):
                    self.banded_attn(h, G, qT[r], ("qT", r), ksT, "ksT", vs, "vs", eb, "eb", "sel", nmT, uz=(2, 3))
                    self.banded_attn(h, G, qT[r], ("qT", r), kwT, "kwT", vw, "vw", eb, "eb", "win", uz=(4, 5))
                    self.attn_finish(h, 1, G, oacc[r], False, uz=(2, 3))
                    self.attn_finish(h, 2, G, oacc[r], False, uz=(4, 5))
                c.op("act", lambda e: e.copy(out=obf[:], in_=oacc[r][:]), reads=[("oacc", id(oacc[r]), G) for G in range(4)], writes=["obf"])
                c.dma("sp", "obf", self.OT[h, :, :], obf[:], reads=["obf"], writes=[("OT", h)])

    def setup_tables2(self):
        c = self.c
        L = self.L
        self.trineg = self.sb("trineg", [128, 128], BF16)
        self.mstrict = self.sb("mstrict", [128, 128], BF16)
        self.masku = self.sb("masku", [128, 64], F32)
        self.rst = self.sb("rst", [128, T], F32)
        tmp = self.sb("tmpc", [128, 128], F32)
        c.op("pool", lambda e: e.memset(tmp[:], -1.0), writes=["tmpc"])
        c.op("pool", lambda e: e.affine_select(out=tmp[:], in_=tmp[:], pattern=[[-1, 128]], compare_op=ALU.is_ge, fill=0.0, base=0, channel_multiplier=1),
             reads=["tmpc"], writes=["tmpc"])
        c.op("pool", lambda e: e.tensor_copy(out=self.trineg[:], in_=tmp[:]), reads=["tmpc"], writes=["trineg"])
        c.op("pool", lambda e: e.memset(tmp[:], 1.0), reads=["tmpc"], writes=["tmpc"])
        c.op("pool", lambda e: e.affine_select(out=tmp[:], in_=tmp[:], pattern=[[1, 128]], compare_op=ALU.is_gt, fill=0.0, base=0, channel_multiplier=-1),
             reads=["tmpc"], writes=["tmpc"])
        c.op("pool", lambda e: e.tensor_copy(out=self.mstrict[:], in_=tmp[:]), reads=["tmpc"], writes=["mstrict"])
        self.mneg = self.sb("mneg", [128, 128], BF16)
        c.op("pool", lambda e: e.memset(tmp[:], -30000.0), reads=["tmpc"], writes=["tmpc"])
        c.op("pool", lambda e: e.affine_select(out=tmp[:], in_=tmp[:], pattern=[[-1, 128]], compare_op=ALU.is_ge, fill=0.0, base=0, channel_multiplier=1),
             reads=["tmpc"], writes=["tmpc"])
        c.op("pool", lambda e: e.tensor_copy(out=self.mneg[:], in_=tmp[:]), reads=["tmpc"], writes=["mneg"])
        c.op("pool", lambda e: e.memset(self.masku[:], 1.0), writes=["masku"])
        for p0 in (0, 64):
            c.op("pool", lambda e: e.affine_select(out=self.masku[p0:p0 + 64, :], in_=self.masku[p0:p0 + 64, :], pattern=[[1, 64]], compare_op=ALU.is_ge,
                                                   fill=0.0, base=0, channel_multiplier=-1), reads=["masku"], writes=["masku"])
        c.op("pool", lambda e: e.memset(self.rst[:], 1.0), writes=["rst"])
        c.op("pool", lambda e: e.memset(self.rst[:].rearrange("p (c t) -> p c t", t=64)[:, :, 0:1], 0.0), reads=["rst"], writes=["rst"])
        hl = self.sb("hl", [128, 4, 8], F32)
        lc = self.sb("lc", [128, L, 4], F32)
        self.lb = self.sb("lb", [128, L, 8], F32)
        self.oml = self.sb("oml", [128, L, 8], F32)
        ssum = self.sb("ssum", [128, 8], F32)
        c.dma("sp", "cst9", hl[:], self.hlb[:, :, :], writes=["hl"])
        c.dma("sp", "cst10", lc[:], self.lcum[:, :, :], writes=["lc"])
        c.op("act", lambda e: e.activation(out=hl[:], in_=hl[:], func=AF.Exp), reads=["hl"], writes=["hl"])
        c.op("dve", lambda e: e.tensor_tensor(out=ssum[:], in0=hl[:, 0, :], in1=hl[:, 1, :], op=ALU.add), reads=["hl"], writes=["ssum"])
        c.op("dve", lambda e: e.tensor_tensor(out=ssum[:], in0=ssum[:], in1=hl[:, 2, :], op=ALU.add), reads=["hl", "ssum"], writes=["ssum"])
        c.op("dve", lambda e: e.tensor_tensor(out=ssum[:], in0=ssum[:], in1=hl[:, 3, :], op=ALU.add), reads=["hl", "ssum"], writes=["ssum"])
        c.op("dve", lambda e: e.reciprocal(out=ssum[:], in_=ssum[:]), reads=["ssum"], writes=["ssum"])
        for l4 in range(4):
            c.op("dve", lambda e: e.tensor_tensor(out=hl[:, l4, :], in0=hl[:, l4, :], in1=ssum[:], op=ALU.mult), reads=["hl", "ssum"], writes=["hl"])
        for li in range(L):
            c.op("dve", lambda e: e.tensor_scalar(out=self.lb[:, li, :], in0=hl[:, 0, :], scalar1=lc[:, li, 0:1], scalar2=None, op0=ALU.mult),
                 reads=["hl", "lc"], writes=["lb"])
            for l4 in range(1, 4):
                c.op("dve", lambda e: e.scalar_tensor_tensor(out=self.lb[:, li, :], in0=hl[:, l4, :], scalar=lc[:, li, l4:l4 + 1], in1=self.lb[:, li, :],
                                                             op0=ALU.mult, op1=ALU.add), reads=["hl", "lc", "lb"], writes=["lb"])
        c.op("dve", lambda e: e.tensor_scalar(out=self.oml[:], in0=self.lb[:], scalar1=-1.0, scalar2=1.0, op0=ALU.mult, op1=ALU.add),
             reads=["lb"], writes=["oml"])
        self.gn_t = self.sb("gn", [128, L, 128], F32)
        c.dma("sp", "cst11", self.gn_t[:], self.gn[:, :, :], writes=["gn"])
        self.gffn = self.sb("gffn", [128, L, 16], F32)
        c.dma("sp", "cst12", self.gffn[:], self.norm_ffn[:, :, :], writes=["gffn"])
        self.convw_t = self.sb("convw", [128, L, 3, 88], F32)
        c.dma("sp", "cst13", self.convw_t[:], self.convw[:, :, :, :], writes=["convw"])
        self.convb_t = self.sb("convb", [128, L, 88], F32)
        c.dma("sp", "cst14", self.convb_t[:], self.convb[:, :, :], writes=["convb"])

    def phase_sb(self, li, s):
        c = self.c
        sb = self.sb
        qTs = [sb("sqT", [128, T], BF16) for _ in range(2)]
        kTs = [sb("skT", [128, T], BF16) for _ in range(2)]
        Vs = [sb("sV", [128, 16, 128], BF16) for _ in range(2)]
        R = sb("sR", [128, 4, 512], F32)
        e32 = [sb("se32", [128, 512], F32) for _ in range(2)]
        arg = sb("sarg", [128, 512], F32)
        spb = [sb("spb", [128, 512], BF16) for _ in range(2)]
        A = [sb("sA", [128, 512], BF16) for _ in range(2)]
        obf = [sb("sobf", [128, T], BF16) for _ in range(2)]
        tmv = self.TM.rearrange("(kt p) n -> p kt n", p=128)

        def load_head(h):
            b = h % 2
            c.dma("sp", f"sqT{b}", qTs[b][:], self.FM[16 + h, :, :], reads=[("FM", 16 + h)], writes=[("sqT", b)])
            c.dma("sp", f"skT{b}", kTs[b][:], self.FM[24 + h, :, :], reads=[("FM", 24 + h)], writes=[("skT", b)])
            c.dma("sp", f"sV{b}", Vs[b][:], tmv[:, :, 512 + h * 128:512 + (h + 1) * 128],
                  reads=[("TM", 512 + (h // 4) * 512, t) for t in range(16)], writes=[("sV", b)])
        load_head(0)
        blocks = []
        for h in range(8):
            for G in range(4):
                kts = list(range(4 * G + 3, -1, -1))
                for kt in kts:
                    blocks.append((h, G, kt, kt == kts[0], kt == kts[-1]))

        def stage_a(k):
            h, G, kt, firstk, lastk = blocks[k]
            hb = h % 2
            qT, kT = qTs[hb], kTs[hb]
            if firstk:
                c.op("pool", lambda e: e.memset(R[:, G, :], 0.0), writes=[("sR", G)])
            qlo = max(kt, 4 * G)
            c0 = (qlo - 4 * G) * 128
            n = 512 - c0
            i = k % 2
            S, kS = self.bk[i], ("bk", i)
            sp_, ksp = spb[i], ("spb", i)
            diag = kt >= 4 * G
            c.op("pe", lambda e: e.matmul(S[:, 0:n], lhsT=kT[:, kt * 128:(kt + 1) * 128], rhs=qT[:, G * 512 + c0:(G + 1) * 512], start=True, stop=False),
                 reads=[("skT", hb), ("sqT", hb)], writes=[kS])
            if diag:
                c.op("pe", lambda e: e.matmul(S[:, 0:128], lhsT=self.ident[:], rhs=self.mneg[:], start=False, stop=False),
                     reads=["ident", "mneg"], writes=[kS])
            c.op("act", lambda e: e.activation(out=e32[i][:, 0:n], in_=S[:, 0:n], func=AF.Exp), reads=[kS], writes=[("se32", i)])
            c.op("act", lambda e: e.activation(out=sp_[:, 0:n], in_=e32[i][:, 0:n], func=AF.Ln, bias=1.0), reads=[("se32", i)], writes=[ksp])

        def stage_b(k):
            h, G, kt, firstk, lastk = blocks[k]
            hb = h % 2
            V = Vs[hb]
            qlo = max(kt, 4 * G)
            c0 = (qlo - 4 * G) * 128
            n = 512 - c0
            i = k % 2
            S, kS = self.bk[i], ("bk", i)
            sp_, ksp = spb[i], ("spb", i)
            A_, kA = A[i], ("sA", i)
            U, kU = (self.bk[2], ("bk", 2)) if G % 2 == 0 else (self.bk[4], ("bk", 4))
            Cs, kCs = (self.bk[3], ("bk", 3)) if k % 2 == 0 else (self.bk[5], ("bk", 5))
            diag = kt >= 4 * G
            if G == 0 and firstk and h + 1 < 8:
                load_head(h + 1)
            c.op("pe", lambda e: e.matmul(S[:, 0:n], lhsT=self.trineg[:], rhs=sp_[:, 0:n], start=False, stop=True), reads=["trineg", ksp], writes=[kS])
            c.op("pe", lambda e: e.matmul(Cs[:, 0:n], lhsT=self.ones_b[:], rhs=sp_[:, 0:n], start=True, stop=True), reads=["ones", ksp], writes=[kCs])
            c.op("dve", lambda e: e.tensor_tensor(out=arg[:, 0:n], in0=S[:, 0:n], in1=R[:, G, c0:512], op=ALU.subtract), reads=[kS, ("sR", G)], writes=["sarg"])
            c.op("act", lambda e: e.activation(out=A_[:, 0:n], in_=arg[:, 0:n], func=AF.Exp), reads=["sarg"], writes=[kA])
            if not lastk:
                c.op("dve", lambda e: e.tensor_tensor(out=R[:, G, c0:512], in0=R[:, G, c0:512], in1=Cs[:, 0:n], op=ALU.add), reads=[("sR", G), kCs], writes=[("sR", G)])
            c.op("pe", lambda e: e.matmul(U[:, c0:512], lhsT=V[:, kt, :], rhs=A_[:, 0:n], start=firstk, stop=lastk), reads=[("sV", hb), kA], writes=[kU])
            if lastk:
                ob = obf[hb]
                c.op("act", lambda e: e.copy(out=ob[:, G * 512:(G + 1) * 512], in_=U[:]), reads=[kU], writes=[("sobf", hb)])
                if G == 3:
                    c.dma("sp", f"sobf{hb}", self.OT[8 + h, :, :], ob[:], reads=[("sobf", hb)], writes=[("OT", 8 + h)])
        stage_a(0)
        for k in range(len(blocks)):
            if k + 1 < len(blocks):
                stage_a(k + 1)
            stage_b(k)

    def phase_hg(self, li, s):
        c = self.c
        sb = self.sb
        qf = sb("hq", [128, T], BF16)
        F = sb("hF", [128, T], F32)
        LF = sb("hLF", [128, T], F32)
        Kk = sb("hK", [128, T], F32)
        b = sb("hb", [128, T], F32)
        d1 = sb("hd1", [128, T], F32)
        E = sb("hE", [128, T], F32)
        qp = sb("hqp", [128, T], BF16)
        kp = sb("hkp", [128, T], BF16)
        kd = sb("hkd", [128, T], BF16)
        kdT = sb("hkdT", [128, 16, 128], BF16)
        V = sb("hV", [128, 16, 128], BF16)
        Gs = sb("hGs", [128, 16, 128], BF16)
        emid = sb("hemid", [128, 32], F32)
        elast = sb("helast", [128, 32], F32)
        S = sb("hS", [128, 128], F32)
        Sb = sb("hSb", [128, 128], BF16)
        am = sb("ham", [128, 64], BF16)
        ss = sb("hss", [128, 4], F32)
        junk = sb("hjunk", [128, 128], F32)
        y = sb("hy", [128, 128], F32)
        ybs = [sb("hyb", [128, 128], BF16) for _ in range(2)]
        ohT = sb("hohT", [128, T], BF16)
        tmv = self.TM.rearrange("(kt p) n -> p kt n", p=128)
        c3 = lambda a: a[:].rearrange("p (c t) -> p c t", t=64)
        for h in range(8):
            c.dma("sp", "hq", qf[:], self.FM[32 + h, :, :], reads=[("FM", 32 + h)], writes=["hq"])
            c.dma("sp", "hF", F[:], self.HF[h, :, :], reads=[("HF", h)], writes=["hF"])
            c.dma("sp", "hV", V[:], tmv[:, :, 1536 + h * 128:1536 + (h + 1) * 128],
                  reads=[("TM", 1536 + (h // 4) * 512, t) for t in range(16)], writes=["hV"])
            c.dma("sp", "hGs", Gs[:], tmv[:, :, 2560 + h * 128:2560 + (h + 1) * 128],
                  reads=[("TM", 2560 + (h // 4) * 512, t) for t in range(16)], writes=["hGs"])
            c.op("act", lambda e: e.activation(out=F[:], in_=F[:], func=AF.Sigmoid), reads=["hF"], writes=["hF"])
            c.op("dve", lambda e: e.tensor_scalar(out=F[:], in0=F[:], scalar1=self.oml[:, li, h:h + 1], scalar2=self.lb[:, li, h:h + 1],
                                                  op0=ALU.mult, op1=ALU.add), reads=["hF", "oml", "lb"], writes=["hF"])
            c.op("act", lambda e: e.activation(out=LF[:], in_=F[:], func=AF.Ln), reads=["hF"], writes=["hLF"])
            c.op("dve", lambda e: e.tensor_scalar(out=Kk[:], in0=F[:], scalar1=-1.0, scalar2=1.0, op0=ALU.mult, op1=ALU.add), reads=["hF"], writes=["hK"])
            c.op("dve", lambda e: e.tensor_tensor_scan(out=b[:], data0=self.rst[:], data1=LF[:], initial=0.0, op0=ALU.mult, op1=ALU.add),
                 reads=["hLF", "rst"], writes=["hb"])
            bmid = c3(b)[:, :, 31:32]
            blast = c3(b)[:, :, 63:64]
            c.op("dve", lambda e: e.tensor_tensor(out=c3(d1), in0=c3(b), in1=bmid.broadcast_to([128, 32, 64]), op=ALU.subtract), reads=["hb"], writes=["hd1"])
            c.op("act", lambda e: e.activation(out=E[:], in_=d1[:], func=AF.Exp), reads=["hd1"], writes=["hE"])
            c.op("dve", lambda e: e.tensor_tensor(out=qp[:], in0=qf[:], in1=E[:], op=ALU.mult), reads=["hq", "hE"], writes=["hqp"])
            c.op("act", lambda e: e.activation(out=E[:], in_=d1[:], func=AF.Exp, scale=-1.0), reads=["hd1", "hqp"], writes=["hE"])
            c.op("dve", lambda e: e.tensor_tensor(out=kp[:], in0=Kk[:], in1=E[:], op=ALU.mult), reads=["hK", "hE"], writes=["hkp"])
            c.op("dve", lambda e: e.tensor_tensor(out=c3(d1), in0=blast.broadcast_to([128, 32, 64]), in1=c3(b), op=ALU.subtract), reads=["hb", "hkp"], writes=["hd1"])
            c.op("act", lambda e: e.activation(out=E[:], in_=d1[:], func=AF.Exp), reads=["hd1", "hkp"], writes=["hE"])
            c.op("dve", lambda e: e.tensor_tensor(out=kd[:], in0=Kk[:], in1=E[:], op=ALU.mult), reads=["hK", "hE"], writes=["hkd"])
            c.op("act", lambda e: e.activation(out=emid[:].rearrange("p (c o) -> p c o", o=1), in_=bmid, func=AF.Exp), reads=["hb"], writes=["hemid"])
            c.op("act", lambda e: e.activation(out=elast[:].rearrange("p (c o) -> p c o", o=1), in_=blast, func=AF.Exp), reads=["hb"], writes=["helast"])
            for tt in range(16):
                pt = self.ptr[tt % 2]
                kp_ = ("ptr", tt % 2)
                c.op("pe", lambda e: e.transpose(out=pt[:, 0, :], in_=kd[:, tt * 128:(tt + 1) * 128], identity=self.ident[:]), reads=["hkd", "ident"], writes=[kp_])
                c.op("act", lambda e: e.copy(out=kdT[:, tt, :], in_=pt[:, 0, :]), reads=[kp_], writes=["hkdT"])
            c.op("pool", lambda e: e.memset(S[:], 0.0), writes=["hS"])
            def pre(ch):
                tt, half = ch // 2, ch % 2
                p0 = half * 64
                cs = slice(ch * 64, (ch + 1) * 64)
                aps, ka = (self.bk[0], ("bk", 0)) if ch % 2 == 0 else (self.bk[5], ("bk", 5))
                KV, kKV = (self.bk[3], ("bk", 3)) if ch % 2 == 0 else (self.bk[2], ("bk", 2))
                c.op("pe", lambda e: e.matmul(aps[p0:p0 + 64, 0:64], lhsT=kp[:, cs], rhs=qp[:, cs], start=True, stop=True), reads=["hkp", "hqp"], writes=[ka])
                c.op("dve", lambda e: e.tensor_tensor(out=am[p0:p0 + 64, :], in0=aps[p0:p0 + 64, 0:64], in1=self.masku[p0:p0 + 64, :], op=ALU.mult),
                     reads=[ka, "masku"], writes=[("ham", half)])
                c.op("pe", lambda e: e.matmul(KV[:, 0:128], lhsT=kdT[p0:p0 + 64, tt, :], rhs=V[p0:p0 + 64, tt, :], start=True, stop=True),
                     reads=["hkdT", "hV"], writes=[kKV])

            def main(ch):
                tt, half = ch // 2, ch % 2
                p0 = half * 64
                cs = slice(ch * 64, (ch + 1) * 64)
                KV, kKV = (self.bk[3], ("bk", 3)) if ch % 2 == 0 else (self.bk[2], ("bk", 2))
                O, kO = (self.bk[1], ("bk", 1)) if tt % 2 == 0 else (self.bk[4], ("bk", 4))
                c.op("act", lambda e: e.activation(out=Sb[:], in_=S[:], func=AF.Copy, scale=emid[:, ch:ch + 1]), reads=["hS", "hemid"], writes=["hSb"])
                c.op("pe", lambda e: e.matmul(O[p0:p0 + 64, 0:128], lhsT=am[p0:p0 + 64, :], rhs=V[p0:p0 + 64, tt, :], start=True, stop=False),
                     reads=[("ham", half), "hV"], writes=[kO])
                c.op("pe", lambda e: e.matmul(O[p0:p0 + 64, 0:128], lhsT=qp[:, cs], rhs=Sb[:], start=False, stop=True), reads=["hqp", "hSb"], writes=[kO])
                c.op("dve", lambda e: e.scalar_tensor_tensor(out=S[:], in0=S[:], scalar=elast[:, ch:ch + 1], in1=KV[:, 0:128], op0=ALU.mult, op1=ALU.add),
                     reads=["hS", "helast", kKV], writes=["hS"])
                if half == 1:
                    c.op("act", lambda e: e.activation(out=junk[:], in_=O[:, 0:128], func=AF.Square, accum_out=ss[:, 0:1]), reads=[kO], writes=["hjunk", "hss"])
                    c.op("act", lambda e: e.activation(out=ss[:, 1:2], in_=ss[:, 0:1], func=AF.Sqrt, scale=1.0 / HD, bias=self.eps_t[:, 0:1]),
                         reads=["hss", "eps"], writes=["hss1"])
                    c.op("dve", lambda e: e.reciprocal(out=ss[:, 2:3], in_=ss[:, 1:2]), reads=["hss1"], writes=["hss2"])
                    c.op("dve", lambda e: e.scalar_tensor_tensor(out=y[:], in0=O[:, 0:128], scalar=ss[:, 2:3], in1=self.gn_t[:, li, :], op0=ALU.mult, op1=ALU.mult),
                         reads=[kO, "hss2", "gn"], writes=["hy"])
                    ybt = ybs[tt % 2]
                    c.op("dve", lambda e: e.tensor_tensor(out=ybt[:], in0=y[:], in1=Gs[:, tt, :], op=ALU.mult), reads=["hy", "hGs"], writes=[("hyb", tt % 2)])

            def fin(tt):
                pt = self.ptr[tt % 2]
                kp_ = ("ptr", tt % 2)
                c.op("pe", lambda e: e.transpose(out=pt[:, 1, :], in_=ybs[tt % 2][:], identity=self.ident[:]), reads=[("hyb", tt % 2), "ident"], writes=[kp_])
                c.op("act", lambda e: e.copy(out=ohT[:, tt * 128:(tt + 1) * 128], in_=pt[:, 1, :]), reads=[kp_], writes=["hohT"])
            pre(0)
            for ch in range(32):
                if ch + 1 < 32:
                    pre(ch + 1)
                main(ch)
                if ch % 2 == 0 and ch >= 2:
                    fin(ch // 2 - 1)
            fin(15)
            c.dma("sp", "hohT", self.OT[16 + h, :, :], ohT[:], reads=["hohT"], writes=[("OT", 16 + h)])

    def phase_d(self, li, s, tb, xsrc, xsname, xdst, xdname, carry):
        c = self.c
        sb = self.sb
        t0 = tb * 512
        xblk = sb("xblk", [128, 4, D], F32)
        h2T = sb("h2T", [128, 16, 512], BF16)
        kxb = "xblk"
        c.dma("sp", "xblk", xblk[:], xsrc[s, t0:t0 + 512, :].rearrange("(ts p) n -> p ts n", p=128), reads=[("X", xsname, s, tb)], writes=[kxb])
        nacc = [0]

        def next_acc():
            i = nacc[0] % 2
            nacc[0] += 1
            return self.bk[i], ("bk", i)
        with self.phase():
            hb = sb("hb", [128, 16, 512], BF16)
            ob = sb("ob", [128, 24, 512], BF16)
            sg = [sb("sg", [128, 4, 512], BF16) for _ in range(3)]
            mT = sb("mT", [128, 16, 512], BF16)
            macc = sb("macc", [128, 4, 512], F32)
            tmp = sb("mtmp", [128, 512], F32)
            wt = [sb("wtd", [128, 16, 512], BF16) for _ in range(2)]
            self.xn = [sb("xn", [128, D], BF16)]
            self.sqj = [sb("sqj", [128, D], BF16)]
            self.ss = [sb("ss", [128, 4], F32) for _ in range(2)]
            nl = [0]

            def load(view, n_k, key):
                i = nl[0] % 2
                nl[0] += 1
                c.dma("sp", f"wtd{i}", wt[i][:, 0:n_k, :], view, reads=[key], writes=[("wtd", i)])
                return wt[i], ("wtd", i)
            c.dma("sp", "hb", hb[:], self.HT[:, :, t0:t0 + 512], reads=["HT"], writes=["hb"])
            c.dma("sp", "ob", ob[:], self.OT[:, :, t0:t0 + 512].rearrange("c p t -> p c t"), reads=[("OT", k) for k in range(24)], writes=["ob"])
            win = self.win_b[li].rearrange("(kc p) n -> p kc n", p=128)
            for dg in range(4):
                self.cvt_some(1)
                for br in range(3):
                    col = C_MG + br * 2048 + dg * 512
                    w, kw = load(win[:, :, col:col + 512], 16, ("win_b", li))
                    for j in range(4):
                        acc, ka = next_acc()
                        for kc in range(16):
                            c.op("pe", lambda e: e.matmul(acc[:], lhsT=w[:, kc, j * 128:(j + 1) * 128], rhs=hb[:, kc, :], start=(kc == 0), stop=(kc == 15)),
                                 reads=[kw, "hb"], writes=[ka])
                        c.op("act", lambda e: e.activation(out=sg[br][:, j, :], in_=acc[:], func=AF.Sigmoid), reads=[ka], writes=[("sg", br, j)])
                for br in range(3):
                    wb = self.wbr_b[li, br * 1024:(br + 1) * 1024, :].rearrange("(kc p) n -> p kc n", p=128)
                    w, kw = load(wb[:, :, dg * 512:(dg + 1) * 512], 8, ("wbr_b", li))
                    for j in range(4):
                        acc, ka = next_acc()
                        for c8 in range(8):
                            c.op("pe", lambda e: e.matmul(acc[:], lhsT=w[:, c8, j * 128:(j + 1) * 128], rhs=ob[:, br * 8 + c8, :], start=(c8 == 0), stop=(c8 == 7)),
                                 reads=[kw, "ob"], writes=[ka])
                        if br == 0:
                            c.op("dve", lambda e: e.tensor_tensor(out=macc[:, j, :], in0=acc[:], in1=sg[0][:, j, :], op=ALU.mult),
                                 reads=[ka, ("sg", 0, j)], writes=[("macc", j)])
                        else:
                            c.op("dve", lambda e: e.tensor_tensor(out=tmp[:], in0=acc[:], in1=sg[br][:, j, :], op=ALU.mult),
                                 reads=[ka, ("sg", br, j)], writes=["mtmp"])
                            c.op("pool", lambda e: e.tensor_tensor(out=macc[:, j, :], in0=macc[:, j, :], in1=tmp[:], op=ALU.add),
                                 reads=["mtmp", ("macc", j)], writes=[("macc", j)])
                        if br == 2:
                            c.op("act", lambda e: e.copy(out=mT[:, dg * 4 + j, :], in_=macc[:, j, :]), reads=[("macc", j)], writes=["mT"])
            wo = self.wout_b[li].rearrange("(kc p) n -> p kc n", p=128)
            for ng in range(4):
                w, kw = load(wo[:, :, ng * 512:(ng + 1) * 512], 16, ("wout_b", li))
                for ts in range(4):
                    acc, ka = next_acc()
                    for kc in range(16):
                        c.op("pe", lambda e: e.matmul(acc[:], lhsT=mT[:, kc, ts * 128:(ts + 1) * 128], rhs=w[:, kc, :], start=(kc == 0), stop=(kc == 15)),
                             reads=[kw, "mT"], writes=[ka])
                    xs = xblk[:, ts, ng * 512:(ng + 1) * 512]
                    c.op("dve", lambda e: e.tensor_tensor(out=xs, in0=acc[:], in1=xs, op=ALU.add), reads=[ka, kxb], writes=[kxb])
            for ts in range(4):
                self.norm_tile(xblk[:, ts, :], kxb, self.gffn[:, li, :], h2T, ts * 128, "h2T")
        with self.phase():
            aT = sb("aT", [128, 44, 512], BF16)
            wt = [sb("wtf", [128, 16, 512], BF16) for _ in range(3)]
            ub = [sb("ub", [128, 514], F32) for _ in range(2)]
            cc = [sb("cc", [128, 512], F32) for _ in range(2)]
            nl = [0]

            def load3(view, n_k, key):
                i = nl[0] % 3
                nl[0] += 1
                c.dma("sp", f"wtf{i}", wt[i][:, 0:n_k, :], view, reads=[key], writes=[("wtf", i)])
                return wt[i], ("wtf", i)
            wu = self.wup_b[li].rearrange("(kc p) n -> p kc n", p=128)
            for i4 in range(11):
                self.cvt_some(1)
                wg_, kwg = load3(wu[:, :, i4 * 512:(i4 + 1) * 512], 16, ("wup_b", li))
                wv_, kwv = load3(wu[:, :, DFF + i4 * 512:DFF + (i4 + 1) * 512], 16, ("wup_b", li))
                for j in range(4):
                    i = i4 * 4 + j
                    for side, (w, kw) in enumerate(((wg_, kwg), (wv_, kwv))):
                        ch = i + 44 * side
                        acc, ka = self.bk[2 + side], ("bk", 2 + side)
                        for kc in range(16):
                            c.op("pe", lambda e: e.matmul(acc[:], lhsT=w[:, kc, j * 128:(j + 1) * 128], rhs=h2T[:, kc, :], start=(kc == 0), stop=(kc == 15)),
                                 reads=[kw, "h2T"], writes=[ka])
                        u, ku = ub[side], ("ub", side)
                        cv, kc_ = cc[side], ("cc", side)
                        c.op("pool", lambda e: e.tensor_copy(out=u[:, 0:2], in_=carry[:, ch, :]), reads=[("carry", ch)], writes=[ku])
                        c.op("act", lambda e: e.copy(out=u[:, 2:514], in_=acc[:]), reads=[ka], writes=[ku])
                        c.op("pool", lambda e: e.tensor_copy(out=carry[:, ch, :], in_=u[:, 512:514]), reads=[ku], writes=[("carry", ch)])
                        cw = self.convw_t
                        c.op("pool", lambda e: e.tensor_scalar(out=cv[:], in0=u[:, 2:514], scalar1=cw[:, li, 2, ch:ch + 1], scalar2=self.convb_t[:, li, ch:ch + 1],
                                                               op0=ALU.mult, op1=ALU.add), reads=[ku, "convw", "convb"], writes=[kc_])
                        c.op("dve", lambda e: e.scalar_tensor_tensor(out=cv[:], in0=u[:, 1:513], scalar=cw[:, li, 1, ch:ch + 1], in1=cv[:], op0=ALU.mult, op1=ALU.add),
                             reads=[ku, "convw", kc_], writes=[kc_])
                        c.op("dve", lambda e: e.scalar_tensor_tensor(out=cv[:], in0=u[:, 0:512], scalar=cw[:, li, 0, ch:ch + 1], in1=cv[:], op0=ALU.mult, op1=ALU.add),
                             reads=[ku, "convw", kc_], writes=[kc_])
                    c.op("act", lambda e: e.activation(out=cc[0][:], in_=cc[0][:], func=AF.Silu), reads=[("cc", 0)], writes=[("cc", 0)])
                    c.op("dve", lambda e: e.tensor_tensor(out=aT[:, i, :], in0=cc[0][:], in1=cc[1][:], op=ALU.mult), reads=[("cc", 0), ("cc", 1)], writes=["aT"])
            wd = self.wdn_b[li].rearrange("(kc p) n -> p kc n", p=128)
            for ng in range(4):
                for kg in range(3):
                    nk = 16 if kg < 2 else 12
                    w, kw = load3(wd[:, kg * 16:kg * 16 + nk, ng * 512:(ng + 1) * 512], nk, ("wdn_b", li))
                    for ts in range(4):
                        acc, ka = self.bk[ts], ("bk", ts)
                        for k2 in range(nk):
                            kc = kg * 16 + k2
                            c.op("pe", lambda e: e.matmul(acc[:], lhsT=aT[:, kc, ts * 128:(ts + 1) * 128], rhs=w[:, k2, :], start=(kc == 0), stop=(kc == 43)),
                                 reads=[kw, "aT"], writes=[ka])
                for ts in range(4):
                    xs = xblk[:, ts, ng * 512:(ng + 1) * 512]
                    c.op("dve", lambda e: e.tensor_tensor(out=xs, in0=self.bk[ts][:], in1=xs, op=ALU.add), reads=[("bk", ts), kxb], writes=[kxb])
            c.dma("sp", "xout", xdst[s, t0:t0 + 512, :].rearrange("(ts p) n -> p ts n", p=128), xblk[:], reads=[kxb], writes=[("X", xdname, s, tb)])

    def build(self):
        c = self.c
        self._stacks = []
        self.setup_consts()
        self.eps_t = self.sb("eps", [128, 1], F32)
        c.op("pool", lambda e: e.memset(self.eps_t[:], EPS), writes=["eps"])
        self.ptr = [self.ps("ptr", [128, 8, 128], BF16) for _ in range(2)]
        self.bk = [self.ps("bk", [128, 512], F32) for _ in range(6)]
        self.acc = self.bk[0:2]
        self.pn = self.bk[2]
        self.sqb = self.sb("sqb", [128, 512], BF16)
        self.rt = self.sb("rt", [128, 512], F32)
        self.setup_tables()
        self.setup_tables2()
        self.cvt_queue = []
        for piece in self.convert_pieces(0):
            piece()
        for li in range(self.L):
            xsrc, xsname = (self.x, "xin") if li == 0 else (self.XS[(li - 1) % 2], f"XS{(li - 1) % 2}")
            xdst, xdname = (self.out, "out") if li == self.L - 1 else (self.XS[li % 2], f"XS{li % 2}")
            if li + 1 < self.L:
                self.cvt_queue = self.convert_pieces(li + 1)
            for s in range(NSEQ):
                with self.phase():
                    self.hT = self.sb("hT", [128, 16, T], BF16)
                    self.xt = [self.sb("xt", [128, D], F32) for _ in range(3)]
                    self.xn = [self.sb("xn", [128, D], BF16) for _ in range(2)]
                    self.sqj = [self.sb("sqj", [128, D], BF16) for _ in range(2)]
                    self.ss = [self.sb("ss", [128, 4], F32) for _ in range(2)]
                    self.wt = [self.sb("wt", [128, 16, 512], BF16) for _ in range(2)]
                    self.fst = [self.sb("fst", [128, T], BF16) for _ in range(2)]
                    self.fst32 = self.sb("fst32", [128, T], F32)
                    self.tst = [self.sb("tst", [128, 512], BF16) for _ in range(2)]
                    self.gst = self.sb("gst", [32, T], F32)
                    self.phase_a(li, s, xsrc, xsname)
                    c.dma("sp", "hts", self.HT[:, :, :], self.hT[:], reads=[("hT", t) for t in range(16)], writes=["HT"])
                    self.phase_b(li, s)
                if "stopB" in self.debug:
                    continue
                with self.phase():
                    self.phase_nsa(li, s)
                with self.phase():
                    self.phase_sb(li, s)
                with self.phase():
                    self.phase_hg(li, s)
                if "stopC" in self.debug:
                    continue
                with self.phase():
                    carry = self.sb("carry", [128, 88, 2], F32)
                    c.op("pool", lambda e: e.memset(carry[:], 0.0), writes=[("carry", ch) for ch in range(88)])
                    for tb in range(4):
                        with self.phase():
                            self.phase_d(li, s, tb, xsrc, xsname, xdst, xdname, carry)
            self.cvt_some(1000)
        c.finish()
        return self.nc


def _t5_bucket(dist):
    import math
    n = np.maximum(dist, 0)
    big = 16 + (np.log(np.maximum(n, 1).astype(np.float32) / np.float32(16)) / np.float32(math.log(8.0)) * np.float32(16)).astype(np.int32)
    return np.where(n < 16, n, np.minimum(big, 31))


def _static_tables(rel_bias):
    i = np.arange(128)[:, None]
    j = np.arange(128)[None, :]
    blocks = []
    for delta in (0, 128, 512):
        dist = delta + j - i
        blocks.append((dist, (dist >= 0) & (dist < 512)))
    for delta in (0, 128):
        dist = delta + j - i
        blocks.append((dist, dist >= 0))
    t = np.arange(T)[None, :]
    dist = t - 16 * i - 31
    blocks.append((dist, (dist >= 0) & (i < 127)))
    dist_all = np.concatenate([b[0] for b in blocks], axis=1)
    valid = np.concatenate([b[1] for b in blocks], axis=1)
    bucket = _t5_bucket(dist_all)
    ebias = np.empty((8, 128, dist_all.shape[1]), np.float32)
    for h in range(8):
        ebias[h] = np.where(valid, rel_bias[bucket, h], np.float32(-1e30))
    cb = np.ascontiguousarray(np.broadcast_to(rel_bias[31][None, :], (128, 8))).astype(np.float32)
    tpos = (8 + np.arange(8))[None, :, None] * 128 + np.arange(128)[:, None, None]
    qblk = tpos // 64
    jb = np.arange(32)[None, None, :]
    forced = (jb <= qblk) & ((jb == 0) | (jb >= qblk - 1))
    addmask = np.where(forced, 1e30, np.where(jb <= qblk, 0.0, -1e30)).astype(np.float32)
    esel = (np.arange(32)[:, None, None] == 2 * np.arange(16)[None, :, None] + (np.arange(128) // 64)[None, None, :])
    c_start = np.arange(127) * 16
    s_start = np.arange(32) * 64
    overlap = ((c_start[:, None] < s_start[None, :] + 64) & (c_start[:, None] + 32 > s_start[None, :]))
    ov33 = np.zeros((128, 33), np.float32)
    ov33[:127, :32] = overlap
    ov33[:, 32] = 1.0
    selg = (np.arange(24)[:, None, None] == np.arange(24)[None, :, None]) & np.ones((1, 1, 128), bool)
    return dict(ebias=ebias, cb=cb, addmask=addmask, esel=esel.astype(ml_dtypes.bfloat16),
                ov33=ov33.astype(ml_dtypes.bfloat16), selg=selg.astype(np.float32))


def prep_shared(inputs, layers):
    ls = list(layers)
    f = lambda a: np.ascontiguousarray(np.asarray(a, dtype=np.float32))
    d = _static_tables(f(inputs["rel_bias"]))
    d["w_in"] = f(inputs["w_in"])[ls]
    d["norm_attn"] = f(np.asarray(inputs["norm_attn"])[ls].reshape(len(ls), 16, 128).transpose(2, 0, 1))
    hg = np.concatenate([np.asarray(inputs["nsa_q_gain"])[ls][:, None, :], np.asarray(inputs["nsa_k_gain"])[ls]], axis=1)
    d["hgain"] = f(hg.transpose(2, 0, 1))
    d["cmp_w1"] = f(inputs["cmp_w1"])[ls]
    d["cmp_w2"] = f(inputs["cmp_w2"])[ls]
    d["posT"] = f(np.asarray(inputs["cmp_pos"])[ls].transpose(3, 0, 1, 2))
    d["hlb"] = f(np.asarray(inputs["hg_lower_bound"]).reshape(4, 8, 128).transpose(2, 0, 1))
    lcum = np.zeros((128, len(ls), 4), np.float32)
    for i, l in enumerate(ls):
        lcum[:, i, 1:l + 1] = 1.0
    d["lcum"] = lcum
    d["gn"] = f(np.broadcast_to(np.asarray(inputs["hg_norm_gain"])[ls][None, :, :], (128, len(ls), 128)))
    d["w_branch"] = f(np.asarray(inputs["w_branch"])[ls].reshape(len(ls), 3072, D))
    d["w_out"] = f(inputs["w_out"])[ls]
    d["norm_ffn"] = f(np.asarray(inputs["norm_ffn"])[ls].reshape(len(ls), 16, 128).transpose(2, 0, 1))
    d["w_up"] = f(inputs["w_up"])[ls]
    d["convw"] = f(np.asarray(inputs["conv_w"])[ls].reshape(len(ls), 3, 88, 128).transpose(3, 0, 1, 2))
    d["convb"] = f(np.asarray(inputs["conv_b"])[ls].reshape(len(ls), 88, 128).transpose(2, 0, 1))
    d["w_down"] = f(inputs["w_down"])[ls]
    return d


_PROGS = {}


def _get_prog(n_layers):
    if n_layers not in _PROGS:
        p = Prog(list(range(n_layers)))
        _PROGS[n_layers] = p.build()
    return _PROGS[n_layers]


FUSED = True


def kernel(**inputs):
    x = np.ascontiguousarray(np.asarray(inputs["x"], dtype=np.float32))
    n_cores = 8
    if FUSED:
        groups = [list(range(DEPTH))]
    else:
        groups = [[l] for l in range(DEPTH)]
    cur = x
    for ls in groups:
        nc = _get_prog(len(ls))
        shared = prep_shared(inputs, ls)
        in_maps = [dict(shared, x=np.ascontiguousarray(cur[NSEQ * i:NSEQ * (i + 1)])) for i in range(n_cores)]
        res = run_bass_kernel_spmd(nc, in_maps, core_ids=list(range(n_cores)))
        cur = np.concatenate([np.asarray(r["out"], dtype=np.float32) for r in res.results], axis=0)
    return cur
```

```python
import numpy as np
from contextlib import ExitStack
import ml_dtypes
import concourse.bass as bass
import concourse.mybir as mybir
from concourse.bass_utils import run_bass_kernel_spmd

F32 = mybir.dt.float32
BF16 = mybir.dt.bfloat16
AF = mybir.ActivationFunctionType
ALU = mybir.AluOpType

D = 2048
T = 2048
NSEQ = 2
DEPTH = 4
HD = 128
IN_COLS = 15896
DFF = 5632
EPS = 1e-6
SCALE = HD ** -0.5
C_QN, C_KC, C_VC, C_KS, C_VS, C_KW, C_VW, C_G = 0, 1024, 1280, 1536, 1792, 2048, 2304, 2560
C_SQ, C_SK, C_SV = 2584, 3608, 4632
C_HQ, C_HF, C_HI, C_HG = 5656, 6680, 7704, 8728
C_MG = 9752


class Ctx:
    def __init__(self, nc):
        self.nc = nc
        self.E = dict(pe=nc.tensor, act=nc.scalar, dve=nc.vector, pool=nc.gpsimd, sp=nc.sync)
        self.sem = {n: nc.alloc_semaphore("s_" + n) for n in self.E}
        self.cnt = {n: 0 for n in self.E}
        self.seen = {n: {} for n in self.E}
        self.res = {}
        self.slots = {}
        self.nwait = 0
        self.nsep = 0
        self.hist = {n: ([0], [{}]) for n in self.E}

    def _slot(self, name):
        if name not in self.slots:
            self.slots[name] = [self.nc.alloc_semaphore("d_" + name), 0]
        return self.slots[name]

    def _need(self, reads, writes):
        need = {}

        def add(p, c):
            if need.get(p, 0) < c:
                need[p] = c
        for k in reads:
            r = self.res.get(k)
            if r and r[0]:
                add(*r[0])
        for k in writes:
            r = self.res.get(k)
            if r:
                if r[0]:
                    add(*r[0])
                for p, c in r[1].items():
                    add(p, c)
        return need

    def _wait(self, eng, need):
        for p, c in need.items():
            if p == eng and eng in ("pe",):
                continue
            if self.seen[eng].get(p, 0) >= c:
                continue
            if isinstance(p, tuple):
                self.E[eng].wait_ge(self.slots[p[1]][0], 16 * c)
            else:
                self.E[eng].wait_ge(self.sem[p], c)
            self.nwait += 1
            self.nsep += 1
            self.seen[eng][p] = c
            self._learn(eng, p, c)

    def _learn(self, eng, p, c):
        if isinstance(p, tuple):
            return
        import bisect
        cnts, snaps = self.hist[p]
        snap = snaps[bisect.bisect_right(cnts, c) - 1]
        se = self.seen[eng]
        for x, v in snap.items():
            if x != eng and se.get(x, 0) < v:
                se[x] = v

    def _snap(self, eng):
        cnts, snaps = self.hist[eng]
        cur = {k: v for k, v in self.seen[eng].items() if not isinstance(k, tuple)}
        if cur != snaps[-1]:
            cnts.append(self.cnt[eng] + 1)
            snaps.append(cur)

    def _mark(self, who, c, reads, writes):
        for k in reads:
            r = self.res.setdefault(k, [None, {}])
            r[1][who] = c
        for k in writes:
            self.res[k] = [(who, c), {}]

    def op(self, eng, fn, reads=(), writes=()):
        need = self._need(reads, writes)
        order = sorted(need.items(), key=lambda kv: (isinstance(kv[0], tuple), -kv[1]))
        pend = []
        for p, cnt_ in order:
            if p == eng and eng in ("pe",):
                continue
            if self.seen[eng].get(p, 0) >= cnt_:
                continue
            pend.append((p, cnt_))
            self.seen[eng][p] = cnt_
            self._learn(eng, p, cnt_)
        for p, cnt_ in pend:
            self.seen[eng].pop(p, None) if False else None
        attach = pend.pop() if pend else None
        for p, cnt_ in pend:
            if isinstance(p, tuple):
                self.E[eng].wait_ge(self.slots[p[1]][0], 16 * cnt_)
            else:
                self.E[eng].wait_ge(self.sem[p], cnt_)
            self.nwait += 1
            self.nsep += 1
        ins = fn(self.E[eng])
        if attach is not None:
            p, cnt_ = attach
            if isinstance(p, tuple):
                ins._wait_ge(self.slots[p[1]][0], 16 * cnt_)
            else:
                ins._wait_ge(self.sem[p], cnt_)
            self.nwait += 1
        self._snap(eng)
        self.cnt[eng] += 1
        ins.then_inc(self.sem[eng], 1)
        self._mark(eng, self.cnt[eng], reads, writes)
        return ins

    def dma(self, q, slot, out, in_, reads=(), writes=(), **kw):
        self._wait(q, self._need(reads, writes))
        s = self._slot(slot)
        ins = self.E[q].dma_start(out=out, in_=in_, **kw)
        s[1] += 1
        ins.then_inc(s[0], 16)
        self._mark(("dma", slot), s[1], reads, writes)
        return ins

    def barrier(self):
        for eng in self.E:
            need = {}
            for n in self.E:
                if n != eng and self.cnt[n]:
                    need[n] = self.cnt[n]
            for name, (sem, cc) in self.slots.items():
                if cc:
                    need[("dma", name)] = cc
            self._wait(eng, need)

    def finish(self):
        for name, (sem, c) in self.slots.items():
            if c:
                self.E["sp"].wait_ge(sem, 16 * c)
        for n in self.E:
            if n != "sp" and self.cnt[n]:
                self.E["sp"].wait_ge(self.sem[n], self.cnt[n])


class Prog:
    def __init__(self, layers, debug=()):
        self.layers = list(layers)
        self.debug = set(debug)
        nc = self.nc = bass.Bass("TRN2", target_bir_lowering=False)
        self.c = Ctx(nc)
        self._n = 0
        L = len(self.layers)
        self.L = L
        di = lambda name, shape, dt=F32: nc.dram_tensor(name, list(shape), dt, kind="ExternalInput").ap()
        self.x = di("x", [NSEQ, T, D])
        self.w_in = di("w_in", [L, D, IN_COLS])
        self.norm_attn = di("norm_attn", [128, L, 16])
        self.hgain = di("hgain", [128, L, 4])
        self.ebias = di("ebias", [8, 128, 2688])
        self.cb = di("cb", [128, 8])
        self.addmask = di("addmask", [128, 8, 32])
        self.esel = di("esel", [32, 16, 128], BF16)
        self.ov33 = di("ov33", [128, 33], BF16)
        self.selg = di("selg", [24, 24, 128])
        self.cmp_w1 = di("cmp_w1", [L, 2, 32, 128, 128])
        self.cmp_w2 = di("cmp_w2", [L, 2, 128, 128])
        self.posT = di("posT", [128, L, 2, 32])
        self.hlb = di("hlb", [128, 4, 8])
        self.lcum = di("lcum", [128, L, 4])
        self.gn = di("gn", [128, L, 128])
        self.w_branch = di("w_branch", [L, 3072, D])
        self.w_out = di("w_out", [L, D, D])
        self.norm_ffn = di("norm_ffn", [128, L, 16])
        self.w_up = di("w_up", [L, D, 2 * DFF])
        self.convw = di("convw", [128, L, 3, 88])
        self.convb = di("convb", [128, L, 88])
        self.w_down = di("w_down", [L, DFF, D])
        self.wbr_b = self.scratch("wbr_b", [L, 3072, D], BF16)
        self.wout_b = self.scratch("wout_b", [L, D, D], BF16)
        self.wup_b = self.scratch("wup_b", [L, D, 2 * DFF], BF16)
        self.wdn_b = self.scratch("wdn_b", [L, DFF, D], BF16)
        self.XS = self.scratch("XS", [2, NSEQ, T, D], F32)
        self.EBD = self.scratch("EBD", [8, 128, 2688], F32)
        self.EBA = self.scratch("EBA", [8, 128, 640], BF16)
        self.OT = self.scratch("OT", [24, 128, T], BF16)
        self.out = nc.dram_tensor("out", [NSEQ, T, D], F32, kind="ExternalOutput").ap()
        self.win_b = self.scratch("win_b", [L, D, IN_COLS], BF16)
        self.HT = self.scratch("HT", [128, 16, T], BF16)
        self.FM = self.scratch("FM", [40, 128, T], BF16)
        self.HF = self.scratch("HF", [8, 128, T], F32)
        self.GS = self.scratch("GS", [24, T], F32)
        self.TM = self.scratch("TM", [T, 3584], BF16)

    def scratch(self, name, shape, dt):
        kind = "ExternalOutput" if name in self.debug else "Internal"
        return self.nc.dram_tensor(name, list(shape), dt, kind=kind).ap()

    def sb(self, name, shape, dt):
        self._n += 1
        if getattr(self, "_stacks", None):
            return self._stacks[-1].enter_context(self.nc.sbuf_tensor(f"{name}_{self._n}", list(shape), dt))
        return self.nc.alloc_sbuf_tensor(f"{name}_{self._n}", list(shape), dt)

    def phase(self):
        prog = self

        class _P:
            def __enter__(self_):
                prog._stacks.append(ExitStack())
                return self_

            def __exit__(self_, *a):
                prog.c.barrier()
                prog._stacks.pop().close()
                return False
        return _P()

    def ps(self, name, shape, dt=F32):
        self._n += 1
        return self.nc.alloc_psum_tensor(f"{name}_{self._n}", list(shape), dt)

    def convert_pieces(self, li):
        c = self.c
        pieces = []

        def mk(dst, src, key):
            return lambda: c.dma("pool", "cvt_" + key[0], dst, src, writes=[key])
        for i in range(D // 128):
            src = self.w_in[li, i * 128:(i + 1) * 128, :].rearrange("p (a b) -> p a b", b=1987)
            dst = self.win_b[li, i * 128:(i + 1) * 128, :].rearrange("p (a b) -> p a b", b=1987)
            pieces.append(mk(dst, src, ("win_b", li)))
        for srcw, dstw, rows, key in ((self.w_branch, self.wbr_b, 3072, "wbr_b"), (self.w_out, self.wout_b, D, "wout_b"),
                                      (self.w_up, self.wup_b, D, "wup_b"), (self.w_down, self.wdn_b, DFF, "wdn_b")):
            for i in range(rows // 128):
                src = srcw[li, i * 128:(i + 1) * 128, :].rearrange("p (a b) -> p a b", b=1024)
                dst = dstw[li, i * 128:(i + 1) * 128, :].rearrange("p (a b) -> p a b", b=1024)
                pieces.append(mk(dst, src, (key, li)))
        return pieces

    def cvt_some(self, n=1):
        for _ in range(n):
            if self.cvt_queue:
                self.cvt_queue.pop(0)()

    def setup_consts(self):
        c = self.c
        self.ident = self.sb("ident", [128, 128], BF16)
        self.ones_b = self.sb("ones", [128, 128], BF16)
        self.gattn = self.sb("gattn", [128, self.L, 16], F32)
        idf = self.sb("idf", [128, 128], F32)
        c.op("pool", lambda e: e.memset(idf[:], 1.0), writes=["idf"])
        c.op("pool", lambda e: e.affine_select(out=idf[:], in_=idf[:], pattern=[[-1, 128]], compare_op=ALU.is_equal,
                                               fill=0.0, base=0, channel_multiplier=1), reads=["idf"], writes=["idf"])
        c.op("pool", lambda e: e.tensor_copy(out=self.ident[:], in_=idf[:]), reads=["idf"], writes=["ident"])
        c.op("pool", lambda e: e.memset(self.ones_b[:], 1.0), writes=["ones"])
        c.dma("sp", "cst", self.gattn[:], self.norm_attn[:, :, :], writes=["gattn"])
        self.hgain_t = self.sb("hgain", [128, self.L, 4], F32)
        c.dma("sp", "cst2", self.hgain_t[:], self.hgain[:, :, :], writes=["hgain"])

    def norm_tile(self, xt, kx, gains, dstT, col0, kdst):
        c = self.c
        self.nnorm = getattr(self, "nnorm", 0) + 1
        b0, b1, b2 = self.nnorm % len(self.ss), self.nnorm % len(self.sqj), self.nnorm % len(self.xn)
        ss, sq, xn = self.ss[b0], self.sqj[b1], self.xn[b2]
        kss, ksq, kxn = ("ss", b0), ("sqj", b1), ("xn", b2)
        c.op("act", lambda e: e.activation(out=sq[:], in_=xt, func=AF.Square, accum_out=ss[:, 0:1]), reads=[kx], writes=[ksq, kss])
        c.op("act", lambda e: e.activation(out=ss[:, 1:2], in_=ss[:, 0:1], func=AF.Sqrt, scale=1.0 / D, bias=self.eps_t[:, 0:1]),
             reads=[kss, "eps"], writes=[kss])
        c.op("dve", lambda e: e.reciprocal(out=ss[:, 2:3], in_=ss[:, 1:2]), reads=[kss], writes=[kss])
        c.op("dve", lambda e: e.tensor_scalar(out=xn[:], in0=xt, scalar1=ss[:, 2:3], scalar2=None, op0=ALU.mult), reads=[kx, kss], writes=[kxn])
        for half in range(2):
            pt = self.ptr[half]
            kp = ("ptr", half)
            for j in range(8):
                kc = half * 8 + j
                c.op("pe", lambda e: e.transpose(out=pt[:, j, :], in_=xn[:, kc * 128:(kc + 1) * 128], identity=self.ident[:]),
                     reads=[kxn, "ident"], writes=[kp])
            for j in range(8):
                kc = half * 8 + j
                dst = dstT[:, kc, col0:col0 + 128]
                g = gains[:, kc:kc + 1]
                if j % 2 == 0:
                    c.op("act", lambda e: e.activation(out=dst, in_=pt[:, j, :], func=AF.Copy, scale=g), reads=[kp, "gattn", "gffn"], writes=[kdst])
                else:
                    c.op("dve", lambda e: e.tensor_scalar(out=dst, in0=pt[:, j, :], scalar1=g, scalar2=None, op0=ALU.mult),
                         reads=[kp, "gattn", "gffn"], writes=[kdst])

    def phase_a(self, li, s, xsrc, xname):
        c = self.c
        for tt in range(16):
            xt = self.xt[tt % 3]
            kx = ("xt", tt % 3)
            c.dma("sp", f"xt{tt % 3}", xt[:], xsrc[s, tt * 128:(tt + 1) * 128, :], reads=[("X", xname, s, tt // 4)], writes=[kx])
            self.norm_tile(xt[:], kx, self.gattn[:, li, :], self.hT, tt * 128, ("hT", tt))

    def phase_b(self, li, s):
        c = self.c
        hT = self.hT
        hT_keys = [("hT", t) for t in range(16)]
        wsrc = self.win_b[li].rearrange("(kc p) n -> p kc n", p=128)
        nload = [0]

        def load_w(col, n):
            i = nload[0] % 2
            nload[0] += 1
            wt = self.wt[i]
            c.dma("sp", f"wt{i}", wt[:, :, 0:n], wsrc[:, :, col:col + n], reads=[("win_b", li)], writes=[("wt", i)])
            return wt, ("wt", i)

        nacc = [0]

        def next_acc():
            i = nacc[0] % 2
            nacc[0] += 1
            return self.acc[i], ("acc", i)

        nst = [0]
        fm_jobs = [
            (C_QN, 8, "hnorm", 0, 0), (C_KC, 2, "plain", 8, None), (C_VC, 2, "plain", 10, None),
            (C_KS, 2, "hnorm", 12, 2), (C_KW, 2, "hnorm", 14, 3),
            (C_SQ, 8, "plainS", 16, None), (C_SK, 8, "plain", 24, None), (C_HQ, 8, "plain", 32, None),
            (C_HF, 8, "plain32", 0, None),
        ]
        for col0, nch, kind, fm0, gi in fm_jobs:
            for c4 in range(0, nch, 4):
                ncc = min(4, nch - c4)
                wt, kw = load_w(col0 + c4 * 128, ncc * 128)
                for j in range(ncc):
                    ch = c4 + j
                    if kind == "plain32":
                        st, kst = self.fst32, "fst32"
                    else:
                        st, kst = self.fst[nst[0] % 2], ("fst", nst[0] % 2)
                        nst[0] += 1
                    for tb in range(4):
                        acc, ka = next_acc()
                        for kc in range(16):
                            c.op("pe", lambda e: e.matmul(acc[:], lhsT=wt[:, kc, j * 128:(j + 1) * 128], rhs=hT[:, kc, tb * 512:(tb + 1) * 512],
                                                          start=(kc == 0), stop=(kc == 15)),
                                 reads=[kw] + hT_keys, writes=[ka])
                        dst = st[:, tb * 512:(tb + 1) * 512]
                        if kind == "plainS":
                            c.op("act", lambda e: e.activation(out=dst, in_=acc[:], func=AF.Copy, scale=SCALE), reads=[ka], writes=[kst])
                        elif kind == "plain":
                            eng = "act" if tb % 2 == 0 else "dve"
                            if eng == "act":
                                c.op("act", lambda e: e.copy(out=dst, in_=acc[:]), reads=[ka], writes=[kst])
                            else:
                                c.op("dve", lambda e: e.tensor_copy(out=dst, in_=acc[:]), reads=[ka], writes=[kst])
                        elif kind == "plain32":
                            c.op("act", lambda e: e.copy(out=dst, in_=acc[:]), reads=[ka], writes=[kst])
                        else:
                            self.hnorm_block(acc, ka, dst, kst, self.hgain_t[:, li, gi:gi + 1])
                    if kind == "plain32":
                        c.dma("act", "fst32", self.HF[ch, :, :], st[:], reads=[kst], writes=[("HF", ch)])
                    else:
                        c.dma("act", f"fst{kst[1]}", self.FM[fm0 + ch, :, :], st[:], reads=[kst], writes=[("FM", fm0 + ch)])
        wt, kw = load_w(C_G, 24)
        for tb in range(4):
            acc, ka = next_acc()
            for kc in range(16):
                c.op("pe", lambda e: e.matmul(acc[0:24, :], lhsT=wt[:, kc, 0:24], rhs=hT[:, kc, tb * 512:(tb + 1) * 512],
                                              start=(kc == 0), stop=(kc == 15)), reads=[kw] + hT_keys, writes=[ka])
            c.op("act", lambda e: e.activation(out=self.gst[0:24, tb * 512:(tb + 1) * 512], in_=acc[0:24, :], func=AF.Sigmoid),
                 reads=[ka], writes=["gst"])
        c.dma("act", "gst", self.GS[:, :], self.gst[0:24, :], reads=["gst"], writes=["GS"])
        tm_jobs = [(C_VS, 256, 0, False), (C_VW, 256, 256, False), (C_SV, 1024, 512, False),
                   (C_HI, 1024, 1536, False), (C_HG, 1024, 2560, True)]
        nts = [0]
        for col0, ncols, tm0, sig in tm_jobs:
            for c0 in range(0, ncols, 512):
                n = min(512, ncols - c0)
                wt, kw = load_w(col0 + c0, n)
                for tt in range(16):
                    acc, ka = next_acc()
                    for kc in range(16):
                        c.op("pe", lambda e: e.matmul(acc[:, 0:n], lhsT=hT[:, kc, tt * 128:(tt + 1) * 128], rhs=wt[:, kc, 0:n],
                                                      start=(kc == 0), stop=(kc == 15)), reads=[kw, ("hT", tt)], writes=[ka])
                    i = nts[0] % 2
                    nts[0] += 1
                    st, kst = self.tst[i], ("tst", i)
                    if sig:
                        c.op("act", lambda e: e.activation(out=st[:, 0:n], in_=acc[:, 0:n], func=AF.Sigmoid), reads=[ka], writes=[kst])
                    elif tt % 2 == 0:
                        c.op("act", lambda e: e.copy(out=st[:, 0:n], in_=acc[:, 0:n]), reads=[ka], writes=[kst])
                    else:
                        c.op("dve", lambda e: e.tensor_copy(out=st[:, 0:n], in_=acc[:, 0:n]), reads=[ka], writes=[kst])
                    c.dma("act", f"tst{i}", self.TM[tt * 128:(tt + 1) * 128, tm0 + c0:tm0 + c0 + n], st[:, 0:n],
                          reads=[kst], writes=[("TM", tm0 + c0, tt)])

    def hnorm_block(self, acc, ka, dst, kdst, gain):
        c = self.c
        c.op("act", lambda e: e.activation(out=self.sqb[:], in_=acc[:], func=AF.Square), reads=[ka], writes=["sqb"])
        c.op("pe", lambda e: e.matmul(self.pn[:], lhsT=self.ones_b[:], rhs=self.sqb[:], start=True, stop=True),
             reads=["sqb", "ones"], writes=["pn"])
        c.op("act", lambda e: e.activation(out=self.rt[:], in_=self.pn[:], func=AF.Sqrt, scale=1.0 / HD, bias=self.eps_t[:, 0:1]),
             reads=["pn", "eps"], writes=["rt"])
        c.op("dve", lambda e: e.reciprocal(out=self.rt[:], in_=self.rt[:]), reads=["rt"], writes=["rt"])
        c.op("dve", lambda e: e.scalar_tensor_tensor(out=dst, in0=acc[:], scalar=gain, in1=self.rt[:], op0=ALU.mult, op1=ALU.mult),
             reads=[ka, "rt", "hgain"], writes=[kdst])

    def setup_tables(self):
        c = self.c
        self.cb_t = self.sb("cb", [128, 8], F32)
        self.negc = self.sb("negc", [128, 8], F32)
        c.dma("sp", "cst3", self.cb_t[:], self.cb[:, :], writes=["cb"])
        c.op("dve", lambda e: e.tensor_scalar(out=self.negc[:], in0=self.cb_t[:], scalar1=-1.0, scalar2=None, op0=ALU.mult),
             reads=["cb"], writes=["negc"])
        self.addmask_t = self.sb("addmask", [128, 8, 32], F32)
        c.dma("sp", "cst4", self.addmask_t[:], self.addmask[:, :, :], writes=["addmask"])
        self.esel_t = self.sb("esel", [32, 16, 128], BF16)
        c.dma("sp", "cst5", self.esel_t[:], self.esel[:, :, :], writes=["esel"])
        self.ov33_t = self.sb("ov33", [128, 33], BF16)
        c.dma("sp", "cst6", self.ov33_t[:], self.ov33[:, :], writes=["ov33"])
        self.posT_t = self.sb("posT", [128, self.L, 2, 32], F32)
        c.dma("sp", "cst8", self.posT_t[:], self.posT[:, :, :, :], writes=["posT"])
        self.posb = self.sb("posb", [128, self.L, 2, 32], BF16)
        c.op("dve", lambda e: e.tensor_copy(out=self.posb[:], in_=self.posT_t[:]), reads=["posT"], writes=["posb"])
        self.idf = self.sb("idf2", [128, 128], F32)
        c.op("pool", lambda e: e.memset(self.idf[:], 1.0), writes=["idf2"])
        c.op("pool", lambda e: e.affine_select(out=self.idf[:], in_=self.idf[:], pattern=[[-1, 128]], compare_op=ALU.is_equal,
                                               fill=0.0, base=0, channel_multiplier=1), reads=["idf2"], writes=["idf2"])
        with self.phase():
            eb = self.sb("ebl", [128, 2688], F32)
            eba_s = self.sb("eba_s", [128, 640], BF16)
            for h in range(8):
                c.dma("sp", "ebl", eb[:], self.ebias[h, :, :], writes=["ebl"])
                c.op("dve", lambda e: e.tensor_scalar(out=eba_s[:], in0=eb[:, 0:640], scalar1=self.negc[:, h:h + 1], scalar2=1.0 / SCALE,
                                                      op0=ALU.add, op1=ALU.mult), reads=["ebl", "negc"], writes=["eba_s"])
                c.dma("sp", "ebas", self.EBA[h, :, :], eba_s[:], reads=["eba_s"], writes=[("EBA", h)])
                c.op("act", lambda e: e.activation(out=eb[:], in_=eb[:], func=AF.Exp, bias=self.negc[:, h:h + 1]),
                     reads=["ebl", "negc"], writes=["ebl"])
                c.dma("sp", "ebs", self.EBD[h, :, :], eb[:], reads=["ebl"], writes=[("EBD", h)])

    def gelu_tanh(self, dst, kdst, src_ps, ksrc, bias, n):
        c = self.c
        x, x2 = self.gx, self.gx2
        c.op("act", lambda e: e.activation(out=x[:, 0:n], in_=src_ps, func=AF.Identity, bias=bias), reads=[ksrc, "cbias"], writes=["gx"])
        c.op("dve", lambda e: e.tensor_tensor(out=x2[:, 0:n], in0=x[:, 0:n], in1=x[:, 0:n], op=ALU.mult), reads=["gx"], writes=["gx2"])
        c.op("dve", lambda e: e.tensor_scalar(out=x2[:, 0:n], in0=x2[:, 0:n], scalar1=0.044715, scalar2=1.0, op0=ALU.mult, op1=ALU.add),
             reads=["gx2"], writes=["gx2"])
        c.op("dve", lambda e: e.tensor_tensor(out=x2[:, 0:n], in0=x2[:, 0:n], in1=x[:, 0:n], op=ALU.mult), reads=["gx", "gx2"], writes=["gx2"])
        c.op("act", lambda e: e.activation(out=x2[:, 0:n], in_=x2[:, 0:n], func=AF.Sigmoid, scale=1.5957691216), reads=["gx2"], writes=["gx2"])
        c.op("dve", lambda e: e.tensor_tensor(out=dst, in0=x2[:, 0:n], in1=x[:, 0:n], op=ALU.mult), reads=["gx", "gx2"], writes=[kdst])

    def compress(self, li, j, xT, kx, w1b, w2b, hid):
        c = self.c
        b5 = self.bk[5]
        k5 = ("bk", 5)
        c.dma("pool", "w1b", w1b[:], self.cmp_w1[li, j].rearrange("l d e -> d l e"), writes=["w1b"])
        c.dma("pool", "w2b", w2b[:], self.cmp_w2[li, j], writes=["w2b"])
        for l in range(32):
            c.op("pe", lambda e: e.matmul(b5[:, 0:127], lhsT=w1b[:, l, :], rhs=xT[:, l:l + 16 * 126 + 1:16], start=(l == 0), stop=(l == 31)),
                 reads=["w1b", kx], writes=[k5])
        for l in range(32):
            c.op("pe", lambda e: e.matmul(b5[:, 128:129], lhsT=w1b[:, l, :], rhs=self.posb[:, li, j, l:l + 1], start=False, stop=(l == 31)),
                 reads=["w1b", "posb"], writes=[k5])
        c.op("dve", lambda e: e.tensor_copy(out=self.cbias[:], in_=b5[:, 128:129]), reads=[k5], writes=["cbias"])
        c.op("pool", lambda e: e.memset(hid[:], 0.0), writes=["hid"])
        self.gelu_tanh(hid[:, 0:127], "hid", b5[:, 0:127], k5, self.cbias[:, 0:1], 127)

    def gate_bcast(self, krow, cols):
        c = self.c
        n = cols[1] - cols[0]
        c.op("pe", lambda e: e.matmul(self.bk[4][:, 0:n], lhsT=self.selg_t[:, krow, :], rhs=self.gsb[0:24, cols[0]:cols[1]], start=True, stop=True),
             reads=["selg", "gsb"], writes=[("bk", 4)])

    def attn_finish(self, h, br, G, oacc, first, guard=False, uz=(2, 3)):
        c = self.c
        U, kU = self.bk[uz[0]], ("bk", uz[0])
        Z, kZ = self.bk[uz[1]], ("bk", uz[1])
        i = self.nfin % 2
        self.nfin += 1
        w, kw_ = self.wvs[i], ("wv", i)
        tmp, kt_ = self.tmps[i], ("tmpv", i)
        gb = self.gbs[br][:, G * 512:(G + 1) * 512]
        if guard:
            c.op("dve", lambda e: e.tensor_scalar(out=w[:], in0=Z[:], scalar1=1e-30, scalar2=None, op0=ALU.max), reads=[kZ], writes=[kw_])
            c.op("dve", lambda e: e.reciprocal(out=w[:], in_=w[:]), reads=[kw_], writes=[kw_])
        else:
            c.op("dve", lambda e: e.reciprocal(out=w[:], in_=Z[:]), reads=[kZ], writes=[kw_])
        c.op("pool", lambda e: e.tensor_tensor(out=w[:], in0=gb, in1=w[:], op=ALU.mult), reads=[kw_, ("gbs", br)], writes=[kw_])
        dst = oacc[:, G * 512:(G + 1) * 512]
        ko = ("oacc", id(oacc), G)
        if first:
            c.op("dve", lambda e: e.tensor_tensor(out=dst, in0=U[:], in1=w[:], op=ALU.mult), reads=[kw_, kU], writes=[ko])
        else:
            c.op("dve", lambda e: e.tensor_tensor(out=tmp[:], in0=U[:], in1=w[:], op=ALU.mult), reads=[kw_, kU], writes=[kt_])
            c.op("pool", lambda e: e.tensor_tensor(out=dst, in0=dst, in1=tmp[:], op=ALU.add), reads=[kt_, ko], writes=[ko])

    def gate_rows(self, h, brs):
        c = self.c
        for br in brs:
            row = h * 3 + br
            c.dma("sp", f"gbs{br}", self.gbs[br][:], self.GS[row:row + 1, :].partition_broadcast(128), reads=["GS"], writes=[("gbs", br)])

    def banded_attn(self, h, G, qT, kq, kT, kk, V, kv, eb, keb, mode, nmT=None, uz=(2, 3)):
        c = self.c
        if mode == "win":
            kts = [kt for kt in range(max(0, 4 * G - 4), 4 * G + 4)]
        else:
            kts = list(range(0, 4 * G + 4))
        use_mask = (mode == "sel" and G >= 2)
        U, kU = self.bk[uz[0]], ("bk", uz[0])
        Z, kZ = self.bk[uz[1]], ("bk", uz[1])
        info = []
        for kt in kts:
            qlo = max(kt, 4 * G)
            qhi = min(kt + 4, 4 * G + 3) if mode == "win" else 4 * G + 3
            c0, c1 = (qlo - 4 * G) * 128, (qhi - 4 * G + 1) * 128
            i = self.nS % 2
            self.nS += 1
            info.append((kt, qlo, qhi, c0, c1, i))

        def stage_a(k):
            kt, qlo, qhi, c0, c1, i = info[k]
            n = c1 - c0
            S, kS = self.bk[i], ("bk", i)
            P, kP = self.P[i], ("P", i)
            subs = []
            for qb in range(qlo, qhi + 1):
                d = qb - kt
                if mode == "win":
                    ti = {0: 0, 1: 1, 4: 2}.get(d)
                    off = 0
                else:
                    ti = {0: 0, 1: 1}.get(d)
                    off = 384
                if ti is None:
                    continue
                subs.append(((qb - qlo) * 128, off + ti * 128))
            c.op("pe", lambda e: e.matmul(S[:, 0:n], lhsT=kT[:, kt * 128:(kt + 1) * 128], rhs=qT[:, G * 512 + c0:G * 512 + c1],
                                          start=True, stop=False), reads=[kk, kq], writes=[kS])
            if use_mask:
                c.op("pe", lambda e: e.matmul(S[:, 0:n], lhsT=self.esel_t[:, kt, :], rhs=nmT[:, G * 512 + c0 - 1024:G * 512 + c1 - 1024],
                                              start=False, stop=False), reads=["esel", "nmT"], writes=[kS])
            for (pc, tc) in subs:
                c.op("pe", lambda e: e.matmul(S[:, pc:pc + 128], lhsT=self.ident[:], rhs=self.eba[:, tc:tc + 128], start=False, stop=False),
                     reads=["ident", "eba"], writes=[kS])
            c.op("pe", lambda e: e.matmul(S[0:32, 0:2], lhsT=self.esel_t[:, 0, 0:32], rhs=self.zero32[:, 0:2], start=False, stop=True),
                 reads=["esel", "zero32"], writes=[kS])
            c.op("act", lambda e: e.activation(out=P[:, 0:n], in_=S[:, 0:n], func=AF.Exp, scale=SCALE, bias=self.cb_t[:, h:h + 1]),
                 reads=[kS, "cb"], writes=[kP])

        def stage_b(k):
            kt, qlo, qhi, c0, c1, i = info[k]
            n = c1 - c0
            P, kP = self.P[i], ("P", i)
            c.op("pe", lambda e: e.matmul(U[:, c0:c1], lhsT=V[:, kt, :], rhs=P[:, 0:n], start=(k == 0), stop=(k == len(info) - 1)),
                 reads=[kv, kP], writes=[kU])
            c.op("pe", lambda e: e.matmul(Z[:, c0:c1], lhsT=self.ones_b[:], rhs=P[:, 0:n], start=(k == 0), stop=(k == len(info) - 1)),
                 reads=["ones", kP], writes=[kZ])
        stage_a(0)
        for k in range(len(info)):
            if k + 1 < len(info):
                stage_a(k + 1)
            stage_b(k)

    def phase_nsa(self, li, s):
        c = self.c
        sb = self.sb
        self.gsb = sb("gsb", [32, T], F32)
        self.wvs = [sb("wv", [128, 512], F32) for _ in range(2)]
        self.tmps = [sb("tmpv", [128, 512], F32) for _ in range(2)]
        self.tmpv = self.tmps[0]
        self.gbs = [sb("gbs", [128, T], F32) for _ in range(3)]
        self.nfin = 0
        self.P = [sb("P", [128, 512], BF16) for _ in range(2)]
        self.gx = sb("gx", [128, 128], F32)
        self.gx2 = sb("gx2", [128, 128], F32)
        self.cbias = sb("cbias", [128, 1], F32)
        self.nS = 0
        self.cexp = sb("cexp", [128, 512], F32)
        w1b = sb("w1b", [128, 32, 128], BF16)
        w2b = sb("w2b", [128, 128], BF16)
        xk = sb("xk", [128, T], BF16)
        xv = sb("xv", [128, T], BF16)
        hid = sb("hid", [128, 128], BF16)
        kcmpT = sb("kcmpT", [128, 128], BF16)
        vcmp = sb("vcmp", [128, 1, 128], BF16)
        ksT = sb("ksT", [128, T], BF16)
        kwT = sb("kwT", [128, T], BF16)
        vs = sb("vs", [128, 16, 128], BF16)
        vw = sb("vw", [128, 16, 128], BF16)
        qT = [sb("qT", [128, T], BF16) for _ in range(4)]
        oacc = [sb("oacc", [128, T], F32) for _ in range(4)]
        obf = sb("obf", [128, T], BF16)
        eb = sb("eb", [128, 2688], F32)
        eba4 = sb("eba", [128, 4, 640], BF16)
        self.zero32 = sb("zero32", [32, 2], BF16)
        c.op("pool", lambda e: e.memset(self.zero32[:], 0.0), writes=["zero32"])
        impacc = sb("impacc", [128, 8, 32], F32)
        imps = sb("imps", [128, 40], F32)
        sc = sb("sc", [128, 32], F32)
        sc2 = sb("sc2", [128, 32], F32)
        m8 = sb("m8", [128, 16], F32)
        nmT = sb("nmT", [32, T // 2], BF16)
        b5, k5 = self.bk[5], ("bk", 5)
        c.dma("sp", "gsb", self.gsb[0:24, :], self.GS[:, :], reads=["GS"], writes=["gsb"])
        for g in range(2):
            c.dma("sp", "xk", xk[:], self.FM[8 + g, :, :], reads=[("FM", 8 + g)], writes=["xk"])
            c.dma("sp", "xv", xv[:], self.FM[10 + g, :, :], reads=[("FM", 10 + g)], writes=["xv"])
            c.dma("sp", "ksT", ksT[:], self.FM[12 + g, :, :], reads=[("FM", 12 + g)], writes=["ksT"])
            c.dma("sp", "kwT", kwT[:], self.FM[14 + g, :, :], reads=[("FM", 14 + g)], writes=["kwT"])
            tmv = self.TM.rearrange("(kt p) n -> p kt n", p=128)
            c.dma("sp", "vs", vs[:], tmv[:, :, g * 128:(g + 1) * 128], reads=[("TM", 0, t) for t in range(16)], writes=["vs"])
            c.dma("sp", "vw", vw[:], tmv[:, :, 256 + g * 128:256 + (g + 1) * 128], reads=[("TM", 256, t) for t in range(16)], writes=["vw"])
            self.compress(li, 0, xk, "xk", w1b, w2b, hid)
            c.op("pe", lambda e: e.matmul(b5[:, 256:384], lhsT=w2b[:], rhs=hid[:], start=True, stop=True), reads=["w2b", "hid"], writes=[k5])
            c.op("act", lambda e: e.activation(out=self.sqb[:, 0:128], in_=b5[:, 256:384], func=AF.Square), reads=[k5], writes=["sqb"])
            c.op("pe", lambda e: e.matmul(self.bk[4][:, 0:128], lhsT=self.ones_b[:], rhs=self.sqb[:, 0:128], start=True, stop=True),
                 reads=["sqb", "ones"], writes=[("bk", 4)])
            c.op("act", lambda e: e.activation(out=self.rt[:, 0:128], in_=self.bk[4][:, 0:128], func=AF.Sqrt, scale=1.0 / HD, bias=self.eps_t[:, 0:1]),
                 reads=[("bk", 4), "eps"], writes=["rt"])
            c.op("dve", lambda e: e.reciprocal(out=self.rt[:, 0:128], in_=self.rt[:, 0:128]), reads=["rt"], writes=["rt"])
            c.op("dve", lambda e: e.scalar_tensor_tensor(out=kcmpT[:], in0=b5[:, 256:384], scalar=self.hgain_t[:, li, 1:2], in1=self.rt[:, 0:128],
                                                         op0=ALU.mult, op1=ALU.mult), reads=[k5, "rt", "hgain"], writes=["kcmpT"])
            self.compress(li, 1, xv, "xv", w1b, w2b, hid)
            c.op("pe", lambda e: e.matmul(b5[:, 256:384], lhsT=hid[:], rhs=w2b[:], start=True, stop=True), reads=["w2b", "hid"], writes=[k5])
            c.op("dve", lambda e: e.tensor_copy(out=vcmp[:, 0, :], in_=b5[:, 256:384]), reads=[k5], writes=["vcmp"])
            c.op("pool", lambda e: e.memset(impacc[:], 0.0), writes=["impacc"])
            for r in range(4):
                h = 4 * g + r
                c.dma("sp", f"qT{r}", qT[r][:], self.FM[h, :, :], reads=[("FM", h)], writes=[("qT", r)])
                c.dma("sp", "eb", eb[:], self.EBD[h, :, :], reads=[("EBD", h)], writes=["eb"])
                self.gate_rows(h, [0])
                for G in range(4):
                    i = self.nS % 2
                    self.nS += 1
                    S, kS = self.bk[i], ("bk", i)
                    P, kP = self.P[i], ("P", i)
                    c.op("pe", lambda e: e.matmul(S[:], lhsT=kcmpT[:], rhs=qT[r][:, G * 512:(G + 1) * 512], start=True, stop=True),
                         reads=["kcmpT", ("qT", r)], writes=[kS])
                    c.op("act", lambda e: e.activation(out=self.cexp[:], in_=S[:], func=AF.Exp, scale=SCALE, bias=self.cb_t[:, h:h + 1]),
                         reads=[kS, "cb"], writes=["cexp"])
                    c.op("dve", lambda e: e.tensor_tensor(out=P[:], in0=self.cexp[:], in1=eb[:, 640 + G * 512:640 + (G + 1) * 512], op=ALU.mult),
                         reads=["cexp", "eb"], writes=[kP])
                    c.op("pe", lambda e: e.matmul(self.bk[2][:], lhsT=vcmp[:, 0, :], rhs=P[:], start=True, stop=True), reads=["vcmp", kP], writes=[("bk", 2)])
                    c.op("pe", lambda e: e.matmul(self.bk[3][:], lhsT=self.ones_b[:], rhs=P[:], start=True, stop=True), reads=["ones", kP], writes=[("bk", 3)])
                    if G >= 2:
                        for q4 in range(4):
                            tt = G * 4 + q4 - 8
                            c.op("pe", lambda e: e.matmul(b5[:, q4 * 64:q4 * 64 + 33], lhsT=P[:, q4 * 128:(q4 + 1) * 128], rhs=self.ov33_t[:],
                                                          start=True, stop=True), reads=["ov33", kP], writes=[k5])
                            c.op("dve", lambda e: e.reciprocal(out=imps[:, 32:33], in_=b5[:, q4 * 64 + 32:q4 * 64 + 33]), reads=[k5], writes=["imps"])
                            c.op("dve", lambda e: e.tensor_scalar(out=imps[:, 0:32], in0=b5[:, q4 * 64:q4 * 64 + 32], scalar1=imps[:, 32:33], scalar2=None,
                                                                  op0=ALU.mult), reads=[k5, "imps"], writes=["imps2"])
                            c.op("dve", lambda e: e.tensor_tensor(out=impacc[:, tt, :], in0=impacc[:, tt, :], in1=imps[:, 0:32], op=ALU.add),
                                 reads=["imps2", "impacc"], writes=["impacc"])
                    self.attn_finish(h, 0, G, oacc[r], True, guard=True)
            for tt in range(8):
                c.op("dve", lambda e: e.tensor_tensor(out=sc[:], in0=impacc[:, tt, :], in1=self.addmask_t[:, tt, :], op=ALU.add),
                     reads=["impacc", "addmask"], writes=["sc"])
                c.op("dve", lambda e: e.max(out=m8[:, 0:8], in_=sc[:]), reads=["sc"], writes=["m8"])
                c.op("dve", lambda e: e.match_replace(out=sc2[:], in_to_replace=m8[:, 0:8], in_values=sc[:], imm_value=-3e38),
                     reads=["sc", "m8"], writes=["sc2"])
                c.op("dve", lambda e: e.max(out=m8[:, 8:16], in_=sc2[:]), reads=["sc2"], writes=["m8b"])
                c.op("dve", lambda e: e.tensor_scalar(out=sc2[:], in0=sc[:], scalar1=m8[:, 15:16], scalar2=None, op0=ALU.is_ge),
                     reads=["sc", "m8b"], writes=["sc2"])
                c.op("dve", lambda e: e.tensor_scalar(out=sc2[:], in0=sc2[:], scalar1=30000.0, scalar2=-30000.0, op0=ALU.mult, op1=ALU.add),
                     reads=["sc2"], writes=["sc2"])
                c.op("pe", lambda e: e.transpose(out=b5[0:32, 384:512], in_=sc2[:], identity=self.idf[:]), reads=["sc2", "idf2"], writes=[k5])
                c.op("act", lambda e: e.copy(out=nmT[:, tt * 128:(tt + 1) * 128], in_=b5[0:32, 384:512]), reads=[k5], writes=["nmT"])
            c.dma("sp", "eba", eba4[:], self.EBA[4 * g:4 * g + 4, :, :].rearrange("h p n -> p h n"), reads=[("EBA", 4 * g + r_) for r_ in range(4)], writes=["eba"])
            for r in range(4):
                h = 4 * g + r
                c.dma("sp", "eb", eb[:], self.EBD[h, :, :], reads=[("EBD", h)], writes=["eb"])
                self.gate_rows(h, [1, 2])
                self.eba = eba4[:, r, :]
                for G in range(4):
                    self.banded_attn(h, G, qT[r], ("qT", r), ksT, "ksT", vs, "vs", eb, "eb", "sel", nmT, uz=(2, 3))
                    self.banded_attn(h, G, qT[r], ("qT", r), kwT, "kwT", vw, "vw", eb, "eb", "win", uz=(4, 5))
                    self.attn_finish(h, 1, G, oacc[r], False, uz=(2, 3))
                    self.attn_finish(h, 2, G, oacc[r], False, uz=(4, 5))
                c.op("act", lambda e: e.copy(out=obf[:], in_=oacc[r][:]), reads=[("oacc", id(oacc[r]), G) for G in range(4)], writes=["obf"])
                c.dma("sp", "obf", self.OT[h, :, :], obf[:], reads=["obf"], writes=[("OT", h)])

    def setup_tables2(self):
        c = self.c
        L = self.L
        self.trineg = self.sb("trineg", [128, 128], BF16)
        self.mstrict = self.sb("mstrict", [128, 128], BF16)
        self.masku = self.sb("masku", [128, 64], F32)
        self.rst = self.sb("rst", [128, T], F32)
        tmp = self.sb("tmpc", [128, 128], F32)
        c.op("pool", lambda e: e.memset(tmp[:], -1.0), writes=["tmpc"])
        c.op("pool", lambda e: e.affine_select(out=tmp[:], in_=tmp[:], pattern=[[-1, 128]], compare_op=ALU.is_ge, fill=0.0, base=0, channel_multiplier=1),
             reads=["tmpc"], writes=["tmpc"])
        c.op("pool", lambda e: e.tensor_copy(out=self.trineg[:], in_=tmp[:]), reads=["tmpc"], writes=["trineg"])
        c.op("pool", lambda e: e.memset(tmp[:], 1.0), reads=["tmpc"], writes=["tmpc"])
        c.op("pool", lambda e: e.affine_select(out=tmp[:], in_=tmp[:], pattern=[[1, 128]], compare_op=ALU.is_gt, fill=0.0, base=0, channel_multiplier=-1),
             reads=["tmpc"], writes=["tmpc"])
        c.op("pool", lambda e: e.tensor_copy(out=self.mstrict[:], in_=tmp[:]), reads=["tmpc"], writes=["mstrict"])
        self.mneg = self.sb("mneg", [128, 128], BF16)
        c.op("pool", lambda e: e.memset(tmp[:], -30000.0), reads=["tmpc"], writes=["tmpc"])
        c.op("pool", lambda e: e.affine_select(out=tmp[:], in_=tmp[:], pattern=[[-1, 128]], compare_op=ALU.is_ge, fill=0.0, base=0, channel_multiplier=1),
             reads=["tmpc"], writes=["tmpc"])
        c.op("pool", lambda e: e.tensor_copy(out=self.mneg[:], in_=tmp[:]), reads=["tmpc"], writes=["mneg"])
        c.op("pool", lambda e: e.memset(self.masku[:], 1.0), writes=["masku"])
        for p0 in (0, 64):
            c.op("pool", lambda e: e.affine_select(out=self.masku[p0:p0 + 64, :], in_=self.masku[p0:p0 + 64, :], pattern=[[1, 64]], compare_op=ALU.is_ge,
                                                   fill=0.0, base=0, channel_multiplier=-1), reads=["masku"], writes=["masku"])
        c.op("pool", lambda e: e.memset(self.rst[:], 1.0), writes=["rst"])
        c.op("pool", lambda e: e.memset(self.rst[:].rearrange("p (c t) -> p c t", t=64)[:, :, 0:1], 0.0), reads=["rst"], writes=["rst"])
        hl = self.sb("hl", [128, 4, 8], F32)
        lc = self.sb("lc", [128, L, 4], F32)
        self.lb = self.sb("lb", [128, L, 8], F32)
        self.oml = self.sb("oml", [128, L, 8], F32)
        ssum = self.sb("ssum", [128, 8], F32)
        c.dma("sp", "cst9", hl[:], self.hlb[:, :, :], writes=["hl"])
        c.dma("sp", "cst10", lc[:], self.lcum[:, :, :], writes=["lc"])
        c.op("act", lambda e: e.activation(out=hl[:], in_=hl[:], func=AF.Exp), reads=["hl"], writes=["hl"])
        c.op("dve", lambda e: e.tensor_tensor(out=ssum[:], in0=hl[:, 0, :], in1=hl[:, 1, :], op=ALU.add), reads=["hl"], writes=["ssum"])
        c.op("dve", lambda e: e.tensor_tensor(out=ssum[:], in0=ssum[:], in1=hl[:, 2, :], op=ALU.add), reads=["hl", "ssum"], writes=["ssum"])
        c.op("dve", lambda e: e.tensor_tensor(out=ssum[:], in0=ssum[:], in1=hl[:, 3, :], op=ALU.add), reads=["hl", "ssum"], writes=["ssum"])
        c.op("dve", lambda e: e.reciprocal(out=ssum[:], in_=ssum[:]), reads=["ssum"], writes=["ssum"])
        for l4 in range(4):
            c.op("dve", lambda e: e.tensor_tensor(out=hl[:, l4, :], in0=hl[:, l4, :], in1=ssum[:], op=ALU.mult), reads=["hl", "ssum"], writes=["hl"])
        for li in range(L):
            c.op("dve", lambda e: e.tensor_scalar(out=self.lb[:, li, :], in0=hl[:, 0, :], scalar1=lc[:, li, 0:1], scalar2=None, op0=ALU.mult),
                 reads=["hl", "lc"], writes=["lb"])
            for l4 in range(1, 4):
                c.op("dve", lambda e: e.scalar_tensor_tensor(out=self.lb[:, li, :], in0=hl[:, l4, :], scalar=lc[:, li, l4:l4 + 1], in1=self.lb[:, li, :],
                                                             op0=ALU.mult, op1=ALU.add), reads=["hl", "lc", "lb"], writes=["lb"])
        c.op("dve", lambda e: e.tensor_scalar(out=self.oml[:], in0=self.lb[:], scalar1=-1.0, scalar2=1.0, op0=ALU.mult, op1=ALU.add),
             reads=["lb"], writes=["oml"])
        self.gn_t = self.sb("gn", [128, L, 128], F32)
        c.dma("sp", "cst11", self.gn_t[:], self.gn[:, :, :], writes=["gn"])
        self.gffn = self.sb("gffn", [128, L, 16], F32)
        c.dma("sp", "cst12", self.gffn[:], self.norm_ffn[:, :, :], writes=["gffn"])
        self.convw_t = self.sb("convw", [128, L, 3, 88], F32)
        c.dma("sp", "cst13", self.convw_t[:], self.convw[:, :, :, :], writes=["convw"])
        self.convb_t = self.sb("convb", [128, L, 88], F32)
        c.dma("sp", "cst14", self.convb_t[:], self.convb[:, :, :], writes=["convb"])

    def phase_sb(self, li, s):
        c = self.c
        sb = self.sb
        qTs = [sb("sqT", [128, T], BF16) for _ in range(2)]
        kTs = [sb("skT", [128, T], BF16) for _ in range(2)]
        Vs = [sb("sV", [128, 16, 128], BF16) for _ in range(2)]
        R = sb("sR", [128, 4, 512], F32)
        e32 = [sb("se32", [128, 512], F32) for _ in range(2)]
        arg = sb("sarg", [128, 512], F32)
        spb = [sb("spb", [128, 512], BF16) for _ in range(2)]
        A = [sb("sA", [128, 512], BF16) for _ in range(2)]
        obf = [sb("sobf", [128, T], BF16) for _ in range(2)]
        tmv = self.TM.rearrange("(kt p) n -> p kt n", p=128)

        def load_head(h):
            b = h % 2
            c.dma("sp", f"sqT{b}", qTs[b][:], self.FM[16 + h, :, :], reads=[("FM", 16 + h)], writes=[("sqT", b)])
            c.dma("sp", f"skT{b}", kTs[b][:], self.FM[24 + h, :, :], reads=[("FM", 24 + h)], writes=[("skT", b)])
            c.dma("sp", f"sV{b}", Vs[b][:], tmv[:, :, 512 + h * 128:512 + (h + 1) * 128],
                  reads=[("TM", 512 + (h // 4) * 512, t) for t in range(16)], writes=[("sV", b)])
        load_head(0)
        blocks = []
        for h in range(8):
            for G in range(4):
                kts = list(range(4 * G + 3, -1, -1))
                for kt in kts:
                    blocks.append((h, G, kt, kt == kts[0], kt == kts[-1]))

        def stage_a(k):
            h, G, kt, firstk, lastk = blocks[k]
            hb = h % 2
            qT, kT = qTs[hb], kTs[hb]
            if firstk:
                c.op("pool", lambda e: e.memset(R[:, G, :], 0.0), writes=[("sR", G)])
            qlo = max(kt, 4 * G)
            c0 = (qlo - 4 * G) * 128
            n = 512 - c0
            i = k % 2
            S, kS = self.bk[i], ("bk", i)
            sp_, ksp = spb[i], ("spb", i)
            diag = kt >= 4 * G
            c.op("pe", lambda e: e.matmul(S[:, 0:n], lhsT=kT[:, kt * 128:(kt + 1) * 128], rhs=qT[:, G * 512 + c0:(G + 1) * 512], start=True, stop=False),
                 reads=[("skT", hb), ("sqT", hb)], writes=[kS])
            if diag:
                c.op("pe", lambda e: e.matmul(S[:, 0:128], lhsT=self.ident[:], rhs=self.mneg[:], start=False, stop=False),
                     reads=["ident", "mneg"], writes=[kS])
            c.op("act", lambda e: e.activation(out=e32[i][:, 0:n], in_=S[:, 0:n], func=AF.Exp), reads=[kS], writes=[("se32", i)])
            c.op("act", lambda e: e.activation(out=sp_[:, 0:n], in_=e32[i][:, 0:n], func=AF.Ln, bias=1.0), reads=[("se32", i)], writes=[ksp])

        def stage_b(k):
            h, G, kt, firstk, lastk = blocks[k]
            hb = h % 2
            V = Vs[hb]
            qlo = max(kt, 4 * G)
            c0 = (qlo - 4 * G) * 128
            n = 512 - c0
            i = k % 2
            S, kS = self.bk[i], ("bk", i)
            sp_, ksp = spb[i], ("spb", i)
            A_, kA = A[i], ("sA", i)
            U, kU = (self.bk[2], ("bk", 2)) if G % 2 == 0 else (self.bk[4], ("bk", 4))
            Cs, kCs = (self.bk[3], ("bk", 3)) if k % 2 == 0 else (self.bk[5], ("bk", 5))
            diag = kt >= 4 * G
            if G == 0 and firstk and h + 1 < 8:
                load_head(h + 1)
            c.op("pe", lambda e: e.matmul(S[:, 0:n], lhsT=self.trineg[:], rhs=sp_[:, 0:n], start=False, stop=True), reads=["trineg", ksp], writes=[kS])
            c.op("pe", lambda e: e.matmul(Cs[:, 0:n], lhsT=self.ones_b[:], rhs=sp_[:, 0:n], start=True, stop=True), reads=["ones", ksp], writes=[kCs])
            c.op("dve", lambda e: e.tensor_tensor(out=arg[:, 0:n], in0=S[:, 0:n], in1=R[:, G, c0:512], op=ALU.subtract), reads=[kS, ("sR", G)], writes=["sarg"])
            c.op("act", lambda e: e.activation(out=A_[:, 0:n], in_=arg[:, 0:n], func=AF.Exp), reads=["sarg"], writes=[kA])
            if not lastk:
                c.op("dve", lambda e: e.tensor_tensor(out=R[:, G, c0:512], in0=R[:, G, c0:512], in1=Cs[:, 0:n], op=ALU.add), reads=[("sR", G), kCs], writes=[("sR", G)])
            c.op("pe", lambda e: e.matmul(U[:, c0:512], lhsT=V[:, kt, :], rhs=A_[:, 0:n], start=firstk, stop=lastk), reads=[("sV", hb), kA], writes=[kU])
            if lastk:
                ob = obf[hb]
                c.op("act", lambda e: e.copy(out=ob[:, G * 512:(G + 1) * 512], in_=U[:]), reads=[kU], writes=[("sobf", hb)])
                if G == 3:
                    c.dma("sp", f"sobf{hb}", self.OT[8 + h, :, :], ob[:], reads=[("sobf", hb)], writes=[("OT", 8 + h)])
        stage_a(0)
        for k in range(len(blocks)):
            if k + 1 < len(blocks):
                stage_a(k + 1)
            stage_b(k)

    def phase_hg(self, li, s):
        c = self.c
        sb = self.sb
        qf = sb("hq", [128, T], BF16)
        F = sb("hF", [128, T], F32)
        LF = sb("hLF", [128, T], F32)
        Kk = sb("hK", [128, T], F32)
        b = sb("hb", [128, T], F32)
        d1 = sb("hd1", [128, T], F32)
        E = sb("hE", [128, T], F32)
        qp = sb("hqp", [128, T], BF16)
        kp = sb("hkp", [128, T], BF16)
        kd = sb("hkd", [128, T], BF16)
        kdT = sb("hkdT", [128, 16, 128], BF16)
        V = sb("hV", [128, 16, 128], BF16)
        Gs = sb("hGs", [128, 16, 128], BF16)
        emid = sb("hemid", [128, 32], F32)
        elast = sb("helast", [128, 32], F32)
        S = sb("hS", [128, 128], F32)
        Sb = sb("hSb", [128, 128], BF16)
        am = sb("ham", [128, 64], BF16)
        ss = sb("hss", [128, 4], F32)
        junk = sb("hjunk", [128, 128], F32)
        y = sb("hy", [128, 128], F32)
        ybs = [sb("hyb", [128, 128], BF16) for _ in range(2)]
        ohT = sb("hohT", [128, T], BF16)
        tmv = self.TM.rearrange("(kt p) n -> p kt n", p=128)
        c3 = lambda a: a[:].rearrange("p (c t) -> p c t", t=64)
        for h in range(8):
            c.dma("sp", "hq", qf[:], self.FM[32 + h, :, :], reads=[("FM", 32 + h)], writes=["hq"])
            c.dma("sp", "hF", F[:], self.HF[h, :, :], reads=[("HF", h)], writes=["hF"])
            c.dma("sp", "hV", V[:], tmv[:, :, 1536 + h * 128:1536 + (h + 1) * 128],
                  reads=[("TM", 1536 + (h // 4) * 512, t) for t in range(16)], writes=["hV"])
            c.dma("sp", "hGs", Gs[:], tmv[:, :, 2560 + h * 128:2560 + (h + 1) * 128],
                  reads=[("TM", 2560 + (h // 4) * 512, t) for t in range(16)], writes=["hGs"])
            c.op("act", lambda e: e.activation(out=F[:], in_=F[:], func=AF.Sigmoid), reads=["hF"], writes=["hF"])
            c.op("dve", lambda e: e.tensor_scalar(out=F[:], in0=F[:], scalar1=self.oml[:, li, h:h + 1], scalar2=self.lb[:, li, h:h + 1],
                                                  op0=ALU.mult, op1=ALU.add), reads=["hF", "oml", "lb"], writes=["hF"])
            c.op("act", lambda e: e.activation(out=LF[:], in_=F[:], func=AF.Ln), reads=["hF"], writes=["hLF"])
            c.op("dve", lambda e: e.tensor_scalar(out=Kk[:], in0=F[:], scalar1=-1.0, scalar2=1.0, op0=ALU.mult, op1=ALU.add), reads=["hF"], writes=["hK"])
            c.op("dve", lambda e: e.tensor_tensor_scan(out=b[:], data0=self.rst[:], data1=LF[:], initial=0.0, op0=ALU.mult, op1=ALU.add),
                 reads=["hLF", "rst"], writes=["hb"])
            bmid = c3(b)[:, :, 31:32]
            blast = c3(b)[:, :, 63:64]
            c.op("dve", lambda e: e.tensor_tensor(out=c3(d1), in0=c3(b), in1=bmid.broadcast_to([128, 32, 64]), op=ALU.subtract), reads=["hb"], writes=["hd1"])
            c.op("act", lambda e: e.activation(out=E[:], in_=d1[:], func=AF.Exp), reads=["hd1"], writes=["hE"])
            c.op("dve", lambda e: e.tensor_tensor(out=qp[:], in0=qf[:], in1=E[:], op=ALU.mult), reads=["hq", "hE"], writes=["hqp"])
            c.op("act", lambda e: e.activation(out=E[:], in_=d1[:], func=AF.Exp, scale=-1.0), reads=["hd1", "hqp"], writes=["hE"])
            c.op("dve", lambda e: e.tensor_tensor(out=kp[:], in0=Kk[:], in1=E[:], op=ALU.mult), reads=["hK", "hE"], writes=["hkp"])
            c.op("dve", lambda e: e.tensor_tensor(out=c3(d1), in0=blast.broadcast_to([128, 32, 64]), in1=c3(b), op=ALU.subtract), reads=["hb", "hkp"], writes=["hd1"])
            c.op("act", lambda e: e.activation(out=E[:], in_=d1[:], func=AF.Exp), reads=["hd1", "hkp"], writes=["hE"])
            c.op("dve", lambda e: e.tensor_tensor(out=kd[:], in0=Kk[:], in1=E[:], op=ALU.mult), reads=["hK", "hE"], writes=["hkd"])
            c.op("act", lambda e: e.activation(out=emid[:].rearrange("p (c o) -> p c o", o=1), in_=bmid, func=AF.Exp), reads=["hb"], writes=["hemid"])
            c.op("act", lambda e: e.activation(out=elast[:].rearrange("p (c o) -> p c o", o=1), in_=blast, func=AF.Exp), reads=["hb"], writes=["helast"])
            for tt in range(16):
                pt = self.ptr[tt % 2]
                kp_ = ("ptr", tt % 2)
                c.op("pe", lambda e: e.transpose(out=pt[:, 0, :], in_=kd[:, tt * 128:(tt + 1) * 128], identity=self.ident[:]), reads=["hkd", "ident"], writes=[kp_])
                c.op("act", lambda e: e.copy(out=kdT[:, tt, :], in_=pt[:, 0, :]), reads=[kp_], writes=["hkdT"])
            c.op("pool", lambda e: e.memset(S[:], 0.0), writes=["hS"])
            def pre(ch):
                tt, half = ch // 2, ch % 2
                p0 = half * 64
                cs = slice(ch * 64, (ch + 1) * 64)
                aps, ka = (self.bk[0], ("bk", 0)) if ch % 2 == 0 else (self.bk[5], ("bk", 5))
                KV, kKV = (self.bk[3], ("bk", 3)) if ch % 2 == 0 else (self.bk[2], ("bk", 2))
                c.op("pe", lambda e: e.matmul(aps[p0:p0 + 64, 0:64], lhsT=kp[:, cs], rhs=qp[:, cs], start=True, stop=True), reads=["hkp", "hqp"], writes=[ka])
                c.op("dve", lambda e: e.tensor_tensor(out=am[p0:p0 + 64, :], in0=aps[p0:p0 + 64, 0:64], in1=self.masku[p0:p0 + 64, :], op=ALU.mult),
                     reads=[ka, "masku"], writes=[("ham", half)])
                c.op("pe", lambda e: e.matmul(KV[:, 0:128], lhsT=kdT[p0:p0 + 64, tt, :], rhs=V[p0:p0 + 64, tt, :], start=True, stop=True),
                     reads=["hkdT", "hV"], writes=[kKV])

            def main(ch):
                tt, half = ch // 2, ch % 2
                p0 = half * 64
                cs = slice(ch * 64, (ch + 1) * 64)
                KV, kKV = (self.bk[3], ("bk", 3)) if ch % 2 == 0 else (self.bk[2], ("bk", 2))
                O, kO = (self.bk[1], ("bk", 1)) if tt % 2 == 0 else (self.bk[4], ("bk", 4))
                c.op("act", lambda e: e.activation(out=Sb[:], in_=S[:], func=AF.Copy, scale=emid[:, ch:ch + 1]), reads=["hS", "hemid"], writes=["hSb"])
                c.op("pe", lambda e: e.matmul(O[p0:p0 + 64, 0:128], lhsT=am[p0:p0 + 64, :], rhs=V[p0:p0 + 64, tt, :], start=True, stop=False),
                     reads=[("ham", half), "hV"], writes=[kO])
                c.op("pe", lambda e: e.matmul(O[p0:p0 + 64, 0:128], lhsT=qp[:, cs], rhs=Sb[:], start=False, stop=True), reads=["hqp", "hSb"], writes=[kO])
                c.op("dve", lambda e: e.scalar_tensor_tensor(out=S[:], in0=S[:], scalar=elast[:, ch:ch + 1], in1=KV[:, 0:128], op0=ALU.mult, op1=ALU.add),
                     reads=["hS", "helast", kKV], writes=["hS"])
                if half == 1:
                    c.op("act", lambda e: e.activation(out=junk[:], in_=O[:, 0:128], func=AF.Square, accum_out=ss[:, 0:1]), reads=[kO], writes=["hjunk", "hss"])
                    c.op("act", lambda e: e.activation(out=ss[:, 1:2], in_=ss[:, 0:1], func=AF.Sqrt, scale=1.0 / HD, bias=self.eps_t[:, 0:1]),
                         reads=["hss", "eps"], writes=["hss1"])
                    c.op("dve", lambda e: e.reciprocal(out=ss[:, 2:3], in_=ss[:, 1:2]), reads=["hss1"], writes=["hss2"])
                    c.op("dve", lambda e: e.scalar_tensor_tensor(out=y[:], in0=O[:, 0:128], scalar=ss[:, 2:3], in1=self.gn_t[:, li, :], op0=ALU.mult, op1=ALU.mult),
                         reads=[kO, "hss2", "gn"], writes=["hy"])
                    ybt = ybs[tt % 2]
                    c.op("dve", lambda e: e.tensor_tensor(out=ybt[:], in0=y[:], in1=Gs[:, tt, :], op=ALU.mult), reads=["hy", "hGs"], writes=[("hyb", tt % 2)])

            def fin(tt):
                pt = self.ptr[tt % 2]
                kp_ = ("ptr", tt % 2)
                c.op("pe", lambda e: e.transpose(out=pt[:, 1, :], in_=ybs[tt % 2][:], identity=self.ident[:]), reads=[("hyb", tt % 2), "ident"], writes=[kp_])
                c.op("act", lambda e: e.copy(out=ohT[:, tt * 128:(tt + 1) * 128], in_=pt[:, 1, :]), reads=[kp_], writes=["hohT"])
            pre(0)
            for ch in range(32):
                if ch + 1 < 32:
                    pre(ch + 1)
                main(ch)
                if ch % 2 == 0 and ch >= 2:
                    fin(ch // 2 - 1)
            fin(15)
            c.dma("sp", "hohT", self.OT[16 + h, :, :], ohT[:], reads=["hohT"], writes=[("OT", 16 + h)])

    def phase_d(self, li, s, tb, xsrc, xsname, xdst, xdname, carry):
        c = self.c
        sb = self.sb
        t0 = tb * 512
        xblk = sb("xblk", [128, 4, D], F32)
        h2T = sb("h2T", [128, 16, 512], BF16)
        kxb = "xblk"
        c.dma("sp", "xblk", xblk[:], xsrc[s, t0:t0 + 512, :].rearrange("(ts p) n -> p ts n", p=128), reads=[("X", xsname, s, tb)], writes=[kxb])
        nacc = [0]

        def next_acc():
            i = nacc[0] % 2
            nacc[0] += 1
            return self.bk[i], ("bk", i)
        with self.phase():
            hb = sb("hb", [128, 16, 512], BF16)
            ob = sb("ob", [128, 24, 512], BF16)
            sg = [sb("sg", [128, 4, 512], BF16) for _ in range(3)]
            mT = sb("mT", [128, 16, 512], BF16)
            macc = sb("macc", [128, 4, 512], F32)
            tmp = sb("mtmp", [128, 512], F32)
            wt = [sb("wtd", [128, 16, 512], BF16) for _ in range(2)]
            self.xn = [sb("xn", [128, D], BF16)]
            self.sqj = [sb("sqj", [128, D], BF16)]
            self.ss = [sb("ss", [128, 4], F32) for _ in range(2)]
            nl = [0]

            def load(view, n_k, key):
                i = nl[0] % 2
                nl[0] += 1
                c.dma("sp", f"wtd{i}", wt[i][:, 0:n_k, :], view, reads=[key], writes=[("wtd", i)])
                return wt[i], ("wtd", i)
            c.dma("sp", "hb", hb[:], self.HT[:, :, t0:t0 + 512], reads=["HT"], writes=["hb"])
            c.dma("sp", "ob", ob[:], self.OT[:, :, t0:t0 + 512].rearrange("c p t -> p c t"), reads=[("OT", k) for k in range(24)], writes=["ob"])
            win = self.win_b[li].rearrange("(kc p) n -> p kc n", p=128)
            for dg in range(4):
                self.cvt_some(1)
                for br in range(3):
                    col = C_MG + br * 2048 + dg * 512
                    w, kw = load(win[:, :, col:col + 512], 16, ("win_b", li))
                    for j in range(4):
                        acc, ka = next_acc()
                        for kc in range(16):
                            c.op("pe", lambda e: e.matmul(acc[:], lhsT=w[:, kc, j * 128:(j + 1) * 128], rhs=hb[:, kc, :], start=(kc == 0), stop=(kc == 15)),
                                 reads=[kw, "hb"], writes=[ka])
                        c.op("act", lambda e: e.activation(out=sg[br][:, j, :], in_=acc[:], func=AF.Sigmoid), reads=[ka], writes=[("sg", br, j)])
                for br in range(3):
                    wb = self.wbr_b[li, br * 1024:(br + 1) * 1024, :].rearrange("(kc p) n -> p kc n", p=128)
                    w, kw = load(wb[:, :, dg * 512:(dg + 1) * 512], 8, ("wbr_b", li))
                    for j in range(4):
                        acc, ka = next_acc()
                        for c8 in range(8):
                            c.op("pe", lambda e: e.matmul(acc[:], lhsT=w[:, c8, j * 128:(j + 1) * 128], rhs=ob[:, br * 8 + c8, :], start=(c8 == 0), stop=(c8 == 7)),
                                 reads=[kw, "ob"], writes=[ka])
                        if br == 0:
                            c.op("dve", lambda e: e.tensor_tensor(out=macc[:, j, :], in0=acc[:], in1=sg[0][:, j, :], op=ALU.mult),
                                 reads=[ka, ("sg", 0, j)], writes=[("macc", j)])
                        else:
                            c.op("dve", lambda e: e.tensor_tensor(out=tmp[:], in0=acc[:], in1=sg[br][:, j, :], op=ALU.mult),
                                 reads=[ka, ("sg", br, j)], writes=["mtmp"])
                            c.op("pool", lambda e: e.tensor_tensor(out=macc[:, j, :], in0=macc[:, j, :], in1=tmp[:], op=ALU.add),
                                 reads=["mtmp", ("macc", j)], writes=[("macc", j)])
                        if br == 2:
                            c.op("act", lambda e: e.copy(out=mT[:, dg * 4 + j, :], in_=macc[:, j, :]), reads=[("macc", j)], writes=["mT"])
            wo = self.wout_b[li].rearrange("(kc p) n -> p kc n", p=128)
            for ng in range(4):
                w, kw = load(wo[:, :, ng * 512:(ng + 1) * 512], 16, ("wout_b", li))
                for ts in range(4):
                    acc, ka = next_acc()
                    for kc in range(16):
                        c.op("pe", lambda e: e.matmul(acc[:], lhsT=mT[:, kc, ts * 128:(ts + 1) * 128], rhs=w[:, kc, :], start=(kc == 0), stop=(kc == 15)),
                             reads=[kw, "mT"], writes=[ka])
                    xs = xblk[:, ts, ng * 512:(ng + 1) * 512]
                    c.op("dve", lambda e: e.tensor_tensor(out=xs, in0=acc[:], in1=xs, op=ALU.add), reads=[ka, kxb], writes=[kxb])
            for ts in range(4):
                self.norm_tile(xblk[:, ts, :], kxb, self.gffn[:, li, :], h2T, ts * 128, "h2T")
        with self.phase():
            aT = sb("aT", [128, 44, 512], BF16)
            wt = [sb("wtf", [128, 16, 512], BF16) for _ in range(3)]
            ub = [sb("ub", [128, 514], F32) for _ in range(2)]
            cc = [sb("cc", [128, 512], F32) for _ in range(2)]
            nl = [0]

            def load3(view, n_k, key):
                i = nl[0] % 3
                nl[0] += 1
                c.dma("sp", f"wtf{i}", wt[i][:, 0:n_k, :], view, reads=[key], writes=[("wtf", i)])
                return wt[i], ("wtf", i)
            wu = self.wup_b[li].rearrange("(kc p) n -> p kc n", p=128)
            for i4 in range(11):
                self.cvt_some(1)
                wg_, kwg = load3(wu[:, :, i4 * 512:(i4 + 1) * 512], 16, ("wup_b", li))
                wv_, kwv = load3(wu[:, :, DFF + i4 * 512:DFF + (i4 + 1) * 512], 16, ("wup_b", li))
                for j in range(4):
                    i = i4 * 4 + j
                    for side, (w, kw) in enumerate(((wg_, kwg), (wv_, kwv))):
                        ch = i + 44 * side
                        acc, ka = self.bk[2 + side], ("bk", 2 + side)
                        for kc in range(16):
                            c.op("pe", lambda e: e.matmul(acc[:], lhsT=w[:, kc, j * 128:(j + 1) * 128], rhs=h2T[:, kc, :], start=(kc == 0), stop=(kc == 15)),
                                 reads=[kw, "h2T"], writes=[ka])
                        u, ku = ub[side], ("ub", side)
                        cv, kc_ = cc[side], ("cc", side)
                        c.op("pool", lambda e: e.tensor_copy(out=u[:, 0:2], in_=carry[:, ch, :]), reads=[("carry", ch)], writes=[ku])
                        c.op("act", lambda e: e.copy(out=u[:, 2:514], in_=acc[:]), reads=[ka], writes=[ku])
                        c.op("pool", lambda e: e.tensor_copy(out=carry[:, ch, :], in_=u[:, 512:514]), reads=[ku], writes=[("carry", ch)])
                        cw = self.convw_t
                        c.op("pool", lambda e: e.tensor_scalar(out=cv[:], in0=u[:, 2:514], scalar1=cw[:, li, 2, ch:ch + 1], scalar2=self.convb_t[:, li, ch:ch + 1],
                                                               op0=ALU.mult, op1=ALU.add), reads=[ku, "convw", "convb"], writes=[kc_])
                        c.op("dve", lambda e: e.scalar_tensor_tensor(out=cv[:], in0=u[:, 1:513], scalar=cw[:, li, 1, ch:ch + 1], in1=cv[:], op0=ALU.mult, op1=ALU.add),
                             reads=[ku, "convw", kc_], writes=[kc_])
                        c.op("dve", lambda e: e.scalar_tensor_tensor(out=cv[:], in0=u[:, 0:512], scalar=cw[:, li, 0, ch:ch + 1], in1=cv[:], op0=ALU.mult, op1=ALU.add),
                             reads=[ku, "convw", kc_], writes=[kc_])
                    c.op("act", lambda e: e.activation(out=cc[0][:], in_=cc[0][:], func=AF.Silu), reads=[("cc", 0)], writes=[("cc", 0)])
                    c.op("dve", lambda e: e.tensor_tensor(out=aT[:, i, :], in0=cc[0][:], in1=cc[1][:], op=ALU.mult), reads=[("cc", 0), ("cc", 1)], writes=["aT"])
            wd = self.wdn_b[li].rearrange("(kc p) n -> p kc n", p=128)
            for ng in range(4):
                for kg in range(3):
                    nk = 16 if kg < 2 else 12
                    w, kw = load3(wd[:, kg * 16:kg * 16 + nk, ng * 512:(ng + 1) * 512], nk, ("wdn_b", li))
                    for ts in range(4):
                        acc, ka = self.bk[ts], ("bk", ts)
                        for k2 in range(nk):
                            kc = kg * 16 + k2
                            c.op("pe", lambda e: e.matmul(acc[:], lhsT=aT[:, kc, ts * 128:(ts + 1) * 128], rhs=w[:, k2, :], start=(kc == 0), stop=(kc == 43)),
                                 reads=[kw, "aT"], writes=[ka])
                for ts in range(4):
                    xs = xblk[:, ts, ng * 512:(ng + 1) * 512]
                    c.op("dve", lambda e: e.tensor_tensor(out=xs, in0=self.bk[ts][:], in1=xs, op=ALU.add), reads=[("bk", ts), kxb], writes=[kxb])
            c.dma("sp", "xout", xdst[s, t0:t0 + 512, :].rearrange("(ts p) n -> p ts n", p=128), xblk[:], reads=[kxb], writes=[("X", xdname, s, tb)])

    def build(self):
        c = self.c
        self._stacks = []
        self.setup_consts()
        self.eps_t = self.sb("eps", [128, 1], F32)
        c.op("pool", lambda e: e.memset(self.eps_t[:], EPS), writes=["eps"])
        self.ptr = [self.ps("ptr", [128, 8, 128], BF16) for _ in range(2)]
        self.bk = [self.ps("bk", [128, 512], F32) for _ in range(6)]
        self.acc = self.bk[0:2]
        self.pn = self.bk[2]
        self.sqb = self.sb("sqb", [128, 512], BF16)
        self.rt = self.sb("rt", [128, 512], F32)
        self.setup_tables()
        self.setup_tables2()
        self.cvt_queue = []
        for piece in self.convert_pieces(0):
            piece()
        for li in range(self.L):
            xsrc, xsname = (self.x, "xin") if li == 0 else (self.XS[(li - 1) % 2], f"XS{(li - 1) % 2}")
            xdst, xdname = (self.out, "out") if li == self.L - 1 else (self.XS[li % 2], f"XS{li % 2}")
            if li + 1 < self.L:
                self.cvt_queue = self.convert_pieces(li + 1)
            for s in range(NSEQ):
                with self.phase():
                    self.hT = self.sb("hT", [128, 16, T], BF16)
                    self.xt = [self.sb("xt", [128, D], F32) for _ in range(3)]
                    self.xn = [self.sb("xn", [128, D], BF16) for _ in range(2)]
                    self.sqj = [self.sb("sqj", [128, D], BF16) for _ in range(2)]
                    self.ss = [self.sb("ss", [128, 4], F32) for _ in range(2)]
                    self.wt = [self.sb("wt", [128, 16, 512], BF16) for _ in range(2)]
                    self.fst = [self.sb("fst", [128, T], BF16) for _ in range(2)]
                    self.fst32 = self.sb("fst32", [128, T], F32)
                    self.tst = [self.sb("tst", [128, 512], BF16) for _ in range(2)]
                    self.gst = self.sb("gst", [32, T], F32)
                    self.phase_a(li, s, xsrc, xsname)
                    c.dma("sp", "hts", self.HT[:, :, :], self.hT[:], reads=[("hT", t) for t in range(16)], writes=["HT"])
                    self.phase_b(li, s)
                if "stopB" in self.debug:
                    continue
                with self.phase():
                    self.phase_nsa(li, s)
                with self.phase():
                    self.phase_sb(li, s)
                with self.phase():
                    self.phase_hg(li, s)
                if "stopC" in self.debug:
                    continue
                with self.phase():
                    carry = self.sb("carry", [128, 88, 2], F32)
                    c.op("pool", lambda e: e.memset(carry[:], 0.0), writes=[("carry", ch) for ch in range(88)])
                    for tb in range(4):
                        with self.phase():
                            self.phase_d(li, s, tb, xsrc, xsname, xdst, xdname, carry)
            self.cvt_some(1000)
        c.finish()
        return self.nc


def _t5_bucket(dist):
    import math
    n = np.maximum(dist, 0)
    big = 16 + (np.log(np.maximum(n, 1).astype(np.float32) / np.float32(16)) / np.float32(math.log(8.0)) * np.float32(16)).astype(np.int32)
    return np.where(n < 16, n, np.minimum(big, 31))


def _static_tables(rel_bias):
    i = np.arange(128)[:, None]
    j = np.arange(128)[None, :]
    blocks = []
    for delta in (0, 128, 512):
        dist = delta + j - i
        blocks.append((dist, (dist >= 0) & (dist < 512)))
    for delta in (0, 128):
        dist = delta + j - i
        blocks.append((dist, dist >= 0))
    t = np.arange(T)[None, :]
    dist = t - 16 * i - 31
    blocks.append((dist, (dist >= 0) & (i < 127)))
    dist_all = np.concatenate([b[0] for b in blocks], axis=1)
    valid = np.concatenate([b[1] for b in blocks], axis=1)
    bucket = _t5_bucket(dist_all)
    ebias = np.empty((8, 128, dist_all.shape[1]), np.float32)
    for h in range(8):
        ebias[h] = np.where(valid, rel_bias[bucket, h], np.float32(-1e30))
    cb = np.ascontiguousarray(np.broadcast_to(rel_bias[31][None, :], (128, 8))).astype(np.float32)
    tpos = (8 + np.arange(8))[None, :, None] * 128 + np.arange(128)[:, None, None]
    qblk = tpos // 64
    jb = np.arange(32)[None, None, :]
    forced = (jb <= qblk) & ((jb == 0) | (jb >= qblk - 1))
    addmask = np.where(forced, 1e30, np.where(jb <= qblk, 0.0, -1e30)).astype(np.float32)
    esel = (np.arange(32)[:, None, None] == 2 * np.arange(16)[None, :, None] + (np.arange(128) // 64)[None, None, :])
    c_start = np.arange(127) * 16
    s_start = np.arange(32) * 64
    overlap = ((c_start[:, None] < s_start[None, :] + 64) & (c_start[:, None] + 32 > s_start[None, :]))
    ov33 = np.zeros((128, 33), np.float32)
    ov33[:127, :32] = overlap
    ov33[:, 32] = 1.0
    selg = (np.arange(24)[:, None, None] == np.arange(24)[None, :, None]) & np.ones((1, 1, 128), bool)
    return dict(ebias=ebias, cb=cb, addmask=addmask, esel=esel.astype(ml_dtypes.bfloat16),
                ov33=ov33.astype(ml_dtypes.bfloat16), selg=selg.astype(np.float32))


def prep_shared(inputs, layers):
    ls = list(layers)
    f = lambda a: np.ascontiguousarray(np.asarray(a, dtype=np.float32))
    d = _static_tables(f(inputs["rel_bias"]))
    d["w_in"] = f(inputs["w_in"])[ls]
    d["norm_attn"] = f(np.asarray(inputs["norm_attn"])[ls].reshape(len(ls), 16, 128).transpose(2, 0, 1))
    hg = np.concatenate([np.asarray(inputs["nsa_q_gain"])[ls][:, None, :], np.asarray(inputs["nsa_k_gain"])[ls]], axis=1)
    d["hgain"] = f(hg.transpose(2, 0, 1))
    d["cmp_w1"] = f(inputs["cmp_w1"])[ls]
    d["cmp_w2"] = f(inputs["cmp_w2"])[ls]
    d["posT"] = f(np.asarray(inputs["cmp_pos"])[ls].transpose(3, 0, 1, 2))
    d["hlb"] = f(np.asarray(inputs["hg_lower_bound"]).reshape(4, 8, 128).transpose(2, 0, 1))
    lcum = np.zeros((128, len(ls), 4), np.float32)
    for i, l in enumerate(ls):
        lcum[:, i, 1:l + 1] = 1.0
    d["lcum"] = lcum
    d["gn"] = f(np.broadcast_to(np.asarray(inputs["hg_norm_gain"])[ls][None, :, :], (128, len(ls), 128)))
    d["w_branch"] = f(np.asarray(inputs["w_branch"])[ls].reshape(len(ls), 3072, D))
    d["w_out"] = f(inputs["w_out"])[ls]
    d["norm_ffn"] = f(np.asarray(inputs["norm_ffn"])[ls].reshape(len(ls), 16, 128).transpose(2, 0, 1))
    d["w_up"] = f(inputs["w_up"])[ls]
    d["convw"] = f(np.asarray(inputs["conv_w"])[ls].reshape(len(ls), 3, 88, 128).transpose(3, 0, 1, 2))
    d["convb"] = f(np.asarray(inputs["conv_b"])[ls].reshape(len(ls), 88, 128).transpose(2, 0, 1))
    d["w_down"] = f(inputs["w_down"])[ls]
    return d


_PROGS = {}


def _get_prog(n_layers):
    if n_layers not in _PROGS:
        p = Prog(list(range(n_layers)))
        _PROGS[n_layers] = p.build()
    return _PROGS[n_layers]


FUSED = True


def kernel(**inputs):
    x = np.ascontiguousarray(np.asarray(inputs["x"], dtype=np.float32))
    n_cores = 8
    if FUSED:
        groups = [list(range(DEPTH))]
    else:
        groups = [[l] for l in range(DEPTH)]
    cur = x
    for ls in groups:
        nc = _get_prog(len(ls))
        shared = prep_shared(inputs, ls)
        in_maps = [dict(shared, x=np.ascontiguousarray(cur[NSEQ * i:NSEQ * (i + 1)])) for i in range(n_cores)]
        res = run_bass_kernel_spmd(nc, in_maps, core_ids=list(range(n_cores)))
        cur = np.concatenate([np.asarray(r["out"], dtype=np.float32) for r in res.results], axis=0)
    return cur
```
